# Optimizing a Trainium2 kernel written in Bass

```python
import math
import jax, jax.numpy as jnp
from jax import lax
import numpy as np

D_MODEL = 2048
BATCH = 2
SEQ = 8192
DEPTH = 1

DIFF_HEADS = 8
DIFF_HEAD_DIM = 64
MOBA_HEADS = 8
MOBA_HEAD_DIM = 128
MOBA_BLOCK = 256
MOBA_TOPK = 3
MOBA_Q_CHUNK = 64
ATTN_Q_BLOCK = 128
REL_BUCKETS = 32
REL_MAX_DIST = 128
N_EXPERTS = 64
N_GROUPS = 8
TOPK_GROUPS = 4
TOPK_EXPERTS = 8
EXPERT_DIM = 512
SHARED_DIM = 512
ROUTED_SCALE = 2.5
MOE_ROW_BLOCK = 128
RMS_EPS = 1e-6

DIFF_QK_W = DIFF_HEADS * 2 * DIFF_HEAD_DIM
DIFF_V_W = DIFF_HEADS * 2 * DIFF_HEAD_DIM
MOBA_W = MOBA_HEADS * MOBA_HEAD_DIM
IN_COLS = 2 * DIFF_QK_W + DIFF_V_W + 3 * MOBA_W + 2 * D_MODEL

kernel_name = 'hybrid_diffattn_moba_moe_adaln'


def _rmsnorm(x, gain):
    xf = x.astype(jnp.float32)
    y = xf * lax.rsqrt(jnp.mean(xf * xf, axis=-1, keepdims=True) + RMS_EPS)
    return (y * gain.astype(jnp.float32)).astype(x.dtype)


def _t5_bucket(rel):
    n = jnp.maximum(rel, 0)
    max_exact = REL_BUCKETS // 2
    nf = jnp.maximum(n, 1).astype(jnp.float32)
    large = max_exact + (jnp.log(nf / max_exact) / math.log(REL_MAX_DIST / max_exact)
                         * (REL_BUCKETS - max_exact)).astype(jnp.int32)
    large = jnp.minimum(large, REL_BUCKETS - 1)
    return jnp.where(n < max_exact, n, large)


def _diff_attention(q, k, v, lam_vecs, subln_g, table, layer_idx):
    B, S = q.shape[0], q.shape[1]
    H, dh, QB = DIFF_HEADS, DIFF_HEAD_DIM, ATTN_Q_BLOCK
    lam_init = 0.8 - 0.6 * math.exp(-0.3 * layer_idx)
    lv = lam_vecs.astype(jnp.float32)
    lam = jnp.exp(jnp.sum(lv[0] * lv[1])) - jnp.exp(jnp.sum(lv[2] * lv[3])) + lam_init
    qh = q.transpose(0, 2, 3, 1, 4)
    kh = k.transpose(0, 2, 3, 1, 4)
    vh = v.transpose(0, 2, 1, 3)
    nqb = S // QB
    q_blocks = qh.reshape(B, H, 2, nqb, QB, dh).transpose(3, 0, 1, 2, 4, 5)
    k_pos = jnp.arange(S)
    scale = dh ** -0.5

    def one_block(args):
        qb, start = args
        q_pos = start + jnp.arange(QB)
        rel = q_pos[:, None] - k_pos[None, :]
        bias = table[_t5_bucket(rel)].astype(jnp.float32).transpose(2, 0, 1)
        logits = jnp.einsum('bhmqd,bhmkd->bhmqk', qb, kh).astype(jnp.float32) * scale + bias[None, :, None]
        logits = jnp.where(rel >= 0, logits, -jnp.inf)
        p = jax.nn.softmax(logits, axis=-1)
        a = p[:, :, 0] - lam * p[:, :, 1]
        return jnp.einsum('bhqk,bhkd->bhqd', a.astype(vh.dtype), vh)

    o = lax.map(one_block, (q_blocks, jnp.arange(nqb, dtype=jnp.int32) * QB))
    o = _rmsnorm(o, subln_g) * (1.0 - lam_init)
    return o.transpose(1, 0, 3, 2, 4).reshape(B, S, H * 2 * dh)


def _moba_attention(q, k, v, table):
    B, S = q.shape[0], q.shape[1]
    H, dh, BLK, QC = MOBA_HEADS, MOBA_HEAD_DIM, MOBA_BLOCK, MOBA_Q_CHUNK
    qh = q.transpose(0, 2, 1, 3)
    kh = k.transpose(0, 2, 1, 3)
    vh = v.transpose(0, 2, 1, 3)
    nb = -(-S // BLK)
    pad = nb * BLK - S
    kblk = jnp.pad(kh, ((0, 0), (0, 0), (0, pad), (0, 0))).reshape(B, H, nb, BLK, dh)
    vblk = jnp.pad(vh, ((0, 0), (0, 0), (0, pad), (0, 0))).reshape(B, H, nb, BLK, dh)
    kmean = jnp.mean(kblk.astype(jnp.float32), axis=3)
    topk = min(MOBA_TOPK, nb)
    nqc = S // QC
    q_chunks = qh.reshape(B, H, nqc, QC, dh).transpose(2, 0, 1, 3, 4)
    blk_pos = jnp.arange(BLK)
    head_ids = jnp.arange(H)
    scale = dh ** -0.5
    gather = jax.vmap(jax.vmap(lambda t, i: t[i]))

    def one_chunk(args):
        qc, start = args
        cur = start // BLK
        q_pos = start + jnp.arange(QC)
        gate = jnp.einsum('bhqd,bhnd->bhqn', qc.astype(jnp.float32), kmean)
        gate = jnp.where(jnp.arange(nb) < cur, gate, -jnp.inf)
        _, idx = lax.top_k(gate, topk)
        valid = idx < cur
        ksel = gather(kblk, idx)
        vsel = gather(vblk, idx)
        kown = lax.dynamic_index_in_dim(kblk, cur, axis=2, keepdims=False)
        vown = lax.dynamic_index_in_dim(vblk, cur, axis=2, keepdims=False)
        s_sel = jnp.einsum('bhqd,bhqtkd->bhqtk', qc, ksel).astype(jnp.float32) * scale
        rel_sel = q_pos[None, None, :, None, None] - (idx[..., None] * BLK + blk_pos)
        s_sel = s_sel + table[head_ids[None, :, None, None, None], _t5_bucket(rel_sel)].astype(jnp.float32)
        s_sel = jnp.where(valid[..., None], s_sel, -jnp.inf)
        s_own = jnp.einsum('bhqd,bhkd->bhqk', qc, kown).astype(jnp.float32) * scale
        rel_own = q_pos[:, None] - (cur * BLK + blk_pos)[None, :]
        s_own = s_own + table[:, _t5_bucket(rel_own)].astype(jnp.float32)[None]
        s_own = jnp.where(rel_own >= 0, s_own, -jnp.inf)
        logits = jnp.concatenate([s_own, s_sel.reshape(B, H, QC, topk * BLK)], axis=-1)
        p = jax.nn.softmax(logits, axis=-1).astype(vh.dtype)
        p_sel = p[..., BLK:].reshape(B, H, QC, topk, BLK)
        return (jnp.einsum('bhqk,bhkd->bhqd', p[..., :BLK], vown)
                + jnp.einsum('bhqtk,bhqtkd->bhqd', p_sel, vsel))

    o = lax.map(one_chunk, (q_chunks, jnp.arange(nqc, dtype=jnp.int32) * QC))
    return o.transpose(1, 0, 3, 2, 4).reshape(B, S, H * dh)


def _moe(h, w_router, router_bias, w_gate, w_up, w_down, w_sh_gate, w_sh_up, w_sh_down):
    B, S, D = h.shape
    T = B * S
    E, K, M = N_EXPERTS, TOPK_EXPERTS, MOE_ROW_BLOCK
    ht = h.reshape(T, D)
    scores = jax.nn.sigmoid(jnp.matmul(ht, w_router).astype(jnp.float32))
    sel = scores + router_bias.astype(jnp.float32)
    grp = sel.reshape(T, N_GROUPS, E // N_GROUPS)
    grp_score = jnp.sum(lax.top_k(grp, 2)[0], axis=-1)
    _, gidx = lax.top_k(grp_score, TOPK_GROUPS)
    gmask = jnp.sum(jax.nn.one_hot(gidx, N_GROUPS, dtype=jnp.int32), axis=-2) > 0
    sel = jnp.where(jnp.repeat(gmask, E // N_GROUPS, axis=-1), sel, -jnp.inf)
    _, eidx = lax.top_k(sel, K)
    w = jnp.take_along_axis(scores, eidx, axis=-1)
    w = w / jnp.sum(w, axis=-1, keepdims=True) * ROUTED_SCALE
    A = T * K
    flat_e = eidx.reshape(A)
    flat_tok = jnp.repeat(jnp.arange(T, dtype=jnp.int32), K)
    order = jnp.argsort(flat_e)
    e_sorted = flat_e[order]
    tok_sorted = flat_tok[order]
    w_sorted = w.reshape(A)[order]
    counts = jnp.bincount(flat_e, length=E)
    starts = jnp.cumsum(counts) - counts
    padded = ((counts + M - 1) // M) * M
    pad_ends = jnp.cumsum(padded)
    dest = (pad_ends - padded)[e_sorted] + jnp.arange(A) - starts[e_sorted]
    n_blocks = -(-A // M) + E
    P = n_blocks * M
    row_tok = jnp.zeros((P,), jnp.int32).at[dest].set(tok_sorted)
    row_w = jnp.zeros((P,), jnp.float32).at[dest].set(w_sorted)
    block_expert = jnp.minimum(jnp.searchsorted(pad_ends // M, jnp.arange(n_blocks), side='right'), E - 1)
    x_pad = ht[row_tok].reshape(n_blocks, M, D)

    def expert_block(args):
        xb, e = args
        g = jnp.matmul(xb, w_gate[e])
        u = jnp.matmul(xb, w_up[e])
        return jnp.matmul(jax.nn.silu(g) * u, w_down[e])

    y_pad = lax.map(expert_block, (x_pad, block_expert)).reshape(P, D)
    routed = jax.ops.segment_sum(y_pad.astype(jnp.float32) * row_w[:, None], row_tok, num_segments=T)
    shared = jnp.matmul(jax.nn.silu(jnp.matmul(ht, w_sh_gate)) * jnp.matmul(ht, w_sh_up), w_sh_down)
    return (routed.astype(h.dtype) + shared).reshape(B, S, D)


def setup_inputs(seed: int = 0) -> dict:
    key = jax.random.key(seed)
    ks = jax.random.split(key, 24)
    f32 = jnp.float32
    D, L = D_MODEL, DEPTH
    nrm = lambda k, shape, s: jax.random.normal(k, shape, f32) * s
    return {
        'x': nrm(ks[0], (BATCH, SEQ, D), 1.0),
        'c': nrm(ks[1], (BATCH, D), 1.0),
        'w_ada': nrm(ks[2], (L, D, 6 * D), 0.5 * D ** -0.5),
        'b_ada': nrm(ks[3], (L, 6 * D), 0.02),
        'g_mix': 1.0 + nrm(ks[4], (L, D), 0.05),
        'g_ffn': 1.0 + nrm(ks[5], (L, D), 0.05),
        'w_in': nrm(ks[6], (L, D, IN_COLS), D ** -0.5),
        'diff_lambda': nrm(ks[7], (L, 4, DIFF_HEAD_DIM), 0.1),
        'diff_subln_g': 1.0 + nrm(ks[8], (L, 2 * DIFF_HEAD_DIM), 0.05),
        'rel_bias': nrm(ks[9], (REL_BUCKETS, DIFF_HEADS + MOBA_HEADS), 0.5),
        'w_o_diff': nrm(ks[10], (L, DIFF_V_W, D), DIFF_V_W ** -0.5),
        'w_o_moba': nrm(ks[11], (L, MOBA_W, D), MOBA_W ** -0.5),
        'w_out': nrm(ks[12], (L, D, D), D ** -0.5),
        'w_router': nrm(ks[13], (L, D, N_EXPERTS), D ** -0.5),
        'router_bias': nrm(ks[14], (L, N_EXPERTS), 0.01),
        'w_exp_gate': nrm(ks[15], (L, N_EXPERTS, D, EXPERT_DIM), D ** -0.5),
        'w_exp_up': nrm(ks[16], (L, N_EXPERTS, D, EXPERT_DIM), D ** -0.5),
        'w_exp_down': nrm(ks[17], (L, N_EXPERTS, EXPERT_DIM, D), EXPERT_DIM ** -0.5),
        'w_sh_gate': nrm(ks[18], (L, D, SHARED_DIM), D ** -0.5),
        'w_sh_up': nrm(ks[19], (L, D, SHARED_DIM), D ** -0.5),
        'w_sh_down': nrm(ks[20], (L, SHARED_DIM, D), SHARED_DIM ** -0.5),
        'g_final': 1.0 + nrm(ks[21], (D,), 0.05),
    }


def reference(x, c, w_ada, b_ada, g_mix, g_ffn, w_in, diff_lambda, diff_subln_g, rel_bias,
              w_o_diff, w_o_moba, w_out, w_router, router_bias, w_exp_gate, w_exp_up, w_exp_down,
              w_sh_gate, w_sh_up, w_sh_down, g_final):
    B, S, D = x.shape
    table_diff = rel_bias[:, :DIFF_HEADS]
    table_moba = rel_bias[:, DIFF_HEADS:].T
    c_act = jax.nn.silu(c)
    o_qd = 0
    o_kd = o_qd + DIFF_QK_W
    o_vd = o_kd + DIFF_QK_W
    o_qm = o_vd + DIFF_V_W
    o_km = o_qm + MOBA_W
    o_vm = o_km + MOBA_W
    o_gd = o_vm + MOBA_W
    o_gm = o_gd + D
    for l in range(DEPTH):
        mod = jnp.matmul(c_act, w_ada[l]) + b_ada[l]
        shift1, scale1, gate1, shift2, scale2, gate2 = jnp.split(mod[:, None, :], 6, axis=-1)
        h = _rmsnorm(x, g_mix[l]) * (1.0 + scale1) + shift1
        proj = jnp.matmul(h, w_in[l])
        qd = proj[..., o_qd:o_kd].reshape(B, S, DIFF_HEADS, 2, DIFF_HEAD_DIM)
        kd = proj[..., o_kd:o_vd].reshape(B, S, DIFF_HEADS, 2, DIFF_HEAD_DIM)
        vd = proj[..., o_vd:o_qm].reshape(B, S, DIFF_HEADS, 2 * DIFF_HEAD_DIM)
        qm = proj[..., o_qm:o_km].reshape(B, S, MOBA_HEADS, MOBA_HEAD_DIM)
        km = proj[..., o_km:o_vm].reshape(B, S, MOBA_HEADS, MOBA_HEAD_DIM)
        vm = proj[..., o_vm:o_gd].reshape(B, S, MOBA_HEADS, MOBA_HEAD_DIM)
        gd = jax.nn.sigmoid(proj[..., o_gd:o_gm])
        gm = jax.nn.sigmoid(proj[..., o_gm:])
        y_d = jnp.matmul(_diff_attention(qd, kd, vd, diff_lambda[l], diff_subln_g[l], table_diff, l), w_o_diff[l])
        y_m = jnp.matmul(_moba_attention(qm, km, vm, table_moba), w_o_moba[l])
        mixed = jnp.matmul(gd * y_d + gm * y_m, w_out[l])
        x = x + gate1 * mixed
        h2 = _rmsnorm(x, g_ffn[l]) * (1.0 + scale2) + shift2
        x = x + gate2 * _moe(h2, w_router[l], router_bias[l], w_exp_gate[l], w_exp_up[l], w_exp_down[l],
                             w_sh_gate[l], w_sh_up[l], w_sh_down[l])
    return _rmsnorm(x, g_final)
```

```python
import os
import contextlib
import numpy as np
import ml_dtypes
import concourse.bass as bass
import concourse.mybir as mybir
from concourse.bass_utils import run_bass_kernel_spmd

F32 = mybir.dt.float32
BF16 = mybir.dt.bfloat16
I32 = mybir.dt.int32
AF = mybir.ActivationFunctionType
ALU = mybir.AluOpType
AX = mybir.AxisListType

D = 2048
KC = 16
S = 8192
NTA = 64
NTO = 16
TO = 2048
E = 64
CAP = 896
JMAX = CAP // 128
NSLOT = E * CAP
BIG = 30000.0
EPS = 1e-6
NCORES = int(os.environ.get("MK_CORES", "8"))
DEBUG = os.environ.get("MK_DEBUG", "")

COMPUTE = ("pe", "act", "dve", "pool")
NDSEM = 6
_SYNC = [None]
USED_INPUTS = set()


class Sync:
    def __init__(self, nc, st):
        self.nc = nc
        self.cnt = {e: 0 for e in COMPUTE}
        self.dcnt = {}
        self.seen = {e: {} for e in ("pe", "act", "dve", "pool", "sp")}
        self.all_tokens = {}
        self.sems = {}
        keys = list(COMPUTE) + ["d_%s_%d" % (q, j) for q in ("sp", "pool", "poolw", "act") for j in range(NDSEM)]
        for k in keys:
            self.sems[k] = st.enter_context(nc.semaphore("s_" + k))


class Phase:
    def __init__(self, nc, name):
        self.nc = nc
        self.name = name
        self.sy = _SYNC[0]
        self.streams = {e: [] for e in ("pe", "act", "dve", "pool", "sp")}
        self.last_w = {}
        self.readers = {}
        self.region = None
        self.cnt_ap = None

    def begin_region(self, expert, thresh):
        self.region = {"expert": expert, "thresh": thresh, "before": dict(self.sy.all_tokens)}

    def end_region(self):
        self.region = None

    def load_cond_reg(self, expert, res):
        t = self.last_w.get(res)
        for eng in self.streams:
            w = self._waits(eng, [t] if t is not None else [])
            self.streams[eng].append((w, ("reg", expert), None, None))

    def _deps(self, reads, writes):
        deps = []
        for r in reads:
            t = self.last_w.get(r)
            if t is not None:
                deps.append(t)
        for w in writes:
            t = self.last_w.get(w)
            if t is not None:
                deps.append(t)
            deps.extend(self.readers.get(w, ()))
        return deps

    def _waits(self, eng, deps):
        out = {}
        seen = self.sy.seen[eng]
        for (k, v) in deps:
            if eng == "pe" and k == "pe":
                continue
            if seen.get(k, 0) >= v:
                continue
            if out.get(k, 0) < v:
                out[k] = v
        for k, v in out.items():
            seen[k] = v
        return list(out.items())

    def _commit(self, tok, reads, writes):
        for r in reads:
            self.readers.setdefault(r, []).append(tok)
        for w in writes:
            self.last_w[w] = tok
            self.readers[w] = []
        k, v = tok
        if self.sy.all_tokens.get(k, 0) < v:
            self.sy.all_tokens[k] = v

    def op(self, eng, fn, reads=(), writes=()):
        deps = self._deps(reads, writes)
        waits = self._waits(eng, deps)
        self.sy.cnt[eng] += 1
        tok = (eng, self.sy.cnt[eng])
        self.streams[eng].append((waits, fn, tok, self.region))
        self._commit(tok, reads, writes)
        return tok

    def dma(self, q, fn, reads=(), writes=(), cls=""):
        deps = self._deps(reads, writes)
        i = self.sy.dcnt.get(q + cls, 0)
        self.sy.dcnt[q + cls] = i + 1
        semkey = "d_%s%s_%d" % (q, cls, i % NDSEM)
        tok = (semkey, 16 * (i // NDSEM + 1))
        if i >= NDSEM:
            deps.append((semkey, 16 * (i // NDSEM)))
        waits = self._waits(q, deps)
        self.streams[q].append((waits, fn, tok, self.region))
        self._commit(tok, reads, writes)
        return tok

    def run(self):
        nc = self.nc
        sems = self.sy.sems
        toks = list(self.sy.all_tokens.items())
        for eng in self.streams:
            w = self._waits(eng, toks)
            if w:
                self.streams[eng].append((w, None, None, None))
        cnt_ap = self.cnt_ap
        with nc.Block() as block:

            def emit(engine, entry):
                waits, fn, tok, _ = entry
                for (k, v) in waits:
                    engine.wait_ge(sems[k], v)
                if fn is None:
                    return
                ins = fn(engine)
                k, v = tok
                ins.then_inc(sems[k], 1 if k in COMPUTE else 16)

            def replay(engine, stream):
                reg = None
                i = 0
                n = len(stream)
                while i < n:
                    entry = stream[i]
                    waits, fn, tok, region = entry
                    if isinstance(fn, tuple):
                        for (k, v) in waits:
                            engine.wait_ge(sems[k], v)
                        if reg is None:
                            reg = engine.alloc_register("cnt_reg")
                        engine.reg_load(reg, cnt_ap[0:1, fn[1]:fn[1] + 1])
                        i += 1
                        continue
                    if region is None:
                        emit(engine, entry)
                        i += 1
                        continue
                    groups = []
                    j = i
                    while j < n and stream[j][3] is not None and stream[j][3]["expert"] == region["expert"]:
                        r_ = stream[j][3]
                        j2 = j
                        while j2 < n and stream[j2][3] is r_:
                            j2 += 1
                        groups.append((r_, stream[j:j2]))
                        j = j2

                    def compensate(rest):
                        before = rest[0][0]["before"]
                        ext = {}
                        incs = {}
                        for (_, grp) in rest:
                            for (w_, f_, t_, _r) in grp:
                                for (k, v) in w_:
                                    v2 = min(v, before.get(k, 0))
                                    if v2 > 0 and ext.get(k, 0) < v2:
                                        ext[k] = v2
                                if t_ is not None:
                                    k, v = t_
                                    incs[k] = incs.get(k, 0) + (1 if k in COMPUTE else 16)
                        for k in incs:
                            b = before.get(k, 0)
                            if b > 0 and ext.get(k, 0) < b:
                                ext[k] = b
                        for k, v in ext.items():
                            engine.wait_ge(sems[k], v)
                        for k, v in incs.items():
                            engine.sem_inc(sems[k], v)

                    def chain(gi_):
                        if gi_ == len(groups):
                            return
                        r_, grp = groups[gi_]
                        with engine.If_lt(reg, r_["thresh"] + 1):
                            compensate(groups[gi_:])
                        with engine.Else():
                            for e_ in grp:
                                emit(engine, e_)
                            chain(gi_ + 1)
                    chain(0)
                    i = j

            @block.tensor
            def _(e):
                replay(e, self.streams["pe"])

            @block.scalar
            def _(e):
                replay(e, self.streams["act"])

            @block.vector
            def _(e):
                replay(e, self.streams["dve"])

            @block.gpsimd
            def _(e):
                replay(e, self.streams["pool"])

            @block.sync
            def _(e):
                replay(e, self.streams["sp"])


def sb(st, nc, name, shape, dt):
    return st.enter_context(nc.sbuf_tensor(name, shape, dt))


def ps(st, nc, name, shape, dt):
    return st.enter_context(nc.psum_tensor(name, shape, dt))


def build_program(stop_after=99):
    nc = bass.Bass("TRN2", target_bir_lowering=False)
    USED_INPUTS.clear()

    def din(name, shape, dt=F32):
        A[name] = nc.dram_tensor(name, list(shape), dt, kind="ExternalInput").ap()

    def dscr(name, shape, dt):
        kind = "ExternalOutput" if name in DEBUG.split(",") else "Internal"
        A[name] = nc.dram_tensor(name, list(shape), dt, kind=kind).ap()

    in_specs = {
        "x_all": ([S, D], F32),
        "x_own": ([TO, D], F32),
        "cT": ([128, KC], F32),
        "w_ada": ([D, 6 * D], F32),
        "b_ada_b": ([128, 6 * D], F32),
        "g_mix_b": ([128, D], F32),
        "g_ffn_b": ([128, D], F32),
        "g_final_b": ([128, D], F32),
        "w_in": ([D, 10240], F32),
        "lam_b": ([128, 256], F32),
        "subln_b": ([128, 128], F32),
        "BT": ([128, 16 * 5 * 128], BF16),
        "b31": ([128, 16], F32),
        "pen": ([128, NTO * 32], F32),
        "past": ([128, NTO * 32], F32),
        "own": ([128, NTO * 32], F32),
        "w_o_diff": ([1024, D], F32),
        "w_o_moba": ([1024, D], F32),
        "w_out": ([D, D], F32),
        "w_router": ([D, E], F32),
        "rbias_b": ([128, E], F32),
        "w_exp_gate": ([E, D, 512], F32),
        "w_exp_up": ([E, D, 512], F32),
        "w_exp_down": ([E, 512, D], F32),
        "w_sh_gate": ([D, 512], F32),
        "w_sh_up": ([D, 512], F32),
        "w_sh_down": ([512, D], F32),
        "ident_bf": ([128, 128], BF16),
        "ident_f": ([128, 128], F32),
        "ltri": ([128, 128], BF16),
        "selc": ([32, 32 * 128], BF16),
        "ecap": ([128, E], F32),
        "dumpidx": ([128, 1], F32),
        "tokid": ([128, NTO], I32),
    }

    class LazyA(dict):
        def __missing__(self, name):
            shape, dt = in_specs[name]
            ap = nc.dram_tensor(name, list(shape), dt, kind="ExternalInput").ap()
            self[name] = ap
            USED_INPUTS.add(name)
            return ap
    A = LazyA()
    A["out"] = nc.dram_tensor("out", [TO, D], F32, kind="ExternalOutput").ap()

    dscr("modb", [128, 6 * D], F32)
    dscr("hT_all", [NTA, 128, KC * 128], BF16)
    dscr("hT_own", [NTO, 128, KC * 128], BF16)
    dscr("QTd", [8, 128, TO], BF16); dscr("KTd", [8, 128, S], BF16); dscr("Vd", [S, 1024], BF16)
    dscr("QTm", [8, 128, TO], BF16); dscr("KTm", [8, 128, S], BF16); dscr("Vm", [S, 1024], BF16)
    dscr("KMT", [8, 128, 32], F32)
    dscr("GT", [32, 128, TO], BF16)
    dscr("o_d", [TO, D], BF16)
    dscr("zT", [4, 128, KC * 512], BF16)
    dscr("x1", [TO, D], F32)
    dscr("h2", [TO, D], BF16)
    dscr("h2T", [NTO, 128, KC * 128], BF16)
    dscr("rowtok", [NSLOT + 128, 8], I32)
    dscr("Ybuf", [NSLOT + 128, D], BF16)
    dscr("Ysh", [TO, D], BF16)
    dscr("route", [128, NTO * 16], F32)
    dscr("cnt_d", [1, E], I32)

    gst = contextlib.ExitStack()
    gst.__enter__()
    _SYNC[0] = Sync(nc, gst)
    if stop_after >= 1:
        phase_mod(nc, A)
    if stop_after >= 2:
        phase_h(nc, A)
    if stop_after >= 3:
        phase_proj(nc, A)
    if stop_after >= 4:
        phase_attn(nc, A)
    if stop_after >= 5:
        phase_oproj(nc, A)
    if stop_after >= 6:
        phase_wout(nc, A)
    if stop_after >= 7:
        phase_route(nc, A)
    if stop_after >= 8:
        phase_experts(nc, A)
    if stop_after >= 9:
        phase_final(nc, A)
    gst.__exit__(None, None, None)
    return nc


def phase_mod(nc, A):
    with contextlib.ExitStack() as st:
        P = Phase(nc, "p1")
        cT = sb(st, nc, "p1_cT", [128, KC], F32)
        cact = sb(st, nc, "p1_cact", [128, KC], F32)
        ones = sb(st, nc, "p1_ones", [128, 128], F32)
        L = sb(st, nc, "p1_L", [128, KC, 128], BF16)
        wt = [sb(st, nc, "p1_w%d" % i, [128, KC, 512], BF16) for i in range(2)]
        bt = [sb(st, nc, "p1_b%d" % i, [128, 512], F32) for i in range(2)]
        gt = [sb(st, nc, "p1_g%d" % i, [128, 512], F32) for i in range(2)]
        ot = [sb(st, nc, "p1_o%d" % i, [128, 512], F32) for i in range(2)]
        pp = [ps(st, nc, "p1_ps%d" % i, [128, 512], F32) for i in range(2)]
        P.dma("sp", lambda e: e.dma_start(out=cT[:], in_=A["cT"][:, :]), writes=["cT"])
        P.op("dve", lambda e: e.memset(ones[:], 1.0), writes=["ones"])
        P.op("act", lambda e: e.activation(out=cact[:], in_=cT[:], func=AF.Silu), reads=["cT"], writes=["cact"])
        for j in range(KC):
            P.op("dve", lambda e, j=j: e.tensor_scalar(out=L[:, j, :], in0=ones[:], scalar1=cact[:, j:j + 1], scalar2=None, op0=ALU.mult),
                 reads=["cact", "ones"], writes=["L"])
        w_view = A["w_ada"].rearrange("(k p) n -> p k n", p=128)
        for n in range(24):
            i = n % 2
            P.dma("pool", lambda e, n=n, i=i: e.dma_start(out=wt[i][:], in_=w_view[:, :, n * 512:(n + 1) * 512]), writes=["w%d" % i])
            P.dma("sp", lambda e, n=n, i=i: e.dma_start(out=bt[i][:], in_=A["b_ada_b"][:, n * 512:(n + 1) * 512]), writes=["b%d" % i])
            kind = n // 4
            if kind in (1, 4):
                gsrc = A["g_mix_b"] if kind == 1 else A["g_ffn_b"]
                c0 = (n % 4) * 512
                P.dma("sp", lambda e, i=i, gsrc=gsrc, c0=c0: e.dma_start(out=gt[i][:], in_=gsrc[:, c0:c0 + 512]), writes=["g%d" % i])

            def mm(e, i=i):
                ins = None
                for k in range(KC):
                    ins = e.matmul(pp[i][:], lhsT=L[:, k, :], rhs=wt[i][:, k, :], start=(k == 0), stop=(k == KC - 1))
                return ins
            P.op("pe", mm, reads=["L", "w%d" % i], writes=["ps%d" % i])
            if kind in (1, 4):
                P.op("dve", lambda e, i=i: e.tensor_tensor(out=ot[i][:], in0=pp[i][:], in1=bt[i][:], op=ALU.add),
                     reads=["ps%d" % i, "b%d" % i], writes=["o%d" % i])
                P.op("dve", lambda e, i=i: e.scalar_tensor_tensor(out=ot[i][:], in0=ot[i][:], scalar=1.0, in1=gt[i][:], op0=ALU.add, op1=ALU.mult),
                     reads=["o%d" % i, "g%d" % i], writes=["o%d" % i])
            else:
                P.op("dve", lambda e, i=i: e.tensor_tensor(out=ot[i][:], in0=pp[i][:], in1=bt[i][:], op=ALU.add),
                     reads=["ps%d" % i, "b%d" % i], writes=["o%d" % i])
            P.dma("sp", lambda e, n=n, i=i: e.dma_start(out=A["modb"][:, n * 512:(n + 1) * 512], in_=ot[i][:]), reads=["o%d" % i], writes=["modb"])
        P.run()


MOD_SHIFT1, MOD_A1, MOD_G1, MOD_SHIFT2, MOD_A2, MOD_G2 = [i * D for i in range(6)]


def emit_norm_mod_T(P, nc, xt, sq, ss, rstd, hf, hb, pT, hT, Amod, Smod, ident, tag, xres, extra_reads=()):
    P.op("act", lambda e: e.activation(out=sq[:], in_=xt[:], func=AF.Square, accum_out=ss[:]),
         reads=[xres], writes=["sq" + tag, "ss" + tag])
    P.op("dve", lambda e: e.tensor_scalar(out=rstd[:], in0=ss[:], scalar1=1.0 / D, scalar2=EPS, op0=ALU.mult, op1=ALU.add),
         reads=["ss" + tag], writes=["rstd" + tag])
    P.op("act", lambda e: e.activation(out=rstd[:], in_=rstd[:], func=AF.Sqrt), reads=["rstd" + tag], writes=["rstd" + tag])
    P.op("dve", lambda e: e.reciprocal(out=rstd[:], in_=rstd[:]), reads=["rstd" + tag], writes=["rstd" + tag])
    P.op("dve", lambda e: e.scalar_tensor_tensor(out=hf[:], in0=xt[:], scalar=rstd[:, 0:1], in1=Amod[:], op0=ALU.mult, op1=ALU.mult),
         reads=[xres, "rstd" + tag, "Amod"] + list(extra_reads), writes=["hf" + tag])
    P.op("pool", lambda e: e.tensor_tensor(out=hb[:], in0=hf[:], in1=Smod[:], op=ALU.add),
         reads=["hf" + tag, "Smod"], writes=["hb" + tag])

    def tr(e):
        ins = None
        for k in range(KC):
            ins = e.transpose(out=pT[:, k, :], in_=hb[:, k * 128:(k + 1) * 128], identity=ident[:])
        return ins
    P.op("pe", tr, reads=["hb" + tag, "ident"], writes=["pT" + tag])
    P.op("act", lambda e: e.activation(out=hT[:], in_=pT[:], func=AF.Copy), reads=["pT" + tag], writes=["hT" + tag])


def phase_h(nc, A):
    with contextlib.ExitStack() as st:
        P = Phase(nc, "p2")
        ident = sb(st, nc, "p2_ident", [128, 128], BF16)
        Amod = sb(st, nc, "p2_A", [128, D], F32)
        Smod = sb(st, nc, "p2_S", [128, D], F32)
        xt = [sb(st, nc, "p2_x%d" % i, [128, D], F32) for i in range(3)]
        sq = [sb(st, nc, "p2_sq%d" % i, [128, D], BF16) for i in range(3)]
        ss = [sb(st, nc, "p2_ss%d" % i, [128, 1], F32) for i in range(3)]
        rstd = [sb(st, nc, "p2_rs%d" % i, [128, 1], F32) for i in range(3)]
        hf = [sb(st, nc, "p2_hf%d" % i, [128, D], F32) for i in range(3)]
        hb = [sb(st, nc, "p2_hb%d" % i, [128, D], BF16) for i in range(3)]
        hT = [sb(st, nc, "p2_hT%d" % i, [128, KC, 128], BF16) for i in range(3)]
        pT = [ps(st, nc, "p2_pT%d" % i, [128, KC, 128], BF16) for i in range(3)]
        P.dma("sp", lambda e: e.dma_start(out=ident[:], in_=A["ident_bf"][:, :]), writes=["ident"])
        P.dma("sp", lambda e: e.dma_start(out=Amod[:], in_=A["modb"][:, MOD_A1:MOD_A1 + D]), writes=["Amod"])
        P.dma("sp", lambda e: e.dma_start(out=Smod[:], in_=A["modb"][:, MOD_SHIFT1:MOD_SHIFT1 + D]), writes=["Smod"])
        for t in range(NTA + NTO):
            i = t % 3
            tag = str(i)
            if t < NTA:
                src = A["x_all"][t * 128:(t + 1) * 128, :]
                dst = A["hT_all"][t]
            else:
                src = A["x_own"][(t - NTA) * 128:(t - NTA + 1) * 128, :]
                dst = A["hT_own"][t - NTA]
            P.dma("sp", lambda e, i=i, src=src: e.dma_start(out=xt[i][:], in_=src), writes=["x" + tag])
            emit_norm_mod_T(P, nc, xt[i], sq[i], ss[i], rstd[i], hf[i], hb[i], pT[i], hT[i], Amod, Smod, ident, tag, "x" + tag)
            P.dma("pool", lambda e, i=i, dst=dst: e.dma_start(out=dst, in_=hT[i][:].rearrange("p k t -> p (k t)")),
                  reads=["hT" + tag], writes=["hTd"])
        P.run()


def phase_proj(nc, A):
    with contextlib.ExitStack() as st:
        P = Phase(nc, "p3")
        W = [sb(st, nc, "p3_W%d" % i, [128, KC, 1024], BF16) for i in range(2)]
        H = [sb(st, nc, "p3_H%d" % i, [128, 4, KC * 128], BF16) for i in range(2)]
        O = [sb(st, nc, "p3_O%d" % i, [128, 8, 512], BF16) for i in range(2)]
        KM = sb(st, nc, "p3_KM", [128, 8, 32], F32)
        pp = [ps(st, nc, "p3_ps%d" % i, [128, 512], F32) for i in range(6)]
        w_view = A["w_in"].rearrange("(k p) n -> p k n", p=128)
        passes = [
            ("qd", 0, "own", "fm"), ("kd", 1024, "all", "fm"), ("vd", 2048, "all", "tm"),
            ("qm", 3072, "own", "fm"), ("km", 4096, "all", "fm"), ("vm", 5120, "all", "tm"),
            ("g0", 6144, "own", "fm"), ("g1", 7168, "own", "fm"), ("g2", 8192, "own", "fm"), ("g3", 9216, "own", "fm"),
        ]
        gcount = 0
        pcount = 0
        if os.environ.get("MK_P3"):
            passes = [passes[int(i)] for i in os.environ["MK_P3"].split(",")]
        for pi, (pname, c0, tset, kind) in enumerate(passes):
            wi = pi % 2
            wres = "W%d" % wi

            def loadW(pj):
                wj = pj % 2
                cj = passes[pj][1]
                for q4 in range(4):
                    P.dma("pool", lambda e, wj=wj, cj=cj, q4=q4: e.dma_start(out=W[wj][:, q4 * 4:(q4 + 1) * 4, :], in_=w_view[:, q4 * 4:(q4 + 1) * 4, cj:cj + 1024]),
                          writes=["W%d" % wj], cls="w")
            if pi == 0:
                loadW(0)
            if pi + 1 < len(passes):
                loadW(pi + 1)
            ngroups = 16 if tset == "all" else 4
            src = A["hT_all"] if tset == "all" else A["hT_own"]
            for g in range(ngroups):
                hi = gcount % 2
                gcount += 1
                hres = "H%d" % hi
                P.dma("sp", lambda e, hi=hi, src=src, g=g: e.dma_start(out=H[hi][:], in_=src[g * 4:(g + 1) * 4].rearrange("t p f -> p t f")),
                      writes=[hres])
                oi = g % 2
                ores = "O%d" % oi
                if kind == "fm":
                    for ch in range(8):
                        pidx = pcount % 6
                        pcount += 1
                        pres = "ps%d" % pidx

                        def mm(e, wi=wi, hi=hi, ch=ch, pidx=pidx):
                            ins = None
                            for k in range(KC):
                                ins = e.matmul(pp[pidx][:].rearrange("p (t q) -> p t q", t=4), lhsT=W[wi][:, k, ch * 128:(ch + 1) * 128],
                                               rhs=H[hi][:, :, k * 128:(k + 1) * 128], start=(k == 0), stop=(k == KC - 1))
                            return ins
                        P.op("pe", mm, reads=[wres, hres], writes=[pres])
                        if pname in ("qd", "qm"):
                            sc = 0.125 if pname == "qd" else float(128 ** -0.5)
                            P.op("act", lambda e, oi=oi, ch=ch, pidx=pidx, sc=sc: e.activation(out=O[oi][:, ch, :], in_=pp[pidx][:], func=AF.Copy, scale=sc),
                                 reads=[pres], writes=[ores])
                        elif pname.startswith("g"):
                            P.op("act", lambda e, oi=oi, ch=ch, pidx=pidx: e.activation(out=O[oi][:, ch, :], in_=pp[pidx][:], func=AF.Sigmoid),
                                 reads=[pres], writes=[ores])
                        elif pname == "km":
                            for bb in range(2):
                                P.op("act", lambda e, oi=oi, ch=ch, pidx=pidx, g=g, bb=bb: e.activation(out=O[oi][:, ch, bb * 256:(bb + 1) * 256], in_=pp[pidx][:, bb * 256:(bb + 1) * 256],
                                                                                                    func=AF.Copy, accum_out=KM[:, ch, 2 * g + bb:2 * g + bb + 1]),
                                     reads=[pres], writes=[ores, "KM"])
                        else:
                            eng = "dve" if ch % 2 == 0 else "act"
                            if eng == "dve":
                                P.op("dve", lambda e, oi=oi, ch=ch, pidx=pidx: e.tensor_copy(out=O[oi][:, ch, :], in_=pp[pidx][:]), reads=[pres], writes=[ores])
                            else:
                                P.op("act", lambda e, oi=oi, ch=ch, pidx=pidx: e.activation(out=O[oi][:, ch, :], in_=pp[pidx][:], func=AF.Copy), reads=[pres], writes=[ores])
                    if pname == "qd":
                        dst = A["QTd"][:, :, g * 512:(g + 1) * 512]
                    elif pname == "kd":
                        dst = A["KTd"][:, :, g * 512:(g + 1) * 512]
                    elif pname == "qm":
                        dst = A["QTm"][:, :, g * 512:(g + 1) * 512]
                    elif pname == "km":
                        dst = A["KTm"][:, :, g * 512:(g + 1) * 512]
                    else:
                        gi = int(pname[1])
                        dst = A["GT"][gi * 8:(gi + 1) * 8, :, g * 512:(g + 1) * 512]
                    P.dma("pool", lambda e, oi=oi, dst=dst: e.dma_start(out=dst.rearrange("c p t -> p c t"), in_=O[oi][:]), reads=[ores], writes=["dram_" + pname])
                else:
                    Ov = O[oi][:].rearrange("p c t -> p (c t)").rearrange("p (t n) -> p t n", t=4)
                    for tt in range(4):
                        for half in range(2):
                            pidx = pcount % 6
                            pcount += 1
                            pres = "ps%d" % pidx

                            def mm(e, wi=wi, hi=hi, tt=tt, half=half, pidx=pidx):
                                ins = None
                                for k in range(KC):
                                    ins = e.matmul(pp[pidx][:], lhsT=H[hi][:, tt, k * 128:(k + 1) * 128], rhs=W[wi][:, k, half * 512:(half + 1) * 512],
                                                   start=(k == 0), stop=(k == KC - 1))
                                return ins
                            P.op("pe", mm, reads=[wres, hres], writes=[pres])
                            if (tt * 2 + half) % 2 == 0:
                                P.op("dve", lambda e, Ov=Ov, tt=tt, half=half, pidx=pidx: e.tensor_copy(out=Ov[:, tt, half * 512:(half + 1) * 512], in_=pp[pidx][:]),
                                     reads=[pres], writes=[ores])
                            else:
                                P.op("act", lambda e, Ov=Ov, tt=tt, half=half, pidx=pidx: e.activation(out=Ov[:, tt, half * 512:(half + 1) * 512], in_=pp[pidx][:], func=AF.Copy),
                                     reads=[pres], writes=[ores])
                    dstT = A["Vd"] if pname == "vd" else A["Vm"]
                    dst = dstT[g * 512:(g + 1) * 512, :].rearrange("(t p) n -> p t n", p=128)
                    P.dma("pool", lambda e, Ov=Ov, dst=dst: e.dma_start(out=dst, in_=Ov), reads=[ores], writes=["dram_" + pname])
            if pname == "km":
                P.op("dve", lambda e: e.tensor_scalar(out=KM[:], in0=KM[:], scalar1=1.0 / 256.0, scalar2=None, op0=ALU.mult), reads=["KM"], writes=["KM"])
                P.dma("sp", lambda e: e.dma_start(out=A["KMT"].rearrange("h p n -> p h n"), in_=KM[:]), reads=["KM"], writes=["dram_KMT"])
        P.run()


def phase_attn(nc, A):
    with contextlib.ExitStack() as st:
        P = Phase(nc, "p4")
        ident = sb(st, nc, "p4_ident", [128, 128], BF16)
        identf = sb(st, nc, "p4_identf", [128, 128], F32)
        BT = sb(st, nc, "p4_BT", [128, 16, 5, 128], BF16)
        b31 = sb(st, nc, "p4_b31", [128, 16], F32)
        selc = sb(st, nc, "p4_sel", [32, 32, 128], BF16)
        pen = sb(st, nc, "p4_pen", [128, NTO, 32], F32)
        past = sb(st, nc, "p4_past", [128, NTO, 32], F32)
        own = sb(st, nc, "p4_own", [128, NTO, 32], F32)
        lamb = sb(st, nc, "p4_lamb", [128, 256], F32)
        lamt = sb(st, nc, "p4_lamt", [128, 128], F32)
        lam2 = sb(st, nc, "p4_lam2", [128, 2], F32)
        nlam = sb(st, nc, "p4_nlam", [128, 1], F32)
        subg = sb(st, nc, "p4_subg", [128, 128], F32)
        KTb = [sb(st, nc, "p4_KT%d" % i, [128, 2 * S], BF16) for i in range(2)]
        QTb = [sb(st, nc, "p4_QT%d" % i, [128, 2 * TO], BF16) for i in range(2)]
        Vb = [sb(st, nc, "p4_V%d" % i, [128, NTA, 130], BF16) for i in range(2)]
        KMb = [sb(st, nc, "p4_KM%d" % i, [128, 32], F32) for i in range(2)]
        QTf = [sb(st, nc, "p4_QTf%d" % i, [128, 128], F32) for i in range(2)]
        Pb = [sb(st, nc, "p4_P%d" % i, [128, 512], BF16) for i in range(6)]
        oh = [sb(st, nc, "p4_oh%d" % i, [128, NTO, 128], BF16) for i in range(2)]
        gate = [sb(st, nc, "p4_gate%d" % i, [128, 32], F32) for i in range(2)]
        top8 = [sb(st, nc, "p4_top8%d" % i, [128, 8], F32) for i in range(2)]
        mb = [sb(st, nc, "p4_mb%d" % i, [128, 32], F32) for i in range(2)]
        mbT = [sb(st, nc, "p4_mbT%d" % i, [32, 128], BF16) for i in range(2)]
        rl = [sb(st, nc, "p4_rl%d" % i, [128, 2], F32) for i in range(2)]
        o1 = [sb(st, nc, "p4_o1%d" % i, [128, 128], F32) for i in range(2)]
        o2 = [sb(st, nc, "p4_o2%d" % i, [128, 128], F32) for i in range(2)]
        junk = [sb(st, nc, "p4_junk%d" % i, [128, 128], F32) for i in range(2)]
        ssq = [sb(st, nc, "p4_ssq%d" % i, [128, 1], F32) for i in range(2)]
        pS = [ps(st, nc, "p4_pS%d" % i, [128, 512], F32) for i in range(4)]
        pO = [ps(st, nc, "p4_pO%d" % i, [128, 512], F32) for i in range(2)]
        pOb = [ps(st, nc, "p4_pOb%d" % i, [128, 512], F32) for i in range(2)]
        pG = pOb[0][:, 0:32]
        pM = pOb[1][0:32, 0:128]

        P.dma("sp", lambda e: e.dma_start(out=ident[:], in_=A["ident_bf"][:, :]), writes=["ident"])
        P.dma("sp", lambda e: e.dma_start(out=identf[:], in_=A["ident_f"][:, :]), writes=["identf"])
        P.dma("sp", lambda e: e.dma_start(out=BT[:].rearrange("p h j q -> p (h j q)"), in_=A["BT"][:, :]), writes=["BT"])
        P.dma("sp", lambda e: e.dma_start(out=b31[:], in_=A["b31"][:, :]), writes=["b31"])
        P.dma("sp", lambda e: e.dma_start(out=selc[:].rearrange("p n k -> p (n k)"), in_=A["selc"][:, :]), writes=["selc"])
        P.dma("sp", lambda e: e.dma_start(out=pen[:].rearrange("p m n -> p (m n)"), in_=A["pen"][:, :]), writes=["pen"])
        P.dma("sp", lambda e: e.dma_start(out=past[:].rearrange("p m n -> p (m n)"), in_=A["past"][:, :]), writes=["past"])
        P.dma("sp", lambda e: e.dma_start(out=own[:].rearrange("p m n -> p (m n)"), in_=A["own"][:, :]), writes=["own"])
        P.dma("sp", lambda e: e.dma_start(out=lamb[:], in_=A["lam_b"][:, :]), writes=["lamb"])
        P.dma("sp", lambda e: e.dma_start(out=subg[:], in_=A["subln_b"][:, :]), writes=["subg"])
        lv = lamb[:].rearrange("p (a d) -> p a d", a=4)
        P.op("dve", lambda e: e.tensor_tensor(out=lamt[:, 0:64], in0=lv[:, 0, :], in1=lv[:, 1, :], op=ALU.mult), reads=["lamb"], writes=["lamt"])
        P.op("dve", lambda e: e.tensor_tensor(out=lamt[:, 64:128], in0=lv[:, 2, :], in1=lv[:, 3, :], op=ALU.mult), reads=["lamb", "lamt"], writes=["lamt"])
        for a_ in range(2):
            P.op("act", lambda e, a_=a_: e.activation(out=lamb[:, a_ * 64:(a_ + 1) * 64], in_=lamt[:, a_ * 64:(a_ + 1) * 64], func=AF.Copy, accum_out=lam2[:, a_:a_ + 1]),
                 reads=["lamt"], writes=["lam2", "lamb"])
        P.op("act", lambda e: e.activation(out=lam2[:], in_=lam2[:], func=AF.Exp), reads=["lam2"], writes=["lam2"])
        P.op("dve", lambda e: e.tensor_tensor(out=nlam[:], in0=lam2[:, 1:2], in1=lam2[:, 0:1], op=ALU.subtract), reads=["lam2"], writes=["nlam"])
        P.op("dve", lambda e: e.tensor_scalar(out=nlam[:], in0=nlam[:], scalar1=-0.2, scalar2=None, op0=ALU.add), reads=["nlam"], writes=["nlam"])
        P.op("dve", lambda e: e.tensor_scalar(out=subg[:], in0=subg[:], scalar1=0.8, scalar2=None, op0=ALU.mult), reads=["subg"], writes=["subg"])
        for i in range(2):
            P.op("pool", lambda e, i=i: e.memset(Vb[i][:, :, 128:130], 1.0), writes=["V%d" % i])

        scount = 0
        pcount = 0
        qcount = 0
        def load_head(h):
            hb_ = h % 2
            diff = h < 8
            hh = h if diff else h - 8
            kres, qres, vres, kmres = "KT%d" % hb_, "QT%d" % hb_, "V%d" % hb_, "KM%d" % hb_
            if diff:
                for mp in range(2):
                    P.dma("sp", lambda e, hb_=hb_, hh=hh, mp=mp: e.dma_start(out=KTb[hb_][0:64, mp * S:(mp + 1) * S], in_=A["KTd"][hh, mp * 64:(mp + 1) * 64, :]), writes=[kres])
                    P.dma("sp", lambda e, hb_=hb_, hh=hh, mp=mp: e.dma_start(out=QTb[hb_][0:64, mp * TO:(mp + 1) * TO], in_=A["QTd"][hh, mp * 64:(mp + 1) * 64, :]), writes=[qres])
                vsrc = A["Vd"]
            else:
                P.dma("sp", lambda e, hb_=hb_, hh=hh: e.dma_start(out=KTb[hb_][:, 0:S], in_=A["KTm"][hh]), writes=[kres])
                P.dma("sp", lambda e, hb_=hb_, hh=hh: e.dma_start(out=QTb[hb_][:, 0:TO], in_=A["QTm"][hh]), writes=[qres])
                P.dma("sp", lambda e, hb_=hb_, hh=hh: e.dma_start(out=KMb[hb_][:], in_=A["KMT"][hh]), writes=[kmres])
                vsrc = A["Vm"]
            P.dma("sp", lambda e, hb_=hb_, hh=hh, vsrc=vsrc: e.dma_start(out=Vb[hb_][:, :, 0:128], in_=vsrc[:, hh * 128:(hh + 1) * 128].rearrange("(t p) d -> p t d", p=128)),
                  writes=[vres])

        load_head(0)
        for h in range(16):
            hb_ = h % 2
            diff = h < 8
            hh = h if diff else h - 8
            kres, qres, vres, kmres = "KT%d" % hb_, "QT%d" % hb_, "V%d" % hb_, "KM%d" % hb_
            if h + 1 < 16:
                load_head(h + 1)
            nmaps = 2 if diff else 1
            ohres = "oh%d" % hb_

            def emit_pre(m, hb_=hb_, kmres=kmres, qres=qres, diff=diff):
                qb = m % 2
                if diff:
                    return
                gres, tres, mres, mtres = "gate%d" % qb, "top8%d" % qb, "mb%d" % qb, "mbT%d" % qb
                P.op("act", lambda e, qb=qb, hb_=hb_, m=m: e.activation(out=QTf[qb][:], in_=QTb[hb_][:, m * 128:(m + 1) * 128], func=AF.Copy), reads=[qres], writes=["QTf%d" % qb])
                P.op("pe", lambda e, qb=qb, hb_=hb_: e.matmul(pG, lhsT=QTf[qb][:], rhs=KMb[hb_][:], start=True, stop=True), reads=["QTf%d" % qb, kmres], writes=["pOb0"])
                P.op("dve", lambda e, qb=qb, m=m: e.tensor_tensor(out=gate[qb][:], in0=pG, in1=pen[:, m, :], op=ALU.add), reads=["pOb0", "pen"], writes=[gres])
                P.op("dve", lambda e, qb=qb: e.max(out=top8[qb][:], in_=gate[qb][:]), reads=[gres], writes=[tres])
                P.op("dve", lambda e, qb=qb: e.tensor_scalar(out=mb[qb][:], in0=gate[qb][:], scalar1=top8[qb][:, 2:3], scalar2=None, op0=ALU.is_ge), reads=[gres, tres], writes=[mres])
                P.op("dve", lambda e, qb=qb, m=m: e.tensor_tensor(out=mb[qb][:], in0=mb[qb][:], in1=past[:, m, :], op=ALU.mult), reads=[mres, "past"], writes=[mres])
                P.op("dve", lambda e, qb=qb, m=m: e.tensor_tensor(out=mb[qb][:], in0=mb[qb][:], in1=own[:, m, :], op=ALU.add), reads=[mres, "own"], writes=[mres])
                P.op("dve", lambda e, qb=qb: e.tensor_scalar(out=mb[qb][:], in0=mb[qb][:], scalar1=-1.0, scalar2=BIG, op0=ALU.add, op1=ALU.mult), reads=[mres], writes=[mres])
                P.op("pe", lambda e, qb=qb: e.transpose(out=pM, in_=mb[qb][:], identity=identf[:]), reads=[mres, "identf"], writes=["pOb1"])
                P.op("dve", lambda e, qb=qb: e.tensor_copy(out=mbT[qb][:], in_=pM), reads=["pOb1"], writes=[mtres])

            def emit_qk(m, g, hb_=hb_, h=h, diff=diff, nmaps=nmaps, kres=kres, qres=qres):
                nonlocal scount
                qb = m % 2
                near_last = (g == m)
                prev_grp = (g == m - 1)
                sidx = []
                for mp in range(nmaps):
                    si = scount % 4
                    scount += 1
                    sidx.append(si)
                    sres = "pS%d" % si

                    def mmqk(e, si=si, mp=mp, g=g, m=m, hb_=hb_, h=h, diff=diff, near_last=near_last, prev_grp=prev_grp, qb=qb):
                        ins = None
                        for j4 in range(4):
                            kj = 4 * g + j4
                            if diff:
                                lhs = KTb[hb_][0:64, mp * S + kj * 128: mp * S + (kj + 1) * 128]
                                rhs = QTb[hb_][0:64, mp * TO + m * 128: mp * TO + (m + 1) * 128]
                            else:
                                lhs = KTb[hb_][:, kj * 128:(kj + 1) * 128]
                                rhs = QTb[hb_][:, m * 128:(m + 1) * 128]
                            bt_j = None
                            if near_last:
                                bt_j = j4 + 1
                            elif prev_grp and j4 == 3:
                                bt_j = 0
                            last = (bt_j is None) and diff
                            out = pS[si][:, j4 * 128:(j4 + 1) * 128]
                            ins = e.matmul(out, lhsT=lhs, rhs=rhs, start=True, stop=last)
                            if not diff:
                                ins = e.matmul(out, lhsT=selc[:, kj // 2, :], rhs=mbT[qb][:], start=False, stop=(bt_j is None))
                            if bt_j is not None:
                                ins = e.matmul(out, lhsT=ident[:], rhs=BT[:, h, bt_j, :], start=False, stop=True)
                        return ins
                    rd = [kres, qres, "ident", "BT"]
                    if not diff:
                        rd += ["selc", "mbT%d" % qb]
                    P.op("pe", mmqk, reads=rd, writes=[sres])
                return sidx

            def emit_exp_pv(m, g, sidx, hb_=hb_, h=h, diff=diff, nmaps=nmaps, vres=vres):
                nonlocal pcount
                qb = m % 2
                pOres = "pO%d" % qb
                pObres = "pOb%d" % qb
                near_last = (g == m)
                prev_grp = (g == m - 1)
                pidx = []
                for mp in range(nmaps):
                    si = sidx[mp]
                    pi = pcount % 6
                    pcount += 1
                    pidx.append(pi)
                    sres, pres = "pS%d" % si, "P%d" % pi
                    if near_last:
                        P.op("act", lambda e, pi=pi, si=si: e.activation(out=Pb[pi][:], in_=pS[si][:], func=AF.Exp), reads=[sres], writes=[pres])
                    elif prev_grp:
                        P.op("act", lambda e, pi=pi, si=si, h=h: e.activation(out=Pb[pi][:, 0:384], in_=pS[si][:, 0:384], func=AF.Exp, bias=b31[:, h:h + 1]), reads=[sres, "b31"], writes=[pres])
                        P.op("act", lambda e, pi=pi, si=si: e.activation(out=Pb[pi][:, 384:512], in_=pS[si][:, 384:512], func=AF.Exp), reads=[sres], writes=[pres])
                    else:
                        P.op("act", lambda e, pi=pi, si=si, h=h: e.activation(out=Pb[pi][:], in_=pS[si][:], func=AF.Exp, bias=b31[:, h:h + 1]), reads=[sres, "b31"], writes=[pres])
                for mp in range(nmaps):
                    pi = pidx[mp]

                    def mmpv(e, pi=pi, mp=mp, g=g, qb=qb, hb_=hb_, m=m):
                        ins = None
                        for j4 in range(4):
                            kj = 4 * g + j4
                            ins = e.matmul((pO if mp == 0 else pOb)[qb][:, 0:130], lhsT=Pb[pi][:, j4 * 128:(j4 + 1) * 128], rhs=Vb[hb_][:, kj, :],
                                           start=(kj == 0), stop=(kj == 4 * m + 3))
                        return ins
                    P.op("pe", mmpv, reads=["P%d" % pi, vres], writes=[pOres if mp == 0 else pObres])

            def emit_epi(m, hb_=hb_, diff=diff, ohres=ohres):
                qb = m % 2
                pOres = "pO%d" % qb
                pObres = "pOb%d" % qb
                rres, o1res, o2res = "rl%d" % qb, "o1%d" % qb, "o2%d" % qb
                if diff:
                    P.op("dve", lambda e, qb=qb: e.reciprocal(out=rl[qb][:, 0:1], in_=pO[qb][:, 128:129]), reads=[pOres], writes=[rres])
                    P.op("dve", lambda e, qb=qb: e.reciprocal(out=rl[qb][:, 1:2], in_=pOb[qb][:, 128:129]), reads=[pObres, rres], writes=[rres])
                    P.op("dve", lambda e, qb=qb: e.tensor_scalar(out=o1[qb][:], in0=pO[qb][:, 0:128], scalar1=rl[qb][:, 0:1], scalar2=None, op0=ALU.mult), reads=[pOres, rres], writes=[o1res])
                    P.op("dve", lambda e, qb=qb: e.tensor_scalar(out=o2[qb][:], in0=pOb[qb][:, 0:128], scalar1=rl[qb][:, 1:2], scalar2=nlam[:, 0:1], op0=ALU.mult, op1=ALU.mult),
                         reads=[pObres, rres, "nlam"], writes=[o2res])
                    P.op("dve", lambda e, qb=qb: e.tensor_tensor(out=o1[qb][:], in0=o1[qb][:], in1=o2[qb][:], op=ALU.add), reads=[o1res, o2res], writes=[o1res])
                    P.op("act", lambda e, qb=qb: e.activation(out=junk[qb][:], in_=o1[qb][:], func=AF.Square, accum_out=ssq[qb][:]), reads=[o1res], writes=["junk%d" % qb, "ssq%d" % qb])
                    P.op("dve", lambda e, qb=qb: e.tensor_scalar(out=ssq[qb][:], in0=ssq[qb][:], scalar1=1.0 / 128.0, scalar2=EPS, op0=ALU.mult, op1=ALU.add), reads=["ssq%d" % qb], writes=["ssq%d" % qb])
                    P.op("act", lambda e, qb=qb: e.activation(out=ssq[qb][:], in_=ssq[qb][:], func=AF.Sqrt), reads=["ssq%d" % qb], writes=["ssq%d" % qb])
                    P.op("dve", lambda e, qb=qb: e.reciprocal(out=ssq[qb][:], in_=ssq[qb][:]), reads=["ssq%d" % qb], writes=["ssq%d" % qb])
                    P.op("dve", lambda e, qb=qb, hb_=hb_, m=m: e.scalar_tensor_tensor(out=oh[hb_][:, m, :], in0=o1[qb][:], scalar=ssq[qb][:, 0:1], in1=subg[:], op0=ALU.mult, op1=ALU.mult),
                         reads=[o1res, "ssq%d" % qb, "subg"], writes=[ohres])
                else:
                    P.op("dve", lambda e, qb=qb: e.reciprocal(out=rl[qb][:, 0:1], in_=pO[qb][:, 128:129]), reads=[pOres], writes=[rres])
                    P.op("dve", lambda e, qb=qb, hb_=hb_, m=m: e.tensor_scalar(out=oh[hb_][:, m, :], in0=pO[qb][:, 0:128], scalar1=rl[qb][:, 0:1], scalar2=None, op0=ALU.mult),
                         reads=[pOres, rres], writes=[ohres])

            pending = None
            for m in range(NTO):
                for g in range(m + 1):
                    if g == 0:
                        emit_pre(m)
                    sidx = emit_qk(m, g)
                    if pending is not None:
                        emit_exp_pv(*pending)
                        if pending[1] == pending[0]:
                            emit_epi(pending[0])
                    pending = (m, g, sidx)
            emit_exp_pv(*pending)
            emit_epi(pending[0])
            P.dma("sp", lambda e, hb_=hb_, h=h: e.dma_start(out=A["o_d"][:, h * 128:(h + 1) * 128].rearrange("(m p) d -> p m d", p=128), in_=oh[hb_][:]),
                  reads=[ohres], writes=["dram_o"])
        P.run()


def phase_oproj(nc, A):
    with contextlib.ExitStack() as st:
        P = Phase(nc, "p5")
        ident = sb(st, nc, "p5_ident", [128, 128], BF16)
        Wd = sb(st, nc, "p5_Wd", [128, 8, D], BF16)
        Wm = sb(st, nc, "p5_Wm", [128, 8, D], BF16)
        ot = [sb(st, nc, "p5_ot%d" % i, [128, D], BF16) for i in range(2)]
        oT = [sb(st, nc, "p5_oT%d" % i, [128, KC, 512], BF16) for i in range(1)]
        Gd = [sb(st, nc, "p5_Gd%d" % i, [128, KC, 512], BF16) for i in range(1)]
        Gm = [sb(st, nc, "p5_Gm%d" % i, [128, KC, 512], BF16) for i in range(1)]
        zT = [sb(st, nc, "p5_zT%d" % i, [128, KC, 512], BF16) for i in range(1)]
        t1 = [sb(st, nc, "p5_t1%d" % i, [128, 512], F32) for i in range(2)]
        pT = [ps(st, nc, "p5_pT%d" % i, [128, KC, 128], BF16) for i in range(1)]
        pY = [ps(st, nc, "p5_pY%d" % i, [128, 512], F32) for i in range(4)]
        P.dma("sp", lambda e: e.dma_start(out=ident[:], in_=A["ident_bf"][:, :]), writes=["ident"])
        for q4 in range(2):
            P.dma("pool", lambda e, q4=q4: e.dma_start(out=Wd[:, q4 * 4:(q4 + 1) * 4, :], in_=A["w_o_diff"].rearrange("(k p) n -> p k n", p=128)[:, q4 * 4:(q4 + 1) * 4, :]), writes=["Wd"])
            P.dma("pool", lambda e, q4=q4: e.dma_start(out=Wm[:, q4 * 4:(q4 + 1) * 4, :], in_=A["w_o_moba"].rearrange("(k p) n -> p k n", p=128)[:, q4 * 4:(q4 + 1) * 4, :]), writes=["Wm"])
        tcount = 0
        ycount = 0
        for g in range(4):
            gi = 0
            P.dma("sp", lambda e, gi=gi, g=g: e.dma_start(out=Gd[gi][:], in_=A["GT"][0:16, :, g * 512:(g + 1) * 512].rearrange("c p t -> p c t")), writes=["Gd%d" % gi])
            P.dma("sp", lambda e, gi=gi, g=g: e.dma_start(out=Gm[gi][:], in_=A["GT"][16:32, :, g * 512:(g + 1) * 512].rearrange("c p t -> p c t")), writes=["Gm%d" % gi])
            for tt in range(4):
                ti = tcount % 2
                tcount += 1
                tile = g * 4 + tt
                P.dma("sp", lambda e, ti=ti, tile=tile: e.dma_start(out=ot[ti][:], in_=A["o_d"][tile * 128:(tile + 1) * 128, :]), writes=["ot%d" % ti])

                def tr(e, ti=ti):
                    ins = None
                    for k in range(KC):
                        ins = e.transpose(out=pT[0][:, k, :], in_=ot[ti][:, k * 128:(k + 1) * 128], identity=ident[:])
                    return ins
                P.op("pe", tr, reads=["ot%d" % ti, "ident"], writes=["pT"])
                P.op("act", lambda e, gi=gi, tt=tt: e.activation(out=oT[gi][:, :, tt * 128:(tt + 1) * 128], in_=pT[0][:], func=AF.Copy), reads=["pT"], writes=["oT%d" % gi])
            for c in range(KC):
                yd_i = ycount % 4
                ym_i = (ycount + 1) % 4
                ycount += 2
                t1i = c % 2

                def mmd(e, gi=gi, c=c, yd_i=yd_i):
                    ins = None
                    for k in range(8):
                        ins = e.matmul(pY[yd_i][:], lhsT=Wd[:, k, c * 128:(c + 1) * 128], rhs=oT[gi][:, k, :], start=(k == 0), stop=(k == 7))
                    return ins

                def mmm(e, gi=gi, c=c, ym_i=ym_i):
                    ins = None
                    for k in range(8):
                        ins = e.matmul(pY[ym_i][:], lhsT=Wm[:, k, c * 128:(c + 1) * 128], rhs=oT[gi][:, 8 + k, :], start=(k == 0), stop=(k == 7))
                    return ins
                P.op("pe", mmd, reads=["Wd", "oT%d" % gi], writes=["pY%d" % yd_i])
                P.op("pe", mmm, reads=["Wm", "oT%d" % gi], writes=["pY%d" % ym_i])
                P.op("dve", lambda e, gi=gi, c=c, yd_i=yd_i, t1i=t1i: e.tensor_tensor(out=t1[t1i][:], in0=pY[yd_i][:], in1=Gd[gi][:, c, :], op=ALU.mult),
                     reads=["pY%d" % yd_i, "Gd%d" % gi], writes=["t1%d" % t1i])
                P.op("dve", lambda e, gi=gi, c=c, ym_i=ym_i, t1i=t1i: e.tensor_tensor(out=zT[gi][:, c, :], in0=pY[ym_i][:], in1=Gm[gi][:, c, :], op=ALU.mult),
                     reads=["pY%d" % ym_i, "Gm%d" % gi], writes=["zT%d" % gi])
                P.op("pool", lambda e, gi=gi, c=c, t1i=t1i: e.tensor_tensor(out=zT[gi][:, c, :], in0=zT[gi][:, c, :], in1=t1[t1i][:], op=ALU.add),
                     reads=["t1%d" % t1i, "zT%d" % gi], writes=["zT%d" % gi])
            P.dma("sp", lambda e, gi=gi, g=g: e.dma_start(out=A["zT"][g], in_=zT[gi][:].rearrange("p c t -> p (c t)")), reads=["zT%d" % gi], writes=["dram_zT"])
        P.run()


def phase_wout(nc, A):
    with contextlib.ExitStack() as st:
        P = Phase(nc, "p6")
        Wo = sb(st, nc, "p6_Wo", [128, KC, D], BF16)
        G1 = sb(st, nc, "p6_G1", [128, D], F32)
        zT = [sb(st, nc, "p6_zT%d" % i, [128, KC, 512], BF16) for i in range(2)]
        xt = [sb(st, nc, "p6_x%d" % i, [128, D], F32) for i in range(2)]
        pY = [ps(st, nc, "p6_pY%d" % i, [128, 512], F32) for i in range(4)]
        for q4 in range(4):
            P.dma("pool", lambda e, q4=q4: e.dma_start(out=Wo[:, q4 * 4:(q4 + 1) * 4, :], in_=A["w_out"].rearrange("(k p) n -> p k n", p=128)[:, q4 * 4:(q4 + 1) * 4, :]), writes=["Wo"])
        P.dma("sp", lambda e: e.dma_start(out=G1[:], in_=A["modb"][:, MOD_G1:MOD_G1 + D]), writes=["G1"])
        ycount = 0
        tcount = 0
        for g in range(4):
            gi = g % 2
            P.dma("sp", lambda e, gi=gi, g=g: e.dma_start(out=zT[gi][:].rearrange("p c t -> p (c t)"), in_=A["zT"][g]), writes=["zT%d" % gi])
            for tt in range(4):
                ti = tcount % 2
                tcount += 1
                tile = g * 4 + tt
                P.dma("sp", lambda e, ti=ti, tile=tile: e.dma_start(out=xt[ti][:], in_=A["x_own"][tile * 128:(tile + 1) * 128, :]), writes=["x%d" % ti])
                for cg in range(4):
                    yi = ycount % 4
                    ycount += 1

                    def mm(e, gi=gi, tt=tt, cg=cg, yi=yi):
                        ins = None
                        for k in range(KC):
                            ins = e.matmul(pY[yi][:], lhsT=zT[gi][:, k, tt * 128:(tt + 1) * 128], rhs=Wo[:, k, cg * 512:(cg + 1) * 512], start=(k == 0), stop=(k == KC - 1))
                        return ins
                    P.op("pe", mm, reads=["zT%d" % gi, "Wo"], writes=["pY%d" % yi])
                    P.op("dve", lambda e, yi=yi, cg=cg, ti=ti: e.tensor_tensor(out=pY[yi][:], in0=pY[yi][:], in1=G1[:, cg * 512:(cg + 1) * 512], op=ALU.mult),
                         reads=["pY%d" % yi, "G1"], writes=["pY%d" % yi])
                    P.op("dve", lambda e, yi=yi, cg=cg, ti=ti: e.tensor_tensor(out=xt[ti][:, cg * 512:(cg + 1) * 512], in0=pY[yi][:], in1=xt[ti][:, cg * 512:(cg + 1) * 512], op=ALU.add),
                         reads=["pY%d" % yi, "x%d" % ti], writes=["x%d" % ti])
                P.dma("sp", lambda e, ti=ti, tile=tile: e.dma_start(out=A["x1"][tile * 128:(tile + 1) * 128, :], in_=xt[ti][:]), reads=["x%d" % ti], writes=["dram_x1"])
        P.run()


def phase_route(nc, A):
    with contextlib.ExitStack() as st:
        P = Phase(nc, "p7")
        ident = sb(st, nc, "p7_ident", [128, 128], BF16)
        ltri = sb(st, nc, "p7_ltri", [128, 128], BF16)
        onesb = sb(st, nc, "p7_ones", [128, 128], BF16)
        Amod = sb(st, nc, "p7_A", [128, D], F32)
        Smod = sb(st, nc, "p7_S", [128, D], F32)
        Wr = sb(st, nc, "p7_Wr", [128, KC, E], BF16)
        rbias = sb(st, nc, "p7_rbias", [128, E], F32)
        ecap = sb(st, nc, "p7_ecap", [128, E], F32)
        dumpidx = sb(st, nc, "p7_dump", [128, 1], F32)
        tokid = sb(st, nc, "p7_tokid", [128, NTO], I32)
        zero_i = sb(st, nc, "p7_zero", [128, CAP * 8], I32)
        zero_b = sb(st, nc, "p7_zerob", [128, D], BF16)
        xt = [sb(st, nc, "p7_x%d" % i, [128, D], F32) for i in range(2)]
        sq = [sb(st, nc, "p7_sq%d" % i, [128, D], BF16) for i in range(2)]
        ss = [sb(st, nc, "p7_ss%d" % i, [128, 1], F32) for i in range(2)]
        rstd = [sb(st, nc, "p7_rs%d" % i, [128, 1], F32) for i in range(2)]
        hf = [sb(st, nc, "p7_hf%d" % i, [128, D], F32) for i in range(2)]
        hb = [sb(st, nc, "p7_hb%d" % i, [128, D], BF16) for i in range(2)]
        hT = [sb(st, nc, "p7_hT%d" % i, [128, KC, 128], BF16) for i in range(2)]
        emask = sb(st, nc, "p7_emask", [128, NTO, E], BF16)
        scores = [sb(st, nc, "p7_sc%d" % i, [128, E], F32) for i in range(2)]
        selv = [sb(st, nc, "p7_sel%d" % i, [128, E], F32) for i in range(2)]
        g8 = [sb(st, nc, "p7_g8%d" % i, [128, 8], F32) for i in range(2)]
        gsc = [sb(st, nc, "p7_gsc%d" % i, [128, 8], F32) for i in range(2)]
        gm8 = [sb(st, nc, "p7_gm%d" % i, [128, 8], F32) for i in range(2)]
        gmask = [sb(st, nc, "p7_gmask%d" % i, [128, 8], F32) for i in range(2)]
        t8 = [sb(st, nc, "p7_t8%d" % i, [128, 8], F32) for i in range(2)]
        em = [sb(st, nc, "p7_em%d" % i, [128, E], F32) for i in range(2)]
        wt = sb(st, nc, "p7_wt", [128, NTO, E], F32)
        wsum = [sb(st, nc, "p7_ws%d" % i, [128, 1], F32) for i in range(2)]
        key = [sb(st, nc, "p7_key%d" % i, [128, E], F32) for i in range(2)]
        k8 = [sb(st, nc, "p7_k8%d" % i, [128, 8], F32) for i in range(2)]
        oh_ = [sb(st, nc, "p7_oh%d" % i, [128, E], F32) for i in range(2)]
        junk_ = [sb(st, nc, "p7_junk%d" % i, [128, E], F32) for i in range(2)]
        cnt_i = sb(st, nc, "p7_cnt_i", [128, E], I32)
        route = [sb(st, nc, "p7_route%d" % i, [128, 16], F32) for i in range(2)]
        sidx = [sb(st, nc, "p7_sidx%d" % i, [128, 8], I32) for i in range(2)]
        tokrow = [sb(st, nc, "p7_tokrow%d" % i, [128, 8], I32) for i in range(2)]
        valid = [sb(st, nc, "p7_valid%d" % i, [128, 8], F32) for i in range(2)]
        pT = [ps(st, nc, "p7_pT%d" % i, [128, KC, 128], BF16) for i in range(2)]
        pL = [ps(st, nc, "p7_pL%d" % i, [128, E], F32) for i in range(2)]
        pR = [ps(st, nc, "p7_pR%d" % i, [128, E], F32) for i in range(2)]

        P.dma("sp", lambda e: e.dma_start(out=ident[:], in_=A["ident_bf"][:, :]), writes=["ident"])
        P.dma("sp", lambda e: e.dma_start(out=ltri[:], in_=A["ltri"][:, :]), writes=["ltri"])
        P.dma("sp", lambda e: e.dma_start(out=Amod[:], in_=A["modb"][:, MOD_A2:MOD_A2 + D]), writes=["Amod"])
        P.dma("sp", lambda e: e.dma_start(out=Smod[:], in_=A["modb"][:, MOD_SHIFT2:MOD_SHIFT2 + D]), writes=["Smod"])
        P.dma("pool", lambda e: e.dma_start(out=Wr[:], in_=A["w_router"].rearrange("(k p) n -> p k n", p=128)), writes=["Wr"])
        P.dma("sp", lambda e: e.dma_start(out=rbias[:], in_=A["rbias_b"][:, :]), writes=["rbias"])
        P.dma("sp", lambda e: e.dma_start(out=ecap[:], in_=A["ecap"][:, :]), writes=["ecap"])
        P.dma("sp", lambda e: e.dma_start(out=dumpidx[:], in_=A["dumpidx"][:, :]), writes=["dumpidx"])
        P.dma("sp", lambda e: e.dma_start(out=tokid[:], in_=A["tokid"][:, :]), writes=["tokid"])
        P.op("dve", lambda e: e.memset(onesb[:], 1.0), writes=["onesb"])
        P.op("pool", lambda e: e.memset(zero_i[:], 0), writes=["zero_i"])
        P.op("pool", lambda e: e.memset(zero_b[:], 0.0), writes=["zero_b"])
        rt_view = A["rowtok"][0:NSLOT, :].rearrange("(e c) w -> e (c w)", e=E)
        P.dma("sp", lambda e: e.dma_start(out=rt_view, in_=zero_i[0:E, :]), reads=["zero_i"], writes=["dram_rowtok"])
        P.dma("sp", lambda e: e.dma_start(out=A["rowtok"][NSLOT:NSLOT + 128, :], in_=zero_i[:, 0:8]), reads=["zero_i"], writes=["dram_rowtok"])
        P.dma("sp", lambda e: e.dma_start(out=A["Ybuf"][NSLOT:NSLOT + 128, :], in_=zero_b[:]), reads=["zero_b"], writes=["dram_Ybuf"])

        for t in range(NTO):
            i = t % 2
            tag = str(i)
            P.dma("sp", lambda e, i=i, t=t: e.dma_start(out=xt[i][:], in_=A["x1"][t * 128:(t + 1) * 128, :]), writes=["x" + tag])
            emit_norm_mod_T(P, nc, xt[i], sq[i], ss[i], rstd[i], hf[i], hb[i], pT[i], hT[i], Amod, Smod, ident, tag, "x" + tag)
            P.dma("sp", lambda e, i=i, t=t: e.dma_start(out=A["h2"][t * 128:(t + 1) * 128, :], in_=hb[i][:]), reads=["hb" + tag], writes=["dram_h2"])
            P.dma("sp", lambda e, i=i, t=t: e.dma_start(out=A["h2T"][t], in_=hT[i][:].rearrange("p k t -> p (k t)")), reads=["hT" + tag], writes=["dram_h2T"])

            def mml(e, i=i):
                ins = None
                for k in range(KC):
                    ins = e.matmul(pL[i][:], lhsT=hT[i][:, k, :], rhs=Wr[:, k, :], start=(k == 0), stop=(k == KC - 1))
                return ins
            P.op("pe", mml, reads=["hT" + tag, "Wr"], writes=["pL" + tag])
            P.op("act", lambda e, i=i: e.activation(out=scores[i][:], in_=pL[i][:], func=AF.Sigmoid), reads=["pL" + tag], writes=["sc" + tag])
            P.op("dve", lambda e, i=i: e.tensor_tensor(out=selv[i][:], in0=scores[i][:], in1=rbias[:], op=ALU.add), reads=["sc" + tag, "rbias"], writes=["sel" + tag])
            for gq in range(8):
                P.op("dve", lambda e, i=i, gq=gq: e.max(out=g8[i][:], in_=selv[i][:, gq * 8:(gq + 1) * 8]), reads=["sel" + tag, "gsc" + tag], writes=["g8" + tag])
                P.op("dve", lambda e, i=i, gq=gq: e.tensor_tensor(out=gsc[i][:, gq:gq + 1], in0=g8[i][:, 0:1], in1=g8[i][:, 1:2], op=ALU.add), reads=["g8" + tag], writes=["gsc" + tag])
            P.op("dve", lambda e, i=i: e.max(out=gm8[i][:], in_=gsc[i][:]), reads=["gsc" + tag], writes=["gm8" + tag])
            P.op("dve", lambda e, i=i: e.tensor_scalar(out=gmask[i][:], in0=gsc[i][:], scalar1=gm8[i][:, 3:4], scalar2=None, op0=ALU.is_ge), reads=["gsc" + tag, "gm8" + tag], writes=["gmask" + tag])
            for gq in range(8):
                P.op("dve", lambda e, i=i, gq=gq: e.tensor_scalar(out=selv[i][:, gq * 8:(gq + 1) * 8], in0=selv[i][:, gq * 8:(gq + 1) * 8], scalar1=2.0, scalar2=gmask[i][:, gq:gq + 1],
                                                                 op0=ALU.add, op1=ALU.mult), reads=["sel" + tag, "gmask" + tag], writes=["sel" + tag])
            P.op("dve", lambda e, i=i: e.max(out=t8[i][:], in_=selv[i][:]), reads=["sel" + tag], writes=["t8" + tag])
            P.op("dve", lambda e, i=i: e.tensor_scalar(out=em[i][:], in0=selv[i][:], scalar1=t8[i][:, 7:8], scalar2=None, op0=ALU.is_ge), reads=["sel" + tag, "t8" + tag], writes=["em" + tag])
            P.op("dve", lambda e, i=i, t=t: e.tensor_copy(out=emask[:, t, :], in_=em[i][:]), reads=["em" + tag], writes=["emask"])
            P.op("dve", lambda e, i=i, t=t: e.tensor_tensor(out=wt[:, t, :], in0=scores[i][:], in1=em[i][:], op=ALU.mult), reads=["sc" + tag, "em" + tag], writes=["wt"])
            P.op("act", lambda e, i=i, t=t: e.activation(out=junk_[i][:], in_=wt[:, t, :], func=AF.Copy, accum_out=wsum[i][:]), reads=["wt"], writes=["ws" + tag, "junk" + tag])
            P.op("dve", lambda e, i=i: e.reciprocal(out=wsum[i][:], in_=wsum[i][:]), reads=["ws" + tag], writes=["ws" + tag])
            P.op("dve", lambda e, i=i, t=t: e.tensor_scalar(out=wt[:, t, :], in0=wt[:, t, :], scalar1=wsum[i][:, 0:1], scalar2=2.5, op0=ALU.mult, op1=ALU.mult), reads=["wt", "ws" + tag], writes=["wt"])

            def mmr(e, i=i, t=t):
                ins = None
                for j in range(t):
                    ins = e.matmul(pR[i][:], lhsT=onesb[:], rhs=emask[:, j, :], start=(j == 0), stop=False)
                ins = e.matmul(pR[i][:], lhsT=ltri[:], rhs=emask[:, t, :], start=(t == 0), stop=True)
                return ins
            P.op("pe", mmr, reads=["emask", "onesb", "ltri"], writes=["pR" + tag])
            P.op("dve", lambda e, i=i: e.scalar_tensor_tensor(out=key[i][:], in0=pR[i][:], scalar=1.0, in1=ecap[:], op0=ALU.add, op1=ALU.add), reads=["pR" + tag, "ecap"], writes=["key" + tag])
            P.op("dve", lambda e, i=i: e.tensor_scalar(out=oh_[i][:], in0=pR[i][:], scalar1=float(CAP) - 0.5, scalar2=None, op0=ALU.is_lt), reads=["pR" + tag], writes=["oh" + tag])
            P.op("dve", lambda e, i=i: e.tensor_tensor(out=oh_[i][:], in0=oh_[i][:], in1=em[i][:], op=ALU.mult), reads=["oh" + tag, "em" + tag], writes=["oh" + tag])
            P.op("dve", lambda e, i=i: e.tensor_tensor(out=key[i][:], in0=key[i][:], in1=oh_[i][:], op=ALU.mult), reads=["key" + tag, "oh" + tag], writes=["key" + tag])
            P.op("dve", lambda e, i=i: e.max(out=k8[i][:], in_=key[i][:]), reads=["key" + tag], writes=["k8" + tag])
            for kk in range(8):
                P.op("dve", lambda e, i=i, kk=kk: e.tensor_scalar(out=oh_[i][:], in0=key[i][:], scalar1=k8[i][:, kk:kk + 1], scalar2=None, op0=ALU.is_equal), reads=["key" + tag, "k8" + tag, "route" + tag], writes=["oh" + tag])
                P.op("dve", lambda e, i=i, kk=kk, t=t: e.tensor_tensor(out=oh_[i][:], in0=oh_[i][:], in1=wt[:, t, :], op=ALU.mult), reads=["oh" + tag, "wt"], writes=["oh" + tag])
                P.op("act", lambda e, i=i, kk=kk: e.activation(out=junk_[i][:], in_=oh_[i][:], func=AF.Copy, accum_out=route[i][:, 8 + kk:9 + kk]), reads=["oh" + tag], writes=["route" + tag, "junk" + tag])
            P.op("dve", lambda e, i=i: e.tensor_scalar(out=valid[i][:], in0=k8[i][:], scalar1=0.5, scalar2=None, op0=ALU.is_gt), reads=["k8" + tag], writes=["valid" + tag])
            P.op("dve", lambda e, i=i: e.tensor_tensor(out=route[i][:, 8:16], in0=route[i][:, 8:16], in1=valid[i][:], op=ALU.mult), reads=["route" + tag, "valid" + tag], writes=["route" + tag])
            P.op("dve", lambda e, i=i: e.tensor_scalar(out=route[i][:, 0:8], in0=k8[i][:], scalar1=-1.0, scalar2=dumpidx[:, 0:1], op0=ALU.add, op1=ALU.subtract), reads=["k8" + tag, "dumpidx", "route" + tag], writes=["route" + tag])
            P.op("dve", lambda e, i=i: e.tensor_tensor(out=route[i][:, 0:8], in0=route[i][:, 0:8], in1=valid[i][:], op=ALU.mult), reads=["route" + tag, "valid" + tag], writes=["route" + tag])
            P.op("dve", lambda e, i=i: e.tensor_scalar(out=route[i][:, 0:8], in0=route[i][:, 0:8], scalar1=dumpidx[:, 0:1], scalar2=None, op0=ALU.add), reads=["route" + tag, "dumpidx"], writes=["route" + tag])
            P.op("dve", lambda e, i=i: e.tensor_copy(out=sidx[i][:], in_=route[i][:, 0:8]), reads=["route" + tag], writes=["sidx" + tag])
            P.dma("sp", lambda e, i=i, t=t: e.dma_start(out=A["route"][:, t * 16:(t + 1) * 16], in_=route[i][:]), reads=["route" + tag], writes=["dram_route"])
            if t == NTO - 1:
                def mmc(e):
                    ins = None
                    for j in range(NTO):
                        ins = e.matmul(pL[0][:], lhsT=onesb[:], rhs=emask[:, j, :], start=(j == 0), stop=(j == NTO - 1))
                    return ins
                P.op("pe", mmc, reads=["emask", "onesb"], writes=["pL0"])
                P.op("dve", lambda e: e.tensor_copy(out=cnt_i[:], in_=pL[0][:]), reads=["pL0"], writes=["cnt_i"])
                P.dma("sp", lambda e: e.dma_start(out=A["cnt_d"][:, :], in_=cnt_i[0:1, :]), reads=["cnt_i"], writes=["dram_cnt"])
            for kk in range(8):
                P.op("pool", lambda e, i=i, kk=kk, t=t: e.tensor_copy(out=tokrow[i][:, kk:kk + 1], in_=tokid[:, t:t + 1]), reads=["tokid", "tokrow" + tag], writes=["tokrow" + tag])
            for kk in range(8):
                P.dma("pool", lambda e, i=i, kk=kk: e.indirect_dma_start(out=A["rowtok"][:, :], out_offset=bass.IndirectOffsetOnAxis(ap=sidx[i][:, kk:kk + 1], axis=0),
                                                                         in_=tokrow[i][:], in_offset=None),
                      reads=["sidx" + tag, "tokrow" + tag, "dram_rowtok"], writes=["dram_rowtok_s"])
        P.run()


def phase_experts(nc, A):
    NST = 4
    with contextlib.ExitStack() as st:
        P = Phase(nc, "p8")
        ident = sb(st, nc, "p8_ident", [128, 128], BF16)
        cnt_sb = sb(st, nc, "p8_cnt", [1, E], I32)
        P.cnt_ap = cnt_sb
        Wg = [sb(st, nc, "p8_Wg%d" % i, [128, KC, 512], BF16) for i in range(2)]
        Wu = [sb(st, nc, "p8_Wu%d" % i, [128, KC, 512], BF16) for i in range(2)]
        Wd = [sb(st, nc, "p8_Wd%d" % i, [128, 4, D], BF16) for i in range(2)]
        stage = [sb(st, nc, "p8_st%d" % i, [128, 2048], F32) for i in range(NST)]
        idx_all = sb(st, nc, "p8_idxall", [128, E * JMAX, 8], I32)
        xg = [sb(st, nc, "p8_xg%d" % i, [128, D], BF16) for i in range(3)]
        xT = [sb(st, nc, "p8_xT%d" % i, [128, KC, 128], BF16) for i in range(2)]
        sg = [sb(st, nc, "p8_sg%d" % i, [128, 512], F32) for i in range(2)]
        aT = [sb(st, nc, "p8_aT%d" % i, [128, 4, 128], BF16) for i in range(2)]
        Y = [sb(st, nc, "p8_Y%d" % i, [128, D], BF16) for i in range(2)]
        pT = [ps(st, nc, "p8_pT%d" % i, [128, KC, 128], BF16) for i in range(1)]
        pG = [ps(st, nc, "p8_pG%d" % i, [128, 512], F32) for i in range(2)]
        pU = [ps(st, nc, "p8_pU%d" % i, [128, 512], F32) for i in range(2)]
        pY = [ps(st, nc, "p8_pY%d" % i, [128, 512], F32) for i in range(2)]
        P.dma("pool", lambda e: e.dma_start(out=ident[:], in_=A["ident_bf"][:, :]), writes=["ident"])
        P.dma("pool", lambda e: e.dma_start(out=cnt_sb[:], in_=A["cnt_d"][:, :]), writes=["cnt_sb"])
        cn = {"g": 0, "y": 0, "yb": 0, "x": 0, "a": 0, "gu": 0, "st": 0, "ce": 0}

        def wsrc(ei):
            if ei < E:
                return A["w_exp_gate"][ei], A["w_exp_up"][ei], A["w_exp_down"][ei]
            return A["w_sh_gate"], A["w_sh_up"], A["w_sh_down"]

        def weight_chunks(ei):
            wi = ei % 2
            gsrc, usrc, dsrc = wsrc(ei)
            out = []
            for c in range(4):
                out.append((gsrc.rearrange("(k p) n -> p k n", p=128)[:, 4 * c:4 * c + 4, :], Wg[wi][:, 4 * c:4 * c + 4, :], "Wg%d_%d" % (wi, c), (4, 512)))
            for c in range(4):
                out.append((usrc.rearrange("(k p) n -> p k n", p=128)[:, 4 * c:4 * c + 4, :], Wu[wi][:, 4 * c:4 * c + 4, :], "Wu%d_%d" % (wi, c), (4, 512)))
            for c in range(4):
                out.append((dsrc.rearrange("(k p) n -> p k n", p=128)[:, c:c + 1, :], Wd[wi][:, c:c + 1, :], "Wd%d_%d" % (wi, c), (1, 2048)))
            return out

        def issue_chunk_dma(ch):
            src, dst, res, (a, b) = ch
            si = cn["st"] % NST
            cn["st"] += 1
            P.dma("sp", lambda e, si=si, src=src, a=a: e.dma_start(out=stage[si][:].rearrange("p (a b) -> p a b", a=a), in_=src), writes=["st%d" % si])
            return si

        def issue_chunk_cast(ch, si):
            src, dst, res, (a, b) = ch
            eng = ("dve", "act", "pool")[cn["ce"] % 3]
            cn["ce"] += 1
            view = stage[si][:].rearrange("p (a b) -> p a b", a=a)
            if eng == "act":
                P.op("act", lambda e, dst=dst, view=view: e.activation(out=dst, in_=view, func=AF.Copy), reads=["st%d" % si], writes=[res])
            else:
                P.op(eng, lambda e, dst=dst, view=view: e.tensor_copy(out=dst, in_=view), reads=["st%d" % si], writes=[res])

        def ffn(wi, xi, dst):
            xres = "xT%d" % xi
            ai = cn["a"] % 2
            cn["a"] += 1
            ares = "aT%d" % ai
            gb = cn["gu"] % 2
            cn["gu"] += 1

            def mmgu(e, wi=wi, xi=xi, gb=gb):
                ins = None
                for fc in range(4):
                    for k in range(KC):
                        ins = e.matmul(pG[gb][:, fc * 128:(fc + 1) * 128], lhsT=Wg[wi][:, k, fc * 128:(fc + 1) * 128], rhs=xT[xi][:, k, :], start=(k == 0), stop=(k == KC - 1))
                for fc in range(4):
                    for k in range(KC):
                        ins = e.matmul(pU[gb][:, fc * 128:(fc + 1) * 128], lhsT=Wu[wi][:, k, fc * 128:(fc + 1) * 128], rhs=xT[xi][:, k, :], start=(k == 0), stop=(k == KC - 1))
                return ins
            P.op("pe", mmgu, reads=["Wg%d_%d" % (wi, c) for c in range(4)] + ["Wu%d_%d" % (wi, c) for c in range(4)] + [xres], writes=["pG%d" % gb, "pU%d" % gb])
            P.op("act", lambda e, gb=gb: e.activation(out=sg[gb][:], in_=pG[gb][:], func=AF.Silu), reads=["pG%d" % gb], writes=["sg%d" % gb])
            P.op("dve", lambda e, gb=gb, ai=ai: e.tensor_tensor(out=aT[ai][:], in0=pU[gb][:].rearrange("p (f t) -> p f t", f=4), in1=sg[gb][:].rearrange("p (f t) -> p f t", f=4), op=ALU.mult),
                 reads=["pU%d" % gb, "sg%d" % gb], writes=[ares])
            yi = cn["yb"] % 2
            cn["yb"] += 1
            for cg in range(4):
                pi = cn["y"] % 2
                cn["y"] += 1

                def mmy(e, wi=wi, ai=ai, cg=cg, pi=pi):
                    ins = None
                    for fc in range(4):
                        ins = e.matmul(pY[pi][:], lhsT=aT[ai][:, fc, :], rhs=Wd[wi][:, fc, cg * 512:(cg + 1) * 512], start=(fc == 0), stop=(fc == 3))
                    return ins
                P.op("pe", mmy, reads=[ares] + ["Wd%d_%d" % (wi, c) for c in range(4)], writes=["pY%d" % pi])
                if cg % 2 == 0:
                    P.op("act", lambda e, yi=yi, cg=cg, pi=pi: e.activation(out=Y[yi][:, cg * 512:(cg + 1) * 512], in_=pY[pi][:], func=AF.Copy), reads=["pY%d" % pi], writes=["Y%d" % yi])
                else:
                    P.op("dve", lambda e, yi=yi, cg=cg, pi=pi: e.tensor_copy(out=Y[yi][:, cg * 512:(cg + 1) * 512], in_=pY[pi][:]), reads=["pY%d" % pi], writes=["Y%d" % yi])
            P.dma("act", lambda e, yi=yi, dst=dst: e.dma_start(out=dst, in_=Y[yi][:]), reads=["Y%d" % yi], writes=["dram_Y"])

        for ei in range(E):
            P.dma("pool", lambda e, ei=ei: e.dma_start(out=idx_all[:, ei * JMAX:(ei + 1) * JMAX, :], in_=A["rowtok"][ei * CAP:(ei + 1) * CAP, :].rearrange("(t p) w -> p t w", p=128)),
                  writes=["idxall%d" % ei])
        for ch in weight_chunks(0):
            si = issue_chunk_dma(ch)
            issue_chunk_cast(ch, si)
        for ei in range(E):
            wi = ei % 2
            P.load_cond_reg(ei, "cnt_sb")
            nxt = weight_chunks(ei + 1)
            pend = []
            for j in range(JMAX):
                for _ in range(2):
                    if nxt:
                        ch = nxt.pop(0)
                        pend.append((ch, issue_chunk_dma(ch)))
                P.begin_region(ei, 128 * j)
                gi = cn["g"] % 3
                cn["g"] += 1
                xi = cn["x"] % 2
                cn["x"] += 1
                s0 = ei * CAP + j * 128
                P.dma("pool", lambda e, ei=ei, j=j, gi=gi: e.indirect_dma_start(out=xg[gi][:], out_offset=None, in_=A["h2"][:, :],
                                                                              in_offset=bass.IndirectOffsetOnAxis(ap=idx_all[:, ei * JMAX + j, 0:1], axis=0)),
                      reads=["idxall%d" % ei], writes=["xg%d" % gi])

                def tr(e, gi=gi):
                    ins = None
                    for k in range(KC):
                        ins = e.transpose(out=pT[0][:, k, :], in_=xg[gi][:, k * 128:(k + 1) * 128], identity=ident[:])
                    return ins
                P.op("pe", tr, reads=["xg%d" % gi, "ident"], writes=["pT"])
                if j % 2 == 0:
                    P.op("act", lambda e, xi=xi: e.activation(out=xT[xi][:], in_=pT[0][:], func=AF.Copy), reads=["pT"], writes=["xT%d" % xi])
                else:
                    P.op("dve", lambda e, xi=xi: e.tensor_copy(out=xT[xi][:], in_=pT[0][:]), reads=["pT"], writes=["xT%d" % xi])
                ffn(wi, xi, A["Ybuf"][s0:s0 + 128, :])
                P.end_region()
                while pend:
                    ch, si = pend.pop(0)
                    issue_chunk_cast(ch, si)
            assert not nxt
        wi = E % 2
        for t in range(NTO):
            xi = cn["x"] % 2
            cn["x"] += 1
            P.dma("pool", lambda e, xi=xi, t=t: e.dma_start(out=xT[xi][:], in_=A["h2T"][t].rearrange("p (k q) -> p k q", k=KC)), writes=["xT%d" % xi])
            ffn(wi, xi, A["Ysh"][t * 128:(t + 1) * 128, :])
        P.run()


def phase_final(nc, A):
    with contextlib.ExitStack() as st:
        P = Phase(nc, "p9")
        G2 = sb(st, nc, "p9_G2", [128, D], F32)
        gf = sb(st, nc, "p9_gf", [128, D], F32)
        xt = [sb(st, nc, "p9_x%d" % i, [128, D], F32) for i in range(2)]
        ysh = [sb(st, nc, "p9_ysh%d" % i, [128, D], BF16) for i in range(2)]
        yk = [sb(st, nc, "p9_yk%d" % i, [128, D], BF16) for i in range(4)]
        acc = [sb(st, nc, "p9_acc%d" % i, [128, D], F32) for i in range(2)]
        route = [sb(st, nc, "p9_route%d" % i, [128, 16], F32) for i in range(2)]
        sidx = [sb(st, nc, "p9_sidx%d" % i, [128, 8], I32) for i in range(2)]
        sq = [sb(st, nc, "p9_sq%d" % i, [128, D], BF16) for i in range(2)]
        ss = [sb(st, nc, "p9_ss%d" % i, [128, 1], F32) for i in range(2)]
        P.dma("sp", lambda e: e.dma_start(out=G2[:], in_=A["modb"][:, MOD_G2:MOD_G2 + D]), writes=["G2"])
        P.dma("sp", lambda e: e.dma_start(out=gf[:], in_=A["g_final_b"][:, :]), writes=["gf"])
        kc = 0
        for t in range(NTO):
            i = t % 2
            tag = str(i)
            P.dma("sp", lambda e, i=i, t=t: e.dma_start(out=xt[i][:], in_=A["x1"][t * 128:(t + 1) * 128, :]), writes=["x" + tag])
            P.dma("sp", lambda e, i=i, t=t: e.dma_start(out=ysh[i][:], in_=A["Ysh"][t * 128:(t + 1) * 128, :]), writes=["ysh" + tag])
            P.dma("sp", lambda e, i=i, t=t: e.dma_start(out=route[i][:], in_=A["route"][:, t * 16:(t + 1) * 16]), writes=["route" + tag])
            P.op("dve", lambda e, i=i: e.tensor_copy(out=sidx[i][:], in_=route[i][:, 0:8]), reads=["route" + tag], writes=["sidx" + tag])
            P.op("dve", lambda e, i=i: e.tensor_copy(out=acc[i][:], in_=ysh[i][:]), reads=["ysh" + tag], writes=["acc" + tag])
            for kk in range(8):
                ki = kc % 4
                kc += 1
                P.dma("pool", lambda e, i=i, kk=kk, ki=ki: e.indirect_dma_start(out=yk[ki][:], out_offset=None, in_=A["Ybuf"][:, :],
                                                                              in_offset=bass.IndirectOffsetOnAxis(ap=sidx[i][:, kk:kk + 1], axis=0)),
                      reads=["sidx" + tag], writes=["yk%d" % ki])
                P.op("dve", lambda e, i=i, kk=kk, ki=ki: e.scalar_tensor_tensor(out=acc[i][:], in0=yk[ki][:], scalar=route[i][:, 8 + kk:9 + kk], in1=acc[i][:], op0=ALU.mult, op1=ALU.add),
                     reads=["yk%d" % ki, "route" + tag, "acc" + tag], writes=["acc" + tag])
            P.op("pool", lambda e, i=i: e.tensor_tensor(out=acc[i][:], in0=acc[i][:], in1=G2[:], op=ALU.mult), reads=["acc" + tag, "G2"], writes=["acc" + tag])
            P.op("pool", lambda e, i=i: e.tensor_tensor(out=xt[i][:], in0=xt[i][:], in1=acc[i][:], op=ALU.add), reads=["acc" + tag, "x" + tag], writes=["x" + tag])
            P.op("act", lambda e, i=i: e.activation(out=sq[i][:], in_=xt[i][:], func=AF.Square, accum_out=ss[i][:]), reads=["x" + tag], writes=["sq" + tag, "ss" + tag])
            P.op("dve", lambda e, i=i: e.tensor_scalar(out=ss[i][:], in0=ss[i][:], scalar1=1.0 / D, scalar2=EPS, op0=ALU.mult, op1=ALU.add), reads=["ss" + tag], writes=["ss" + tag])
            P.op("act", lambda e, i=i: e.activation(out=ss[i][:], in_=ss[i][:], func=AF.Sqrt), reads=["ss" + tag], writes=["ss" + tag])
            P.op("dve", lambda e, i=i: e.reciprocal(out=ss[i][:], in_=ss[i][:]), reads=["ss" + tag], writes=["ss" + tag])
            P.op("dve", lambda e, i=i: e.scalar_tensor_tensor(out=acc[i][:], in0=xt[i][:], scalar=ss[i][:, 0:1], in1=gf[:], op0=ALU.mult, op1=ALU.mult),
                 reads=["x" + tag, "ss" + tag, "gf", "acc" + tag], writes=["acc" + tag])
            P.dma("sp", lambda e, i=i, t=t: e.dma_start(out=A["out"][t * 128:(t + 1) * 128, :], in_=acc[i][:]), reads=["acc" + tag], writes=["dram_out"])
        P.run()


def _t5_bucket_np(rel):
    n = np.maximum(rel, 0)
    nf = np.maximum(n, 1).astype(np.float32)
    large = 16 + (np.log(nf / 16) / np.float32(np.log(128 / 16)) * 16).astype(np.int32)
    large = np.minimum(large, 31)
    return np.where(n < 16, n, large)


def _bucket_table():
    n = np.arange(0, 700, dtype=np.int64)
    nf = np.maximum(n, 1).astype(np.float32)
    large = 16 + (np.log(nf / np.float32(16)) / np.float32(np.log(8.0)) * np.float32(16)).astype(np.int32)
    large = np.minimum(large, 31)
    return np.where(n < 16, n, large)


def make_core_inputs(inp, c, shared):
    b, r = c // 4, c % 4
    f32 = np.float32
    x = inp["x"]
    own_tiles = [4 * m + r for m in range(NTO)]
    xb = np.ascontiguousarray(x[b])
    m_ = dict(shared)
    m_["x_all"] = xb
    m_["x_own"] = np.ascontiguousarray(xb.reshape(NTA, 128, D)[own_tiles].reshape(TO, D))
    m_["cT"] = np.ascontiguousarray(inp["c"][b].reshape(KC, 128).T)
    ext = shared["_ext_table"]
    bidx = np.zeros((128, 5, 128), np.int64)
    kk = np.arange(128)[:, None]
    qq = np.arange(128)[None, :]
    for jj in range(5):
        delta = r - (jj - 1)
        rel = delta * 128 + qq - kk
        bi = shared["_bucket"][np.clip(rel, 0, 699)]
        bidx[:, jj, :] = np.where(rel >= 0, bi, 32)
    BT = ext[bidx]
    m_["BT"] = np.ascontiguousarray(BT.transpose(0, 3, 1, 2).reshape(128, 16 * 5 * 128)).astype(ml_dtypes.bfloat16)
    cur = np.array([(4 * m + r) // 2 for m in range(NTO)])
    n = np.arange(32)[None, :]
    past = (n < cur[:, None])
    ownm = (n == cur[:, None])
    const_tbl = np.array([0.0, 1.0, -BIG], f32)
    m_["past"] = np.ascontiguousarray(np.broadcast_to(const_tbl[past.astype(np.int64)].reshape(1, NTO * 32), (128, NTO * 32)))
    m_["own"] = np.ascontiguousarray(np.broadcast_to(const_tbl[ownm.astype(np.int64)].reshape(1, NTO * 32), (128, NTO * 32)))
    m_["pen"] = np.ascontiguousarray(np.broadcast_to(const_tbl[np.where(past, 0, 2)].reshape(1, NTO * 32), (128, NTO * 32)))
    for k in list(m_.keys()):
        if k.startswith("_"):
            del m_[k]
    return m_


def make_shared(inp):
    f32 = np.float32
    bc = lambda v, n: np.ascontiguousarray(np.broadcast_to(np.asarray(v, f32).reshape(1, n), (128, n)))
    sh = {}
    sh["w_ada"] = np.ascontiguousarray(inp["w_ada"][0])
    sh["b_ada_b"] = bc(inp["b_ada"][0], 6 * D)
    sh["g_mix_b"] = bc(inp["g_mix"][0], D)
    sh["g_ffn_b"] = bc(inp["g_ffn"][0], D)
    sh["g_final_b"] = bc(inp["g_final"], D)
    sh["w_in"] = np.ascontiguousarray(inp["w_in"][0])
    sh["lam_b"] = bc(inp["diff_lambda"][0].reshape(-1), 256)
    sh["subln_b"] = bc(inp["diff_subln_g"][0], 128)
    rel_bias = np.asarray(inp["rel_bias"], f32)
    sh["b31"] = bc(rel_bias[31], 16)
    sh["_ext_table"] = np.concatenate([rel_bias, np.full((1, 16), -BIG, f32)], axis=0)
    sh["_bucket"] = _bucket_table()
    sh["w_o_diff"] = np.ascontiguousarray(inp["w_o_diff"][0])
    sh["w_o_moba"] = np.ascontiguousarray(inp["w_o_moba"][0])
    sh["w_out"] = np.ascontiguousarray(inp["w_out"][0])
    sh["w_router"] = np.ascontiguousarray(inp["w_router"][0])
    sh["rbias_b"] = bc(inp["router_bias"][0], E)
    sh["w_exp_gate"] = np.ascontiguousarray(inp["w_exp_gate"][0])
    sh["w_exp_up"] = np.ascontiguousarray(inp["w_exp_up"][0])
    sh["w_exp_down"] = np.ascontiguousarray(inp["w_exp_down"][0])
    sh["w_sh_gate"] = np.ascontiguousarray(inp["w_sh_gate"][0])
    sh["w_sh_up"] = np.ascontiguousarray(inp["w_sh_up"][0])
    sh["w_sh_down"] = np.ascontiguousarray(inp["w_sh_down"][0])
    sh["ident_bf"] = np.eye(128, dtype=f32).astype(ml_dtypes.bfloat16)
    sh["ident_f"] = np.eye(128, dtype=f32)
    tp = np.arange(128)
    sh["ltri"] = (tp[:, None] < tp[None, :]).astype(f32).astype(ml_dtypes.bfloat16)
    sel = np.zeros((32, 32, 128), f32)
    for n in range(32):
        sel[n, n, :] = 1.0
    sh["selc"] = sel.reshape(32, 32 * 128).astype(ml_dtypes.bfloat16)
    sh["ecap"] = bc(np.arange(E, dtype=f32) * CAP, E)
    sh["dumpidx"] = (NSLOT + np.arange(128, dtype=f32)).reshape(128, 1)
    sh["tokid"] = (np.arange(NTO, dtype=np.int32)[None, :] * 128 + np.arange(128, dtype=np.int32)[:, None]).astype(np.int32)
    return sh


_NC_CACHE = {}


def kernel(**inputs):
    inp = {k: np.asarray(v) for k, v in inputs.items()}
    stop_after = int(os.environ.get("MK_STOP", "99"))
    key = (stop_after, DEBUG)
    if key not in _NC_CACHE:
        _NC_CACHE[key] = build_program(stop_after)
    nc = _NC_CACHE[key]
    shared = make_shared(inp)
    in_maps = [make_core_inputs(inp, c, shared) for c in range(NCORES)]
    used = set(USED_INPUTS)
    in_maps = [{k: v for k, v in m.items() if k in used} for m in in_maps]
    res = run_bass_kernel_spmd(nc, in_maps, core_ids=list(range(NCORES)))
    out = np.zeros((2, S, D), np.float32)
    for c in range(NCORES):
        b, r = c // 4, c % 4
        if "out" not in res.results[c]:
            continue
        o = np.asarray(res.results[c]["out"]).reshape(NTO, 128, D)
        ov = out[b].reshape(NTA, 128, D)
        for m in range(NTO):
            ov[4 * m + r] = o[m]
    if DEBUG:
        kernel.last_results = res.results
    return out
```

```python
import os
import contextlib
import numpy as np
import ml_dtypes
import concourse.bass as bass
import concourse.mybir as mybir
from concourse.bass_utils import run_bass_kernel_spmd

F32 = mybir.dt.float32
BF16 = mybir.dt.bfloat16
I32 = mybir.dt.int32
AF = mybir.ActivationFunctionType
ALU = mybir.AluOpType
AX = mybir.AxisListType

D = 2048
KC = 16
S = 8192
NTA = 64
NTO = 16
TO = 2048
E = 64
CAP = 896
JMAX = CAP // 128
NSLOT = E * CAP
BIG = 30000.0
EPS = 1e-6
NCORES = int(os.environ.get("MK_CORES", "8"))
DEBUG = os.environ.get("MK_DEBUG", "")

COMPUTE = ("pe", "act", "dve", "pool")
NDSEM = 6
_SYNC = [None]
USED_INPUTS = set()


class Sync:
    def __init__(self, nc, st):
        self.nc = nc
        self.cnt = {e: 0 for e in COMPUTE}
        self.dcnt = {}
        self.seen = {e: {} for e in ("pe", "act", "dve", "pool", "sp")}
        self.all_tokens = {}
        self.sems = {}
        keys = list(COMPUTE) + ["d_%s_%d" % (q, j) for q in ("sp", "pool", "poolw", "act") for j in range(NDSEM)]
        for k in keys:
            self.sems[k] = st.enter_context(nc.semaphore("s_" + k))


class Phase:
    def __init__(self, nc, name):
        self.nc = nc
        self.name = name
        self.sy = _SYNC[0]
        self.streams = {e: [] for e in ("pe", "act", "dve", "pool", "sp")}
        self.last_w = {}
        self.readers = {}
        self.region = None
        self.cnt_ap = None

    def begin_region(self, expert, thresh):
        self.region = {"expert": expert, "thresh": thresh, "before": dict(self.sy.all_tokens)}

    def end_region(self):
        self.region = None

    def load_cond_reg(self, expert, res):
        t = self.last_w.get(res)
        for eng in self.streams:
            w = self._waits(eng, [t] if t is not None else [])
            self.streams[eng].append((w, ("reg", expert), None, None))

    def _deps(self, reads, writes):
        deps = []
        for r in reads:
            t = self.last_w.get(r)
            if t is not None:
                deps.append(t)
        for w in writes:
            t = self.last_w.get(w)
            if t is not None:
                deps.append(t)
            deps.extend(self.readers.get(w, ()))
        return deps

    def _waits(self, eng, deps):
        out = {}
        seen = self.sy.seen[eng]
        for (k, v) in deps:
            if eng == "pe" and k == "pe":
                continue
            if seen.get(k, 0) >= v:
                continue
            if out.get(k, 0) < v:
                out[k] = v
        for k, v in out.items():
            seen[k] = v
        return list(out.items())

    def _commit(self, tok, reads, writes):
        for r in reads:
            self.readers.setdefault(r, []).append(tok)
        for w in writes:
            self.last_w[w] = tok
            self.readers[w] = []
        k, v = tok
        if self.sy.all_tokens.get(k, 0) < v:
            self.sy.all_tokens[k] = v

    def op(self, eng, fn, reads=(), writes=()):
        deps = self._deps(reads, writes)
        waits = self._waits(eng, deps)
        self.sy.cnt[eng] += 1
        tok = (eng, self.sy.cnt[eng])
        self.streams[eng].append((waits, fn, tok, self.region))
        self._commit(tok, reads, writes)
        return tok

    def dma(self, q, fn, reads=(), writes=(), cls=""):
        deps = self._deps(reads, writes)
        i = self.sy.dcnt.get(q + cls, 0)
        self.sy.dcnt[q + cls] = i + 1
        semkey = "d_%s%s_%d" % (q, cls, i % NDSEM)
        tok = (semkey, 16 * (i // NDSEM + 1))
        if i >= NDSEM:
            deps.append((semkey, 16 * (i // NDSEM)))
        waits = self._waits(q, deps)
        self.streams[q].append((waits, fn, tok, self.region))
        self._commit(tok, reads, writes)
        return tok

    def run(self):
        nc = self.nc
        sems = self.sy.sems
        toks = list(self.sy.all_tokens.items())
        for eng in self.streams:
            w = self._waits(eng, toks)
            if w:
                self.streams[eng].append((w, None, None, None))
        cnt_ap = self.cnt_ap
        with nc.Block() as block:

            def emit(engine, entry):
                waits, fn, tok, _ = entry
                for (k, v) in waits:
                    engine.wait_ge(sems[k], v)
                if fn is None:
                    return
                ins = fn(engine)
                k, v = tok
                ins.then_inc(sems[k], 1 if k in COMPUTE else 16)

            def replay(engine, stream):
                reg = None
                i = 0
                n = len(stream)
                while i < n:
                    entry = stream[i]
                    waits, fn, tok, region = entry
                    if isinstance(fn, tuple):
                        for (k, v) in waits:
                            engine.wait_ge(sems[k], v)
                        if reg is None:
                            reg = engine.alloc_register("cnt_reg")
                        engine.reg_load(reg, cnt_ap[0:1, fn[1]:fn[1] + 1])
                        i += 1
                        continue
                    if region is None:
                        emit(engine, entry)
                        i += 1
                        continue
                    groups = []
                    j = i
                    while j < n and stream[j][3] is not None and stream[j][3]["expert"] == region["expert"]:
                        r_ = stream[j][3]
                        j2 = j
                        while j2 < n and stream[j2][3] is r_:
                            j2 += 1
                        groups.append((r_, stream[j:j2]))
                        j = j2

                    def compensate(rest):
                        before = rest[0][0]["before"]
                        ext = {}
                        incs = {}
                        for (_, grp) in rest:
                            for (w_, f_, t_, _r) in grp:
                                for (k, v) in w_:
                                    v2 = min(v, before.get(k, 0))
                                    if v2 > 0 and ext.get(k, 0) < v2:
                                        ext[k] = v2
                                if t_ is not None:
                                    k, v = t_
                                    incs[k] = incs.get(k, 0) + (1 if k in COMPUTE else 16)
                        for k in incs:
                            b = before.get(k, 0)
                            if b > 0 and ext.get(k, 0) < b:
                                ext[k] = b
                        for k, v in ext.items():
                            engine.wait_ge(sems[k], v)
                        for k, v in incs.items():
                            engine.sem_inc(sems[k], v)

                    def chain(gi_):
                        if gi_ == len(groups):
                            return
                        r_, grp = groups[gi_]
                        with engine.If_lt(reg, r_["thresh"] + 1):
                            compensate(groups[gi_:])
                        with engine.Else():
                            for e_ in grp:
                                emit(engine, e_)
                            chain(gi_ + 1)
                    chain(0)
                    i = j

            @block.tensor
            def _(e):
                replay(e, self.streams["pe"])

            @block.scalar
            def _(e):
                replay(e, self.streams["act"])

            @block.vector
            def _(e):
                replay(e, self.streams["dve"])

            @block.gpsimd
            def _(e):
                replay(e, self.streams["pool"])

            @block.sync
            def _(e):
                replay(e, self.streams["sp"])


def sb(st, nc, name, shape, dt):
    return st.enter_context(nc.sbuf_tensor(name, shape, dt))


def ps(st, nc, name, shape, dt):
    return st.enter_context(nc.psum_tensor(name, shape, dt))


def build_program(stop_after=99):
    nc = bass.Bass("TRN2", target_bir_lowering=False)
    USED_INPUTS.clear()

    def din(name, shape, dt=F32):
        A[name] = nc.dram_tensor(name, list(shape), dt, kind="ExternalInput").ap()

    def dscr(name, shape, dt):
        kind = "ExternalOutput" if name in DEBUG.split(",") else "Internal"
        A[name] = nc.dram_tensor(name, list(shape), dt, kind=kind).ap()

    in_specs = {
        "x_all": ([S, D], F32),
        "x_own": ([TO, D], F32),
        "cT": ([128, KC], F32),
        "w_ada": ([D, 6 * D], F32),
        "b_ada_b": ([128, 6 * D], F32),
        "g_mix_b": ([128, D], F32),
        "g_ffn_b": ([128, D], F32),
        "g_final_b": ([128, D], F32),
        "w_in": ([D, 10240], F32),
        "lam_b": ([128, 256], F32),
        "subln_b": ([128, 128], F32),
        "BT": ([128, 16 * 5 * 128], BF16),
        "b31": ([128, 16], F32),
        "pen": ([128, NTO * 32], F32),
        "past": ([128, NTO * 32], F32),
        "own": ([128, NTO * 32], F32),
        "w_o_diff": ([1024, D], F32),
        "w_o_moba": ([1024, D], F32),
        "w_out": ([D, D], F32),
        "w_router": ([D, E], F32),
        "rbias_b": ([128, E], F32),
        "w_exp_gate": ([E, D, 512], F32),
        "w_exp_up": ([E, D, 512], F32),
        "w_exp_down": ([E, 512, D], F32),
        "w_sh_gate": ([D, 512], F32),
        "w_sh_up": ([D, 512], F32),
        "w_sh_down": ([512, D], F32),
        "ident_bf": ([128, 128], BF16),
        "ident_f": ([128, 128], F32),
        "ltri": ([128, 128], BF16),
        "selc": ([32, 32 * 128], BF16),
        "ecap": ([128, E], F32),
        "dumpidx": ([128, 1], F32),
        "tokid": ([128, NTO], I32),
    }

    class LazyA(dict):
        def __missing__(self, name):
            shape, dt = in_specs[name]
            ap = nc.dram_tensor(name, list(shape), dt, kind="ExternalInput").ap()
            self[name] = ap
            USED_INPUTS.add(name)
            return ap
    A = LazyA()
    A["out"] = nc.dram_tensor("out", [TO, D], F32, kind="ExternalOutput").ap()

    dscr("modb", [128, 6 * D], F32)
    dscr("hT_all", [NTA, 128, KC * 128], BF16)
    dscr("hT_own", [NTO, 128, KC * 128], BF16)
    dscr("QTd", [8, 128, TO], BF16); dscr("KTd", [8, 128, S], BF16); dscr("Vd", [S, 1024], BF16)
    dscr("QTm", [8, 128, TO], BF16); dscr("KTm", [8, 128, S], BF16); dscr("Vm", [S, 1024], BF16)
    dscr("KMT", [8, 128, 32], F32)
    dscr("GT", [32, 128, TO], BF16)
    dscr("o_d", [TO, D], BF16)
    dscr("zT", [4, 128, KC * 512], BF16)
    dscr("x1", [TO, D], F32)
    dscr("h2", [TO, D], BF16)
    dscr("h2T", [NTO, 128, KC * 128], BF16)
    dscr("rowtok", [NSLOT + 128, 8], I32)
    dscr("Ybuf", [NSLOT + 128, D], BF16)
    dscr("Ysh", [TO, D], BF16)
    dscr("route", [128, NTO * 16], F32)
    dscr("cnt_d", [1, E], I32)

    gst = contextlib.ExitStack()
    gst.__enter__()
    _SYNC[0] = Sync(nc, gst)
    if stop_after >= 1:
        phase_mod(nc, A)
    if stop_after >= 2:
        phase_h(nc, A)
    if stop_after >= 3:
        phase_proj(nc, A)
    if stop_after >= 4:
        phase_attn(nc, A)
    if stop_after >= 5:
        phase_oproj(nc, A)
    if stop_after >= 6:
        phase_wout(nc, A)
    if stop_after >= 7:
        phase_route(nc, A)
    if stop_after >= 8:
        phase_experts(nc, A)
    if stop_after >= 9:
        phase_final(nc, A)
    gst.__exit__(None, None, None)
    return nc


def phase_mod(nc, A):
    with contextlib.ExitStack() as st:
        P = Phase(nc, "p1")
        cT = sb(st, nc, "p1_cT", [128, KC], F32)
        cact = sb(st, nc, "p1_cact", [128, KC], F32)
        ones = sb(st, nc, "p1_ones", [128, 128], F32)
        L = sb(st, nc, "p1_L", [128, KC, 128], BF16)
        wt = [sb(st, nc, "p1_w%d" % i, [128, KC, 512], BF16) for i in range(2)]
        bt = [sb(st, nc, "p1_b%d" % i, [128, 512], F32) for i in range(2)]
        gt = [sb(st, nc, "p1_g%d" % i, [128, 512], F32) for i in range(2)]
        ot = [sb(st, nc, "p1_o%d" % i, [128, 512], F32) for i in range(2)]
        pp = [ps(st, nc, "p1_ps%d" % i, [128, 512], F32) for i in range(2)]
        P.dma("sp", lambda e: e.dma_start(out=cT[:], in_=A["cT"][:, :]), writes=["cT"])
        P.op("dve", lambda e: e.memset(ones[:], 1.0), writes=["ones"])
        P.op("act", lambda e: e.activation(out=cact[:], in_=cT[:], func=AF.Silu), reads=["cT"], writes=["cact"])
        for j in range(KC):
            P.op("dve", lambda e, j=j: e.tensor_scalar(out=L[:, j, :], in0=ones[:], scalar1=cact[:, j:j + 1], scalar2=None, op0=ALU.mult),
                 reads=["cact", "ones"], writes=["L"])
        w_view = A["w_ada"].rearrange("(k p) n -> p k n", p=128)
        for n in range(24):
            i = n % 2
            P.dma("pool", lambda e, n=n, i=i: e.dma_start(out=wt[i][:], in_=w_view[:, :, n * 512:(n + 1) * 512]), writes=["w%d" % i])
            P.dma("sp", lambda e, n=n, i=i: e.dma_start(out=bt[i][:], in_=A["b_ada_b"][:, n * 512:(n + 1) * 512]), writes=["b%d" % i])
            kind = n // 4
            if kind in (1, 4):
                gsrc = A["g_mix_b"] if kind == 1 else A["g_ffn_b"]
                c0 = (n % 4) * 512
                P.dma("sp", lambda e, i=i, gsrc=gsrc, c0=c0: e.dma_start(out=gt[i][:], in_=gsrc[:, c0:c0 + 512]), writes=["g%d" % i])

            def mm(e, i=i):
                ins = None
                for k in range(KC):
                    ins = e.matmul(pp[i][:], lhsT=L[:, k, :], rhs=wt[i][:, k, :], start=(k == 0), stop=(k == KC - 1))
                return ins
            P.op("pe", mm, reads=["L", "w%d" % i], writes=["ps%d" % i])
            if kind in (1, 4):
                P.op("dve", lambda e, i=i: e.tensor_tensor(out=ot[i][:], in0=pp[i][:], in1=bt[i][:], op=ALU.add),
                     reads=["ps%d" % i, "b%d" % i], writes=["o%d" % i])
                P.op("dve", lambda e, i=i: e.scalar_tensor_tensor(out=ot[i][:], in0=ot[i][:], scalar=1.0, in1=gt[i][:], op0=ALU.add, op1=ALU.mult),
                     reads=["o%d" % i, "g%d" % i], writes=["o%d" % i])
            else:
                P.op("dve", lambda e, i=i: e.tensor_tensor(out=ot[i][:], in0=pp[i][:], in1=bt[i][:], op=ALU.add),
                     reads=["ps%d" % i, "b%d" % i], writes=["o%d" % i])
            P.dma("sp", lambda e, n=n, i=i: e.dma_start(out=A["modb"][:, n * 512:(n + 1) * 512], in_=ot[i][:]), reads=["o%d" % i], writes=["modb"])
        P.run()


MOD_SHIFT1, MOD_A1, MOD_G1, MOD_SHIFT2, MOD_A2, MOD_G2 = [i * D for i in range(6)]


def emit_norm_mod_T(P, nc, xt, sq, ss, rstd, hf, hb, pT, hT, Amod, Smod, ident, tag, xres, extra_reads=()):
    P.op("act", lambda e: e.activation(out=sq[:], in_=xt[:], func=AF.Square, accum_out=ss[:]),
         reads=[xres], writes=["sq" + tag, "ss" + tag])
    P.op("dve", lambda e: e.tensor_scalar(out=rstd[:], in0=ss[:], scalar1=1.0 / D, scalar2=EPS, op0=ALU.mult, op1=ALU.add),
         reads=["ss" + tag], writes=["rstd" + tag])
    P.op("act", lambda e: e.activation(out=rstd[:], in_=rstd[:], func=AF.Sqrt), reads=["rstd" + tag], writes=["rstd" + tag])
    P.op("dve", lambda e: e.reciprocal(out=rstd[:], in_=rstd[:]), reads=["rstd" + tag], writes=["rstd" + tag])
    P.op("dve", lambda e: e.scalar_tensor_tensor(out=hf[:], in0=xt[:], scalar=rstd[:, 0:1], in1=Amod[:], op0=ALU.mult, op1=ALU.mult),
         reads=[xres, "rstd" + tag, "Amod"] + list(extra_reads), writes=["hf" + tag])
    P.op("pool", lambda e: e.tensor_tensor(out=hb[:], in0=hf[:], in1=Smod[:], op=ALU.add),
         reads=["hf" + tag, "Smod"], writes=["hb" + tag])

    def tr(e):
        ins = None
        for k in range(KC):
            ins = e.transpose(out=pT[:, k, :], in_=hb[:, k * 128:(k + 1) * 128], identity=ident[:])
        return ins
    P.op("pe", tr, reads=["hb" + tag, "ident"], writes=["pT" + tag])
    P.op("act", lambda e: e.activation(out=hT[:], in_=pT[:], func=AF.Copy), reads=["pT" + tag], writes=["hT" + tag])


def phase_h(nc, A):
    with contextlib.ExitStack() as st:
        P = Phase(nc, "p2")
        ident = sb(st, nc, "p2_ident", [128, 128], BF16)
        Amod = sb(st, nc, "p2_A", [128, D], F32)
        Smod = sb(st, nc, "p2_S", [128, D], F32)
        xt = [sb(st, nc, "p2_x%d" % i, [128, D], F32) for i in range(3)]
        sq = [sb(st, nc, "p2_sq%d" % i, [128, D], BF16) for i in range(3)]
        ss = [sb(st, nc, "p2_ss%d" % i, [128, 1], F32) for i in range(3)]
        rstd = [sb(st, nc, "p2_rs%d" % i, [128, 1], F32) for i in range(3)]
        hf = [sb(st, nc, "p2_hf%d" % i, [128, D], F32) for i in range(3)]
        hb = [sb(st, nc, "p2_hb%d" % i, [128, D], BF16) for i in range(3)]
        hT = [sb(st, nc, "p2_hT%d" % i, [128, KC, 128], BF16) for i in range(3)]
        pT = [ps(st, nc, "p2_pT%d" % i, [128, KC, 128], BF16) for i in range(3)]
        P.dma("sp", lambda e: e.dma_start(out=ident[:], in_=A["ident_bf"][:, :]), writes=["ident"])
        P.dma("sp", lambda e: e.dma_start(out=Amod[:], in_=A["modb"][:, MOD_A1:MOD_A1 + D]), writes=["Amod"])
        P.dma("sp", lambda e: e.dma_start(out=Smod[:], in_=A["modb"][:, MOD_SHIFT1:MOD_SHIFT1 + D]), writes=["Smod"])
        for t in range(NTA + NTO):
            i = t % 3
            tag = str(i)
            if t < NTA:
                src = A["x_all"][t * 128:(t + 1) * 128, :]
                dst = A["hT_all"][t]
            else:
                src = A["x_own"][(t - NTA) * 128:(t - NTA + 1) * 128, :]
                dst = A["hT_own"][t - NTA]
            P.dma("sp", lambda e, i=i, src=src: e.dma_start(out=xt[i][:], in_=src), writes=["x" + tag])
            emit_norm_mod_T(P, nc, xt[i], sq[i], ss[i], rstd[i], hf[i], hb[i], pT[i], hT[i], Amod, Smod, ident, tag, "x" + tag)
            P.dma("pool", lambda e, i=i, dst=dst: e.dma_start(out=dst, in_=hT[i][:].rearrange("p k t -> p (k t)")),
                  reads=["hT" + tag], writes=["hTd"])
        P.run()


def phase_proj(nc, A):
    with contextlib.ExitStack() as st:
        P = Phase(nc, "p3")
        W = [sb(st, nc, "p3_W%d" % i, [128, KC, 1024], BF16) for i in range(2)]
        H = [sb(st, nc, "p3_H%d" % i, [128, 4, KC * 128], BF16) for i in range(2)]
        O = [sb(st, nc, "p3_O%d" % i, [128, 8, 512], BF16) for i in range(2)]
        KM = sb(st, nc, "p3_KM", [128, 8, 32], F32)
        pp = [ps(st, nc, "p3_ps%d" % i, [128, 512], F32) for i in range(6)]
        w_view = A["w_in"].rearrange("(k p) n -> p k n", p=128)
        passes = [
            ("qd", 0, "own", "fm"), ("kd", 1024, "all", "fm"), ("vd", 2048, "all", "tm"),
            ("qm", 3072, "own", "fm"), ("km", 4096, "all", "fm"), ("vm", 5120, "all", "tm"),
            ("g0", 6144, "own", "fm"), ("g1", 7168, "own", "fm"), ("g2", 8192, "own", "fm"), ("g3", 9216, "own", "fm"),
        ]
        gcount = 0
        pcount = 0
        if os.environ.get("MK_P3"):
            passes = [passes[int(i)] for i in os.environ["MK_P3"].split(",")]
        for pi, (pname, c0, tset, kind) in enumerate(passes):
            wi = pi % 2
            wres = "W%d" % wi

            def loadW(pj):
                wj = pj % 2
                cj = passes[pj][1]
                for q4 in range(4):
                    P.dma("pool", lambda e, wj=wj, cj=cj, q4=q4: e.dma_start(out=W[wj][:, q4 * 4:(q4 + 1) * 4, :], in_=w_view[:, q4 * 4:(q4 + 1) * 4, cj:cj + 1024]),
                          writes=["W%d" % wj], cls="w")
            if pi == 0:
                loadW(0)
            if pi + 1 < len(passes):
                loadW(pi + 1)
            ngroups = 16 if tset == "all" else 4
            src = A["hT_all"] if tset == "all" else A["hT_own"]
            for g in range(ngroups):
                hi = gcount % 2
                gcount += 1
                hres = "H%d" % hi
                P.dma("sp", lambda e, hi=hi, src=src, g=g: e.dma_start(out=H[hi][:], in_=src[g * 4:(g + 1) * 4].rearrange("t p f -> p t f")),
                      writes=[hres])
                oi = g % 2
                ores = "O%d" % oi
                if kind == "fm":
                    for ch in range(8):
                        pidx = pcount % 6
                        pcount += 1
                        pres = "ps%d" % pidx

                        def mm(e, wi=wi, hi=hi, ch=ch, pidx=pidx):
                            ins = None
                            for k in range(KC):
                                ins = e.matmul(pp[pidx][:].rearrange("p (t q) -> p t q", t=4), lhsT=W[wi][:, k, ch * 128:(ch + 1) * 128],
                                               rhs=H[hi][:, :, k * 128:(k + 1) * 128], start=(k == 0), stop=(k == KC - 1))
                            return ins
                        P.op("pe", mm, reads=[wres, hres], writes=[pres])
                        if pname in ("qd", "qm"):
                            sc = 0.125 if pname == "qd" else float(128 ** -0.5)
                            P.op("act", lambda e, oi=oi, ch=ch, pidx=pidx, sc=sc: e.activation(out=O[oi][:, ch, :], in_=pp[pidx][:], func=AF.Copy, scale=sc),
                                 reads=[pres], writes=[ores])
                        elif pname.startswith("g"):
                            P.op("act", lambda e, oi=oi, ch=ch, pidx=pidx: e.activation(out=O[oi][:, ch, :], in_=pp[pidx][:], func=AF.Sigmoid),
                                 reads=[pres], writes=[ores])
                        elif pname == "km":
                            for bb in range(2):
                                P.op("act", lambda e, oi=oi, ch=ch, pidx=pidx, g=g, bb=bb: e.activation(out=O[oi][:, ch, bb * 256:(bb + 1) * 256], in_=pp[pidx][:, bb * 256:(bb + 1) * 256],
                                                                                                    func=AF.Copy, accum_out=KM[:, ch, 2 * g + bb:2 * g + bb + 1]),
                                     reads=[pres], writes=[ores, "KM"])
                        else:
                            eng = "dve" if ch % 2 == 0 else "act"
                            if eng == "dve":
                                P.op("dve", lambda e, oi=oi, ch=ch, pidx=pidx: e.tensor_copy(out=O[oi][:, ch, :], in_=pp[pidx][:]), reads=[pres], writes=[ores])
                            else:
                                P.op("act", lambda e, oi=oi, ch=ch, pidx=pidx: e.activation(out=O[oi][:, ch, :], in_=pp[pidx][:], func=AF.Copy), reads=[pres], writes=[ores])
                    if pname == "qd":
                        dst = A["QTd"][:, :, g * 512:(g + 1) * 512]
                    elif pname == "kd":
                        dst = A["KTd"][:, :, g * 512:(g + 1) * 512]
                    elif pname == "qm":
                        dst = A["QTm"][:, :, g * 512:(g + 1) * 512]
                    elif pname == "km":
                        dst = A["KTm"][:, :, g * 512:(g + 1) * 512]
                    else:
                        gi = int(pname[1])
                        dst = A["GT"][gi * 8:(gi + 1) * 8, :, g * 512:(g + 1) * 512]
                    P.dma("pool", lambda e, oi=oi, dst=dst: e.dma_start(out=dst.rearrange("c p t -> p c t"), in_=O[oi][:]), reads=[ores], writes=["dram_" + pname])
                else:
                    Ov = O[oi][:].rearrange("p c t -> p (c t)").rearrange("p (t n) -> p t n", t=4)
                    for tt in range(4):
                        for half in range(2):
                            pidx = pcount % 6
                            pcount += 1
                            pres = "ps%d" % pidx

                            def mm(e, wi=wi, hi=hi, tt=tt, half=half, pidx=pidx):
                                ins = None
                                for k in range(KC):
                                    ins = e.matmul(pp[pidx][:], lhsT=H[hi][:, tt, k * 128:(k + 1) * 128], rhs=W[wi][:, k, half * 512:(half + 1) * 512],
                                                   start=(k == 0), stop=(k == KC - 1))
                                return ins
                            P.op("pe", mm, reads=[wres, hres], writes=[pres])
                            if (tt * 2 + half) % 2 == 0:
                                P.op("dve", lambda e, Ov=Ov, tt=tt, half=half, pidx=pidx: e.tensor_copy(out=Ov[:, tt, half * 512:(half + 1) * 512], in_=pp[pidx][:]),
                                     reads=[pres], writes=[ores])
                            else:
                                P.op("act", lambda e, Ov=Ov, tt=tt, half=half, pidx=pidx: e.activation(out=Ov[:, tt, half * 512:(half + 1) * 512], in_=pp[pidx][:], func=AF.Copy),
                                     reads=[pres], writes=[ores])
                    dstT = A["Vd"] if pname == "vd" else A["Vm"]
                    dst = dstT[g * 512:(g + 1) * 512, :].rearrange("(t p) n -> p t n", p=128)
                    P.dma("pool", lambda e, Ov=Ov, dst=dst: e.dma_start(out=dst, in_=Ov), reads=[ores], writes=["dram_" + pname])
            if pname == "km":
                P.op("dve", lambda e: e.tensor_scalar(out=KM[:], in0=KM[:], scalar1=1.0 / 256.0, scalar2=None, op0=ALU.mult), reads=["KM"], writes=["KM"])
                P.dma("sp", lambda e: e.dma_start(out=A["KMT"].rearrange("h p n -> p h n"), in_=KM[:]), reads=["KM"], writes=["dram_KMT"])
        P.run()


def phase_attn(nc, A):
    with contextlib.ExitStack() as st:
        P = Phase(nc, "p4")
        ident = sb(st, nc, "p4_ident", [128, 128], BF16)
        identf = sb(st, nc, "p4_identf", [128, 128], F32)
        BT = sb(st, nc, "p4_BT", [128, 16, 5, 128], BF16)
        b31 = sb(st, nc, "p4_b31", [128, 16], F32)
        selc = sb(st, nc, "p4_sel", [32, 32, 128], BF16)
        pen = sb(st, nc, "p4_pen", [128, NTO, 32], F32)
        past = sb(st, nc, "p4_past", [128, NTO, 32], F32)
        own = sb(st, nc, "p4_own", [128, NTO, 32], F32)
        lamb = sb(st, nc, "p4_lamb", [128, 256], F32)
        lamt = sb(st, nc, "p4_lamt", [128, 128], F32)
        lam2 = sb(st, nc, "p4_lam2", [128, 2], F32)
        nlam = sb(st, nc, "p4_nlam", [128, 1], F32)
        subg = sb(st, nc, "p4_subg", [128, 128], F32)
        KTb = [sb(st, nc, "p4_KT%d" % i, [128, 2 * S], BF16) for i in range(2)]
        QTb = [sb(st, nc, "p4_QT%d" % i, [128, 2 * TO], BF16) for i in range(2)]
        Vb = [sb(st, nc, "p4_V%d" % i, [128, NTA, 130], BF16) for i in range(2)]
        KMb = [sb(st, nc, "p4_KM%d" % i, [128, 32], F32) for i in range(2)]
        QTf = [sb(st, nc, "p4_QTf%d" % i, [128, 128], F32) for i in range(2)]
        Pb = [sb(st, nc, "p4_P%d" % i, [128, 512], BF16) for i in range(6)]
        oh = [sb(st, nc, "p4_oh%d" % i, [128, NTO, 128], BF16) for i in range(2)]
        gate = [sb(st, nc, "p4_gate%d" % i, [128, 32], F32) for i in range(2)]
        top8 = [sb(st, nc, "p4_top8%d" % i, [128, 8], F32) for i in range(2)]
        mb = [sb(st, nc, "p4_mb%d" % i, [128, 32], F32) for i in range(2)]
        mbT = [sb(st, nc, "p4_mbT%d" % i, [32, 128], BF16) for i in range(2)]
        rl = [sb(st, nc, "p4_rl%d" % i, [128, 2], F32) for i in range(2)]
        o1 = [sb(st, nc, "p4_o1%d" % i, [128, 128], F32) for i in range(2)]
        o2 = [sb(st, nc, "p4_o2%d" % i, [128, 128], F32) for i in range(2)]
        junk = [sb(st, nc, "p4_junk%d" % i, [128, 128], F32) for i in range(2)]
        ssq = [sb(st, nc, "p4_ssq%d" % i, [128, 1], F32) for i in range(2)]
        pS = [ps(st, nc, "p4_pS%d" % i, [128, 512], F32) for i in range(4)]
        pO = [ps(st, nc, "p4_pO%d" % i, [128, 512], F32) for i in range(2)]
        pOb = [ps(st, nc, "p4_pOb%d" % i, [128, 512], F32) for i in range(2)]
        pG = pOb[0][:, 0:32]
        pM = pOb[1][0:32, 0:128]

        P.dma("sp", lambda e: e.dma_start(out=ident[:], in_=A["ident_bf"][:, :]), writes=["ident"])
        P.dma("sp", lambda e: e.dma_start(out=identf[:], in_=A["ident_f"][:, :]), writes=["identf"])
        P.dma("sp", lambda e: e.dma_start(out=BT[:].rearrange("p h j q -> p (h j q)"), in_=A["BT"][:, :]), writes=["BT"])
        P.dma("sp", lambda e: e.dma_start(out=b31[:], in_=A["b31"][:, :]), writes=["b31"])
        P.dma("sp", lambda e: e.dma_start(out=selc[:].rearrange("p n k -> p (n k)"), in_=A["selc"][:, :]), writes=["selc"])
        P.dma("sp", lambda e: e.dma_start(out=pen[:].rearrange("p m n -> p (m n)"), in_=A["pen"][:, :]), writes=["pen"])
        P.dma("sp", lambda e: e.dma_start(out=past[:].rearrange("p m n -> p (m n)"), in_=A["past"][:, :]), writes=["past"])
        P.dma("sp", lambda e: e.dma_start(out=own[:].rearrange("p m n -> p (m n)"), in_=A["own"][:, :]), writes=["own"])
        P.dma("sp", lambda e: e.dma_start(out=lamb[:], in_=A["lam_b"][:, :]), writes=["lamb"])
        P.dma("sp", lambda e: e.dma_start(out=subg[:], in_=A["subln_b"][:, :]), writes=["subg"])
        lv = lamb[:].rearrange("p (a d) -> p a d", a=4)
        P.op("dve", lambda e: e.tensor_tensor(out=lamt[:, 0:64], in0=lv[:, 0, :], in1=lv[:, 1, :], op=ALU.mult), reads=["lamb"], writes=["lamt"])
        P.op("dve", lambda e: e.tensor_tensor(out=lamt[:, 64:128], in0=lv[:, 2, :], in1=lv[:, 3, :], op=ALU.mult), reads=["lamb", "lamt"], writes=["lamt"])
        for a_ in range(2):
            P.op("act", lambda e, a_=a_: e.activation(out=lamb[:, a_ * 64:(a_ + 1) * 64], in_=lamt[:, a_ * 64:(a_ + 1) * 64], func=AF.Copy, accum_out=lam2[:, a_:a_ + 1]),
                 reads=["lamt"], writes=["lam2", "lamb"])
        P.op("act", lambda e: e.activation(out=lam2[:], in_=lam2[:], func=AF.Exp), reads=["lam2"], writes=["lam2"])
        P.op("dve", lambda e: e.tensor_tensor(out=nlam[:], in0=lam2[:, 1:2], in1=lam2[:, 0:1], op=ALU.subtract), reads=["lam2"], writes=["nlam"])
        P.op("dve", lambda e: e.tensor_scalar(out=nlam[:], in0=nlam[:], scalar1=-0.2, scalar2=None, op0=ALU.add), reads=["nlam"], writes=["nlam"])
        P.op("dve", lambda e: e.tensor_scalar(out=subg[:], in0=subg[:], scalar1=0.8, scalar2=None, op0=ALU.mult), reads=["subg"], writes=["subg"])
        for i in range(2):
            P.op("pool", lambda e, i=i: e.memset(Vb[i][:, :, 128:130], 1.0), writes=["V%d" % i])

        scount = 0
        pcount = 0
        qcount = 0
        def load_head(h):
            hb_ = h % 2
            diff = h < 8
            hh = h if diff else h - 8
            kres, qres, vres, kmres = "KT%d" % hb_, "QT%d" % hb_, "V%d" % hb_, "KM%d" % hb_
            if diff:
                for mp in range(2):
                    P.dma("sp", lambda e, hb_=hb_, hh=hh, mp=mp: e.dma_start(out=KTb[hb_][0:64, mp * S:(mp + 1) * S], in_=A["KTd"][hh, mp * 64:(mp + 1) * 64, :]), writes=[kres])
                    P.dma("sp", lambda e, hb_=hb_, hh=hh, mp=mp: e.dma_start(out=QTb[hb_][0:64, mp * TO:(mp + 1) * TO], in_=A["QTd"][hh, mp * 64:(mp + 1) * 64, :]), writes=[qres])
                vsrc = A["Vd"]
            else:
                P.dma("sp", lambda e, hb_=hb_, hh=hh: e.dma_start(out=KTb[hb_][:, 0:S], in_=A["KTm"][hh]), writes=[kres])
                P.dma("sp", lambda e, hb_=hb_, hh=hh: e.dma_start(out=QTb[hb_][:, 0:TO], in_=A["QTm"][hh]), writes=[qres])
                P.dma("sp", lambda e, hb_=hb_, hh=hh: e.dma_start(out=KMb[hb_][:], in_=A["KMT"][hh]), writes=[kmres])
                vsrc = A["Vm"]
            P.dma("sp", lambda e, hb_=hb_, hh=hh, vsrc=vsrc: e.dma_start(out=Vb[hb_][:, :, 0:128], in_=vsrc[:, hh * 128:(hh + 1) * 128].rearrange("(t p) d -> p t d", p=128)),
                  writes=[vres])

        load_head(0)
        for h in range(16):
            hb_ = h % 2
            diff = h < 8
            hh = h if diff else h - 8
            kres, qres, vres, kmres = "KT%d" % hb_, "QT%d" % hb_, "V%d" % hb_, "KM%d" % hb_
            if h + 1 < 16:
                load_head(h + 1)
            nmaps = 2 if diff else 1
            ohres = "oh%d" % hb_

            def emit_pre(m, hb_=hb_, kmres=kmres, qres=qres, diff=diff):
                qb = m % 2
                if diff:
                    return
                gres, tres, mres, mtres = "gate%d" % qb, "top8%d" % qb, "mb%d" % qb, "mbT%d" % qb
                P.op("act", lambda e, qb=qb, hb_=hb_, m=m: e.activation(out=QTf[qb][:], in_=QTb[hb_][:, m * 128:(m + 1) * 128], func=AF.Copy), reads=[qres], writes=["QTf%d" % qb])
                P.op("pe", lambda e, qb=qb, hb_=hb_: e.matmul(pG, lhsT=QTf[qb][:], rhs=KMb[hb_][:], start=True, stop=True), reads=["QTf%d" % qb, kmres], writes=["pOb0"])
                P.op("dve", lambda e, qb=qb, m=m: e.tensor_tensor(out=gate[qb][:], in0=pG, in1=pen[:, m, :], op=ALU.add), reads=["pOb0", "pen"], writes=[gres])
                P.op("dve", lambda e, qb=qb: e.max(out=top8[qb][:], in_=gate[qb][:]), reads=[gres], writes=[tres])
                P.op("dve", lambda e, qb=qb: e.tensor_scalar(out=mb[qb][:], in0=gate[qb][:], scalar1=top8[qb][:, 2:3], scalar2=None, op0=ALU.is_ge), reads=[gres, tres], writes=[mres])
                P.op("dve", lambda e, qb=qb, m=m: e.tensor_tensor(out=mb[qb][:], in0=mb[qb][:], in1=past[:, m, :], op=ALU.mult), reads=[mres, "past"], writes=[mres])
                P.op("dve", lambda e, qb=qb, m=m: e.tensor_tensor(out=mb[qb][:], in0=mb[qb][:], in1=own[:, m, :], op=ALU.add), reads=[mres, "own"], writes=[mres])
                P.op("dve", lambda e, qb=qb: e.tensor_scalar(out=mb[qb][:], in0=mb[qb][:], scalar1=-1.0, scalar2=BIG, op0=ALU.add, op1=ALU.mult), reads=[mres], writes=[mres])
                P.op("pe", lambda e, qb=qb: e.transpose(out=pM, in_=mb[qb][:], identity=identf[:]), reads=[mres, "identf"], writes=["pOb1"])
                P.op("dve", lambda e, qb=qb: e.tensor_copy(out=mbT[qb][:], in_=pM), reads=["pOb1"], writes=[mtres])

            def emit_qk(m, g, hb_=hb_, h=h, diff=diff, nmaps=nmaps, kres=kres, qres=qres):
                nonlocal scount
                qb = m % 2
                near_last = (g == m)
                prev_grp = (g == m - 1)
                sidx = []
                for mp in range(nmaps):
                    si = scount % 4
                    scount += 1
                    sidx.append(si)
                    sres = "pS%d" % si

                    def mmqk(e, si=si, mp=mp, g=g, m=m, hb_=hb_, h=h, diff=diff, near_last=near_last, prev_grp=prev_grp, qb=qb):
                        ins = None
                        for j4 in range(4):
                            kj = 4 * g + j4
                            if diff:
                                lhs = KTb[hb_][0:64, mp * S + kj * 128: mp * S + (kj + 1) * 128]
                                rhs = QTb[hb_][0:64, mp * TO + m * 128: mp * TO + (m + 1) * 128]
                            else:
                                lhs = KTb[hb_][:, kj * 128:(kj + 1) * 128]
                                rhs = QTb[hb_][:, m * 128:(m + 1) * 128]
                            bt_j = None
                            if near_last:
                                bt_j = j4 + 1
                            elif prev_grp and j4 == 3:
                                bt_j = 0
                            last = (bt_j is None) and diff
                            out = pS[si][:, j4 * 128:(j4 + 1) * 128]
                            ins = e.matmul(out, lhsT=lhs, rhs=rhs, start=True, stop=last)
                            if not diff:
                                ins = e.matmul(out, lhsT=selc[:, kj // 2, :], rhs=mbT[qb][:], start=False, stop=(bt_j is None))
                            if bt_j is not None:
                                ins = e.matmul(out, lhsT=ident[:], rhs=BT[:, h, bt_j, :], start=False, stop=True)
                        return ins
                    rd = [kres, qres, "ident", "BT"]
                    if not diff:
                        rd += ["selc", "mbT%d" % qb]
                    P.op("pe", mmqk, reads=rd, writes=[sres])
                return sidx

            def emit_exp_pv(m, g, sidx, hb_=hb_, h=h, diff=diff, nmaps=nmaps, vres=vres):
                nonlocal pcount
                qb = m % 2
                pOres = "pO%d" % qb
                pObres = "pOb%d" % qb
                near_last = (g == m)
                prev_grp = (g == m - 1)
                pidx = []
                for mp in range(nmaps):
                    si = sidx[mp]
                    pi = pcount % 6
                    pcount += 1
                    pidx.append(pi)
                    sres, pres = "pS%d" % si, "P%d" % pi
                    if near_last:
                        P.op("act", lambda e, pi=pi, si=si: e.activation(out=Pb[pi][:], in_=pS[si][:], func=AF.Exp), reads=[sres], writes=[pres])
                    elif prev_grp:
                        P.op("act", lambda e, pi=pi, si=si, h=h: e.activation(out=Pb[pi][:, 0:384], in_=pS[si][:, 0:384], func=AF.Exp, bias=b31[:, h:h + 1]), reads=[sres, "b31"], writes=[pres])
                        P.op("act", lambda e, pi=pi, si=si: e.activation(out=Pb[pi][:, 384:512], in_=pS[si][:, 384:512], func=AF.Exp), reads=[sres], writes=[pres])
                    else:
                        P.op("act", lambda e, pi=pi, si=si, h=h: e.activation(out=Pb[pi][:], in_=pS[si][:], func=AF.Exp, bias=b31[:, h:h + 1]), reads=[sres, "b31"], writes=[pres])
                for mp in range(nmaps):
                    pi = pidx[mp]

                    def mmpv(e, pi=pi, mp=mp, g=g, qb=qb, hb_=hb_, m=m):
                        ins = None
                        for j4 in range(4):
                            kj = 4 * g + j4
                            ins = e.matmul((pO if mp == 0 else pOb)[qb][:, 0:130], lhsT=Pb[pi][:, j4 * 128:(j4 + 1) * 128], rhs=Vb[hb_][:, kj, :],
                                           start=(kj == 0), stop=(kj == 4 * m + 3))
                        return ins
                    P.op("pe", mmpv, reads=["P%d" % pi, vres], writes=[pOres if mp == 0 else pObres])

            def emit_epi(m, hb_=hb_, diff=diff, ohres=ohres):
                qb = m % 2
                pOres = "pO%d" % qb
                pObres = "pOb%d" % qb
                rres, o1res, o2res = "rl%d" % qb, "o1%d" % qb, "o2%d" % qb
                if diff:
                    P.op("dve", lambda e, qb=qb: e.reciprocal(out=rl[qb][:, 0:1], in_=pO[qb][:, 128:129]), reads=[pOres], writes=[rres])
                    P.op("dve", lambda e, qb=qb: e.reciprocal(out=rl[qb][:, 1:2], in_=pOb[qb][:, 128:129]), reads=[pObres, rres], writes=[rres])
                    P.op("dve", lambda e, qb=qb: e.tensor_scalar(out=o1[qb][:], in0=pO[qb][:, 0:128], scalar1=rl[qb][:, 0:1], scalar2=None, op0=ALU.mult), reads=[pOres, rres], writes=[o1res])
                    P.op("dve", lambda e, qb=qb: e.tensor_scalar(out=o2[qb][:], in0=pOb[qb][:, 0:128], scalar1=rl[qb][:, 1:2], scalar2=nlam[:, 0:1], op0=ALU.mult, op1=ALU.mult),
                         reads=[pObres, rres, "nlam"], writes=[o2res])
                    P.op("dve", lambda e, qb=qb: e.tensor_tensor(out=o1[qb][:], in0=o1[qb][:], in1=o2[qb][:], op=ALU.add), reads=[o1res, o2res], writes=[o1res])
                    P.op("act", lambda e, qb=qb: e.activation(out=junk[qb][:], in_=o1[qb][:], func=AF.Square, accum_out=ssq[qb][:]), reads=[o1res], writes=["junk%d" % qb, "ssq%d" % qb])
                    P.op("dve", lambda e, qb=qb: e.tensor_scalar(out=ssq[qb][:], in0=ssq[qb][:], scalar1=1.0 / 128.0, scalar2=EPS, op0=ALU.mult, op1=ALU.add), reads=["ssq%d" % qb], writes=["ssq%d" % qb])
                    P.op("act", lambda e, qb=qb: e.activation(out=ssq[qb][:], in_=ssq[qb][:], func=AF.Sqrt), reads=["ssq%d" % qb], writes=["ssq%d" % qb])
                    P.op("dve", lambda e, qb=qb: e.reciprocal(out=ssq[qb][:], in_=ssq[qb][:]), reads=["ssq%d" % qb], writes=["ssq%d" % qb])
                    P.op("dve", lambda e, qb=qb, hb_=hb_, m=m: e.scalar_tensor_tensor(out=oh[hb_][:, m, :], in0=o1[qb][:], scalar=ssq[qb][:, 0:1], in1=subg[:], op0=ALU.mult, op1=ALU.mult),
                         reads=[o1res, "ssq%d" % qb, "subg"], writes=[ohres])
                else:
                    P.op("dve", lambda e, qb=qb: e.reciprocal(out=rl[qb][:, 0:1], in_=pO[qb][:, 128:129]), reads=[pOres], writes=[rres])
                    P.op("dve", lambda e, qb=qb, hb_=hb_, m=m: e.tensor_scalar(out=oh[hb_][:, m, :], in0=pO[qb][:, 0:128], scalar1=rl[qb][:, 0:1], scalar2=None, op0=ALU.mult),
                         reads=[pOres, rres], writes=[ohres])

            pending = None
            for m in range(NTO):
                for g in range(m + 1):
                    if g == 0:
                        emit_pre(m)
                    sidx = emit_qk(m, g)
                    if pending is not None:
                        emit_exp_pv(*pending)
                        if pending[1] == pending[0]:
                            emit_epi(pending[0])
                    pending = (m, g, sidx)
            emit_exp_pv(*pending)
            emit_epi(pending[0])
            P.dma("sp", lambda e, hb_=hb_, h=h: e.dma_start(out=A["o_d"][:, h * 128:(h + 1) * 128].rearrange("(m p) d -> p m d", p=128), in_=oh[hb_][:]),
                  reads=[ohres], writes=["dram_o"])
        P.run()


def phase_oproj(nc, A):
    with contextlib.ExitStack() as st:
        P = Phase(nc, "p5")
        ident = sb(st, nc, "p5_ident", [128, 128], BF16)
        Wd = sb(st, nc, "p5_Wd", [128, 8, D], BF16)
        Wm = sb(st, nc, "p5_Wm", [128, 8, D], BF16)
        ot = [sb(st, nc, "p5_ot%d" % i, [128, D], BF16) for i in range(2)]
        oT = [sb(st, nc, "p5_oT%d" % i, [128, KC, 512], BF16) for i in range(1)]
        Gd = [sb(st, nc, "p5_Gd%d" % i, [128, KC, 512], BF16) for i in range(1)]
        Gm = [sb(st, nc, "p5_Gm%d" % i, [128, KC, 512], BF16) for i in range(1)]
        zT = [sb(st, nc, "p5_zT%d" % i, [128, KC, 512], BF16) for i in range(1)]
        t1 = [sb(st, nc, "p5_t1%d" % i, [128, 512], F32) for i in range(2)]
        pT = [ps(st, nc, "p5_pT%d" % i, [128, KC, 128], BF16) for i in range(1)]
        pY = [ps(st, nc, "p5_pY%d" % i, [128, 512], F32) for i in range(4)]
        P.dma("sp", lambda e: e.dma_start(out=ident[:], in_=A["ident_bf"][:, :]), writes=["ident"])
        for q4 in range(2):
            P.dma("pool", lambda e, q4=q4: e.dma_start(out=Wd[:, q4 * 4:(q4 + 1) * 4, :], in_=A["w_o_diff"].rearrange("(k p) n -> p k n", p=128)[:, q4 * 4:(q4 + 1) * 4, :]), writes=["Wd"])
            P.dma("pool", lambda e, q4=q4: e.dma_start(out=Wm[:, q4 * 4:(q4 + 1) * 4, :], in_=A["w_o_moba"].rearrange("(k p) n -> p k n", p=128)[:, q4 * 4:(q4 + 1) * 4, :]), writes=["Wm"])
        tcount = 0
        ycount = 0
        for g in range(4):
            gi = 0
            P.dma("sp", lambda e, gi=gi, g=g: e.dma_start(out=Gd[gi][:], in_=A["GT"][0:16, :, g * 512:(g + 1) * 512].rearrange("c p t -> p c t")), writes=["Gd%d" % gi])
            P.dma("sp", lambda e, gi=gi, g=g: e.dma_start(out=Gm[gi][:], in_=A["GT"][16:32, :, g * 512:(g + 1) * 512].rearrange("c p t -> p c t")), writes=["Gm%d" % gi])
            for tt in range(4):
                ti = tcount % 2
                tcount += 1
                tile = g * 4 + tt
                P.dma("sp", lambda e, ti=ti, tile=tile: e.dma_start(out=ot[ti][:], in_=A["o_d"][tile * 128:(tile + 1) * 128, :]), writes=["ot%d" % ti])

                def tr(e, ti=ti):
                    ins = None
                    for k in range(KC):
                        ins = e.transpose(out=pT[0][:, k, :], in_=ot[ti][:, k * 128:(k + 1) * 128], identity=ident[:])
                    return ins
                P.op("pe", tr, reads=["ot%d" % ti, "ident"], writes=["pT"])
                P.op("act", lambda e, gi=gi, tt=tt: e.activation(out=oT[gi][:, :, tt * 128:(tt + 1) * 128], in_=pT[0][:], func=AF.Copy), reads=["pT"], writes=["oT%d" % gi])
            for c in range(KC):
                yd_i = ycount % 4
                ym_i = (ycount + 1) % 4
                ycount += 2
                t1i = c % 2

                def mmd(e, gi=gi, c=c, yd_i=yd_i):
                    ins = None
                    for k in range(8):
                        ins = e.matmul(pY[yd_i][:], lhsT=Wd[:, k, c * 128:(c + 1) * 128], rhs=oT[gi][:, k, :], start=(k == 0), stop=(k == 7))
                    return ins

                def mmm(e, gi=gi, c=c, ym_i=ym_i):
                    ins = None
                    for k in range(8):
                        ins = e.matmul(pY[ym_i][:], lhsT=Wm[:, k, c * 128:(c + 1) * 128], rhs=oT[gi][:, 8 + k, :], start=(k == 0), stop=(k == 7))
                    return ins
                P.op("pe", mmd, reads=["Wd", "oT%d" % gi], writes=["pY%d" % yd_i])
                P.op("pe", mmm, reads=["Wm", "oT%d" % gi], writes=["pY%d" % ym_i])
                P.op("dve", lambda e, gi=gi, c=c, yd_i=yd_i, t1i=t1i: e.tensor_tensor(out=t1[t1i][:], in0=pY[yd_i][:], in1=Gd[gi][:, c, :], op=ALU.mult),
                     reads=["pY%d" % yd_i, "Gd%d" % gi], writes=["t1%d" % t1i])
                P.op("dve", lambda e, gi=gi, c=c, ym_i=ym_i, t1i=t1i: e.tensor_tensor(out=zT[gi][:, c, :], in0=pY[ym_i][:], in1=Gm[gi][:, c, :], op=ALU.mult),
                     reads=["pY%d" % ym_i, "Gm%d" % gi], writes=["zT%d" % gi])
                P.op("pool", lambda e, gi=gi, c=c, t1i=t1i: e.tensor_tensor(out=zT[gi][:, c, :], in0=zT[gi][:, c, :], in1=t1[t1i][:], op=ALU.add),
                     reads=["t1%d" % t1i, "zT%d" % gi], writes=["zT%d" % gi])
            P.dma("sp", lambda e, gi=gi, g=g: e.dma_start(out=A["zT"][g], in_=zT[gi][:].rearrange("p c t -> p (c t)")), reads=["zT%d" % gi], writes=["dram_zT"])
        P.run()


def phase_wout(nc, A):
    with contextlib.ExitStack() as st:
        P = Phase(nc, "p6")
        Wo = sb(st, nc, "p6_Wo", [128, KC, D], BF16)
        G1 = sb(st, nc, "p6_G1", [128, D], F32)
        zT = [sb(st, nc, "p6_zT%d" % i, [128, KC, 512], BF16) for i in range(2)]
        xt = [sb(st, nc, "p6_x%d" % i, [128, D], F32) for i in range(2)]
        pY = [ps(st, nc, "p6_pY%d" % i, [128, 512], F32) for i in range(4)]
        for q4 in range(4):
            P.dma("pool", lambda e, q4=q4: e.dma_start(out=Wo[:, q4 * 4:(q4 + 1) * 4, :], in_=A["w_out"].rearrange("(k p) n -> p k n", p=128)[:, q4 * 4:(q4 + 1) * 4, :]), writes=["Wo"])
        P.dma("sp", lambda e: e.dma_start(out=G1[:], in_=A["modb"][:, MOD_G1:MOD_G1 + D]), writes=["G1"])
        ycount = 0
        tcount = 0
        for g in range(4):
            gi = g % 2
            P.dma("sp", lambda e, gi=gi, g=g: e.dma_start(out=zT[gi][:].rearrange("p c t -> p (c t)"), in_=A["zT"][g]), writes=["zT%d" % gi])
            for tt in range(4):
                ti = tcount % 2
                tcount += 1
                tile = g * 4 + tt
                P.dma("sp", lambda e, ti=ti, tile=tile: e.dma_start(out=xt[ti][:], in_=A["x_own"][tile * 128:(tile + 1) * 128, :]), writes=["x%d" % ti])
                for cg in range(4):
                    yi = ycount % 4
                    ycount += 1

                    def mm(e, gi=gi, tt=tt, cg=cg, yi=yi):
                        ins = None
                        for k in range(KC):
                            ins = e.matmul(pY[yi][:], lhsT=zT[gi][:, k, tt * 128:(tt + 1) * 128], rhs=Wo[:, k, cg * 512:(cg + 1) * 512], start=(k == 0), stop=(k == KC - 1))
                        return ins
                    P.op("pe", mm, reads=["zT%d" % gi, "Wo"], writes=["pY%d" % yi])
                    P.op("dve", lambda e, yi=yi, cg=cg, ti=ti: e.tensor_tensor(out=pY[yi][:], in0=pY[yi][:], in1=G1[:, cg * 512:(cg + 1) * 512], op=ALU.mult),
                         reads=["pY%d" % yi, "G1"], writes=["pY%d" % yi])
                    P.op("dve", lambda e, yi=yi, cg=cg, ti=ti: e.tensor_tensor(out=xt[ti][:, cg * 512:(cg + 1) * 512], in0=pY[yi][:], in1=xt[ti][:, cg * 512:(cg + 1) * 512], op=ALU.add),
                         reads=["pY%d" % yi, "x%d" % ti], writes=["x%d" % ti])
                P.dma("sp", lambda e, ti=ti, tile=tile: e.dma_start(out=A["x1"][tile * 128:(tile + 1) * 128, :], in_=xt[ti][:]), reads=["x%d" % ti], writes=["dram_x1"])
        P.run()


def phase_route(nc, A):
    with contextlib.ExitStack() as st:
        P = Phase(nc, "p7")
        ident = sb(st, nc, "p7_ident", [128, 128], BF16)
        ltri = sb(st, nc, "p7_ltri", [128, 128], BF16)
        onesb = sb(st, nc, "p7_ones", [128, 128], BF16)
        Amod = sb(st, nc, "p7_A", [128, D], F32)
        Smod = sb(st, nc, "p7_S", [128, D], F32)
        Wr = sb(st, nc, "p7_Wr", [128, KC, E], BF16)
        rbias = sb(st, nc, "p7_rbias", [128, E], F32)
        ecap = sb(st, nc, "p7_ecap", [128, E], F32)
        dumpidx = sb(st, nc, "p7_dump", [128, 1], F32)
        tokid = sb(st, nc, "p7_tokid", [128, NTO], I32)
        zero_i = sb(st, nc, "p7_zero", [128, CAP * 8], I32)
        zero_b = sb(st, nc, "p7_zerob", [128, D], BF16)
        xt = [sb(st, nc, "p7_x%d" % i, [128, D], F32) for i in range(2)]
        sq = [sb(st, nc, "p7_sq%d" % i, [128, D], BF16) for i in range(2)]
        ss = [sb(st, nc, "p7_ss%d" % i, [128, 1], F32) for i in range(2)]
        rstd = [sb(st, nc, "p7_rs%d" % i, [128, 1], F32) for i in range(2)]
        hf = [sb(st, nc, "p7_hf%d" % i, [128, D], F32) for i in range(2)]
        hb = [sb(st, nc, "p7_hb%d" % i, [128, D], BF16) for i in range(2)]
        hT = [sb(st, nc, "p7_hT%d" % i, [128, KC, 128], BF16) for i in range(2)]
        emask = sb(st, nc, "p7_emask", [128, NTO, E], BF16)
        scores = [sb(st, nc, "p7_sc%d" % i, [128, E], F32) for i in range(2)]
        selv = [sb(st, nc, "p7_sel%d" % i, [128, E], F32) for i in range(2)]
        g8 = [sb(st, nc, "p7_g8%d" % i, [128, 8], F32) for i in range(2)]
        gsc = [sb(st, nc, "p7_gsc%d" % i, [128, 8], F32) for i in range(2)]
        gm8 = [sb(st, nc, "p7_gm%d" % i, [128, 8], F32) for i in range(2)]
        gmask = [sb(st, nc, "p7_gmask%d" % i, [128, 8], F32) for i in range(2)]
        t8 = [sb(st, nc, "p7_t8%d" % i, [128, 8], F32) for i in range(2)]
        em = [sb(st, nc, "p7_em%d" % i, [128, E], F32) for i in range(2)]
        wt = sb(st, nc, "p7_wt", [128, NTO, E], F32)
        wsum = [sb(st, nc, "p7_ws%d" % i, [128, 1], F32) for i in range(2)]
        key = [sb(st, nc, "p7_key%d" % i, [128, E], F32) for i in range(2)]
        k8 = [sb(st, nc, "p7_k8%d" % i, [128, 8], F32) for i in range(2)]
        oh_ = [sb(st, nc, "p7_oh%d" % i, [128, E], F32) for i in range(2)]
        junk_ = [sb(st, nc, "p7_junk%d" % i, [128, E], F32) for i in range(2)]
        cnt_i = sb(st, nc, "p7_cnt_i", [128, E], I32)
        route = [sb(st, nc, "p7_route%d" % i, [128, 16], F32) for i in range(2)]
        sidx = [sb(st, nc, "p7_sidx%d" % i, [128, 8], I32) for i in range(2)]
        tokrow = [sb(st, nc, "p7_tokrow%d" % i, [128, 8], I32) for i in range(2)]
        valid = [sb(st, nc, "p7_valid%d" % i, [128, 8], F32) for i in range(2)]
        pT = [ps(st, nc, "p7_pT%d" % i, [128, KC, 128], BF16) for i in range(2)]
        pL = [ps(st, nc, "p7_pL%d" % i, [128, E], F32) for i in range(2)]
        pR = [ps(st, nc, "p7_pR%d" % i, [128, E], F32) for i in range(2)]

        P.dma("sp", lambda e: e.dma_start(out=ident[:], in_=A["ident_bf"][:, :]), writes=["ident"])
        P.dma("sp", lambda e: e.dma_start(out=ltri[:], in_=A["ltri"][:, :]), writes=["ltri"])
        P.dma("sp", lambda e: e.dma_start(out=Amod[:], in_=A["modb"][:, MOD_A2:MOD_A2 + D]), writes=["Amod"])
        P.dma("sp", lambda e: e.dma_start(out=Smod[:], in_=A["modb"][:, MOD_SHIFT2:MOD_SHIFT2 + D]), writes=["Smod"])
        P.dma("pool", lambda e: e.dma_start(out=Wr[:], in_=A["w_router"].rearrange("(k p) n -> p k n", p=128)), writes=["Wr"])
        P.dma("sp", lambda e: e.dma_start(out=rbias[:], in_=A["rbias_b"][:, :]), writes=["rbias"])
        P.dma("sp", lambda e: e.dma_start(out=ecap[:], in_=A["ecap"][:, :]), writes=["ecap"])
        P.dma("sp", lambda e: e.dma_start(out=dumpidx[:], in_=A["dumpidx"][:, :]), writes=["dumpidx"])
        P.dma("sp", lambda e: e.dma_start(out=tokid[:], in_=A["tokid"][:, :]), writes=["tokid"])
        P.op("dve", lambda e: e.memset(onesb[:], 1.0), writes=["onesb"])
        P.op("pool", lambda e: e.memset(zero_i[:], 0), writes=["zero_i"])
        P.op("pool", lambda e: e.memset(zero_b[:], 0.0), writes=["zero_b"])
        rt_view = A["rowtok"][0:NSLOT, :].rearrange("(e c) w -> e (c w)", e=E)
        P.dma("sp", lambda e: e.dma_start(out=rt_view, in_=zero_i[0:E, :]), reads=["zero_i"], writes=["dram_rowtok"])
        P.dma("sp", lambda e: e.dma_start(out=A["rowtok"][NSLOT:NSLOT + 128, :], in_=zero_i[:, 0:8]), reads=["zero_i"], writes=["dram_rowtok"])
        P.dma("sp", lambda e: e.dma_start(out=A["Ybuf"][NSLOT:NSLOT + 128, :], in_=zero_b[:]), reads=["zero_b"], writes=["dram_Ybuf"])

        for t in range(NTO):
            i = t % 2
            tag = str(i)
            P.dma("sp", lambda e, i=i, t=t: e.dma_start(out=xt[i][:], in_=A["x1"][t * 128:(t + 1) * 128, :]), writes=["x" + tag])
            emit_norm_mod_T(P, nc, xt[i], sq[i], ss[i], rstd[i], hf[i], hb[i], pT[i], hT[i], Amod, Smod, ident, tag, "x" + tag)
            P.dma("sp", lambda e, i=i, t=t: e.dma_start(out=A["h2"][t * 128:(t + 1) * 128, :], in_=hb[i][:]), reads=["hb" + tag], writes=["dram_h2"])
            P.dma("sp", lambda e, i=i, t=t: e.dma_start(out=A["h2T"][t], in_=hT[i][:].rearrange("p k t -> p (k t)")), reads=["hT" + tag], writes=["dram_h2T"])

            def mml(e, i=i):
                ins = None
                for k in range(KC):
                    ins = e.matmul(pL[i][:], lhsT=hT[i][:, k, :], rhs=Wr[:, k, :], start=(k == 0), stop=(k == KC - 1))
                return ins
            P.op("pe", mml, reads=["hT" + tag, "Wr"], writes=["pL" + tag])
            P.op("act", lambda e, i=i: e.activation(out=scores[i][:], in_=pL[i][:], func=AF.Sigmoid), reads=["pL" + tag], writes=["sc" + tag])
            P.op("dve", lambda e, i=i: e.tensor_tensor(out=selv[i][:], in0=scores[i][:], in1=rbias[:], op=ALU.add), reads=["sc" + tag, "rbias"], writes=["sel" + tag])
            for gq in range(8):
                P.op("dve", lambda e, i=i, gq=gq: e.max(out=g8[i][:], in_=selv[i][:, gq * 8:(gq + 1) * 8]), reads=["sel" + tag, "gsc" + tag], writes=["g8" + tag])
                P.op("dve", lambda e, i=i, gq=gq: e.tensor_tensor(out=gsc[i][:, gq:gq + 1], in0=g8[i][:, 0:1], in1=g8[i][:, 1:2], op=ALU.add), reads=["g8" + tag], writes=["gsc" + tag])
            P.op("dve", lambda e, i=i: e.max(out=gm8[i][:], in_=gsc[i][:]), reads=["gsc" + tag], writes=["gm8" + tag])
            P.op("dve", lambda e, i=i: e.tensor_scalar(out=gmask[i][:], in0=gsc[i][:], scalar1=gm8[i][:, 3:4], scalar2=None, op0=ALU.is_ge), reads=["gsc" + tag, "gm8" + tag], writes=["gmask" + tag])
            for gq in range(8):
                P.op("dve", lambda e, i=i, gq=gq: e.tensor_scalar(out=selv[i][:, gq * 8:(gq + 1) * 8], in0=selv[i][:, gq * 8:(gq + 1) * 8], scalar1=2.0, scalar2=gmask[i][:, gq:gq + 1],
                                                                 op0=ALU.add, op1=ALU.mult), reads=["sel" + tag, "gmask" + tag], writes=["sel" + tag])
            P.op("dve", lambda e, i=i: e.max(out=t8[i][:], in_=selv[i][:]), reads=["sel" + tag], writes=["t8" + tag])
            P.op("dve", lambda e, i=i: e.tensor_scalar(out=em[i][:], in0=selv[i][:], scalar1=t8[i][:, 7:8], scalar2=None, op0=ALU.is_ge), reads=["sel" + tag, "t8" + tag], writes=["em" + tag])
            P.op("dve", lambda e, i=i, t=t: e.tensor_copy(out=emask[:, t, :], in_=em[i][:]), reads=["em" + tag], writes=["emask"])
            P.op("dve", lambda e, i=i, t=t: e.tensor_tensor(out=wt[:, t, :], in0=scores[i][:], in1=em[i][:], op=ALU.mult), reads=["sc" + tag, "em" + tag], writes=["wt"])
            P.op("act", lambda e, i=i, t=t: e.activation(out=junk_[i][:], in_=wt[:, t, :], func=AF.Copy, accum_out=wsum[i][:]), reads=["wt"], writes=["ws" + tag, "junk" + tag])
            P.op("dve", lambda e, i=i: e.reciprocal(out=wsum[i][:], in_=wsum[i][:]), reads=["ws" + tag], writes=["ws" + tag])
            P.op("dve", lambda e, i=i, t=t: e.tensor_scalar(out=wt[:, t, :], in0=wt[:, t, :], scalar1=wsum[i][:, 0:1], scalar2=2.5, op0=ALU.mult, op1=ALU.mult), reads=["wt", "ws" + tag], writes=["wt"])

            def mmr(e, i=i, t=t):
                ins = None
                for j in range(t):
                    ins = e.matmul(pR[i][:], lhsT=onesb[:], rhs=emask[:, j, :], start=(j == 0), stop=False)
                ins = e.matmul(pR[i][:], lhsT=ltri[:], rhs=emask[:, t, :], start=(t == 0), stop=True)
                return ins
            P.op("pe", mmr, reads=["emask", "onesb", "ltri"], writes=["pR" + tag])
            P.op("dve", lambda e, i=i: e.scalar_tensor_tensor(out=key[i][:], in0=pR[i][:], scalar=1.0, in1=ecap[:], op0=ALU.add, op1=ALU.add), reads=["pR" + tag, "ecap"], writes=["key" + tag])
            P.op("dve", lambda e, i=i: e.tensor_scalar(out=oh_[i][:], in0=pR[i][:], scalar1=float(CAP) - 0.5, scalar2=None, op0=ALU.is_lt), reads=["pR" + tag], writes=["oh" + tag])
            P.op("dve", lambda e, i=i: e.tensor_tensor(out=oh_[i][:], in0=oh_[i][:], in1=em[i][:], op=ALU.mult), reads=["oh" + tag, "em" + tag], writes=["oh" + tag])
            P.op("dve", lambda e, i=i: e.tensor_tensor(out=key[i][:], in0=key[i][:], in1=oh_[i][:], op=ALU.mult), reads=["key" + tag, "oh" + tag], writes=["key" + tag])
            P.op("dve", lambda e, i=i: e.max(out=k8[i][:], in_=key[i][:]), reads=["key" + tag], writes=["k8" + tag])
            for kk in range(8):
                P.op("dve", lambda e, i=i, kk=kk: e.tensor_scalar(out=oh_[i][:], in0=key[i][:], scalar1=k8[i][:, kk:kk + 1], scalar2=None, op0=ALU.is_equal), reads=["key" + tag, "k8" + tag, "route" + tag], writes=["oh" + tag])
                P.op("dve", lambda e, i=i, kk=kk, t=t: e.tensor_tensor(out=oh_[i][:], in0=oh_[i][:], in1=wt[:, t, :], op=ALU.mult), reads=["oh" + tag, "wt"], writes=["oh" + tag])
                P.op("act", lambda e, i=i, kk=kk: e.activation(out=junk_[i][:], in_=oh_[i][:], func=AF.Copy, accum_out=route[i][:, 8 + kk:9 + kk]), reads=["oh" + tag], writes=["route" + tag, "junk" + tag])
            P.op("dve", lambda e, i=i: e.tensor_scalar(out=valid[i][:], in0=k8[i][:], scalar1=0.5, scalar2=None, op0=ALU.is_gt), reads=["k8" + tag], writes=["valid" + tag])
            P.op("dve", lambda e, i=i: e.tensor_tensor(out=route[i][:, 8:16], in0=route[i][:, 8:16], in1=valid[i][:], op=ALU.mult), reads=["route" + tag, "valid" + tag], writes=["route" + tag])
            P.op("dve", lambda e, i=i: e.tensor_scalar(out=route[i][:, 0:8], in0=k8[i][:], scalar1=-1.0, scalar2=dumpidx[:, 0:1], op0=ALU.add, op1=ALU.subtract), reads=["k8" + tag, "dumpidx", "route" + tag], writes=["route" + tag])
            P.op("dve", lambda e, i=i: e.tensor_tensor(out=route[i][:, 0:8], in0=route[i][:, 0:8], in1=valid[i][:], op=ALU.mult), reads=["route" + tag, "valid" + tag], writes=["route" + tag])
            P.op("dve", lambda e, i=i: e.tensor_scalar(out=route[i][:, 0:8], in0=route[i][:, 0:8], scalar1=dumpidx[:, 0:1], scalar2=None, op0=ALU.add), reads=["route" + tag, "dumpidx"], writes=["route" + tag])
            P.op("dve", lambda e, i=i: e.tensor_copy(out=sidx[i][:], in_=route[i][:, 0:8]), reads=["route" + tag], writes=["sidx" + tag])
            P.dma("sp", lambda e, i=i, t=t: e.dma_start(out=A["route"][:, t * 16:(t + 1) * 16], in_=route[i][:]), reads=["route" + tag], writes=["dram_route"])
            if t == NTO - 1:
                def mmc(e):
                    ins = None
                    for j in range(NTO):
                        ins = e.matmul(pL[0][:], lhsT=onesb[:], rhs=emask[:, j, :], start=(j == 0), stop=(j == NTO - 1))
                    return ins
                P.op("pe", mmc, reads=["emask", "onesb"], writes=["pL0"])
                P.op("dve", lambda e: e.tensor_copy(out=cnt_i[:], in_=pL[0][:]), reads=["pL0"], writes=["cnt_i"])
                P.dma("sp", lambda e: e.dma_start(out=A["cnt_d"][:, :], in_=cnt_i[0:1, :]), reads=["cnt_i"], writes=["dram_cnt"])
            for kk in range(8):
                P.op("pool", lambda e, i=i, kk=kk, t=t: e.tensor_copy(out=tokrow[i][:, kk:kk + 1], in_=tokid[:, t:t + 1]), reads=["tokid", "tokrow" + tag], writes=["tokrow" + tag])
            for kk in range(8):
                P.dma("pool", lambda e, i=i, kk=kk: e.indirect_dma_start(out=A["rowtok"][:, :], out_offset=bass.IndirectOffsetOnAxis(ap=sidx[i][:, kk:kk + 1], axis=0),
                                                                         in_=tokrow[i][:], in_offset=None),
                      reads=["sidx" + tag, "tokrow" + tag, "dram_rowtok"], writes=["dram_rowtok_s"])
        P.run()


def phase_experts(nc, A):
    NST = 4
    with contextlib.ExitStack() as st:
        P = Phase(nc, "p8")
        ident = sb(st, nc, "p8_ident", [128, 128], BF16)
        cnt_sb = sb(st, nc, "p8_cnt", [1, E], I32)
        P.cnt_ap = cnt_sb
        Wg = [sb(st, nc, "p8_Wg%d" % i, [128, KC, 512], BF16) for i in range(2)]
        Wu = [sb(st, nc, "p8_Wu%d" % i, [128, KC, 512], BF16) for i in range(2)]
        Wd = [sb(st, nc, "p8_Wd%d" % i, [128, 4, D], BF16) for i in range(2)]
        stage = [sb(st, nc, "p8_st%d" % i, [128, 2048], F32) for i in range(NST)]
        idx_all = sb(st, nc, "p8_idxall", [128, E * JMAX, 8], I32)
        xg = [sb(st, nc, "p8_xg%d" % i, [128, D], BF16) for i in range(3)]
        xT = [sb(st, nc, "p8_xT%d" % i, [128, KC, 128], BF16) for i in range(2)]
        sg = [sb(st, nc, "p8_sg%d" % i, [128, 512], F32) for i in range(2)]
        aT = [sb(st, nc, "p8_aT%d" % i, [128, 4, 128], BF16) for i in range(2)]
        Y = [sb(st, nc, "p8_Y%d" % i, [128, D], BF16) for i in range(2)]
        pT = [ps(st, nc, "p8_pT%d" % i, [128, KC, 128], BF16) for i in range(1)]
        pG = [ps(st, nc, "p8_pG%d" % i, [128, 512], F32) for i in range(2)]
        pU = [ps(st, nc, "p8_pU%d" % i, [128, 512], F32) for i in range(2)]
        pY = [ps(st, nc, "p8_pY%d" % i, [128, 512], F32) for i in range(2)]
        P.dma("pool", lambda e: e.dma_start(out=ident[:], in_=A["ident_bf"][:, :]), writes=["ident"])
        P.dma("pool", lambda e: e.dma_start(out=cnt_sb[:], in_=A["cnt_d"][:, :]), writes=["cnt_sb"])
        cn = {"g": 0, "y": 0, "yb": 0, "x": 0, "a": 0, "gu": 0, "st": 0, "ce": 0}

        def wsrc(ei):
            if ei < E:
                return A["w_exp_gate"][ei], A["w_exp_up"][ei], A["w_exp_down"][ei]
            return A["w_sh_gate"], A["w_sh_up"], A["w_sh_down"]

        def weight_chunks(ei):
            wi = ei % 2
            gsrc, usrc, dsrc = wsrc(ei)
            out = []
            for c in range(4):
                out.append((gsrc.rearrange("(k p) n -> p k n", p=128)[:, 4 * c:4 * c + 4, :], Wg[wi][:, 4 * c:4 * c + 4, :], "Wg%d_%d" % (wi, c), (4, 512)))
            for c in range(4):
                out.append((usrc.rearrange("(k p) n -> p k n", p=128)[:, 4 * c:4 * c + 4, :], Wu[wi][:, 4 * c:4 * c + 4, :], "Wu%d_%d" % (wi, c), (4, 512)))
            for c in range(4):
                out.append((dsrc.rearrange("(k p) n -> p k n", p=128)[:, c:c + 1, :], Wd[wi][:, c:c + 1, :], "Wd%d_%d" % (wi, c), (1, 2048)))
            return out

        def issue_chunk_dma(ch):
            src, dst, res, (a, b) = ch
            si = cn["st"] % NST
            cn["st"] += 1
            P.dma("sp", lambda e, si=si, src=src, a=a: e.dma_start(out=stage[si][:].rearrange("p (a b) -> p a b", a=a), in_=src), writes=["st%d" % si])
            return si

        def issue_chunk_cast(ch, si):
            src, dst, res, (a, b) = ch
            eng = "dve"
            view = stage[si][:].rearrange("p (a b) -> p a b", a=a)
            if eng == "act":
                P.op("act", lambda e, dst=dst, view=view: e.activation(out=dst, in_=view, func=AF.Copy), reads=["st%d" % si], writes=[res])
            else:
                P.op(eng, lambda e, dst=dst, view=view: e.tensor_copy(out=dst, in_=view), reads=["st%d" % si], writes=[res])

        def ffn(wi, xi, dst):
            xres = "xT%d" % xi
            ai = cn["a"] % 2
            cn["a"] += 1
            ares = "aT%d" % ai
            gb = cn["gu"] % 2
            cn["gu"] += 1

            def mmgu(e, wi=wi, xi=xi, gb=gb):
                ins = None
                for fc in range(4):
                    for k in range(KC):
                        ins = e.matmul(pG[gb][:, fc * 128:(fc + 1) * 128], lhsT=Wg[wi][:, k, fc * 128:(fc + 1) * 128], rhs=xT[xi][:, k, :], start=(k == 0), stop=(k == KC - 1))
                for fc in range(4):
                    for k in range(KC):
                        ins = e.matmul(pU[gb][:, fc * 128:(fc + 1) * 128], lhsT=Wu[wi][:, k, fc * 128:(fc + 1) * 128], rhs=xT[xi][:, k, :], start=(k == 0), stop=(k == KC - 1))
                return ins
            P.op("pe", mmgu, reads=["Wg%d_%d" % (wi, c) for c in range(4)] + ["Wu%d_%d" % (wi, c) for c in range(4)] + [xres], writes=["pG%d" % gb, "pU%d" % gb])
            P.op("act", lambda e, gb=gb: e.activation(out=sg[gb][:], in_=pG[gb][:], func=AF.Silu), reads=["pG%d" % gb], writes=["sg%d" % gb])
            P.op("dve", lambda e, gb=gb, ai=ai: e.tensor_tensor(out=aT[ai][:], in0=pU[gb][:].rearrange("p (f t) -> p f t", f=4), in1=sg[gb][:].rearrange("p (f t) -> p f t", f=4), op=ALU.mult),
                 reads=["pU%d" % gb, "sg%d" % gb], writes=[ares])
            yi = cn["yb"] % 2
            cn["yb"] += 1
            for cg in range(4):
                pi = cn["y"] % 2
                cn["y"] += 1

                def mmy(e, wi=wi, ai=ai, cg=cg, pi=pi):
                    ins = None
                    for fc in range(4):
                        ins = e.matmul(pY[pi][:], lhsT=aT[ai][:, fc, :], rhs=Wd[wi][:, fc, cg * 512:(cg + 1) * 512], start=(fc == 0), stop=(fc == 3))
                    return ins
                P.op("pe", mmy, reads=[ares] + ["Wd%d_%d" % (wi, c) for c in range(4)], writes=["pY%d" % pi])
                P.op("dve", lambda e, yi=yi, cg=cg, pi=pi: e.tensor_copy(out=Y[yi][:, cg * 512:(cg + 1) * 512], in_=pY[pi][:]), reads=["pY%d" % pi], writes=["Y%d" % yi])
            P.dma("act", lambda e, yi=yi, dst=dst: e.dma_start(out=dst, in_=Y[yi][:]), reads=["Y%d" % yi], writes=["dram_Y"])

        for ei in range(E):
            P.dma("pool", lambda e, ei=ei: e.dma_start(out=idx_all[:, ei * JMAX:(ei + 1) * JMAX, :], in_=A["rowtok"][ei * CAP:(ei + 1) * CAP, :].rearrange("(t p) w -> p t w", p=128)),
                  writes=["idxall%d" % ei])
        for ch in weight_chunks(0):
            si = issue_chunk_dma(ch)
            issue_chunk_cast(ch, si)
        for ei in range(E):
            wi = ei % 2
            P.load_cond_reg(ei, "cnt_sb")
            nxt = weight_chunks(ei + 1)
            pend = []

            def pump(ncast):
                for _ in range(ncast):
                    if pend:
                        ch, si = pend.pop(0)
                        issue_chunk_cast(ch, si)
                while nxt and len(pend) < NST:
                    ch = nxt.pop(0)
                    pend.append((ch, issue_chunk_dma(ch)))
            for j in range(JMAX):
                pump(0 if j == 0 else 2)
                P.begin_region(ei, 128 * j)
                gi = cn["g"] % 3
                cn["g"] += 1
                xi = cn["x"] % 2
                cn["x"] += 1
                s0 = ei * CAP + j * 128
                P.dma("pool", lambda e, ei=ei, j=j, gi=gi: e.indirect_dma_start(out=xg[gi][:], out_offset=None, in_=A["h2"][:, :],
                                                                              in_offset=bass.IndirectOffsetOnAxis(ap=idx_all[:, ei * JMAX + j, 0:1], axis=0)),
                      reads=["idxall%d" % ei], writes=["xg%d" % gi])

                def tr(e, gi=gi):
                    ins = None
                    for k in range(KC):
                        ins = e.transpose(out=pT[0][:, k, :], in_=xg[gi][:, k * 128:(k + 1) * 128], identity=ident[:])
                    return ins
                P.op("pe", tr, reads=["xg%d" % gi, "ident"], writes=["pT"])
                P.op("dve", lambda e, xi=xi: e.tensor_copy(out=xT[xi][:], in_=pT[0][:]), reads=["pT"], writes=["xT%d" % xi])
                ffn(wi, xi, A["Ybuf"][s0:s0 + 128, :])
                P.end_region()
            while pend or nxt:
                pump(2)
        wi = E % 2
        for t in range(NTO):
            xi = cn["x"] % 2
            cn["x"] += 1
            P.dma("pool", lambda e, xi=xi, t=t: e.dma_start(out=xT[xi][:], in_=A["h2T"][t].rearrange("p (k q) -> p k q", k=KC)), writes=["xT%d" % xi])
            ffn(wi, xi, A["Ysh"][t * 128:(t + 1) * 128, :])
        P.run()


def phase_final(nc, A):
    with contextlib.ExitStack() as st:
        P = Phase(nc, "p9")
        G2 = sb(st, nc, "p9_G2", [128, D], F32)
        gf = sb(st, nc, "p9_gf", [128, D], F32)
        xt = [sb(st, nc, "p9_x%d" % i, [128, D], F32) for i in range(2)]
        ysh = [sb(st, nc, "p9_ysh%d" % i, [128, D], BF16) for i in range(2)]
        yk = [sb(st, nc, "p9_yk%d" % i, [128, D], BF16) for i in range(4)]
        acc = [sb(st, nc, "p9_acc%d" % i, [128, D], F32) for i in range(2)]
        route = [sb(st, nc, "p9_route%d" % i, [128, 16], F32) for i in range(2)]
        sidx = [sb(st, nc, "p9_sidx%d" % i, [128, 8], I32) for i in range(2)]
        sq = [sb(st, nc, "p9_sq%d" % i, [128, D], BF16) for i in range(2)]
        ss = [sb(st, nc, "p9_ss%d" % i, [128, 1], F32) for i in range(2)]
        P.dma("sp", lambda e: e.dma_start(out=G2[:], in_=A["modb"][:, MOD_G2:MOD_G2 + D]), writes=["G2"])
        P.dma("sp", lambda e: e.dma_start(out=gf[:], in_=A["g_final_b"][:, :]), writes=["gf"])
        kc = 0
        for t in range(NTO):
            i = t % 2
            tag = str(i)
            P.dma("sp", lambda e, i=i, t=t: e.dma_start(out=xt[i][:], in_=A["x1"][t * 128:(t + 1) * 128, :]), writes=["x" + tag])
            P.dma("sp", lambda e, i=i, t=t: e.dma_start(out=ysh[i][:], in_=A["Ysh"][t * 128:(t + 1) * 128, :]), writes=["ysh" + tag])
            P.dma("sp", lambda e, i=i, t=t: e.dma_start(out=route[i][:], in_=A["route"][:, t * 16:(t + 1) * 16]), writes=["route" + tag])
            P.op("dve", lambda e, i=i: e.tensor_copy(out=sidx[i][:], in_=route[i][:, 0:8]), reads=["route" + tag], writes=["sidx" + tag])
            P.op("dve", lambda e, i=i: e.tensor_copy(out=acc[i][:], in_=ysh[i][:]), reads=["ysh" + tag], writes=["acc" + tag])
            for kk in range(8):
                ki = kc % 4
                kc += 1
                P.dma("pool", lambda e, i=i, kk=kk, ki=ki: e.indirect_dma_start(out=yk[ki][:], out_offset=None, in_=A["Ybuf"][:, :],
                                                                              in_offset=bass.IndirectOffsetOnAxis(ap=sidx[i][:, kk:kk + 1], axis=0)),
                      reads=["sidx" + tag], writes=["yk%d" % ki])
                P.op("dve", lambda e, i=i, kk=kk, ki=ki: e.scalar_tensor_tensor(out=acc[i][:], in0=yk[ki][:], scalar=route[i][:, 8 + kk:9 + kk], in1=acc[i][:], op0=ALU.mult, op1=ALU.add),
                     reads=["yk%d" % ki, "route" + tag, "acc" + tag], writes=["acc" + tag])
            P.op("pool", lambda e, i=i: e.tensor_tensor(out=acc[i][:], in0=acc[i][:], in1=G2[:], op=ALU.mult), reads=["acc" + tag, "G2"], writes=["acc" + tag])
            P.op("pool", lambda e, i=i: e.tensor_tensor(out=xt[i][:], in0=xt[i][:], in1=acc[i][:], op=ALU.add), reads=["acc" + tag, "x" + tag], writes=["x" + tag])
            P.op("act", lambda e, i=i: e.activation(out=sq[i][:], in_=xt[i][:], func=AF.Square, accum_out=ss[i][:]), reads=["x" + tag], writes=["sq" + tag, "ss" + tag])
            P.op("dve", lambda e, i=i: e.tensor_scalar(out=ss[i][:], in0=ss[i][:], scalar1=1.0 / D, scalar2=EPS, op0=ALU.mult, op1=ALU.add), reads=["ss" + tag], writes=["ss" + tag])
            P.op("act", lambda e, i=i: e.activation(out=ss[i][:], in_=ss[i][:], func=AF.Sqrt), reads=["ss" + tag], writes=["ss" + tag])
            P.op("dve", lambda e, i=i: e.reciprocal(out=ss[i][:], in_=ss[i][:]), reads=["ss" + tag], writes=["ss" + tag])
            P.op("dve", lambda e, i=i: e.scalar_tensor_tensor(out=acc[i][:], in0=xt[i][:], scalar=ss[i][:, 0:1], in1=gf[:], op0=ALU.mult, op1=ALU.mult),
                 reads=["x" + tag, "ss" + tag, "gf", "acc" + tag], writes=["acc" + tag])
            P.dma("sp", lambda e, i=i, t=t: e.dma_start(out=A["out"][t * 128:(t + 1) * 128, :], in_=acc[i][:]), reads=["acc" + tag], writes=["dram_out"])
        P.run()


def _t5_bucket_np(rel):
    n = np.maximum(rel, 0)
    nf = np.maximum(n, 1).astype(np.float32)
    large = 16 + (np.log(nf / 16) / np.float32(np.log(128 / 16)) * 16).astype(np.int32)
    large = np.minimum(large, 31)
    return np.where(n < 16, n, large)


def _bucket_table():
    n = np.arange(0, 700, dtype=np.int64)
    nf = np.maximum(n, 1).astype(np.float32)
    large = 16 + (np.log(nf / np.float32(16)) / np.float32(np.log(8.0)) * np.float32(16)).astype(np.int32)
    large = np.minimum(large, 31)
    return np.where(n < 16, n, large)


def make_core_inputs(inp, c, shared):
    b, r = c // 4, c % 4
    f32 = np.float32
    x = inp["x"]
    own_tiles = [4 * m + r for m in range(NTO)]
    xb = np.ascontiguousarray(x[b])
    m_ = dict(shared)
    m_["x_all"] = xb
    m_["x_own"] = np.ascontiguousarray(xb.reshape(NTA, 128, D)[own_tiles].reshape(TO, D))
    m_["cT"] = np.ascontiguousarray(inp["c"][b].reshape(KC, 128).T)
    ext = shared["_ext_table"]
    bidx = np.zeros((128, 5, 128), np.int64)
    kk = np.arange(128)[:, None]
    qq = np.arange(128)[None, :]
    for jj in range(5):
        delta = r - (jj - 1)
        rel = delta * 128 + qq - kk
        bi = shared["_bucket"][np.clip(rel, 0, 699)]
        bidx[:, jj, :] = np.where(rel >= 0, bi, 32)
    BT = ext[bidx]
    m_["BT"] = np.ascontiguousarray(BT.transpose(0, 3, 1, 2).reshape(128, 16 * 5 * 128)).astype(ml_dtypes.bfloat16)
    cur = np.array([(4 * m + r) // 2 for m in range(NTO)])
    n = np.arange(32)[None, :]
    past = (n < cur[:, None])
    ownm = (n == cur[:, None])
    const_tbl = np.array([0.0, 1.0, -BIG], f32)
    m_["past"] = np.ascontiguousarray(np.broadcast_to(const_tbl[past.astype(np.int64)].reshape(1, NTO * 32), (128, NTO * 32)))
    m_["own"] = np.ascontiguousarray(np.broadcast_to(const_tbl[ownm.astype(np.int64)].reshape(1, NTO * 32), (128, NTO * 32)))
    m_["pen"] = np.ascontiguousarray(np.broadcast_to(const_tbl[np.where(past, 0, 2)].reshape(1, NTO * 32), (128, NTO * 32)))
    for k in list(m_.keys()):
        if k.startswith("_"):
            del m_[k]
    return m_


def make_shared(inp):
    f32 = np.float32
    bc = lambda v, n: np.ascontiguousarray(np.broadcast_to(np.asarray(v, f32).reshape(1, n), (128, n)))
    sh = {}
    sh["w_ada"] = np.ascontiguousarray(inp["w_ada"][0])
    sh["b_ada_b"] = bc(inp["b_ada"][0], 6 * D)
    sh["g_mix_b"] = bc(inp["g_mix"][0], D)
    sh["g_ffn_b"] = bc(inp["g_ffn"][0], D)
    sh["g_final_b"] = bc(inp["g_final"], D)
    sh["w_in"] = np.ascontiguousarray(inp["w_in"][0])
    sh["lam_b"] = bc(inp["diff_lambda"][0].reshape(-1), 256)
    sh["subln_b"] = bc(inp["diff_subln_g"][0], 128)
    rel_bias = np.asarray(inp["rel_bias"], f32)
    sh["b31"] = bc(rel_bias[31], 16)
    sh["_ext_table"] = np.concatenate([rel_bias, np.full((1, 16), -BIG, f32)], axis=0)
    sh["_bucket"] = _bucket_table()
    sh["w_o_diff"] = np.ascontiguousarray(inp["w_o_diff"][0])
    sh["w_o_moba"] = np.ascontiguousarray(inp["w_o_moba"][0])
    sh["w_out"] = np.ascontiguousarray(inp["w_out"][0])
    sh["w_router"] = np.ascontiguousarray(inp["w_router"][0])
    sh["rbias_b"] = bc(inp["router_bias"][0], E)
    sh["w_exp_gate"] = np.ascontiguousarray(inp["w_exp_gate"][0])
    sh["w_exp_up"] = np.ascontiguousarray(inp["w_exp_up"][0])
    sh["w_exp_down"] = np.ascontiguousarray(inp["w_exp_down"][0])
    sh["w_sh_gate"] = np.ascontiguousarray(inp["w_sh_gate"][0])
    sh["w_sh_up"] = np.ascontiguousarray(inp["w_sh_up"][0])
    sh["w_sh_down"] = np.ascontiguousarray(inp["w_sh_down"][0])
    sh["ident_bf"] = np.eye(128, dtype=f32).astype(ml_dtypes.bfloat16)
    sh["ident_f"] = np.eye(128, dtype=f32)
    tp = np.arange(128)
    sh["ltri"] = (tp[:, None] < tp[None, :]).astype(f32).astype(ml_dtypes.bfloat16)
    sel = np.zeros((32, 32, 128), f32)
    for n in range(32):
        sel[n, n, :] = 1.0
    sh["selc"] = sel.reshape(32, 32 * 128).astype(ml_dtypes.bfloat16)
    sh["ecap"] = bc(np.arange(E, dtype=f32) * CAP, E)
    sh["dumpidx"] = (NSLOT + np.arange(128, dtype=f32)).reshape(128, 1)
    sh["tokid"] = (np.arange(NTO, dtype=np.int32)[None, :] * 128 + np.arange(128, dtype=np.int32)[:, None]).astype(np.int32)
    return sh


_NC_CACHE = {}


def kernel(**inputs):
    inp = {k: np.asarray(v) for k, v in inputs.items()}
    stop_after = int(os.environ.get("MK_STOP", "99"))
    key = (stop_after, DEBUG)
    if key not in _NC_CACHE:
        _NC_CACHE[key] = build_program(stop_after)
    nc = _NC_CACHE[key]
    shared = make_shared(inp)
    in_maps = [make_core_inputs(inp, c, shared) for c in range(NCORES)]
    used = set(USED_INPUTS)
    in_maps = [{k: v for k, v in m.items() if k in used} for m in in_maps]
    res = run_bass_kernel_spmd(nc, in_maps, core_ids=list(range(NCORES)))
    out = np.zeros((2, S, D), np.float32)
    for c in range(NCORES):
        b, r = c // 4, c % 4
        if "out" not in res.results[c]:
            continue
        o = np.asarray(res.results[c]["out"]).reshape(NTO, 128, D)
        ov = out[b].reshape(NTA, 128, D)
        for m in range(NTO):
            ov[4 * m + r] = o[m]
    if DEBUG:
        kernel.last_results = res.results
    return out
```

```python
import os
import contextlib
import numpy as np
import ml_dtypes
import concourse.bass as bass
import concourse.mybir as mybir
from concourse.bass_utils import run_bass_kernel_spmd

F32 = mybir.dt.float32
BF16 = mybir.dt.bfloat16
I32 = mybir.dt.int32
AF = mybir.ActivationFunctionType
ALU = mybir.AluOpType
AX = mybir.AxisListType

D = 2048
KC = 16
S = 8192
NTA = 64
NTO = 16
TO = 2048
E = 64
CAP = 896
JMAX = CAP // 128
NSLOT = E * CAP
BIG = 30000.0
EPS = 1e-6
NCORES = int(os.environ.get("MK_CORES", "8"))
DEBUG = os.environ.get("MK_DEBUG", "")

COMPUTE = ("pe", "act", "dve", "pool")
NDSEM = 6
_SYNC = [None]
USED_INPUTS = set()


class Sync:
    def __init__(self, nc, st):
        self.nc = nc
        self.cnt = {e: 0 for e in COMPUTE}
        self.dcnt = {}
        self.seen = {e: {} for e in ("pe", "act", "dve", "pool", "sp")}
        self.all_tokens = {}
        self.sems = {}
        keys = list(COMPUTE) + ["d_%s_%d" % (q, j) for q in ("sp", "pool", "poolw", "act") for j in range(NDSEM)]
        for k in keys:
            self.sems[k] = st.enter_context(nc.semaphore("s_" + k))


class Phase:
    def __init__(self, nc, name):
        self.nc = nc
        self.name = name
        self.sy = _SYNC[0]
        self.streams = {e: [] for e in ("pe", "act", "dve", "pool", "sp")}
        self.last_w = {}
        self.readers = {}
        self.region = None
        self.cnt_ap = None

    def begin_region(self, expert, thresh):
        self.region = {"expert": expert, "thresh": thresh, "before": dict(self.sy.all_tokens)}

    def end_region(self):
        self.region = None

    def load_cond_reg(self, expert, res):
        t = self.last_w.get(res)
        for eng in self.streams:
            w = self._waits(eng, [t] if t is not None else [])
            self.streams[eng].append((w, ("reg", expert), None, None))

    def _deps(self, reads, writes):
        deps = []
        for r in reads:
            t = self.last_w.get(r)
            if t is not None:
                deps.append(t)
        for w in writes:
            t = self.last_w.get(w)
            if t is not None:
                deps.append(t)
            deps.extend(self.readers.get(w, ()))
        return deps

    def _waits(self, eng, deps):
        out = {}
        seen = self.sy.seen[eng]
        for (k, v) in deps:
            if eng == "pe" and k == "pe":
                continue
            if seen.get(k, 0) >= v:
                continue
            if out.get(k, 0) < v:
                out[k] = v
        for k, v in out.items():
            seen[k] = v
        return list(out.items())

    def _commit(self, tok, reads, writes):
        for r in reads:
            self.readers.setdefault(r, []).append(tok)
        for w in writes:
            self.last_w[w] = tok
            self.readers[w] = []
        k, v = tok
        if self.sy.all_tokens.get(k, 0) < v:
            self.sy.all_tokens[k] = v

    def op(self, eng, fn, reads=(), writes=()):
        deps = self._deps(reads, writes)
        waits = self._waits(eng, deps)
        self.sy.cnt[eng] += 1
        tok = (eng, self.sy.cnt[eng])
        self.streams[eng].append((waits, fn, tok, self.region))
        self._commit(tok, reads, writes)
        return tok

    def dma(self, q, fn, reads=(), writes=(), cls=""):
        deps = self._deps(reads, writes)
        i = self.sy.dcnt.get(q + cls, 0)
        self.sy.dcnt[q + cls] = i + 1
        semkey = "d_%s%s_%d" % (q, cls, i % NDSEM)
        tok = (semkey, 16 * (i // NDSEM + 1))
        if i >= NDSEM:
            deps.append((semkey, 16 * (i // NDSEM)))
        waits = self._waits(q, deps)
        self.streams[q].append((waits, fn, tok, self.region))
        self._commit(tok, reads, writes)
        return tok

    def run(self):
        nc = self.nc
        sems = self.sy.sems
        toks = list(self.sy.all_tokens.items())
        for eng in self.streams:
            w = self._waits(eng, toks)
            if w:
                self.streams[eng].append((w, None, None, None))
        cnt_ap = self.cnt_ap
        with nc.Block() as block:

            def emit(engine, entry):
                waits, fn, tok, _ = entry
                for (k, v) in waits:
                    engine.wait_ge(sems[k], v)
                if fn is None:
                    return
                ins = fn(engine)
                k, v = tok
                ins.then_inc(sems[k], 1 if k in COMPUTE else 16)

            def replay(engine, stream):
                reg = None
                i = 0
                n = len(stream)
                while i < n:
                    entry = stream[i]
                    waits, fn, tok, region = entry
                    if isinstance(fn, tuple):
                        for (k, v) in waits:
                            engine.wait_ge(sems[k], v)
                        if reg is None:
                            reg = engine.alloc_register("cnt_reg")
                        engine.reg_load(reg, cnt_ap[0:1, fn[1]:fn[1] + 1])
                        i += 1
                        continue
                    if region is None:
                        emit(engine, entry)
                        i += 1
                        continue
                    groups = []
                    j = i
                    while j < n and stream[j][3] is not None and stream[j][3]["expert"] == region["expert"]:
                        r_ = stream[j][3]
                        j2 = j
                        while j2 < n and stream[j2][3] is r_:
                            j2 += 1
                        groups.append((r_, stream[j:j2]))
                        j = j2

                    def compensate(rest):
                        before = rest[0][0]["before"]
                        ext = {}
                        incs = {}
                        for (_, grp) in rest:
                            for (w_, f_, t_, _r) in grp:
                                for (k, v) in w_:
                                    v2 = min(v, before.get(k, 0))
                                    if v2 > 0 and ext.get(k, 0) < v2:
                                        ext[k] = v2
                                if t_ is not None:
                                    k, v = t_
                                    incs[k] = incs.get(k, 0) + (1 if k in COMPUTE else 16)
                        for k in incs:
                            b = before.get(k, 0)
                            if b > 0 and ext.get(k, 0) < b:
                                ext[k] = b
                        for k, v in ext.items():
                            engine.wait_ge(sems[k], v)
                        for k, v in incs.items():
                            engine.sem_inc(sems[k], v)

                    def chain(gi_):
                        if gi_ == len(groups):
                            return
                        r_, grp = groups[gi_]
                        with engine.If_lt(reg, r_["thresh"] + 1):
                            compensate(groups[gi_:])
                        with engine.Else():
                            for e_ in grp:
                                emit(engine, e_)
                            chain(gi_ + 1)
                    chain(0)
                    i = j

            @block.tensor
            def _(e):
                replay(e, self.streams["pe"])

            @block.scalar
            def _(e):
                replay(e, self.streams["act"])

            @block.vector
            def _(e):
                replay(e, self.streams["dve"])

            @block.gpsimd
            def _(e):
                replay(e, self.streams["pool"])

            @block.sync
            def _(e):
                replay(e, self.streams["sp"])


def sb(st, nc, name, shape, dt):
    return st.enter_context(nc.sbuf_tensor(name, shape, dt))


def ps(st, nc, name, shape, dt):
    return st.enter_context(nc.psum_tensor(name, shape, dt))


def build_program(stop_after=99):
    nc = bass.Bass("TRN2", target_bir_lowering=False)
    USED_INPUTS.clear()

    def din(name, shape, dt=F32):
        A[name] = nc.dram_tensor(name, list(shape), dt, kind="ExternalInput").ap()

    def dscr(name, shape, dt):
        kind = "ExternalOutput" if name in DEBUG.split(",") else "Internal"
        A[name] = nc.dram_tensor(name, list(shape), dt, kind=kind).ap()

    in_specs = {
        "x_all": ([S, D], F32),
        "x_own": ([TO, D], F32),
        "cT": ([128, KC], F32),
        "w_ada": ([D, 6 * D], F32),
        "b_ada_b": ([128, 6 * D], F32),
        "g_mix_b": ([128, D], F32),
        "g_ffn_b": ([128, D], F32),
        "g_final_b": ([128, D], F32),
        "w_in": ([D, 10240], F32),
        "lam_b": ([128, 256], F32),
        "subln_b": ([128, 128], F32),
        "BT": ([128, 16 * 5 * 128], BF16),
        "b31": ([128, 16], F32),
        "pen": ([128, NTO * 32], F32),
        "past": ([128, NTO * 32], F32),
        "own": ([128, NTO * 32], F32),
        "w_o_diff": ([1024, D], F32),
        "w_o_moba": ([1024, D], F32),
        "w_out": ([D, D], F32),
        "w_router": ([D, E], F32),
        "rbias_b": ([128, E], F32),
        "w_exp_gate": ([E, D, 512], F32),
        "w_exp_up": ([E, D, 512], F32),
        "w_exp_down": ([E, 512, D], F32),
        "w_sh_gate": ([D, 512], F32),
        "w_sh_up": ([D, 512], F32),
        "w_sh_down": ([512, D], F32),
        "ident_bf": ([128, 128], BF16),
        "ident_f": ([128, 128], F32),
        "ltri": ([128, 128], BF16),
        "selc": ([32, 32 * 128], BF16),
        "ecap": ([128, E], F32),
        "dumpidx": ([128, 1], F32),
        "tokid": ([128, NTO], I32),
    }

    class LazyA(dict):
        def __missing__(self, name):
            shape, dt = in_specs[name]
            ap = nc.dram_tensor(name, list(shape), dt, kind="ExternalInput").ap()
            self[name] = ap
            USED_INPUTS.add(name)
            return ap
    A = LazyA()
    A["out"] = nc.dram_tensor("out", [TO, D], F32, kind="ExternalOutput").ap()

    dscr("modb", [128, 6 * D], F32)
    dscr("hT_all", [NTA, 128, KC * 128], BF16)
    dscr("hT_own", [NTO, 128, KC * 128], BF16)
    dscr("QTd", [8, 128, TO], BF16); dscr("KTd", [8, 128, S], BF16); dscr("Vd", [S, 1024], BF16)
    dscr("QTm", [8, 128, TO], BF16); dscr("KTm", [8, 128, S], BF16); dscr("Vm", [S, 1024], BF16)
    dscr("KMT", [8, 128, 32], F32)
    dscr("GT", [32, 128, TO], BF16)
    dscr("o_d", [TO, D], BF16)
    dscr("zT", [4, 128, KC * 512], BF16)
    dscr("x1", [TO, D], F32)
    dscr("h2", [TO, D], BF16)
    dscr("h2T", [NTO, 128, KC * 128], BF16)
    dscr("rowtok", [NSLOT + 128, 8], I32)
    dscr("Ybuf", [NSLOT + 128, D], BF16)
    dscr("Ysh", [TO, D], BF16)
    dscr("route", [128, NTO * 16], F32)
    dscr("cnt_d", [1, E], I32)

    gst = contextlib.ExitStack()
    gst.__enter__()
    _SYNC[0] = Sync(nc, gst)
    if stop_after >= 1:
        phase_mod(nc, A)
    if stop_after >= 2:
        phase_h(nc, A)
    if stop_after >= 3:
        phase_proj(nc, A)
    if stop_after >= 4:
        phase_attn(nc, A)
    if stop_after >= 5:
        phase_oproj(nc, A)
    if stop_after >= 6:
        phase_wout(nc, A)
    if stop_after >= 7:
        phase_route(nc, A)
    if stop_after >= 8:
        phase_experts(nc, A)
    if stop_after >= 9:
        phase_final(nc, A)
    gst.__exit__(None, None, None)
    return nc


def phase_mod(nc, A):
    with contextlib.ExitStack() as st:
        P = Phase(nc, "p1")
        cT = sb(st, nc, "p1_cT", [128, KC], F32)
        cact = sb(st, nc, "p1_cact", [128, KC], F32)
        ones = sb(st, nc, "p1_ones", [128, 128], F32)
        L = sb(st, nc, "p1_L", [128, KC, 128], BF16)
        wt = [sb(st, nc, "p1_w%d" % i, [128, KC, 512], BF16) for i in range(2)]
        bt = [sb(st, nc, "p1_b%d" % i, [128, 512], F32) for i in range(2)]
        gt = [sb(st, nc, "p1_g%d" % i, [128, 512], F32) for i in range(2)]
        ot = [sb(st, nc, "p1_o%d" % i, [128, 512], F32) for i in range(2)]
        pp = [ps(st, nc, "p1_ps%d" % i, [128, 512], F32) for i in range(2)]
        P.dma("sp", lambda e: e.dma_start(out=cT[:], in_=A["cT"][:, :]), writes=["cT"])
        P.op("dve", lambda e: e.memset(ones[:], 1.0), writes=["ones"])
        P.op("act", lambda e: e.activation(out=cact[:], in_=cT[:], func=AF.Silu), reads=["cT"], writes=["cact"])
        for j in range(KC):
            P.op("dve", lambda e, j=j: e.tensor_scalar(out=L[:, j, :], in0=ones[:], scalar1=cact[:, j:j + 1], scalar2=None, op0=ALU.mult),
                 reads=["cact", "ones"], writes=["L"])
        w_view = A["w_ada"].rearrange("(k p) n -> p k n", p=128)
        for n in range(24):
            i = n % 2
            P.dma("pool", lambda e, n=n, i=i: e.dma_start(out=wt[i][:], in_=w_view[:, :, n * 512:(n + 1) * 512]), writes=["w%d" % i])
            P.dma("sp", lambda e, n=n, i=i: e.dma_start(out=bt[i][:], in_=A["b_ada_b"][:, n * 512:(n + 1) * 512]), writes=["b%d" % i])
            kind = n // 4
            if kind in (1, 4):
                gsrc = A["g_mix_b"] if kind == 1 else A["g_ffn_b"]
                c0 = (n % 4) * 512
                P.dma("sp", lambda e, i=i, gsrc=gsrc, c0=c0: e.dma_start(out=gt[i][:], in_=gsrc[:, c0:c0 + 512]), writes=["g%d" % i])

            def mm(e, i=i):
                ins = None
                for k in range(KC):
                    ins = e.matmul(pp[i][:], lhsT=L[:, k, :], rhs=wt[i][:, k, :], start=(k == 0), stop=(k == KC - 1))
                return ins
            P.op("pe", mm, reads=["L", "w%d" % i], writes=["ps%d" % i])
            if kind in (1, 4):
                P.op("dve", lambda e, i=i: e.tensor_tensor(out=ot[i][:], in0=pp[i][:], in1=bt[i][:], op=ALU.add),
                     reads=["ps%d" % i, "b%d" % i], writes=["o%d" % i])
                P.op("dve", lambda e, i=i: e.scalar_tensor_tensor(out=ot[i][:], in0=ot[i][:], scalar=1.0, in1=gt[i][:], op0=ALU.add, op1=ALU.mult),
                     reads=["o%d" % i, "g%d" % i], writes=["o%d" % i])
            else:
                P.op("dve", lambda e, i=i: e.tensor_tensor(out=ot[i][:], in0=pp[i][:], in1=bt[i][:], op=ALU.add),
                     reads=["ps%d" % i, "b%d" % i], writes=["o%d" % i])
            P.dma("sp", lambda e, n=n, i=i: e.dma_start(out=A["modb"][:, n * 512:(n + 1) * 512], in_=ot[i][:]), reads=["o%d" % i], writes=["modb"])
        P.run()


MOD_SHIFT1, MOD_A1, MOD_G1, MOD_SHIFT2, MOD_A2, MOD_G2 = [i * D for i in range(6)]


def emit_norm_mod_T(P, nc, xt, sq, ss, rstd, hf, hb, pT, hT, Amod, Smod, ident, tag, xres, extra_reads=()):
    P.op("act", lambda e: e.activation(out=sq[:], in_=xt[:], func=AF.Square, accum_out=ss[:]),
         reads=[xres], writes=["sq" + tag, "ss" + tag])
    P.op("dve", lambda e: e.tensor_scalar(out=rstd[:], in0=ss[:], scalar1=1.0 / D, scalar2=EPS, op0=ALU.mult, op1=ALU.add),
         reads=["ss" + tag], writes=["rstd" + tag])
    P.op("act", lambda e: e.activation(out=rstd[:], in_=rstd[:], func=AF.Sqrt), reads=["rstd" + tag], writes=["rstd" + tag])
    P.op("dve", lambda e: e.reciprocal(out=rstd[:], in_=rstd[:]), reads=["rstd" + tag], writes=["rstd" + tag])
    P.op("dve", lambda e: e.scalar_tensor_tensor(out=hf[:], in0=xt[:], scalar=rstd[:, 0:1], in1=Amod[:], op0=ALU.mult, op1=ALU.mult),
         reads=[xres, "rstd" + tag, "Amod"] + list(extra_reads), writes=["hf" + tag])
    P.op("pool", lambda e: e.tensor_tensor(out=hb[:], in0=hf[:], in1=Smod[:], op=ALU.add),
         reads=["hf" + tag, "Smod"], writes=["hb" + tag])

    def tr(e):
        ins = None
        for k in range(KC):
            ins = e.transpose(out=pT[:, k, :], in_=hb[:, k * 128:(k + 1) * 128], identity=ident[:])
        return ins
    P.op("pe", tr, reads=["hb" + tag, "ident"], writes=["pT" + tag])
    P.op("act", lambda e: e.activation(out=hT[:], in_=pT[:], func=AF.Copy), reads=["pT" + tag], writes=["hT" + tag])


def phase_h(nc, A):
    with contextlib.ExitStack() as st:
        P = Phase(nc, "p2")
        ident = sb(st, nc, "p2_ident", [128, 128], BF16)
        Amod = sb(st, nc, "p2_A", [128, D], F32)
        Smod = sb(st, nc, "p2_S", [128, D], F32)
        xt = [sb(st, nc, "p2_x%d" % i, [128, D], F32) for i in range(3)]
        sq = [sb(st, nc, "p2_sq%d" % i, [128, D], BF16) for i in range(3)]
        ss = [sb(st, nc, "p2_ss%d" % i, [128, 1], F32) for i in range(3)]
        rstd = [sb(st, nc, "p2_rs%d" % i, [128, 1], F32) for i in range(3)]
        hf = [sb(st, nc, "p2_hf%d" % i, [128, D], F32) for i in range(3)]
        hb = [sb(st, nc, "p2_hb%d" % i, [128, D], BF16) for i in range(3)]
        hT = [sb(st, nc, "p2_hT%d" % i, [128, KC, 128], BF16) for i in range(3)]
        pT = [ps(st, nc, "p2_pT%d" % i, [128, KC, 128], BF16) for i in range(3)]
        P.dma("sp", lambda e: e.dma_start(out=ident[:], in_=A["ident_bf"][:, :]), writes=["ident"])
        P.dma("sp", lambda e: e.dma_start(out=Amod[:], in_=A["modb"][:, MOD_A1:MOD_A1 + D]), writes=["Amod"])
        P.dma("sp", lambda e: e.dma_start(out=Smod[:], in_=A["modb"][:, MOD_SHIFT1:MOD_SHIFT1 + D]), writes=["Smod"])
        for t in range(NTA + NTO):
            i = t % 3
            tag = str(i)
            if t < NTA:
                src = A["x_all"][t * 128:(t + 1) * 128, :]
                dst = A["hT_all"][t]
            else:
                src = A["x_own"][(t - NTA) * 128:(t - NTA + 1) * 128, :]
                dst = A["hT_own"][t - NTA]
            P.dma("sp", lambda e, i=i, src=src: e.dma_start(out=xt[i][:], in_=src), writes=["x" + tag])
            emit_norm_mod_T(P, nc, xt[i], sq[i], ss[i], rstd[i], hf[i], hb[i], pT[i], hT[i], Amod, Smod, ident, tag, "x" + tag)
            P.dma("pool", lambda e, i=i, dst=dst: e.dma_start(out=dst, in_=hT[i][:].rearrange("p k t -> p (k t)")),
                  reads=["hT" + tag], writes=["hTd"])
        P.run()


def phase_proj(nc, A):
    with contextlib.ExitStack() as st:
        P = Phase(nc, "p3")
        W = [sb(st, nc, "p3_W%d" % i, [128, KC, 1024], BF16) for i in range(2)]
        H = [sb(st, nc, "p3_H%d" % i, [128, 4, KC * 128], BF16) for i in range(2)]
        O = [sb(st, nc, "p3_O%d" % i, [128, 8, 512], BF16) for i in range(2)]
        KM = sb(st, nc, "p3_KM", [128, 8, 32], F32)
        pp = [ps(st, nc, "p3_ps%d" % i, [128, 512], F32) for i in range(6)]
        w_view = A["w_in"].rearrange("(k p) n -> p k n", p=128)
        passes = [
            ("qd", 0, "own", "fm"), ("kd", 1024, "all", "fm"), ("vd", 2048, "all", "tm"),
            ("qm", 3072, "own", "fm"), ("km", 4096, "all", "fm"), ("vm", 5120, "all", "tm"),
            ("g0", 6144, "own", "fm"), ("g1", 7168, "own", "fm"), ("g2", 8192, "own", "fm"), ("g3", 9216, "own", "fm"),
        ]
        gcount = 0
        pcount = 0
        if os.environ.get("MK_P3"):
            passes = [passes[int(i)] for i in os.environ["MK_P3"].split(",")]
        for pi, (pname, c0, tset, kind) in enumerate(passes):
            wi = pi % 2
            wres = "W%d" % wi

            def loadW(pj):
                wj = pj % 2
                cj = passes[pj][1]
                for q4 in range(4):
                    P.dma("pool", lambda e, wj=wj, cj=cj, q4=q4: e.dma_start(out=W[wj][:, q4 * 4:(q4 + 1) * 4, :], in_=w_view[:, q4 * 4:(q4 + 1) * 4, cj:cj + 1024]),
                          writes=["W%d" % wj], cls="w")
            if pi == 0:
                loadW(0)
            if pi + 1 < len(passes):
                loadW(pi + 1)
            ngroups = 16 if tset == "all" else 4
            src = A["hT_all"] if tset == "all" else A["hT_own"]
            for g in range(ngroups):
                hi = gcount % 2
                gcount += 1
                hres = "H%d" % hi
                P.dma("sp", lambda e, hi=hi, src=src, g=g: e.dma_start(out=H[hi][:], in_=src[g * 4:(g + 1) * 4].rearrange("t p f -> p t f")),
                      writes=[hres])
                oi = g % 2
                ores = "O%d" % oi
                if kind == "fm":
                    for ch in range(8):
                        pidx = pcount % 6
                        pcount += 1
                        pres = "ps%d" % pidx

                        def mm(e, wi=wi, hi=hi, ch=ch, pidx=pidx):
                            ins = None
                            for k in range(KC):
                                ins = e.matmul(pp[pidx][:].rearrange("p (t q) -> p t q", t=4), lhsT=W[wi][:, k, ch * 128:(ch + 1) * 128],
                                               rhs=H[hi][:, :, k * 128:(k + 1) * 128], start=(k == 0), stop=(k == KC - 1))
                            return ins
                        P.op("pe", mm, reads=[wres, hres], writes=[pres])
                        if pname in ("qd", "qm"):
                            sc = 0.125 if pname == "qd" else float(128 ** -0.5)
                            P.op("act", lambda e, oi=oi, ch=ch, pidx=pidx, sc=sc: e.activation(out=O[oi][:, ch, :], in_=pp[pidx][:], func=AF.Copy, scale=sc),
                                 reads=[pres], writes=[ores])
                        elif pname.startswith("g"):
                            P.op("act", lambda e, oi=oi, ch=ch, pidx=pidx: e.activation(out=O[oi][:, ch, :], in_=pp[pidx][:], func=AF.Sigmoid),
                                 reads=[pres], writes=[ores])
                        elif pname == "km":
                            for bb in range(2):
                                P.op("act", lambda e, oi=oi, ch=ch, pidx=pidx, g=g, bb=bb: e.activation(out=O[oi][:, ch, bb * 256:(bb + 1) * 256], in_=pp[pidx][:, bb * 256:(bb + 1) * 256],
                                                                                                    func=AF.Copy, accum_out=KM[:, ch, 2 * g + bb:2 * g + bb + 1]),
                                     reads=[pres], writes=[ores, "KM"])
                        else:
                            eng = "dve" if ch % 2 == 0 else "act"
                            if eng == "dve":
                                P.op("dve", lambda e, oi=oi, ch=ch, pidx=pidx: e.tensor_copy(out=O[oi][:, ch, :], in_=pp[pidx][:]), reads=[pres], writes=[ores])
                            else:
                                P.op("act", lambda e, oi=oi, ch=ch, pidx=pidx: e.activation(out=O[oi][:, ch, :], in_=pp[pidx][:], func=AF.Copy), reads=[pres], writes=[ores])
                    if pname == "qd":
                        dst = A["QTd"][:, :, g * 512:(g + 1) * 512]
                    elif pname == "kd":
                        dst = A["KTd"][:, :, g * 512:(g + 1) * 512]
                    elif pname == "qm":
                        dst = A["QTm"][:, :, g * 512:(g + 1) * 512]
                    elif pname == "km":
                        dst = A["KTm"][:, :, g * 512:(g + 1) * 512]
                    else:
                        gi = int(pname[1])
                        dst = A["GT"][gi * 8:(gi + 1) * 8, :, g * 512:(g + 1) * 512]
                    P.dma("pool", lambda e, oi=oi, dst=dst: e.dma_start(out=dst.rearrange("c p t -> p c t"), in_=O[oi][:]), reads=[ores], writes=["dram_" + pname])
                else:
                    Ov = O[oi][:].rearrange("p c t -> p (c t)").rearrange("p (t n) -> p t n", t=4)
                    for tt in range(4):
                        for half in range(2):
                            pidx = pcount % 6
                            pcount += 1
                            pres = "ps%d" % pidx

                            def mm(e, wi=wi, hi=hi, tt=tt, half=half, pidx=pidx):
                                ins = None
                                for k in range(KC):
                                    ins = e.matmul(pp[pidx][:], lhsT=H[hi][:, tt, k * 128:(k + 1) * 128], rhs=W[wi][:, k, half * 512:(half + 1) * 512],
                                                   start=(k == 0), stop=(k == KC - 1))
                                return ins
                            P.op("pe", mm, reads=[wres, hres], writes=[pres])
                            if (tt * 2 + half) % 2 == 0:
                                P.op("dve", lambda e, Ov=Ov, tt=tt, half=half, pidx=pidx: e.tensor_copy(out=Ov[:, tt, half * 512:(half + 1) * 512], in_=pp[pidx][:]),
                                     reads=[pres], writes=[ores])
                            else:
                                P.op("act", lambda e, Ov=Ov, tt=tt, half=half, pidx=pidx: e.activation(out=Ov[:, tt, half * 512:(half + 1) * 512], in_=pp[pidx][:], func=AF.Copy),
                                     reads=[pres], writes=[ores])
                    dstT = A["Vd"] if pname == "vd" else A["Vm"]
                    dst = dstT[g * 512:(g + 1) * 512, :].rearrange("(t p) n -> p t n", p=128)
                    P.dma("pool", lambda e, Ov=Ov, dst=dst: e.dma_start(out=dst, in_=Ov), reads=[ores], writes=["dram_" + pname])
            if pname == "km":
                P.op("dve", lambda e: e.tensor_scalar(out=KM[:], in0=KM[:], scalar1=1.0 / 256.0, scalar2=None, op0=ALU.mult), reads=["KM"], writes=["KM"])
                P.dma("sp", lambda e: e.dma_start(out=A["KMT"].rearrange("h p n -> p h n"), in_=KM[:]), reads=["KM"], writes=["dram_KMT"])
        P.run()


def phase_attn(nc, A):
    with contextlib.ExitStack() as st:
        P = Phase(nc, "p4")
        ident = sb(st, nc, "p4_ident", [128, 128], BF16)
        identf = sb(st, nc, "p4_identf", [128, 128], F32)
        BT = sb(st, nc, "p4_BT", [128, 16, 5, 128], BF16)
        b31 = sb(st, nc, "p4_b31", [128, 16], F32)
        selc = sb(st, nc, "p4_sel", [32, 32, 128], BF16)
        pen = sb(st, nc, "p4_pen", [128, NTO, 32], F32)
        past = sb(st, nc, "p4_past", [128, NTO, 32], F32)
        own = sb(st, nc, "p4_own", [128, NTO, 32], F32)
        lamb = sb(st, nc, "p4_lamb", [128, 256], F32)
        lamt = sb(st, nc, "p4_lamt", [128, 128], F32)
        lam2 = sb(st, nc, "p4_lam2", [128, 2], F32)
        nlam = sb(st, nc, "p4_nlam", [128, 1], F32)
        subg = sb(st, nc, "p4_subg", [128, 128], F32)
        KTb = [sb(st, nc, "p4_KT%d" % i, [128, 2 * S], BF16) for i in range(2)]
        QTb = [sb(st, nc, "p4_QT%d" % i, [128, 2 * TO], BF16) for i in range(2)]
        Vb = [sb(st, nc, "p4_V%d" % i, [128, NTA, 130], BF16) for i in range(2)]
        KMb = [sb(st, nc, "p4_KM%d" % i, [128, 32], F32) for i in range(2)]
        QTf = [sb(st, nc, "p4_QTf%d" % i, [128, 128], F32) for i in range(2)]
        Pb = [sb(st, nc, "p4_P%d" % i, [128, 512], BF16) for i in range(6)]
        oh = [sb(st, nc, "p4_oh%d" % i, [128, NTO, 128], BF16) for i in range(2)]
        gate = [sb(st, nc, "p4_gate%d" % i, [128, 32], F32) for i in range(2)]
        top8 = [sb(st, nc, "p4_top8%d" % i, [128, 8], F32) for i in range(2)]
        mb = [sb(st, nc, "p4_mb%d" % i, [128, 32], F32) for i in range(2)]
        mbT = [sb(st, nc, "p4_mbT%d" % i, [32, 128], BF16) for i in range(2)]
        rl = [sb(st, nc, "p4_rl%d" % i, [128, 2], F32) for i in range(2)]
        o1 = [sb(st, nc, "p4_o1%d" % i, [128, 128], F32) for i in range(2)]
        o2 = [sb(st, nc, "p4_o2%d" % i, [128, 128], F32) for i in range(2)]
        junk = [sb(st, nc, "p4_junk%d" % i, [128, 128], F32) for i in range(2)]
        ssq = [sb(st, nc, "p4_ssq%d" % i, [128, 1], F32) for i in range(2)]
        pS = [ps(st, nc, "p4_pS%d" % i, [128, 512], F32) for i in range(4)]
        pO = [ps(st, nc, "p4_pO%d" % i, [128, 512], F32) for i in range(2)]
        pOb = [ps(st, nc, "p4_pOb%d" % i, [128, 512], F32) for i in range(2)]
        pG = pOb[0][:, 0:32]
        pM = pOb[1][0:32, 0:128]

        P.dma("sp", lambda e: e.dma_start(out=ident[:], in_=A["ident_bf"][:, :]), writes=["ident"])
        P.dma("sp", lambda e: e.dma_start(out=identf[:], in_=A["ident_f"][:, :]), writes=["identf"])
        P.dma("sp", lambda e: e.dma_start(out=BT[:].rearrange("p h j q -> p (h j q)"), in_=A["BT"][:, :]), writes=["BT"])
        P.dma("sp", lambda e: e.dma_start(out=b31[:], in_=A["b31"][:, :]), writes=["b31"])
        P.dma("sp", lambda e: e.dma_start(out=selc[:].rearrange("p n k -> p (n k)"), in_=A["selc"][:, :]), writes=["selc"])
        P.dma("sp", lambda e: e.dma_start(out=pen[:].rearrange("p m n -> p (m n)"), in_=A["pen"][:, :]), writes=["pen"])
        P.dma("sp", lambda e: e.dma_start(out=past[:].rearrange("p m n -> p (m n)"), in_=A["past"][:, :]), writes=["past"])
        P.dma("sp", lambda e: e.dma_start(out=own[:].rearrange("p m n -> p (m n)"), in_=A["own"][:, :]), writes=["own"])
        P.dma("sp", lambda e: e.dma_start(out=lamb[:], in_=A["lam_b"][:, :]), writes=["lamb"])
        P.dma("sp", lambda e: e.dma_start(out=subg[:], in_=A["subln_b"][:, :]), writes=["subg"])
        lv = lamb[:].rearrange("p (a d) -> p a d", a=4)
        P.op("dve", lambda e: e.tensor_tensor(out=lamt[:, 0:64], in0=lv[:, 0, :], in1=lv[:, 1, :], op=ALU.mult), reads=["lamb"], writes=["lamt"])
        P.op("dve", lambda e: e.tensor_tensor(out=lamt[:, 64:128], in0=lv[:, 2, :], in1=lv[:, 3, :], op=ALU.mult), reads=["lamb", "lamt"], writes=["lamt"])
        for a_ in range(2):
            P.op("act", lambda e, a_=a_: e.activation(out=lamb[:, a_ * 64:(a_ + 1) * 64], in_=lamt[:, a_ * 64:(a_ + 1) * 64], func=AF.Copy, accum_out=lam2[:, a_:a_ + 1]),
                 reads=["lamt"], writes=["lam2", "lamb"])
        P.op("act", lambda e: e.activation(out=lam2[:], in_=lam2[:], func=AF.Exp), reads=["lam2"], writes=["lam2"])
        P.op("dve", lambda e: e.tensor_tensor(out=nlam[:], in0=lam2[:, 1:2], in1=lam2[:, 0:1], op=ALU.subtract), reads=["lam2"], writes=["nlam"])
        P.op("dve", lambda e: e.tensor_scalar(out=nlam[:], in0=nlam[:], scalar1=-0.2, scalar2=None, op0=ALU.add), reads=["nlam"], writes=["nlam"])
        P.op("dve", lambda e: e.tensor_scalar(out=subg[:], in0=subg[:], scalar1=0.8, scalar2=None, op0=ALU.mult), reads=["subg"], writes=["subg"])
        for i in range(2):
            P.op("pool", lambda e, i=i: e.memset(Vb[i][:, :, 128:130], 1.0), writes=["V%d" % i])

        scount = 0
        pcount = 0
        qcount = 0
        def load_head(h):
            hb_ = h % 2
            diff = h < 8
            hh = h if diff else h - 8
            kres, qres, vres, kmres = "KT%d" % hb_, "QT%d" % hb_, "V%d" % hb_, "KM%d" % hb_
            if diff:
                for mp in range(2):
                    P.dma("sp", lambda e, hb_=hb_, hh=hh, mp=mp: e.dma_start(out=KTb[hb_][0:64, mp * S:(mp + 1) * S], in_=A["KTd"][hh, mp * 64:(mp + 1) * 64, :]), writes=[kres])
                    P.dma("sp", lambda e, hb_=hb_, hh=hh, mp=mp: e.dma_start(out=QTb[hb_][0:64, mp * TO:(mp + 1) * TO], in_=A["QTd"][hh, mp * 64:(mp + 1) * 64, :]), writes=[qres])
                vsrc = A["Vd"]
            else:
                P.dma("sp", lambda e, hb_=hb_, hh=hh: e.dma_start(out=KTb[hb_][:, 0:S], in_=A["KTm"][hh]), writes=[kres])
                P.dma("sp", lambda e, hb_=hb_, hh=hh: e.dma_start(out=QTb[hb_][:, 0:TO], in_=A["QTm"][hh]), writes=[qres])
                P.dma("sp", lambda e, hb_=hb_, hh=hh: e.dma_start(out=KMb[hb_][:], in_=A["KMT"][hh]), writes=[kmres])
                vsrc = A["Vm"]
            P.dma("sp", lambda e, hb_=hb_, hh=hh, vsrc=vsrc: e.dma_start(out=Vb[hb_][:, :, 0:128], in_=vsrc[:, hh * 128:(hh + 1) * 128].rearrange("(t p) d -> p t d", p=128)),
                  writes=[vres])

        load_head(0)
        for h in range(16):
            hb_ = h % 2
            diff = h < 8
            hh = h if diff else h - 8
            kres, qres, vres, kmres = "KT%d" % hb_, "QT%d" % hb_, "V%d" % hb_, "KM%d" % hb_
            if h + 1 < 16:
                load_head(h + 1)
            nmaps = 2 if diff else 1
            ohres = "oh%d" % hb_

            def emit_pre(m, hb_=hb_, kmres=kmres, qres=qres, diff=diff):
                qb = m % 2
                if diff:
                    return
                gres, tres, mres, mtres = "gate%d" % qb, "top8%d" % qb, "mb%d" % qb, "mbT%d" % qb
                P.op("act", lambda e, qb=qb, hb_=hb_, m=m: e.activation(out=QTf[qb][:], in_=QTb[hb_][:, m * 128:(m + 1) * 128], func=AF.Copy), reads=[qres], writes=["QTf%d" % qb])
                P.op("pe", lambda e, qb=qb, hb_=hb_: e.matmul(pG, lhsT=QTf[qb][:], rhs=KMb[hb_][:], start=True, stop=True), reads=["QTf%d" % qb, kmres], writes=["pOb0"])
                P.op("dve", lambda e, qb=qb, m=m: e.tensor_tensor(out=gate[qb][:], in0=pG, in1=pen[:, m, :], op=ALU.add), reads=["pOb0", "pen"], writes=[gres])
                P.op("dve", lambda e, qb=qb: e.max(out=top8[qb][:], in_=gate[qb][:]), reads=[gres], writes=[tres])
                P.op("dve", lambda e, qb=qb: e.tensor_scalar(out=mb[qb][:], in0=gate[qb][:], scalar1=top8[qb][:, 2:3], scalar2=None, op0=ALU.is_ge), reads=[gres, tres], writes=[mres])
                P.op("dve", lambda e, qb=qb, m=m: e.tensor_tensor(out=mb[qb][:], in0=mb[qb][:], in1=past[:, m, :], op=ALU.mult), reads=[mres, "past"], writes=[mres])
                P.op("dve", lambda e, qb=qb, m=m: e.tensor_tensor(out=mb[qb][:], in0=mb[qb][:], in1=own[:, m, :], op=ALU.add), reads=[mres, "own"], writes=[mres])
                P.op("dve", lambda e, qb=qb: e.tensor_scalar(out=mb[qb][:], in0=mb[qb][:], scalar1=-1.0, scalar2=BIG, op0=ALU.add, op1=ALU.mult), reads=[mres], writes=[mres])
                P.op("pe", lambda e, qb=qb: e.transpose(out=pM, in_=mb[qb][:], identity=identf[:]), reads=[mres, "identf"], writes=["pOb1"])
                P.op("dve", lambda e, qb=qb: e.tensor_copy(out=mbT[qb][:], in_=pM), reads=["pOb1"], writes=[mtres])

            def emit_qk(m, g, hb_=hb_, h=h, diff=diff, nmaps=nmaps, kres=kres, qres=qres):
                nonlocal scount
                qb = m % 2
                near_last = (g == m)
                prev_grp = (g == m - 1)
                sidx = []
                for mp in range(nmaps):
                    si = scount % 4
                    scount += 1
                    sidx.append(si)
                    sres = "pS%d" % si

                    def mmqk(e, si=si, mp=mp, g=g, m=m, hb_=hb_, h=h, diff=diff, near_last=near_last, prev_grp=prev_grp, qb=qb):
                        ins = None
                        for j4 in range(4):
                            kj = 4 * g + j4
                            if diff:
                                lhs = KTb[hb_][0:64, mp * S + kj * 128: mp * S + (kj + 1) * 128]
                                rhs = QTb[hb_][0:64, mp * TO + m * 128: mp * TO + (m + 1) * 128]
                            else:
                                lhs = KTb[hb_][:, kj * 128:(kj + 1) * 128]
                                rhs = QTb[hb_][:, m * 128:(m + 1) * 128]
                            bt_j = None
                            if near_last:
                                bt_j = j4 + 1
                            elif prev_grp and j4 == 3:
                                bt_j = 0
                            last = (bt_j is None) and diff
                            out = pS[si][:, j4 * 128:(j4 + 1) * 128]
                            ins = e.matmul(out, lhsT=lhs, rhs=rhs, start=True, stop=last)
                            if not diff:
                                ins = e.matmul(out, lhsT=selc[:, kj // 2, :], rhs=mbT[qb][:], start=False, stop=(bt_j is None))
                            if bt_j is not None:
                                ins = e.matmul(out, lhsT=ident[:], rhs=BT[:, h, bt_j, :], start=False, stop=True)
                        return ins
                    rd = [kres, qres, "ident", "BT"]
                    if not diff:
                        rd += ["selc", "mbT%d" % qb]
                    P.op("pe", mmqk, reads=rd, writes=[sres])
                return sidx

            def emit_exp_pv(m, g, sidx, hb_=hb_, h=h, diff=diff, nmaps=nmaps, vres=vres):
                nonlocal pcount
                qb = m % 2
                pOres = "pO%d" % qb
                pObres = "pOb%d" % qb
                near_last = (g == m)
                prev_grp = (g == m - 1)
                pidx = []
                for mp in range(nmaps):
                    si = sidx[mp]
                    pi = pcount % 6
                    pcount += 1
                    pidx.append(pi)
                    sres, pres = "pS%d" % si, "P%d" % pi
                    if near_last:
                        P.op("act", lambda e, pi=pi, si=si: e.activation(out=Pb[pi][:], in_=pS[si][:], func=AF.Exp), reads=[sres], writes=[pres])
                    elif prev_grp:
                        P.op("act", lambda e, pi=pi, si=si, h=h: e.activation(out=Pb[pi][:, 0:384], in_=pS[si][:, 0:384], func=AF.Exp, bias=b31[:, h:h + 1]), reads=[sres, "b31"], writes=[pres])
                        P.op("act", lambda e, pi=pi, si=si: e.activation(out=Pb[pi][:, 384:512], in_=pS[si][:, 384:512], func=AF.Exp), reads=[sres], writes=[pres])
                    else:
                        P.op("act", lambda e, pi=pi, si=si, h=h: e.activation(out=Pb[pi][:], in_=pS[si][:], func=AF.Exp, bias=b31[:, h:h + 1]), reads=[sres, "b31"], writes=[pres])
                for mp in range(nmaps):
                    pi = pidx[mp]

                    def mmpv(e, pi=pi, mp=mp, g=g, qb=qb, hb_=hb_, m=m):
                        ins = None
                        for j4 in range(4):
                            kj = 4 * g + j4
                            ins = e.matmul((pO if mp == 0 else pOb)[qb][:, 0:130], lhsT=Pb[pi][:, j4 * 128:(j4 + 1) * 128], rhs=Vb[hb_][:, kj, :],
                                           start=(kj == 0), stop=(kj == 4 * m + 3))
                        return ins
                    P.op("pe", mmpv, reads=["P%d" % pi, vres], writes=[pOres if mp == 0 else pObres])

            def emit_epi(m, hb_=hb_, diff=diff, ohres=ohres):
                qb = m % 2
                pOres = "pO%d" % qb
                pObres = "pOb%d" % qb
                rres, o1res, o2res = "rl%d" % qb, "o1%d" % qb, "o2%d" % qb
                if diff:
                    P.op("dve", lambda e, qb=qb: e.reciprocal(out=rl[qb][:, 0:1], in_=pO[qb][:, 128:129]), reads=[pOres], writes=[rres])
                    P.op("dve", lambda e, qb=qb: e.reciprocal(out=rl[qb][:, 1:2], in_=pOb[qb][:, 128:129]), reads=[pObres, rres], writes=[rres])
                    P.op("dve", lambda e, qb=qb: e.tensor_scalar(out=o1[qb][:], in0=pO[qb][:, 0:128], scalar1=rl[qb][:, 0:1], scalar2=None, op0=ALU.mult), reads=[pOres, rres], writes=[o1res])
                    P.op("dve", lambda e, qb=qb: e.tensor_scalar(out=o2[qb][:], in0=pOb[qb][:, 0:128], scalar1=rl[qb][:, 1:2], scalar2=nlam[:, 0:1], op0=ALU.mult, op1=ALU.mult),
                         reads=[pObres, rres, "nlam"], writes=[o2res])
                    P.op("dve", lambda e, qb=qb: e.tensor_tensor(out=o1[qb][:], in0=o1[qb][:], in1=o2[qb][:], op=ALU.add), reads=[o1res, o2res], writes=[o1res])
                    P.op("act", lambda e, qb=qb: e.activation(out=junk[qb][:], in_=o1[qb][:], func=AF.Square, accum_out=ssq[qb][:]), reads=[o1res], writes=["junk%d" % qb, "ssq%d" % qb])
                    P.op("dve", lambda e, qb=qb: e.tensor_scalar(out=ssq[qb][:], in0=ssq[qb][:], scalar1=1.0 / 128.0, scalar2=EPS, op0=ALU.mult, op1=ALU.add), reads=["ssq%d" % qb], writes=["ssq%d" % qb])
                    P.op("act", lambda e, qb=qb: e.activation(out=ssq[qb][:], in_=ssq[qb][:], func=AF.Sqrt), reads=["ssq%d" % qb], writes=["ssq%d" % qb])
                    P.op("dve", lambda e, qb=qb: e.reciprocal(out=ssq[qb][:], in_=ssq[qb][:]), reads=["ssq%d" % qb], writes=["ssq%d" % qb])
                    P.op("dve", lambda e, qb=qb, hb_=hb_, m=m: e.scalar_tensor_tensor(out=oh[hb_][:, m, :], in0=o1[qb][:], scalar=ssq[qb][:, 0:1], in1=subg[:], op0=ALU.mult, op1=ALU.mult),
                         reads=[o1res, "ssq%d" % qb, "subg"], writes=[ohres])
                else:
                    P.op("dve", lambda e, qb=qb: e.reciprocal(out=rl[qb][:, 0:1], in_=pO[qb][:, 128:129]), reads=[pOres], writes=[rres])
                    P.op("dve", lambda e, qb=qb, hb_=hb_, m=m: e.tensor_scalar(out=oh[hb_][:, m, :], in0=pO[qb][:, 0:128], scalar1=rl[qb][:, 0:1], scalar2=None, op0=ALU.mult),
                         reads=[pOres, rres], writes=[ohres])

            pending = None
            for m in range(NTO):
                for g in range(m + 1):
                    if g == 0:
                        emit_pre(m)
                    sidx = emit_qk(m, g)
                    if pending is not None:
                        emit_exp_pv(*pending)
                        if pending[1] == pending[0]:
                            emit_epi(pending[0])
                    pending = (m, g, sidx)
            emit_exp_pv(*pending)
            emit_epi(pending[0])
            P.dma("sp", lambda e, hb_=hb_, h=h: e.dma_start(out=A["o_d"][:, h * 128:(h + 1) * 128].rearrange("(m p) d -> p m d", p=128), in_=oh[hb_][:]),
                  reads=[ohres], writes=["dram_o"])
        P.run()


def phase_oproj(nc, A):
    with contextlib.ExitStack() as st:
        P = Phase(nc, "p5")
        ident = sb(st, nc, "p5_ident", [128, 128], BF16)
        Wd = sb(st, nc, "p5_Wd", [128, 8, D], BF16)
        Wm = sb(st, nc, "p5_Wm", [128, 8, D], BF16)
        ot = [sb(st, nc, "p5_ot%d" % i, [128, D], BF16) for i in range(2)]
        oT = [sb(st, nc, "p5_oT%d" % i, [128, KC, 512], BF16) for i in range(1)]
        Gd = [sb(st, nc, "p5_Gd%d" % i, [128, KC, 512], BF16) for i in range(1)]
        Gm = [sb(st, nc, "p5_Gm%d" % i, [128, KC, 512], BF16) for i in range(1)]
        zT = [sb(st, nc, "p5_zT%d" % i, [128, KC, 512], BF16) for i in range(1)]
        t1 = [sb(st, nc, "p5_t1%d" % i, [128, 512], F32) for i in range(2)]
        pT = [ps(st, nc, "p5_pT%d" % i, [128, KC, 128], BF16) for i in range(1)]
        pY = [ps(st, nc, "p5_pY%d" % i, [128, 512], F32) for i in range(4)]
        P.dma("sp", lambda e: e.dma_start(out=ident[:], in_=A["ident_bf"][:, :]), writes=["ident"])
        for q4 in range(2):
            P.dma("pool", lambda e, q4=q4: e.dma_start(out=Wd[:, q4 * 4:(q4 + 1) * 4, :], in_=A["w_o_diff"].rearrange("(k p) n -> p k n", p=128)[:, q4 * 4:(q4 + 1) * 4, :]), writes=["Wd"])
            P.dma("pool", lambda e, q4=q4: e.dma_start(out=Wm[:, q4 * 4:(q4 + 1) * 4, :], in_=A["w_o_moba"].rearrange("(k p) n -> p k n", p=128)[:, q4 * 4:(q4 + 1) * 4, :]), writes=["Wm"])
        tcount = 0
        ycount = 0
        for g in range(4):
            gi = 0
            P.dma("sp", lambda e, gi=gi, g=g: e.dma_start(out=Gd[gi][:], in_=A["GT"][0:16, :, g * 512:(g + 1) * 512].rearrange("c p t -> p c t")), writes=["Gd%d" % gi])
            P.dma("sp", lambda e, gi=gi, g=g: e.dma_start(out=Gm[gi][:], in_=A["GT"][16:32, :, g * 512:(g + 1) * 512].rearrange("c p t -> p c t")), writes=["Gm%d" % gi])
            for tt in range(4):
                ti = tcount % 2
                tcount += 1
                tile = g * 4 + tt
                P.dma("sp", lambda e, ti=ti, tile=tile: e.dma_start(out=ot[ti][:], in_=A["o_d"][tile * 128:(tile + 1) * 128, :]), writes=["ot%d" % ti])

                def tr(e, ti=ti):
                    ins = None
                    for k in range(KC):
                        ins = e.transpose(out=pT[0][:, k, :], in_=ot[ti][:, k * 128:(k + 1) * 128], identity=ident[:])
                    return ins
                P.op("pe", tr, reads=["ot%d" % ti, "ident"], writes=["pT"])
                P.op("act", lambda e, gi=gi, tt=tt: e.activation(out=oT[gi][:, :, tt * 128:(tt + 1) * 128], in_=pT[0][:], func=AF.Copy), reads=["pT"], writes=["oT%d" % gi])
            for c in range(KC):
                yd_i = ycount % 4
                ym_i = (ycount + 1) % 4
                ycount += 2
                t1i = c % 2

                def mmd(e, gi=gi, c=c, yd_i=yd_i):
                    ins = None
                    for k in range(8):
                        ins = e.matmul(pY[yd_i][:], lhsT=Wd[:, k, c * 128:(c + 1) * 128], rhs=oT[gi][:, k, :], start=(k == 0), stop=(k == 7))
                    return ins

                def mmm(e, gi=gi, c=c, ym_i=ym_i):
                    ins = None
                    for k in range(8):
                        ins = e.matmul(pY[ym_i][:], lhsT=Wm[:, k, c * 128:(c + 1) * 128], rhs=oT[gi][:, 8 + k, :], start=(k == 0), stop=(k == 7))
                    return ins
                P.op("pe", mmd, reads=["Wd", "oT%d" % gi], writes=["pY%d" % yd_i])
                P.op("pe", mmm, reads=["Wm", "oT%d" % gi], writes=["pY%d" % ym_i])
                P.op("dve", lambda e, gi=gi, c=c, yd_i=yd_i, t1i=t1i: e.tensor_tensor(out=t1[t1i][:], in0=pY[yd_i][:], in1=Gd[gi][:, c, :], op=ALU.mult),
                     reads=["pY%d" % yd_i, "Gd%d" % gi], writes=["t1%d" % t1i])
                P.op("dve", lambda e, gi=gi, c=c, ym_i=ym_i, t1i=t1i: e.tensor_tensor(out=zT[gi][:, c, :], in0=pY[ym_i][:], in1=Gm[gi][:, c, :], op=ALU.mult),
                     reads=["pY%d" % ym_i, "Gm%d" % gi], writes=["zT%d" % gi])
                P.op("pool", lambda e, gi=gi, c=c, t1i=t1i: e.tensor_tensor(out=zT[gi][:, c, :], in0=zT[gi][:, c, :], in1=t1[t1i][:], op=ALU.add),
                     reads=["t1%d" % t1i, "zT%d" % gi], writes=["zT%d" % gi])
            P.dma("sp", lambda e, gi=gi, g=g: e.dma_start(out=A["zT"][g], in_=zT[gi][:].rearrange("p c t -> p (c t)")), reads=["zT%d" % gi], writes=["dram_zT"])
        P.run()


def phase_wout(nc, A):
    with contextlib.ExitStack() as st:
        P = Phase(nc, "p6")
        Wo = sb(st, nc, "p6_Wo", [128, KC, D], BF16)
        G1 = sb(st, nc, "p6_G1", [128, D], F32)
        zT = [sb(st, nc, "p6_zT%d" % i, [128, KC, 512], BF16) for i in range(2)]
        xt = [sb(st, nc, "p6_x%d" % i, [128, D], F32) for i in range(2)]
        pY = [ps(st, nc, "p6_pY%d" % i, [128, 512], F32) for i in range(4)]
        for q4 in range(4):
            P.dma("pool", lambda e, q4=q4: e.dma_start(out=Wo[:, q4 * 4:(q4 + 1) * 4, :], in_=A["w_out"].rearrange("(k p) n -> p k n", p=128)[:, q4 * 4:(q4 + 1) * 4, :]), writes=["Wo"])
        P.dma("sp", lambda e: e.dma_start(out=G1[:], in_=A["modb"][:, MOD_G1:MOD_G1 + D]), writes=["G1"])
        ycount = 0
        tcount = 0
        for g in range(4):
            gi = g % 2
            P.dma("sp", lambda e, gi=gi, g=g: e.dma_start(out=zT[gi][:].rearrange("p c t -> p (c t)"), in_=A["zT"][g]), writes=["zT%d" % gi])
            for tt in range(4):
                ti = tcount % 2
                tcount += 1
                tile = g * 4 + tt
                P.dma("sp", lambda e, ti=ti, tile=tile: e.dma_start(out=xt[ti][:], in_=A["x_own"][tile * 128:(tile + 1) * 128, :]), writes=["x%d" % ti])
                for cg in range(4):
                    yi = ycount % 4
                    ycount += 1

                    def mm(e, gi=gi, tt=tt, cg=cg, yi=yi):
                        ins = None
                        for k in range(KC):
                            ins = e.matmul(pY[yi][:], lhsT=zT[gi][:, k, tt * 128:(tt + 1) * 128], rhs=Wo[:, k, cg * 512:(cg + 1) * 512], start=(k == 0), stop=(k == KC - 1))
                        return ins
                    P.op("pe", mm, reads=["zT%d" % gi, "Wo"], writes=["pY%d" % yi])
                    P.op("dve", lambda e, yi=yi, cg=cg, ti=ti: e.tensor_tensor(out=pY[yi][:], in0=pY[yi][:], in1=G1[:, cg * 512:(cg + 1) * 512], op=ALU.mult),
                         reads=["pY%d" % yi, "G1"], writes=["pY%d" % yi])
                    P.op("dve", lambda e, yi=yi, cg=cg, ti=ti: e.tensor_tensor(out=xt[ti][:, cg * 512:(cg + 1) * 512], in0=pY[yi][:], in1=xt[ti][:, cg * 512:(cg + 1) * 512], op=ALU.add),
                         reads=["pY%d" % yi, "x%d" % ti], writes=["x%d" % ti])
                P.dma("sp", lambda e, ti=ti, tile=tile: e.dma_start(out=A["x1"][tile * 128:(tile + 1) * 128, :], in_=xt[ti][:]), reads=["x%d" % ti], writes=["dram_x1"])
        P.run()


def phase_route(nc, A):
    with contextlib.ExitStack() as st:
        P = Phase(nc, "p7")
        ident = sb(st, nc, "p7_ident", [128, 128], BF16)
        ltri = sb(st, nc, "p7_ltri", [128, 128], BF16)
        onesb = sb(st, nc, "p7_ones", [128, 128], BF16)
        Amod = sb(st, nc, "p7_A", [128, D], F32)
        Smod = sb(st, nc, "p7_S", [128, D], F32)
        Wr = sb(st, nc, "p7_Wr", [128, KC, E], BF16)
        rbias = sb(st, nc, "p7_rbias", [128, E], F32)
        ecap = sb(st, nc, "p7_ecap", [128, E], F32)
        dumpidx = sb(st, nc, "p7_dump", [128, 1], F32)
        tokid = sb(st, nc, "p7_tokid", [128, NTO], I32)
        zero_i = sb(st, nc, "p7_zero", [128, CAP * 8], I32)
        zero_b = sb(st, nc, "p7_zerob", [128, D], BF16)
        xt = [sb(st, nc, "p7_x%d" % i, [128, D], F32) for i in range(2)]
        sq = [sb(st, nc, "p7_sq%d" % i, [128, D], BF16) for i in range(2)]
        ss = [sb(st, nc, "p7_ss%d" % i, [128, 1], F32) for i in range(2)]
        rstd = [sb(st, nc, "p7_rs%d" % i, [128, 1], F32) for i in range(2)]
        hf = [sb(st, nc, "p7_hf%d" % i, [128, D], F32) for i in range(2)]
        hb = [sb(st, nc, "p7_hb%d" % i, [128, D], BF16) for i in range(2)]
        hT = [sb(st, nc, "p7_hT%d" % i, [128, KC, 128], BF16) for i in range(2)]
        emask = sb(st, nc, "p7_emask", [128, NTO, E], BF16)
        scores = [sb(st, nc, "p7_sc%d" % i, [128, E], F32) for i in range(2)]
        selv = [sb(st, nc, "p7_sel%d" % i, [128, E], F32) for i in range(2)]
        g8 = [sb(st, nc, "p7_g8%d" % i, [128, 8], F32) for i in range(2)]
        gsc = [sb(st, nc, "p7_gsc%d" % i, [128, 8], F32) for i in range(2)]
        gm8 = [sb(st, nc, "p7_gm%d" % i, [128, 8], F32) for i in range(2)]
        gmask = [sb(st, nc, "p7_gmask%d" % i, [128, 8], F32) for i in range(2)]
        t8 = [sb(st, nc, "p7_t8%d" % i, [128, 8], F32) for i in range(2)]
        em = [sb(st, nc, "p7_em%d" % i, [128, E], F32) for i in range(2)]
        wt = sb(st, nc, "p7_wt", [128, NTO, E], F32)
        wsum = [sb(st, nc, "p7_ws%d" % i, [128, 1], F32) for i in range(2)]
        key = [sb(st, nc, "p7_key%d" % i, [128, E], F32) for i in range(2)]
        k8 = [sb(st, nc, "p7_k8%d" % i, [128, 8], F32) for i in range(2)]
        oh_ = [sb(st, nc, "p7_oh%d" % i, [128, E], F32) for i in range(2)]
        junk_ = [sb(st, nc, "p7_junk%d" % i, [128, E], F32) for i in range(2)]
        cnt_i = sb(st, nc, "p7_cnt_i", [128, E], I32)
        route = [sb(st, nc, "p7_route%d" % i, [128, 16], F32) for i in range(2)]
        sidx = [sb(st, nc, "p7_sidx%d" % i, [128, 8], I32) for i in range(2)]
        tokrow = [sb(st, nc, "p7_tokrow%d" % i, [128, 8], I32) for i in range(2)]
        valid = [sb(st, nc, "p7_valid%d" % i, [128, 8], F32) for i in range(2)]
        pT = [ps(st, nc, "p7_pT%d" % i, [128, KC, 128], BF16) for i in range(2)]
        pL = [ps(st, nc, "p7_pL%d" % i, [128, E], F32) for i in range(2)]
        pR = [ps(st, nc, "p7_pR%d" % i, [128, E], F32) for i in range(2)]

        P.dma("sp", lambda e: e.dma_start(out=ident[:], in_=A["ident_bf"][:, :]), writes=["ident"])
        P.dma("sp", lambda e: e.dma_start(out=ltri[:], in_=A["ltri"][:, :]), writes=["ltri"])
        P.dma("sp", lambda e: e.dma_start(out=Amod[:], in_=A["modb"][:, MOD_A2:MOD_A2 + D]), writes=["Amod"])
        P.dma("sp", lambda e: e.dma_start(out=Smod[:], in_=A["modb"][:, MOD_SHIFT2:MOD_SHIFT2 + D]), writes=["Smod"])
        P.dma("pool", lambda e: e.dma_start(out=Wr[:], in_=A["w_router"].rearrange("(k p) n -> p k n", p=128)), writes=["Wr"])
        P.dma("sp", lambda e: e.dma_start(out=rbias[:], in_=A["rbias_b"][:, :]), writes=["rbias"])
        P.dma("sp", lambda e: e.dma_start(out=ecap[:], in_=A["ecap"][:, :]), writes=["ecap"])
        P.dma("sp", lambda e: e.dma_start(out=dumpidx[:], in_=A["dumpidx"][:, :]), writes=["dumpidx"])
        P.dma("sp", lambda e: e.dma_start(out=tokid[:], in_=A["tokid"][:, :]), writes=["tokid"])
        P.op("dve", lambda e: e.memset(onesb[:], 1.0), writes=["onesb"])
        P.op("pool", lambda e: e.memset(zero_i[:], 0), writes=["zero_i"])
        P.op("pool", lambda e: e.memset(zero_b[:], 0.0), writes=["zero_b"])
        rt_view = A["rowtok"][0:NSLOT, :].rearrange("(e c) w -> e (c w)", e=E)
        P.dma("sp", lambda e: e.dma_start(out=rt_view, in_=zero_i[0:E, :]), reads=["zero_i"], writes=["dram_rowtok"])
        P.dma("sp", lambda e: e.dma_start(out=A["rowtok"][NSLOT:NSLOT + 128, :], in_=zero_i[:, 0:8]), reads=["zero_i"], writes=["dram_rowtok"])
        P.dma("sp", lambda e: e.dma_start(out=A["Ybuf"][NSLOT:NSLOT + 128, :], in_=zero_b[:]), reads=["zero_b"], writes=["dram_Ybuf"])

        for t in range(NTO):
            i = t % 2
            tag = str(i)
            P.dma("sp", lambda e, i=i, t=t: e.dma_start(out=xt[i][:], in_=A["x1"][t * 128:(t + 1) * 128, :]), writes=["x" + tag])
            emit_norm_mod_T(P, nc, xt[i], sq[i], ss[i], rstd[i], hf[i], hb[i], pT[i], hT[i], Amod, Smod, ident, tag, "x" + tag)
            P.dma("sp", lambda e, i=i, t=t: e.dma_start(out=A["h2"][t * 128:(t + 1) * 128, :], in_=hb[i][:]), reads=["hb" + tag], writes=["dram_h2"])
            P.dma("sp", lambda e, i=i, t=t: e.dma_start(out=A["h2T"][t], in_=hT[i][:].rearrange("p k t -> p (k t)")), reads=["hT" + tag], writes=["dram_h2T"])

            def mml(e, i=i):
                ins = None
                for k in range(KC):
                    ins = e.matmul(pL[i][:], lhsT=hT[i][:, k, :], rhs=Wr[:, k, :], start=(k == 0), stop=(k == KC - 1))
                return ins
            P.op("pe", mml, reads=["hT" + tag, "Wr"], writes=["pL" + tag])
            P.op("act", lambda e, i=i: e.activation(out=scores[i][:], in_=pL[i][:], func=AF.Sigmoid), reads=["pL" + tag], writes=["sc" + tag])
            P.op("dve", lambda e, i=i: e.tensor_tensor(out=selv[i][:], in0=scores[i][:], in1=rbias[:], op=ALU.add), reads=["sc" + tag, "rbias"], writes=["sel" + tag])
            for gq in range(8):
                P.op("dve", lambda e, i=i, gq=gq: e.max(out=g8[i][:], in_=selv[i][:, gq * 8:(gq + 1) * 8]), reads=["sel" + tag, "gsc" + tag], writes=["g8" + tag])
                P.op("dve", lambda e, i=i, gq=gq: e.tensor_tensor(out=gsc[i][:, gq:gq + 1], in0=g8[i][:, 0:1], in1=g8[i][:, 1:2], op=ALU.add), reads=["g8" + tag], writes=["gsc" + tag])
            P.op("dve", lambda e, i=i: e.max(out=gm8[i][:], in_=gsc[i][:]), reads=["gsc" + tag], writes=["gm8" + tag])
            P.op("dve", lambda e, i=i: e.tensor_scalar(out=gmask[i][:], in0=gsc[i][:], scalar1=gm8[i][:, 3:4], scalar2=None, op0=ALU.is_ge), reads=["gsc" + tag, "gm8" + tag], writes=["gmask" + tag])
            for gq in range(8):
                P.op("dve", lambda e, i=i, gq=gq: e.tensor_scalar(out=selv[i][:, gq * 8:(gq + 1) * 8], in0=selv[i][:, gq * 8:(gq + 1) * 8], scalar1=2.0, scalar2=gmask[i][:, gq:gq + 1],
                                                                 op0=ALU.add, op1=ALU.mult), reads=["sel" + tag, "gmask" + tag], writes=["sel" + tag])
            P.op("dve", lambda e, i=i: e.max(out=t8[i][:], in_=selv[i][:]), reads=["sel" + tag], writes=["t8" + tag])
            P.op("dve", lambda e, i=i: e.tensor_scalar(out=em[i][:], in0=selv[i][:], scalar1=t8[i][:, 7:8], scalar2=None, op0=ALU.is_ge), reads=["sel" + tag, "t8" + tag], writes=["em" + tag])
            P.op("dve", lambda e, i=i, t=t: e.tensor_copy(out=emask[:, t, :], in_=em[i][:]), reads=["em" + tag], writes=["emask"])
            P.op("dve", lambda e, i=i, t=t: e.tensor_tensor(out=wt[:, t, :], in0=scores[i][:], in1=em[i][:], op=ALU.mult), reads=["sc" + tag, "em" + tag], writes=["wt"])
            P.op("act", lambda e, i=i, t=t: e.activation(out=junk_[i][:], in_=wt[:, t, :], func=AF.Copy, accum_out=wsum[i][:]), reads=["wt"], writes=["ws" + tag, "junk" + tag])
            P.op("dve", lambda e, i=i: e.reciprocal(out=wsum[i][:], in_=wsum[i][:]), reads=["ws" + tag], writes=["ws" + tag])
            P.op("dve", lambda e, i=i, t=t: e.tensor_scalar(out=wt[:, t, :], in0=wt[:, t, :], scalar1=wsum[i][:, 0:1], scalar2=2.5, op0=ALU.mult, op1=ALU.mult), reads=["wt", "ws" + tag], writes=["wt"])

            def mmr(e, i=i, t=t):
                ins = None
                for j in range(t):
                    ins = e.matmul(pR[i][:], lhsT=onesb[:], rhs=emask[:, j, :], start=(j == 0), stop=False)
                ins = e.matmul(pR[i][:], lhsT=ltri[:], rhs=emask[:, t, :], start=(t == 0), stop=True)
                return ins
            P.op("pe", mmr, reads=["emask", "onesb", "ltri"], writes=["pR" + tag])
            P.op("dve", lambda e, i=i: e.scalar_tensor_tensor(out=key[i][:], in0=pR[i][:], scalar=1.0, in1=ecap[:], op0=ALU.add, op1=ALU.add), reads=["pR" + tag, "ecap"], writes=["key" + tag])
            P.op("dve", lambda e, i=i: e.tensor_scalar(out=oh_[i][:], in0=pR[i][:], scalar1=float(CAP) - 0.5, scalar2=None, op0=ALU.is_lt), reads=["pR" + tag], writes=["oh" + tag])
            P.op("dve", lambda e, i=i: e.tensor_tensor(out=oh_[i][:], in0=oh_[i][:], in1=em[i][:], op=ALU.mult), reads=["oh" + tag, "em" + tag], writes=["oh" + tag])
            P.op("dve", lambda e, i=i: e.tensor_tensor(out=key[i][:], in0=key[i][:], in1=oh_[i][:], op=ALU.mult), reads=["key" + tag, "oh" + tag], writes=["key" + tag])
            P.op("dve", lambda e, i=i: e.max(out=k8[i][:], in_=key[i][:]), reads=["key" + tag], writes=["k8" + tag])
            for kk in range(8):
                P.op("dve", lambda e, i=i, kk=kk: e.tensor_scalar(out=oh_[i][:], in0=key[i][:], scalar1=k8[i][:, kk:kk + 1], scalar2=None, op0=ALU.is_equal), reads=["key" + tag, "k8" + tag, "route" + tag], writes=["oh" + tag])
                P.op("dve", lambda e, i=i, kk=kk, t=t: e.tensor_tensor(out=oh_[i][:], in0=oh_[i][:], in1=wt[:, t, :], op=ALU.mult), reads=["oh" + tag, "wt"], writes=["oh" + tag])
                P.op("act", lambda e, i=i, kk=kk: e.activation(out=junk_[i][:], in_=oh_[i][:], func=AF.Copy, accum_out=route[i][:, 8 + kk:9 + kk]), reads=["oh" + tag], writes=["route" + tag, "junk" + tag])
            P.op("dve", lambda e, i=i: e.tensor_scalar(out=valid[i][:], in0=k8[i][:], scalar1=0.5, scalar2=None, op0=ALU.is_gt), reads=["k8" + tag], writes=["valid" + tag])
            P.op("dve", lambda e, i=i: e.tensor_tensor(out=route[i][:, 8:16], in0=route[i][:, 8:16], in1=valid[i][:], op=ALU.mult), reads=["route" + tag, "valid" + tag], writes=["route" + tag])
            P.op("dve", lambda e, i=i: e.tensor_scalar(out=route[i][:, 0:8], in0=k8[i][:], scalar1=-1.0, scalar2=dumpidx[:, 0:1], op0=ALU.add, op1=ALU.subtract), reads=["k8" + tag, "dumpidx", "route" + tag], writes=["route" + tag])
            P.op("dve", lambda e, i=i: e.tensor_tensor(out=route[i][:, 0:8], in0=route[i][:, 0:8], in1=valid[i][:], op=ALU.mult), reads=["route" + tag, "valid" + tag], writes=["route" + tag])
            P.op("dve", lambda e, i=i: e.tensor_scalar(out=route[i][:, 0:8], in0=route[i][:, 0:8], scalar1=dumpidx[:, 0:1], scalar2=None, op0=ALU.add), reads=["route" + tag, "dumpidx"], writes=["route" + tag])
            P.op("dve", lambda e, i=i: e.tensor_copy(out=sidx[i][:], in_=route[i][:, 0:8]), reads=["route" + tag], writes=["sidx" + tag])
            P.dma("sp", lambda e, i=i, t=t: e.dma_start(out=A["route"][:, t * 16:(t + 1) * 16], in_=route[i][:]), reads=["route" + tag], writes=["dram_route"])
            if t == NTO - 1:
                def mmc(e):
                    ins = None
                    for j in range(NTO):
                        ins = e.matmul(pL[0][:], lhsT=onesb[:], rhs=emask[:, j, :], start=(j == 0), stop=(j == NTO - 1))
                    return ins
                P.op("pe", mmc, reads=["emask", "onesb"], writes=["pL0"])
                P.op("dve", lambda e: e.tensor_copy(out=cnt_i[:], in_=pL[0][:]), reads=["pL0"], writes=["cnt_i"])
                P.dma("sp", lambda e: e.dma_start(out=A["cnt_d"][:, :], in_=cnt_i[0:1, :]), reads=["cnt_i"], writes=["dram_cnt"])
            for kk in range(8):
                P.op("pool", lambda e, i=i, kk=kk, t=t: e.tensor_copy(out=tokrow[i][:, kk:kk + 1], in_=tokid[:, t:t + 1]), reads=["tokid", "tokrow" + tag], writes=["tokrow" + tag])
            for kk in range(8):
                P.dma("pool", lambda e, i=i, kk=kk: e.indirect_dma_start(out=A["rowtok"][:, :], out_offset=bass.IndirectOffsetOnAxis(ap=sidx[i][:, kk:kk + 1], axis=0),
                                                                         in_=tokrow[i][:], in_offset=None),
                      reads=["sidx" + tag, "tokrow" + tag, "dram_rowtok"], writes=["dram_rowtok_s"])
        P.run()


def phase_experts(nc, A):
    NST = 4
    with contextlib.ExitStack() as st:
        P = Phase(nc, "p8")
        ident = sb(st, nc, "p8_ident", [128, 128], BF16)
        cnt_sb = sb(st, nc, "p8_cnt", [1, E], I32)
        P.cnt_ap = cnt_sb
        Wg = [sb(st, nc, "p8_Wg%d" % i, [128, KC, 512], BF16) for i in range(2)]
        Wu = [sb(st, nc, "p8_Wu%d" % i, [128, KC, 512], BF16) for i in range(2)]
        Wd = [sb(st, nc, "p8_Wd%d" % i, [128, 4, D], BF16) for i in range(2)]
        stage = [sb(st, nc, "p8_st%d" % i, [128, 2048], F32) for i in range(NST)]
        idx_all = sb(st, nc, "p8_idxall", [128, E * JMAX, 8], I32)
        xg = [sb(st, nc, "p8_xg%d" % i, [128, D], BF16) for i in range(3)]
        xT = [sb(st, nc, "p8_xT%d" % i, [128, KC, 128], BF16) for i in range(2)]
        sg = [sb(st, nc, "p8_sg%d" % i, [128, 512], F32) for i in range(2)]
        aT = [sb(st, nc, "p8_aT%d" % i, [128, 4, 128], BF16) for i in range(2)]
        Y = [sb(st, nc, "p8_Y%d" % i, [128, D], BF16) for i in range(2)]
        pT = [ps(st, nc, "p8_pT%d" % i, [128, KC, 128], BF16) for i in range(1)]
        pG = [ps(st, nc, "p8_pG%d" % i, [128, 512], F32) for i in range(2)]
        pU = [ps(st, nc, "p8_pU%d" % i, [128, 512], F32) for i in range(2)]
        pY = [ps(st, nc, "p8_pY%d" % i, [128, 512], F32) for i in range(2)]
        P.dma("pool", lambda e: e.dma_start(out=ident[:], in_=A["ident_bf"][:, :]), writes=["ident"])
        P.dma("pool", lambda e: e.dma_start(out=cnt_sb[:], in_=A["cnt_d"][:, :]), writes=["cnt_sb"])
        cn = {"g": 0, "y": 0, "yb": 0, "x": 0, "a": 0, "gu": 0, "st": 0, "ce": 0}

        def wsrc(ei):
            if ei < E:
                return A["w_exp_gate"][ei], A["w_exp_up"][ei], A["w_exp_down"][ei]
            return A["w_sh_gate"], A["w_sh_up"], A["w_sh_down"]

        def weight_chunks(ei):
            wi = ei % 2
            gsrc, usrc, dsrc = wsrc(ei)
            out = []
            for c in range(4):
                out.append((gsrc.rearrange("(k p) n -> p k n", p=128)[:, 4 * c:4 * c + 4, :], Wg[wi][:, 4 * c:4 * c + 4, :], "Wg%d_%d" % (wi, c), (4, 512)))
            for c in range(4):
                out.append((usrc.rearrange("(k p) n -> p k n", p=128)[:, 4 * c:4 * c + 4, :], Wu[wi][:, 4 * c:4 * c + 4, :], "Wu%d_%d" % (wi, c), (4, 512)))
            for c in range(4):
                out.append((dsrc.rearrange("(k p) n -> p k n", p=128)[:, c:c + 1, :], Wd[wi][:, c:c + 1, :], "Wd%d_%d" % (wi, c), (1, 2048)))
            return out

        def issue_chunk_dma(ch):
            src, dst, res, (a, b) = ch
            si = cn["st"] % NST
            cn["st"] += 1
            P.dma("sp", lambda e, si=si, src=src, a=a: e.dma_start(out=stage[si][:].rearrange("p (a b) -> p a b", a=a), in_=src), writes=["st%d" % si])
            return si

        def issue_chunk_cast(ch, si):
            src, dst, res, (a, b) = ch
            eng = "act"
            view = stage[si][:].rearrange("p (a b) -> p a b", a=a)
            if eng == "act":
                P.op("act", lambda e, dst=dst, view=view: e.activation(out=dst, in_=view, func=AF.Copy), reads=["st%d" % si], writes=[res])
            else:
                P.op(eng, lambda e, dst=dst, view=view: e.tensor_copy(out=dst, in_=view), reads=["st%d" % si], writes=[res])

        def ffn(wi, xi, dst):
            xres = "xT%d" % xi
            ai = cn["a"] % 2
            cn["a"] += 1
            ares = "aT%d" % ai
            gb = cn["gu"] % 2
            cn["gu"] += 1

            def mmg(e, wi=wi, xi=xi, gb=gb):
                ins = None
                for fc in range(4):
                    for k in range(KC):
                        ins = e.matmul(pG[gb][:, fc * 128:(fc + 1) * 128], lhsT=Wg[wi][:, k, fc * 128:(fc + 1) * 128], rhs=xT[xi][:, k, :], start=(k == 0), stop=(k == KC - 1))
                return ins

            def mmu(e, wi=wi, xi=xi, gb=gb):
                ins = None
                for fc in range(4):
                    for k in range(KC):
                        ins = e.matmul(pU[gb][:, fc * 128:(fc + 1) * 128], lhsT=Wu[wi][:, k, fc * 128:(fc + 1) * 128], rhs=xT[xi][:, k, :], start=(k == 0), stop=(k == KC - 1))
                return ins
            P.op("pe", mmg, reads=["Wg%d_%d" % (wi, c) for c in range(4)] + [xres], writes=["pG%d" % gb])
            P.op("pe", mmu, reads=["Wu%d_%d" % (wi, c) for c in range(4)] + [xres], writes=["pU%d" % gb])
            P.op("act", lambda e, gb=gb: e.activation(out=sg[gb][:], in_=pG[gb][:], func=AF.Sigmoid), reads=["pG%d" % gb], writes=["sg%d" % gb])
            P.op("dve", lambda e, gb=gb: e.tensor_tensor(out=sg[gb][:], in0=pG[gb][:], in1=sg[gb][:], op=ALU.mult), reads=["pG%d" % gb, "sg%d" % gb], writes=["sg%d" % gb])
            P.op("dve", lambda e, gb=gb, ai=ai: e.tensor_tensor(out=aT[ai][:], in0=pU[gb][:].rearrange("p (f t) -> p f t", f=4), in1=sg[gb][:].rearrange("p (f t) -> p f t", f=4), op=ALU.mult),
                 reads=["pU%d" % gb, "sg%d" % gb], writes=[ares])
            yi = cn["yb"] % 2
            cn["yb"] += 1
            for cg in range(4):
                pi = cn["y"] % 2
                cn["y"] += 1

                def mmy(e, wi=wi, ai=ai, cg=cg, pi=pi):
                    ins = None
                    for fc in range(4):
                        ins = e.matmul(pY[pi][:], lhsT=aT[ai][:, fc, :], rhs=Wd[wi][:, fc, cg * 512:(cg + 1) * 512], start=(fc == 0), stop=(fc == 3))
                    return ins
                P.op("pe", mmy, reads=[ares] + ["Wd%d_%d" % (wi, c) for c in range(4)], writes=["pY%d" % pi])
                if cg % 2 == 0:
                    P.op("act", lambda e, yi=yi, cg=cg, pi=pi: e.activation(out=Y[yi][:, cg * 512:(cg + 1) * 512], in_=pY[pi][:], func=AF.Copy), reads=["pY%d" % pi], writes=["Y%d_%d" % (yi, cg)])
                else:
                    P.op("dve", lambda e, yi=yi, cg=cg, pi=pi: e.tensor_copy(out=Y[yi][:, cg * 512:(cg + 1) * 512], in_=pY[pi][:]), reads=["pY%d" % pi], writes=["Y%d_%d" % (yi, cg)])
            P.dma("act", lambda e, yi=yi, dst=dst: e.dma_start(out=dst, in_=Y[yi][:]), reads=["Y%d_%d" % (yi, c) for c in range(4)], writes=["dram_Y"])

        for ei in range(E):
            P.dma("pool", lambda e, ei=ei: e.dma_start(out=idx_all[:, ei * JMAX:(ei + 1) * JMAX, :], in_=A["rowtok"][ei * CAP:(ei + 1) * CAP, :].rearrange("(t p) w -> p t w", p=128)),
                  writes=["idxall%d" % ei])
        for ch in weight_chunks(0):
            si = issue_chunk_dma(ch)
            issue_chunk_cast(ch, si)
        for ei in range(E):
            wi = ei % 2
            P.load_cond_reg(ei, "cnt_sb")
            nxt = weight_chunks(ei + 1)
            pend = []

            def pump(ncast):
                for _ in range(ncast):
                    if pend:
                        ch, si = pend.pop(0)
                        issue_chunk_cast(ch, si)
                while nxt and len(pend) < NST:
                    ch = nxt.pop(0)
                    pend.append((ch, issue_chunk_dma(ch)))
            for j in range(JMAX):
                pump(0 if j == 0 else 2)
                P.begin_region(ei, 128 * j)
                gi = cn["g"] % 3
                cn["g"] += 1
                xi = cn["x"] % 2
                cn["x"] += 1
                s0 = ei * CAP + j * 128
                P.dma("pool", lambda e, ei=ei, j=j, gi=gi: e.indirect_dma_start(out=xg[gi][:], out_offset=None, in_=A["h2"][:, :],
                                                                              in_offset=bass.IndirectOffsetOnAxis(ap=idx_all[:, ei * JMAX + j, 0:1], axis=0)),
                      reads=["idxall%d" % ei], writes=["xg%d" % gi])

                def tr(e, gi=gi):
                    ins = None
                    for k in range(KC):
                        ins = e.transpose(out=pT[0][:, k, :], in_=xg[gi][:, k * 128:(k + 1) * 128], identity=ident[:])
                    return ins
                P.op("pe", tr, reads=["xg%d" % gi, "ident"], writes=["pT"])
                P.op("dve", lambda e, xi=xi: e.tensor_copy(out=xT[xi][:], in_=pT[0][:]), reads=["pT"], writes=["xT%d" % xi])
                ffn(wi, xi, A["Ybuf"][s0:s0 + 128, :])
                P.end_region()
            while pend or nxt:
                pump(2)
        wi = E % 2
        for t in range(NTO):
            xi = cn["x"] % 2
            cn["x"] += 1
            P.dma("pool", lambda e, xi=xi, t=t: e.dma_start(out=xT[xi][:], in_=A["h2T"][t].rearrange("p (k q) -> p k q", k=KC)), writes=["xT%d" % xi])
            ffn(wi, xi, A["Ysh"][t * 128:(t + 1) * 128, :])
        P.run()


def phase_final(nc, A):
    with contextlib.ExitStack() as st:
        P = Phase(nc, "p9")
        G2 = sb(st, nc, "p9_G2", [128, D], F32)
        gf = sb(st, nc, "p9_gf", [128, D], F32)
        xt = [sb(st, nc, "p9_x%d" % i, [128, D], F32) for i in range(2)]
        ysh = [sb(st, nc, "p9_ysh%d" % i, [128, D], BF16) for i in range(2)]
        yk = [sb(st, nc, "p9_yk%d" % i, [128, D], BF16) for i in range(4)]
        acc = [sb(st, nc, "p9_acc%d" % i, [128, D], F32) for i in range(2)]
        route = [sb(st, nc, "p9_route%d" % i, [128, 16], F32) for i in range(2)]
        sidx = [sb(st, nc, "p9_sidx%d" % i, [128, 8], I32) for i in range(2)]
        sq = [sb(st, nc, "p9_sq%d" % i, [128, D], BF16) for i in range(2)]
        ss = [sb(st, nc, "p9_ss%d" % i, [128, 1], F32) for i in range(2)]
        P.dma("sp", lambda e: e.dma_start(out=G2[:], in_=A["modb"][:, MOD_G2:MOD_G2 + D]), writes=["G2"])
        P.dma("sp", lambda e: e.dma_start(out=gf[:], in_=A["g_final_b"][:, :]), writes=["gf"])
        kc = 0
        for t in range(NTO):
            i = t % 2
            tag = str(i)
            P.dma("sp", lambda e, i=i, t=t: e.dma_start(out=xt[i][:], in_=A["x1"][t * 128:(t + 1) * 128, :]), writes=["x" + tag])
            P.dma("sp", lambda e, i=i, t=t: e.dma_start(out=ysh[i][:], in_=A["Ysh"][t * 128:(t + 1) * 128, :]), writes=["ysh" + tag])
            P.dma("sp", lambda e, i=i, t=t: e.dma_start(out=route[i][:], in_=A["route"][:, t * 16:(t + 1) * 16]), writes=["route" + tag])
            P.op("dve", lambda e, i=i: e.tensor_copy(out=sidx[i][:], in_=route[i][:, 0:8]), reads=["route" + tag], writes=["sidx" + tag])
            P.op("dve", lambda e, i=i: e.tensor_copy(out=acc[i][:], in_=ysh[i][:]), reads=["ysh" + tag], writes=["acc" + tag])
            for kk in range(8):
                ki = kc % 4
                kc += 1
                P.dma("pool", lambda e, i=i, kk=kk, ki=ki: e.indirect_dma_start(out=yk[ki][:], out_offset=None, in_=A["Ybuf"][:, :],
                                                                              in_offset=bass.IndirectOffsetOnAxis(ap=sidx[i][:, kk:kk + 1], axis=0)),
                      reads=["sidx" + tag], writes=["yk%d" % ki])
                P.op("dve", lambda e, i=i, kk=kk, ki=ki: e.scalar_tensor_tensor(out=acc[i][:], in0=yk[ki][:], scalar=route[i][:, 8 + kk:9 + kk], in1=acc[i][:], op0=ALU.mult, op1=ALU.add),
                     reads=["yk%d" % ki, "route" + tag, "acc" + tag], writes=["acc" + tag])
            P.op("pool", lambda e, i=i: e.tensor_tensor(out=acc[i][:], in0=acc[i][:], in1=G2[:], op=ALU.mult), reads=["acc" + tag, "G2"], writes=["acc" + tag])
            P.op("pool", lambda e, i=i: e.tensor_tensor(out=xt[i][:], in0=xt[i][:], in1=acc[i][:], op=ALU.add), reads=["acc" + tag, "x" + tag], writes=["x" + tag])
            P.op("act", lambda e, i=i: e.activation(out=sq[i][:], in_=xt[i][:], func=AF.Square, accum_out=ss[i][:]), reads=["x" + tag], writes=["sq" + tag, "ss" + tag])
            P.op("dve", lambda e, i=i: e.tensor_scalar(out=ss[i][:], in0=ss[i][:], scalar1=1.0 / D, scalar2=EPS, op0=ALU.mult, op1=ALU.add), reads=["ss" + tag], writes=["ss" + tag])
            P.op("act", lambda e, i=i: e.activation(out=ss[i][:], in_=ss[i][:], func=AF.Sqrt), reads=["ss" + tag], writes=["ss" + tag])
            P.op("dve", lambda e, i=i: e.reciprocal(out=ss[i][:], in_=ss[i][:]), reads=["ss" + tag], writes=["ss" + tag])
            P.op("dve", lambda e, i=i: e.scalar_tensor_tensor(out=acc[i][:], in0=xt[i][:], scalar=ss[i][:, 0:1], in1=gf[:], op0=ALU.mult, op1=ALU.mult),
                 reads=["x" + tag, "ss" + tag, "gf", "acc" + tag], writes=["acc" + tag])
            P.dma("sp", lambda e, i=i, t=t: e.dma_start(out=A["out"][t * 128:(t + 1) * 128, :], in_=acc[i][:]), reads=["acc" + tag], writes=["dram_out"])
        P.run()


def _t5_bucket_np(rel):
    n = np.maximum(rel, 0)
    nf = np.maximum(n, 1).astype(np.float32)
    large = 16 + (np.log(nf / 16) / np.float32(np.log(128 / 16)) * 16).astype(np.int32)
    large = np.minimum(large, 31)
    return np.where(n < 16, n, large)


def _bucket_table():
    n = np.arange(0, 700, dtype=np.int64)
    nf = np.maximum(n, 1).astype(np.float32)
    large = 16 + (np.log(nf / np.float32(16)) / np.float32(np.log(8.0)) * np.float32(16)).astype(np.int32)
    large = np.minimum(large, 31)
    return np.where(n < 16, n, large)


def make_core_inputs(inp, c, shared):
    b, r = c // 4, c % 4
    f32 = np.float32
    x = inp["x"]
    own_tiles = [4 * m + r for m in range(NTO)]
    xb = np.ascontiguousarray(x[b])
    m_ = dict(shared)
    m_["x_all"] = xb
    m_["x_own"] = np.ascontiguousarray(xb.reshape(NTA, 128, D)[own_tiles].reshape(TO, D))
    m_["cT"] = np.ascontiguousarray(inp["c"][b].reshape(KC, 128).T)
    ext = shared["_ext_table"]
    bidx = np.zeros((128, 5, 128), np.int64)
    kk = np.arange(128)[:, None]
    qq = np.arange(128)[None, :]
    for jj in range(5):
        delta = r - (jj - 1)
        rel = delta * 128 + qq - kk
        bi = shared["_bucket"][np.clip(rel, 0, 699)]
        bidx[:, jj, :] = np.where(rel >= 0, bi, 32)
    BT = ext[bidx]
    m_["BT"] = np.ascontiguousarray(BT.transpose(0, 3, 1, 2).reshape(128, 16 * 5 * 128)).astype(ml_dtypes.bfloat16)
    cur = np.array([(4 * m + r) // 2 for m in range(NTO)])
    n = np.arange(32)[None, :]
    past = (n < cur[:, None])
    ownm = (n == cur[:, None])
    const_tbl = np.array([0.0, 1.0, -BIG], f32)
    m_["past"] = np.ascontiguousarray(np.broadcast_to(const_tbl[past.astype(np.int64)].reshape(1, NTO * 32), (128, NTO * 32)))
    m_["own"] = np.ascontiguousarray(np.broadcast_to(const_tbl[ownm.astype(np.int64)].reshape(1, NTO * 32), (128, NTO * 32)))
    m_["pen"] = np.ascontiguousarray(np.broadcast_to(const_tbl[np.where(past, 0, 2)].reshape(1, NTO * 32), (128, NTO * 32)))
    for k in list(m_.keys()):
        if k.startswith("_"):
            del m_[k]
    return m_


def make_shared(inp):
    f32 = np.float32
    bc = lambda v, n: np.ascontiguousarray(np.broadcast_to(np.asarray(v, f32).reshape(1, n), (128, n)))
    sh = {}
    sh["w_ada"] = np.ascontiguousarray(inp["w_ada"][0])
    sh["b_ada_b"] = bc(inp["b_ada"][0], 6 * D)
    sh["g_mix_b"] = bc(inp["g_mix"][0], D)
    sh["g_ffn_b"] = bc(inp["g_ffn"][0], D)
    sh["g_final_b"] = bc(inp["g_final"], D)
    sh["w_in"] = np.ascontiguousarray(inp["w_in"][0])
    sh["lam_b"] = bc(inp["diff_lambda"][0].reshape(-1), 256)
    sh["subln_b"] = bc(inp["diff_subln_g"][0], 128)
    rel_bias = np.asarray(inp["rel_bias"], f32)
    sh["b31"] = bc(rel_bias[31], 16)
    sh["_ext_table"] = np.concatenate([rel_bias, np.full((1, 16), -BIG, f32)], axis=0)
    sh["_bucket"] = _bucket_table()
    sh["w_o_diff"] = np.ascontiguousarray(inp["w_o_diff"][0])
    sh["w_o_moba"] = np.ascontiguousarray(inp["w_o_moba"][0])
    sh["w_out"] = np.ascontiguousarray(inp["w_out"][0])
    sh["w_router"] = np.ascontiguousarray(inp["w_router"][0])
    sh["rbias_b"] = bc(inp["router_bias"][0], E)
    sh["w_exp_gate"] = np.ascontiguousarray(inp["w_exp_gate"][0])
    sh["w_exp_up"] = np.ascontiguousarray(inp["w_exp_up"][0])
    sh["w_exp_down"] = np.ascontiguousarray(inp["w_exp_down"][0])
    sh["w_sh_gate"] = np.ascontiguousarray(inp["w_sh_gate"][0])
    sh["w_sh_up"] = np.ascontiguousarray(inp["w_sh_up"][0])
    sh["w_sh_down"] = np.ascontiguousarray(inp["w_sh_down"][0])
    sh["ident_bf"] = np.eye(128, dtype=f32).astype(ml_dtypes.bfloat16)
    sh["ident_f"] = np.eye(128, dtype=f32)
    tp = np.arange(128)
    sh["ltri"] = (tp[:, None] < tp[None, :]).astype(f32).astype(ml_dtypes.bfloat16)
    sel = np.zeros((32, 32, 128), f32)
    for n in range(32):
        sel[n, n, :] = 1.0
    sh["selc"] = sel.reshape(32, 32 * 128).astype(ml_dtypes.bfloat16)
    sh["ecap"] = bc(np.arange(E, dtype=f32) * CAP, E)
    sh["dumpidx"] = (NSLOT + np.arange(128, dtype=f32)).reshape(128, 1)
    sh["tokid"] = (np.arange(NTO, dtype=np.int32)[None, :] * 128 + np.arange(128, dtype=np.int32)[:, None]).astype(np.int32)
    return sh


_NC_CACHE = {}


def kernel(**inputs):
    inp = {k: np.asarray(v) for k, v in inputs.items()}
    stop_after = int(os.environ.get("MK_STOP", "99"))
    key = (stop_after, DEBUG)
    if key not in _NC_CACHE:
        _NC_CACHE[key] = build_program(stop_after)
    nc = _NC_CACHE[key]
    shared = make_shared(inp)
    in_maps = [make_core_inputs(inp, c, shared) for c in range(NCORES)]
    used = set(USED_INPUTS)
    in_maps = [{k: v for k, v in m.items() if k in used} for m in in_maps]
    res = run_bass_kernel_spmd(nc, in_maps, core_ids=list(range(NCORES)))
    out = np.zeros((2, S, D), np.float32)
    for c in range(NCORES):
        b, r = c // 4, c % 4
        if "out" not in res.results[c]:
            continue
        o = np.asarray(res.results[c]["out"]).reshape(NTO, 128, D)
        ov = out[b].reshape(NTA, 128, D)
        for m in range(NTO):
            ov[4 * m + r] = o[m]
    if DEBUG:
        kernel.last_results = res.results
    return out
```

```python
import os
import contextlib
import numpy as np
import ml_dtypes
import concourse.bass as bass
import concourse.mybir as mybir
from concourse.bass_utils import run_bass_kernel_spmd

F32 = mybir.dt.float32
BF16 = mybir.dt.bfloat16
I32 = mybir.dt.int32
AF = mybir.ActivationFunctionType
ALU = mybir.AluOpType
AX = mybir.AxisListType

D = 2048
KC = 16
S = 8192
NTA = 64
NTO = 16
TO = 2048
E = 64
CAP = 896
JMAX = CAP // 128
NSLOT = E * CAP
BIG = 30000.0
EPS = 1e-6
NCORES = int(os.environ.get("MK_CORES", "8"))
DEBUG = os.environ.get("MK_DEBUG", "")

COMPUTE = ("pe", "act", "dve", "pool")
NDSEM = 6
_SYNC = [None]
USED_INPUTS = set()


class Sync:
    def __init__(self, nc, st):
        self.nc = nc
        self.cnt = {e: 0 for e in COMPUTE}
        self.dcnt = {}
        self.seen = {e: {} for e in ("pe", "act", "dve", "pool", "sp")}
        self.all_tokens = {}
        self.sems = {}
        keys = list(COMPUTE) + ["d_%s_%d" % (q, j) for q in ("sp", "pool", "poolw", "act") for j in range(NDSEM)]
        for k in keys:
            self.sems[k] = st.enter_context(nc.semaphore("s_" + k))


class Phase:
    def __init__(self, nc, name):
        self.nc = nc
        self.name = name
        self.sy = _SYNC[0]
        self.streams = {e: [] for e in ("pe", "act", "dve", "pool", "sp")}
        self.last_w = {}
        self.readers = {}
        self.region = None
        self.cnt_ap = None

    def begin_region(self, expert, thresh):
        self.region = {"expert": expert, "thresh": thresh, "before": dict(self.sy.all_tokens)}

    def end_region(self):
        self.region = None

    def load_cond_reg(self, expert, res):
        t = self.last_w.get(res)
        for eng in self.streams:
            w = self._waits(eng, [t] if t is not None else [])
            self.streams[eng].append((w, ("reg", expert), None, None))

    def _deps(self, reads, writes):
        deps = []
        for r in reads:
            t = self.last_w.get(r)
            if t is not None:
                deps.append(t)
        for w in writes:
            t = self.last_w.get(w)
            if t is not None:
                deps.append(t)
            deps.extend(self.readers.get(w, ()))
        return deps

    def _waits(self, eng, deps):
        out = {}
        seen = self.sy.seen[eng]
        for (k, v) in deps:
            if eng == "pe" and k == "pe":
                continue
            if seen.get(k, 0) >= v:
                continue
            if out.get(k, 0) < v:
                out[k] = v
        for k, v in out.items():
            seen[k] = v
        return list(out.items())

    def _commit(self, tok, reads, writes):
        for r in reads:
            self.readers.setdefault(r, []).append(tok)
        for w in writes:
            self.last_w[w] = tok
            self.readers[w] = []
        k, v = tok
        if self.sy.all_tokens.get(k, 0) < v:
            self.sy.all_tokens[k] = v

    def op(self, eng, fn, reads=(), writes=()):
        deps = self._deps(reads, writes)
        waits = self._waits(eng, deps)
        self.sy.cnt[eng] += 1
        tok = (eng, self.sy.cnt[eng])
        self.streams[eng].append((waits, fn, tok, self.region))
        self._commit(tok, reads, writes)
        return tok

    def dma(self, q, fn, reads=(), writes=(), cls=""):
        deps = self._deps(reads, writes)
        i = self.sy.dcnt.get(q + cls, 0)
        self.sy.dcnt[q + cls] = i + 1
        semkey = "d_%s%s_%d" % (q, cls, i % NDSEM)
        tok = (semkey, 16 * (i // NDSEM + 1))
        if i >= NDSEM:
            deps.append((semkey, 16 * (i // NDSEM)))
        waits = self._waits(q, deps)
        self.streams[q].append((waits, fn, tok, self.region))
        self._commit(tok, reads, writes)
        return tok

    def run(self):
        nc = self.nc
        sems = self.sy.sems
        toks = list(self.sy.all_tokens.items())
        for eng in self.streams:
            w = self._waits(eng, toks)
            if w:
                self.streams[eng].append((w, None, None, None))
        cnt_ap = self.cnt_ap
        with nc.Block() as block:

            def emit(engine, entry):
                waits, fn, tok, _ = entry
                for (k, v) in waits:
                    engine.wait_ge(sems[k], v)
                if fn is None:
                    return
                ins = fn(engine)
                k, v = tok
                ins.then_inc(sems[k], 1 if k in COMPUTE else 16)

            def replay(engine, stream):
                reg = None
                i = 0
                n = len(stream)
                while i < n:
                    entry = stream[i]
                    waits, fn, tok, region = entry
                    if isinstance(fn, tuple):
                        for (k, v) in waits:
                            engine.wait_ge(sems[k], v)
                        if reg is None:
                            reg = engine.alloc_register("cnt_reg")
                        engine.reg_load(reg, cnt_ap[0:1, fn[1]:fn[1] + 1])
                        i += 1
                        continue
                    if region is None:
                        emit(engine, entry)
                        i += 1
                        continue
                    groups = []
                    j = i
                    while j < n and stream[j][3] is not None and stream[j][3]["expert"] == region["expert"]:
                        r_ = stream[j][3]
                        j2 = j
                        while j2 < n and stream[j2][3] is r_:
                            j2 += 1
                        groups.append((r_, stream[j:j2]))
                        j = j2

                    merged = []
                    for (r_, grp) in groups:
                        if merged and merged[-1][0]["thresh"] == r_["thresh"]:
                            merged[-1] = (merged[-1][0], merged[-1][1] + grp)
                        else:
                            merged.append((r_, list(grp)))
                    groups = merged

                    def compensate(rest):
                        before = rest[0][0]["before"]
                        ext = {}
                        incs = {}
                        for (_, grp) in rest:
                            for (w_, f_, t_, _r) in grp:
                                for (k, v) in w_:
                                    v2 = min(v, before.get(k, 0))
                                    if v2 > 0 and ext.get(k, 0) < v2:
                                        ext[k] = v2
                                if t_ is not None:
                                    k, v = t_
                                    incs[k] = incs.get(k, 0) + (1 if k in COMPUTE else 16)
                        for k in incs:
                            b = before.get(k, 0)
                            if b > 0 and ext.get(k, 0) < b:
                                ext[k] = b
                        for k, v in ext.items():
                            engine.wait_ge(sems[k], v)
                        for k, v in incs.items():
                            engine.sem_inc(sems[k], v)

                    def chain(gi_):
                        if gi_ == len(groups):
                            return
                        r_, grp = groups[gi_]
                        with engine.If_lt(reg, r_["thresh"] + 1):
                            compensate(groups[gi_:])
                        with engine.Else():
                            for e_ in grp:
                                emit(engine, e_)
                            chain(gi_ + 1)
                    chain(0)
                    i = j

            @block.tensor
            def _(e):
                replay(e, self.streams["pe"])

            @block.scalar
            def _(e):
                replay(e, self.streams["act"])

            @block.vector
            def _(e):
                replay(e, self.streams["dve"])

            @block.gpsimd
            def _(e):
                replay(e, self.streams["pool"])

            @block.sync
            def _(e):
                replay(e, self.streams["sp"])


def sb(st, nc, name, shape, dt):
    return st.enter_context(nc.sbuf_tensor(name, shape, dt))


def ps(st, nc, name, shape, dt):
    return st.enter_context(nc.psum_tensor(name, shape, dt))


def build_program(stop_after=99):
    nc = bass.Bass("TRN2", target_bir_lowering=False)
    USED_INPUTS.clear()

    def din(name, shape, dt=F32):
        A[name] = nc.dram_tensor(name, list(shape), dt, kind="ExternalInput").ap()

    def dscr(name, shape, dt):
        kind = "ExternalOutput" if name in DEBUG.split(",") else "Internal"
        A[name] = nc.dram_tensor(name, list(shape), dt, kind=kind).ap()

    in_specs = {
        "x_all": ([S, D], F32),
        "x_own": ([TO, D], F32),
        "cT": ([128, KC], F32),
        "w_ada": ([D, 6 * D], F32),
        "b_ada_b": ([128, 6 * D], F32),
        "g_mix_b": ([128, D], F32),
        "g_ffn_b": ([128, D], F32),
        "g_final_b": ([128, D], F32),
        "w_in": ([D, 10240], F32),
        "lam_b": ([128, 256], F32),
        "subln_b": ([128, 128], F32),
        "BT": ([128, 16 * 5 * 128], BF16),
        "b31": ([128, 16], F32),
        "pen": ([128, NTO * 32], F32),
        "past": ([128, NTO * 32], F32),
        "own": ([128, NTO * 32], F32),
        "w_o_diff": ([1024, D], F32),
        "w_o_moba": ([1024, D], F32),
        "w_out": ([D, D], F32),
        "w_router": ([D, E], F32),
        "rbias_b": ([128, E], F32),
        "w_exp_gate": ([E, D, 512], F32),
        "w_exp_up": ([E, D, 512], F32),
        "w_exp_down": ([E, 512, D], F32),
        "w_sh_gate": ([D, 512], F32),
        "w_sh_up": ([D, 512], F32),
        "w_sh_down": ([512, D], F32),
        "ident_bf": ([128, 128], BF16),
        "ident_f": ([128, 128], F32),
        "ltri": ([128, 128], BF16),
        "selc": ([32, 32 * 128], BF16),
        "ecap": ([128, E], F32),
        "dumpidx": ([128, 1], F32),
        "tokid": ([128, NTO], I32),
    }

    class LazyA(dict):
        def __missing__(self, name):
            shape, dt = in_specs[name]
            ap = nc.dram_tensor(name, list(shape), dt, kind="ExternalInput").ap()
            self[name] = ap
            USED_INPUTS.add(name)
            return ap
    A = LazyA()
    A["out"] = nc.dram_tensor("out", [TO, D], F32, kind="ExternalOutput").ap()

    dscr("modb", [128, 6 * D], F32)
    dscr("hT_all", [NTA, 128, KC * 128], BF16)
    dscr("hT_own", [NTO, 128, KC * 128], BF16)
    dscr("QTd", [8, 128, TO], BF16); dscr("KTd", [8, 128, S], BF16); dscr("Vd", [S, 1024], BF16)
    dscr("QTm", [8, 128, TO], BF16); dscr("KTm", [8, 128, S], BF16); dscr("Vm", [S, 1024], BF16)
    dscr("KMT", [8, 128, 32], F32)
    dscr("GT", [32, 128, TO], BF16)
    dscr("o_d", [TO, D], BF16)
    dscr("zT", [4, 128, KC * 512], BF16)
    dscr("x1", [TO, D], F32)
    dscr("h2", [TO, D], BF16)
    dscr("h2T", [NTO, 128, KC * 128], BF16)
    dscr("rowtok", [NSLOT + 128, 8], I32)
    dscr("Ybuf", [NSLOT + 128, D], BF16)
    dscr("Ysh", [TO, D], BF16)
    dscr("route", [128, NTO * 16], F32)
    dscr("cnt_d", [1, E], I32)

    gst = contextlib.ExitStack()
    gst.__enter__()
    _SYNC[0] = Sync(nc, gst)
    if stop_after >= 1:
        phase_mod(nc, A)
    if stop_after >= 2:
        phase_h(nc, A)
    if stop_after >= 3:
        phase_proj(nc, A)
    if stop_after >= 4:
        phase_attn(nc, A)
    if stop_after >= 5:
        phase_oproj(nc, A)
    if stop_after >= 6:
        phase_wout(nc, A)
    if stop_after >= 7:
        phase_route(nc, A)
    if stop_after >= 8:
        phase_experts(nc, A)
    if stop_after >= 9:
        phase_final(nc, A)
    gst.__exit__(None, None, None)
    return nc


def phase_mod(nc, A):
    with contextlib.ExitStack() as st:
        P = Phase(nc, "p1")
        cT = sb(st, nc, "p1_cT", [128, KC], F32)
        cact = sb(st, nc, "p1_cact", [128, KC], F32)
        ones = sb(st, nc, "p1_ones", [128, 128], F32)
        L = sb(st, nc, "p1_L", [128, KC, 128], BF16)
        wt = [sb(st, nc, "p1_w%d" % i, [128, KC, 512], BF16) for i in range(2)]
        bt = [sb(st, nc, "p1_b%d" % i, [128, 512], F32) for i in range(2)]
        gt = [sb(st, nc, "p1_g%d" % i, [128, 512], F32) for i in range(2)]
        ot = [sb(st, nc, "p1_o%d" % i, [128, 512], F32) for i in range(2)]
        pp = [ps(st, nc, "p1_ps%d" % i, [128, 512], F32) for i in range(2)]
        P.dma("sp", lambda e: e.dma_start(out=cT[:], in_=A["cT"][:, :]), writes=["cT"])
        P.op("dve", lambda e: e.memset(ones[:], 1.0), writes=["ones"])
        P.op("act", lambda e: e.activation(out=cact[:], in_=cT[:], func=AF.Silu), reads=["cT"], writes=["cact"])
        for j in range(KC):
            P.op("dve", lambda e, j=j: e.tensor_scalar(out=L[:, j, :], in0=ones[:], scalar1=cact[:, j:j + 1], scalar2=None, op0=ALU.mult),
                 reads=["cact", "ones"], writes=["L"])
        w_view = A["w_ada"].rearrange("(k p) n -> p k n", p=128)
        for n in range(24):
            i = n % 2
            P.dma("pool", lambda e, n=n, i=i: e.dma_start(out=wt[i][:], in_=w_view[:, :, n * 512:(n + 1) * 512]), writes=["w%d" % i])
            P.dma("sp", lambda e, n=n, i=i: e.dma_start(out=bt[i][:], in_=A["b_ada_b"][:, n * 512:(n + 1) * 512]), writes=["b%d" % i])
            kind = n // 4
            if kind in (1, 4):
                gsrc = A["g_mix_b"] if kind == 1 else A["g_ffn_b"]
                c0 = (n % 4) * 512
                P.dma("sp", lambda e, i=i, gsrc=gsrc, c0=c0: e.dma_start(out=gt[i][:], in_=gsrc[:, c0:c0 + 512]), writes=["g%d" % i])

            def mm(e, i=i):
                ins = None
                for k in range(KC):
                    ins = e.matmul(pp[i][:], lhsT=L[:, k, :], rhs=wt[i][:, k, :], start=(k == 0), stop=(k == KC - 1))
                return ins
            P.op("pe", mm, reads=["L", "w%d" % i], writes=["ps%d" % i])
            if kind in (1, 4):
                P.op("dve", lambda e, i=i: e.tensor_tensor(out=ot[i][:], in0=pp[i][:], in1=bt[i][:], op=ALU.add),
                     reads=["ps%d" % i, "b%d" % i], writes=["o%d" % i])
                P.op("dve", lambda e, i=i: e.scalar_tensor_tensor(out=ot[i][:], in0=ot[i][:], scalar=1.0, in1=gt[i][:], op0=ALU.add, op1=ALU.mult),
                     reads=["o%d" % i, "g%d" % i], writes=["o%d" % i])
            else:
                P.op("dve", lambda e, i=i: e.tensor_tensor(out=ot[i][:], in0=pp[i][:], in1=bt[i][:], op=ALU.add),
                     reads=["ps%d" % i, "b%d" % i], writes=["o%d" % i])
            P.dma("sp", lambda e, n=n, i=i: e.dma_start(out=A["modb"][:, n * 512:(n + 1) * 512], in_=ot[i][:]), reads=["o%d" % i], writes=["modb"])
        P.run()


MOD_SHIFT1, MOD_A1, MOD_G1, MOD_SHIFT2, MOD_A2, MOD_G2 = [i * D for i in range(6)]


def emit_norm_mod_T(P, nc, xt, sq, ss, rstd, hf, hb, pT, hT, Amod, Smod, ident, tag, xres, extra_reads=()):
    P.op("act", lambda e: e.activation(out=sq[:], in_=xt[:], func=AF.Square, accum_out=ss[:]),
         reads=[xres], writes=["sq" + tag, "ss" + tag])
    P.op("dve", lambda e: e.tensor_scalar(out=rstd[:], in0=ss[:], scalar1=1.0 / D, scalar2=EPS, op0=ALU.mult, op1=ALU.add),
         reads=["ss" + tag], writes=["rstd" + tag])
    P.op("act", lambda e: e.activation(out=rstd[:], in_=rstd[:], func=AF.Sqrt), reads=["rstd" + tag], writes=["rstd" + tag])
    P.op("dve", lambda e: e.reciprocal(out=rstd[:], in_=rstd[:]), reads=["rstd" + tag], writes=["rstd" + tag])
    P.op("dve", lambda e: e.scalar_tensor_tensor(out=hf[:], in0=xt[:], scalar=rstd[:, 0:1], in1=Amod[:], op0=ALU.mult, op1=ALU.mult),
         reads=[xres, "rstd" + tag, "Amod"] + list(extra_reads), writes=["hf" + tag])
    P.op("pool", lambda e: e.tensor_tensor(out=hb[:], in0=hf[:], in1=Smod[:], op=ALU.add),
         reads=["hf" + tag, "Smod"], writes=["hb" + tag])

    def tr(e):
        ins = None
        for k in range(KC):
            ins = e.transpose(out=pT[:, k, :], in_=hb[:, k * 128:(k + 1) * 128], identity=ident[:])
        return ins
    P.op("pe", tr, reads=["hb" + tag, "ident"], writes=["pT" + tag])
    P.op("act", lambda e: e.activation(out=hT[:], in_=pT[:], func=AF.Copy), reads=["pT" + tag], writes=["hT" + tag])


def phase_h(nc, A):
    with contextlib.ExitStack() as st:
        P = Phase(nc, "p2")
        ident = sb(st, nc, "p2_ident", [128, 128], BF16)
        Amod = sb(st, nc, "p2_A", [128, D], F32)
        Smod = sb(st, nc, "p2_S", [128, D], F32)
        xt = [sb(st, nc, "p2_x%d" % i, [128, D], F32) for i in range(3)]
        sq = [sb(st, nc, "p2_sq%d" % i, [128, D], BF16) for i in range(3)]
        ss = [sb(st, nc, "p2_ss%d" % i, [128, 1], F32) for i in range(3)]
        rstd = [sb(st, nc, "p2_rs%d" % i, [128, 1], F32) for i in range(3)]
        hf = [sb(st, nc, "p2_hf%d" % i, [128, D], F32) for i in range(3)]
        hb = [sb(st, nc, "p2_hb%d" % i, [128, D], BF16) for i in range(3)]
        hT = [sb(st, nc, "p2_hT%d" % i, [128, KC, 128], BF16) for i in range(3)]
        pT = [ps(st, nc, "p2_pT%d" % i, [128, KC, 128], BF16) for i in range(3)]
        P.dma("sp", lambda e: e.dma_start(out=ident[:], in_=A["ident_bf"][:, :]), writes=["ident"])
        P.dma("sp", lambda e: e.dma_start(out=Amod[:], in_=A["modb"][:, MOD_A1:MOD_A1 + D]), writes=["Amod"])
        P.dma("sp", lambda e: e.dma_start(out=Smod[:], in_=A["modb"][:, MOD_SHIFT1:MOD_SHIFT1 + D]), writes=["Smod"])
        for t in range(NTA + NTO):
            i = t % 3
            tag = str(i)
            if t < NTA:
                src = A["x_all"][t * 128:(t + 1) * 128, :]
                dst = A["hT_all"][t]
            else:
                src = A["x_own"][(t - NTA) * 128:(t - NTA + 1) * 128, :]
                dst = A["hT_own"][t - NTA]
            P.dma("sp", lambda e, i=i, src=src: e.dma_start(out=xt[i][:], in_=src), writes=["x" + tag])
            emit_norm_mod_T(P, nc, xt[i], sq[i], ss[i], rstd[i], hf[i], hb[i], pT[i], hT[i], Amod, Smod, ident, tag, "x" + tag)
            P.dma("pool", lambda e, i=i, dst=dst: e.dma_start(out=dst, in_=hT[i][:].rearrange("p k t -> p (k t)")),
                  reads=["hT" + tag], writes=["hTd"])
        P.run()


def phase_proj(nc, A):
    with contextlib.ExitStack() as st:
        P = Phase(nc, "p3")
        W = [sb(st, nc, "p3_W%d" % i, [128, KC, 1024], BF16) for i in range(2)]
        H = [sb(st, nc, "p3_H%d" % i, [128, 4, KC * 128], BF16) for i in range(2)]
        O = [sb(st, nc, "p3_O%d" % i, [128, 8, 512], BF16) for i in range(2)]
        KM = sb(st, nc, "p3_KM", [128, 8, 32], F32)
        pp = [ps(st, nc, "p3_ps%d" % i, [128, 512], F32) for i in range(6)]
        w_view = A["w_in"].rearrange("(k p) n -> p k n", p=128)
        passes = [
            ("qd", 0, "own", "fm"), ("kd", 1024, "all", "fm"), ("vd", 2048, "all", "tm"),
            ("qm", 3072, "own", "fm"), ("km", 4096, "all", "fm"), ("vm", 5120, "all", "tm"),
            ("g0", 6144, "own", "fm"), ("g1", 7168, "own", "fm"), ("g2", 8192, "own", "fm"), ("g3", 9216, "own", "fm"),
        ]
        gcount = 0
        pcount = 0
        if os.environ.get("MK_P3"):
            passes = [passes[int(i)] for i in os.environ["MK_P3"].split(",")]
        for pi, (pname, c0, tset, kind) in enumerate(passes):
            wi = pi % 2
            wres = "W%d" % wi

            def loadW(pj):
                wj = pj % 2
                cj = passes[pj][1]
                for q4 in range(4):
                    P.dma("pool", lambda e, wj=wj, cj=cj, q4=q4: e.dma_start(out=W[wj][:, q4 * 4:(q4 + 1) * 4, :], in_=w_view[:, q4 * 4:(q4 + 1) * 4, cj:cj + 1024]),
                          writes=["W%d" % wj], cls="w")
            if pi == 0:
                loadW(0)
            if pi + 1 < len(passes):
                loadW(pi + 1)
            ngroups = 16 if tset == "all" else 4
            src = A["hT_all"] if tset == "all" else A["hT_own"]
            for g in range(ngroups):
                hi = gcount % 2
                gcount += 1
                hres = "H%d" % hi
                P.dma("sp", lambda e, hi=hi, src=src, g=g: e.dma_start(out=H[hi][:], in_=src[g * 4:(g + 1) * 4].rearrange("t p f -> p t f")),
                      writes=[hres])
                oi = g % 2
                ores = "O%d" % oi
                if kind == "fm":
                    for ch in range(8):
                        pidx = pcount % 6
                        pcount += 1
                        pres = "ps%d" % pidx

                        def mm(e, wi=wi, hi=hi, ch=ch, pidx=pidx):
                            ins = None
                            for k in range(KC):
                                ins = e.matmul(pp[pidx][:].rearrange("p (t q) -> p t q", t=4), lhsT=W[wi][:, k, ch * 128:(ch + 1) * 128],
                                               rhs=H[hi][:, :, k * 128:(k + 1) * 128], start=(k == 0), stop=(k == KC - 1))
                            return ins
                        P.op("pe", mm, reads=[wres, hres], writes=[pres])
                        if pname in ("qd", "qm"):
                            sc = 0.125 if pname == "qd" else float(128 ** -0.5)
                            P.op("act", lambda e, oi=oi, ch=ch, pidx=pidx, sc=sc: e.activation(out=O[oi][:, ch, :], in_=pp[pidx][:], func=AF.Copy, scale=sc),
                                 reads=[pres], writes=[ores])
                        elif pname.startswith("g"):
                            P.op("act", lambda e, oi=oi, ch=ch, pidx=pidx: e.activation(out=O[oi][:, ch, :], in_=pp[pidx][:], func=AF.Sigmoid),
                                 reads=[pres], writes=[ores])
                        elif pname == "km":
                            for bb in range(2):
                                P.op("act", lambda e, oi=oi, ch=ch, pidx=pidx, g=g, bb=bb: e.activation(out=O[oi][:, ch, bb * 256:(bb + 1) * 256], in_=pp[pidx][:, bb * 256:(bb + 1) * 256],
                                                                                                    func=AF.Copy, accum_out=KM[:, ch, 2 * g + bb:2 * g + bb + 1]),
                                     reads=[pres], writes=[ores, "KM"])
                        else:
                            eng = "dve" if ch % 2 == 0 else "act"
                            if eng == "dve":
                                P.op("dve", lambda e, oi=oi, ch=ch, pidx=pidx: e.tensor_copy(out=O[oi][:, ch, :], in_=pp[pidx][:]), reads=[pres], writes=[ores])
                            else:
                                P.op("act", lambda e, oi=oi, ch=ch, pidx=pidx: e.activation(out=O[oi][:, ch, :], in_=pp[pidx][:], func=AF.Copy), reads=[pres], writes=[ores])
                    if pname == "qd":
                        dst = A["QTd"][:, :, g * 512:(g + 1) * 512]
                    elif pname == "kd":
                        dst = A["KTd"][:, :, g * 512:(g + 1) * 512]
                    elif pname == "qm":
                        dst = A["QTm"][:, :, g * 512:(g + 1) * 512]
                    elif pname == "km":
                        dst = A["KTm"][:, :, g * 512:(g + 1) * 512]
                    else:
                        gi = int(pname[1])
                        dst = A["GT"][gi * 8:(gi + 1) * 8, :, g * 512:(g + 1) * 512]
                    P.dma("pool", lambda e, oi=oi, dst=dst: e.dma_start(out=dst.rearrange("c p t -> p c t"), in_=O[oi][:]), reads=[ores], writes=["dram_" + pname])
                else:
                    Ov = O[oi][:].rearrange("p c t -> p (c t)").rearrange("p (t n) -> p t n", t=4)
                    for tt in range(4):
                        for half in range(2):
                            pidx = pcount % 6
                            pcount += 1
                            pres = "ps%d" % pidx

                            def mm(e, wi=wi, hi=hi, tt=tt, half=half, pidx=pidx):
                                ins = None
                                for k in range(KC):
                                    ins = e.matmul(pp[pidx][:], lhsT=H[hi][:, tt, k * 128:(k + 1) * 128], rhs=W[wi][:, k, half * 512:(half + 1) * 512],
                                                   start=(k == 0), stop=(k == KC - 1))
                                return ins
                            P.op("pe", mm, reads=[wres, hres], writes=[pres])
                            if (tt * 2 + half) % 2 == 0:
                                P.op("dve", lambda e, Ov=Ov, tt=tt, half=half, pidx=pidx: e.tensor_copy(out=Ov[:, tt, half * 512:(half + 1) * 512], in_=pp[pidx][:]),
                                     reads=[pres], writes=[ores])
                            else:
                                P.op("act", lambda e, Ov=Ov, tt=tt, half=half, pidx=pidx: e.activation(out=Ov[:, tt, half * 512:(half + 1) * 512], in_=pp[pidx][:], func=AF.Copy),
                                     reads=[pres], writes=[ores])
                    dstT = A["Vd"] if pname == "vd" else A["Vm"]
                    dst = dstT[g * 512:(g + 1) * 512, :].rearrange("(t p) n -> p t n", p=128)
                    P.dma("pool", lambda e, Ov=Ov, dst=dst: e.dma_start(out=dst, in_=Ov), reads=[ores], writes=["dram_" + pname])
            if pname == "km":
                P.op("dve", lambda e: e.tensor_scalar(out=KM[:], in0=KM[:], scalar1=1.0 / 256.0, scalar2=None, op0=ALU.mult), reads=["KM"], writes=["KM"])
                P.dma("sp", lambda e: e.dma_start(out=A["KMT"].rearrange("h p n -> p h n"), in_=KM[:]), reads=["KM"], writes=["dram_KMT"])
        P.run()


def phase_attn(nc, A):
    with contextlib.ExitStack() as st:
        P = Phase(nc, "p4")
        ident = sb(st, nc, "p4_ident", [128, 128], BF16)
        identf = sb(st, nc, "p4_identf", [128, 128], F32)
        BT = sb(st, nc, "p4_BT", [128, 16, 5, 128], BF16)
        b31 = sb(st, nc, "p4_b31", [128, 16], F32)
        selc = sb(st, nc, "p4_sel", [32, 32, 128], BF16)
        pen = sb(st, nc, "p4_pen", [128, NTO, 32], F32)
        past = sb(st, nc, "p4_past", [128, NTO, 32], F32)
        own = sb(st, nc, "p4_own", [128, NTO, 32], F32)
        lamb = sb(st, nc, "p4_lamb", [128, 256], F32)
        lamt = sb(st, nc, "p4_lamt", [128, 128], F32)
        lam2 = sb(st, nc, "p4_lam2", [128, 2], F32)
        nlam = sb(st, nc, "p4_nlam", [128, 1], F32)
        subg = sb(st, nc, "p4_subg", [128, 128], F32)
        KTb = [sb(st, nc, "p4_KT%d" % i, [128, 2 * S], BF16) for i in range(2)]
        QTb = [sb(st, nc, "p4_QT%d" % i, [128, 2 * TO], BF16) for i in range(2)]
        Vb = [sb(st, nc, "p4_V%d" % i, [128, NTA, 130], BF16) for i in range(2)]
        KMb = [sb(st, nc, "p4_KM%d" % i, [128, 32], F32) for i in range(2)]
        QTf = [sb(st, nc, "p4_QTf%d" % i, [128, 128], F32) for i in range(2)]
        Pb = [sb(st, nc, "p4_P%d" % i, [128, 512], BF16) for i in range(6)]
        oh = [sb(st, nc, "p4_oh%d" % i, [128, NTO, 128], BF16) for i in range(2)]
        gate = [sb(st, nc, "p4_gate%d" % i, [128, 32], F32) for i in range(2)]
        top8 = [sb(st, nc, "p4_top8%d" % i, [128, 8], F32) for i in range(2)]
        mb = [sb(st, nc, "p4_mb%d" % i, [128, 32], F32) for i in range(2)]
        mbT = [sb(st, nc, "p4_mbT%d" % i, [32, 128], BF16) for i in range(2)]
        rl = [sb(st, nc, "p4_rl%d" % i, [128, 2], F32) for i in range(2)]
        o1 = [sb(st, nc, "p4_o1%d" % i, [128, 128], F32) for i in range(2)]
        o2 = [sb(st, nc, "p4_o2%d" % i, [128, 128], F32) for i in range(2)]
        junk = [sb(st, nc, "p4_junk%d" % i, [128, 128], F32) for i in range(2)]
        ssq = [sb(st, nc, "p4_ssq%d" % i, [128, 1], F32) for i in range(2)]
        pS = [ps(st, nc, "p4_pS%d" % i, [128, 512], F32) for i in range(4)]
        pO = [ps(st, nc, "p4_pO%d" % i, [128, 512], F32) for i in range(2)]
        pOb = [ps(st, nc, "p4_pOb%d" % i, [128, 512], F32) for i in range(2)]
        pG = pOb[0][:, 0:32]
        pM = pOb[1][0:32, 0:128]

        P.dma("sp", lambda e: e.dma_start(out=ident[:], in_=A["ident_bf"][:, :]), writes=["ident"])
        P.dma("sp", lambda e: e.dma_start(out=identf[:], in_=A["ident_f"][:, :]), writes=["identf"])
        P.dma("sp", lambda e: e.dma_start(out=BT[:].rearrange("p h j q -> p (h j q)"), in_=A["BT"][:, :]), writes=["BT"])
        P.dma("sp", lambda e: e.dma_start(out=b31[:], in_=A["b31"][:, :]), writes=["b31"])
        P.dma("sp", lambda e: e.dma_start(out=selc[:].rearrange("p n k -> p (n k)"), in_=A["selc"][:, :]), writes=["selc"])
        P.dma("sp", lambda e: e.dma_start(out=pen[:].rearrange("p m n -> p (m n)"), in_=A["pen"][:, :]), writes=["pen"])
        P.dma("sp", lambda e: e.dma_start(out=past[:].rearrange("p m n -> p (m n)"), in_=A["past"][:, :]), writes=["past"])
        P.dma("sp", lambda e: e.dma_start(out=own[:].rearrange("p m n -> p (m n)"), in_=A["own"][:, :]), writes=["own"])
        P.dma("sp", lambda e: e.dma_start(out=lamb[:], in_=A["lam_b"][:, :]), writes=["lamb"])
        P.dma("sp", lambda e: e.dma_start(out=subg[:], in_=A["subln_b"][:, :]), writes=["subg"])
        lv = lamb[:].rearrange("p (a d) -> p a d", a=4)
        P.op("dve", lambda e: e.tensor_tensor(out=lamt[:, 0:64], in0=lv[:, 0, :], in1=lv[:, 1, :], op=ALU.mult), reads=["lamb"], writes=["lamt"])
        P.op("dve", lambda e: e.tensor_tensor(out=lamt[:, 64:128], in0=lv[:, 2, :], in1=lv[:, 3, :], op=ALU.mult), reads=["lamb", "lamt"], writes=["lamt"])
        for a_ in range(2):
            P.op("act", lambda e, a_=a_: e.activation(out=lamb[:, a_ * 64:(a_ + 1) * 64], in_=lamt[:, a_ * 64:(a_ + 1) * 64], func=AF.Copy, accum_out=lam2[:, a_:a_ + 1]),
                 reads=["lamt"], writes=["lam2", "lamb"])
        P.op("act", lambda e: e.activation(out=lam2[:], in_=lam2[:], func=AF.Exp), reads=["lam2"], writes=["lam2"])
        P.op("dve", lambda e: e.tensor_tensor(out=nlam[:], in0=lam2[:, 1:2], in1=lam2[:, 0:1], op=ALU.subtract), reads=["lam2"], writes=["nlam"])
        P.op("dve", lambda e: e.tensor_scalar(out=nlam[:], in0=nlam[:], scalar1=-0.2, scalar2=None, op0=ALU.add), reads=["nlam"], writes=["nlam"])
        P.op("dve", lambda e: e.tensor_scalar(out=subg[:], in0=subg[:], scalar1=0.8, scalar2=None, op0=ALU.mult), reads=["subg"], writes=["subg"])
        for i in range(2):
            P.op("pool", lambda e, i=i: e.memset(Vb[i][:, :, 128:130], 1.0), writes=["V%d" % i])

        scount = 0
        pcount = 0
        qcount = 0
        def load_head(h):
            hb_ = h % 2
            diff = h < 8
            hh = h if diff else h - 8
            kres, qres, vres, kmres = "KT%d" % hb_, "QT%d" % hb_, "V%d" % hb_, "KM%d" % hb_
            if diff:
                for mp in range(2):
                    P.dma("sp", lambda e, hb_=hb_, hh=hh, mp=mp: e.dma_start(out=KTb[hb_][0:64, mp * S:(mp + 1) * S], in_=A["KTd"][hh, mp * 64:(mp + 1) * 64, :]), writes=[kres])
                    P.dma("sp", lambda e, hb_=hb_, hh=hh, mp=mp: e.dma_start(out=QTb[hb_][0:64, mp * TO:(mp + 1) * TO], in_=A["QTd"][hh, mp * 64:(mp + 1) * 64, :]), writes=[qres])
                vsrc = A["Vd"]
            else:
                P.dma("sp", lambda e, hb_=hb_, hh=hh: e.dma_start(out=KTb[hb_][:, 0:S], in_=A["KTm"][hh]), writes=[kres])
                P.dma("sp", lambda e, hb_=hb_, hh=hh: e.dma_start(out=QTb[hb_][:, 0:TO], in_=A["QTm"][hh]), writes=[qres])
                P.dma("sp", lambda e, hb_=hb_, hh=hh: e.dma_start(out=KMb[hb_][:], in_=A["KMT"][hh]), writes=[kmres])
                vsrc = A["Vm"]
            P.dma("sp", lambda e, hb_=hb_, hh=hh, vsrc=vsrc: e.dma_start(out=Vb[hb_][:, :, 0:128], in_=vsrc[:, hh * 128:(hh + 1) * 128].rearrange("(t p) d -> p t d", p=128)),
                  writes=[vres])

        load_head(0)
        for h in range(16):
            hb_ = h % 2
            diff = h < 8
            hh = h if diff else h - 8
            kres, qres, vres, kmres = "KT%d" % hb_, "QT%d" % hb_, "V%d" % hb_, "KM%d" % hb_
            if h + 1 < 16:
                load_head(h + 1)
            nmaps = 2 if diff else 1
            ohres = "oh%d" % hb_

            def emit_pre(m, hb_=hb_, kmres=kmres, qres=qres, diff=diff):
                qb = m % 2
                if diff:
                    return
                gres, tres, mres, mtres = "gate%d" % qb, "top8%d" % qb, "mb%d" % qb, "mbT%d" % qb
                P.op("act", lambda e, qb=qb, hb_=hb_, m=m: e.activation(out=QTf[qb][:], in_=QTb[hb_][:, m * 128:(m + 1) * 128], func=AF.Copy), reads=[qres], writes=["QTf%d" % qb])
                P.op("pe", lambda e, qb=qb, hb_=hb_: e.matmul(pG, lhsT=QTf[qb][:], rhs=KMb[hb_][:], start=True, stop=True), reads=["QTf%d" % qb, kmres], writes=["pOb0"])
                P.op("dve", lambda e, qb=qb, m=m: e.tensor_tensor(out=gate[qb][:], in0=pG, in1=pen[:, m, :], op=ALU.add), reads=["pOb0", "pen"], writes=[gres])
                P.op("dve", lambda e, qb=qb: e.max(out=top8[qb][:], in_=gate[qb][:]), reads=[gres], writes=[tres])
                P.op("dve", lambda e, qb=qb: e.tensor_scalar(out=mb[qb][:], in0=gate[qb][:], scalar1=top8[qb][:, 2:3], scalar2=None, op0=ALU.is_ge), reads=[gres, tres], writes=[mres])
                P.op("dve", lambda e, qb=qb, m=m: e.tensor_tensor(out=mb[qb][:], in0=mb[qb][:], in1=past[:, m, :], op=ALU.mult), reads=[mres, "past"], writes=[mres])
                P.op("dve", lambda e, qb=qb, m=m: e.tensor_tensor(out=mb[qb][:], in0=mb[qb][:], in1=own[:, m, :], op=ALU.add), reads=[mres, "own"], writes=[mres])
                P.op("dve", lambda e, qb=qb: e.tensor_scalar(out=mb[qb][:], in0=mb[qb][:], scalar1=-1.0, scalar2=BIG, op0=ALU.add, op1=ALU.mult), reads=[mres], writes=[mres])
                P.op("pe", lambda e, qb=qb: e.transpose(out=pM, in_=mb[qb][:], identity=identf[:]), reads=[mres, "identf"], writes=["pOb1"])
                P.op("dve", lambda e, qb=qb: e.tensor_copy(out=mbT[qb][:], in_=pM), reads=["pOb1"], writes=[mtres])

            def emit_qk(m, g, hb_=hb_, h=h, diff=diff, nmaps=nmaps, kres=kres, qres=qres):
                nonlocal scount
                qb = m % 2
                near_last = (g == m)
                prev_grp = (g == m - 1)
                sidx = []
                for mp in range(nmaps):
                    si = scount % 4
                    scount += 1
                    sidx.append(si)
                    sres = "pS%d" % si

                    def mmqk(e, si=si, mp=mp, g=g, m=m, hb_=hb_, h=h, diff=diff, near_last=near_last, prev_grp=prev_grp, qb=qb):
                        ins = None
                        for j4 in range(4):
                            kj = 4 * g + j4
                            if diff:
                                lhs = KTb[hb_][0:64, mp * S + kj * 128: mp * S + (kj + 1) * 128]
                                rhs = QTb[hb_][0:64, mp * TO + m * 128: mp * TO + (m + 1) * 128]
                            else:
                                lhs = KTb[hb_][:, kj * 128:(kj + 1) * 128]
                                rhs = QTb[hb_][:, m * 128:(m + 1) * 128]
                            bt_j = None
                            if near_last:
                                bt_j = j4 + 1
                            elif prev_grp and j4 == 3:
                                bt_j = 0
                            last = (bt_j is None) and diff
                            out = pS[si][:, j4 * 128:(j4 + 1) * 128]
                            ins = e.matmul(out, lhsT=lhs, rhs=rhs, start=True, stop=last)
                            if not diff:
                                ins = e.matmul(out, lhsT=selc[:, kj // 2, :], rhs=mbT[qb][:], start=False, stop=(bt_j is None))
                            if bt_j is not None:
                                ins = e.matmul(out, lhsT=ident[:], rhs=BT[:, h, bt_j, :], start=False, stop=True)
                        return ins
                    rd = [kres, qres, "ident", "BT"]
                    if not diff:
                        rd += ["selc", "mbT%d" % qb]
                    P.op("pe", mmqk, reads=rd, writes=[sres])
                return sidx

            def emit_exp_pv(m, g, sidx, hb_=hb_, h=h, diff=diff, nmaps=nmaps, vres=vres):
                nonlocal pcount
                qb = m % 2
                pOres = "pO%d" % qb
                pObres = "pOb%d" % qb
                near_last = (g == m)
                prev_grp = (g == m - 1)
                pidx = []
                for mp in range(nmaps):
                    si = sidx[mp]
                    pi = pcount % 6
                    pcount += 1
                    pidx.append(pi)
                    sres, pres = "pS%d" % si, "P%d" % pi
                    if near_last:
                        P.op("act", lambda e, pi=pi, si=si: e.activation(out=Pb[pi][:], in_=pS[si][:], func=AF.Exp), reads=[sres], writes=[pres])
                    elif prev_grp:
                        P.op("act", lambda e, pi=pi, si=si, h=h: e.activation(out=Pb[pi][:, 0:384], in_=pS[si][:, 0:384], func=AF.Exp, bias=b31[:, h:h + 1]), reads=[sres, "b31"], writes=[pres])
                        P.op("act", lambda e, pi=pi, si=si: e.activation(out=Pb[pi][:, 384:512], in_=pS[si][:, 384:512], func=AF.Exp), reads=[sres], writes=[pres])
                    else:
                        P.op("act", lambda e, pi=pi, si=si, h=h: e.activation(out=Pb[pi][:], in_=pS[si][:], func=AF.Exp, bias=b31[:, h:h + 1]), reads=[sres, "b31"], writes=[pres])
                for mp in range(nmaps):
                    pi = pidx[mp]

                    def mmpv(e, pi=pi, mp=mp, g=g, qb=qb, hb_=hb_, m=m):
                        ins = None
                        for j4 in range(4):
                            kj = 4 * g + j4
                            ins = e.matmul((pO if mp == 0 else pOb)[qb][:, 0:130], lhsT=Pb[pi][:, j4 * 128:(j4 + 1) * 128], rhs=Vb[hb_][:, kj, :],
                                           start=(kj == 0), stop=(kj == 4 * m + 3))
                        return ins
                    P.op("pe", mmpv, reads=["P%d" % pi, vres], writes=[pOres if mp == 0 else pObres])

            def emit_epi(m, hb_=hb_, diff=diff, ohres=ohres):
                qb = m % 2
                pOres = "pO%d" % qb
                pObres = "pOb%d" % qb
                rres, o1res, o2res = "rl%d" % qb, "o1%d" % qb, "o2%d" % qb
                if diff:
                    P.op("dve", lambda e, qb=qb: e.reciprocal(out=rl[qb][:, 0:1], in_=pO[qb][:, 128:129]), reads=[pOres], writes=[rres])
                    P.op("dve", lambda e, qb=qb: e.reciprocal(out=rl[qb][:, 1:2], in_=pOb[qb][:, 128:129]), reads=[pObres, rres], writes=[rres])
                    P.op("dve", lambda e, qb=qb: e.tensor_scalar(out=o1[qb][:], in0=pO[qb][:, 0:128], scalar1=rl[qb][:, 0:1], scalar2=None, op0=ALU.mult), reads=[pOres, rres], writes=[o1res])
                    P.op("dve", lambda e, qb=qb: e.tensor_scalar(out=o2[qb][:], in0=pOb[qb][:, 0:128], scalar1=rl[qb][:, 1:2], scalar2=nlam[:, 0:1], op0=ALU.mult, op1=ALU.mult),
                         reads=[pObres, rres, "nlam"], writes=[o2res])
                    P.op("dve", lambda e, qb=qb: e.tensor_tensor(out=o1[qb][:], in0=o1[qb][:], in1=o2[qb][:], op=ALU.add), reads=[o1res, o2res], writes=[o1res])
                    P.op("act", lambda e, qb=qb: e.activation(out=junk[qb][:], in_=o1[qb][:], func=AF.Square, accum_out=ssq[qb][:]), reads=[o1res], writes=["junk%d" % qb, "ssq%d" % qb])
                    P.op("dve", lambda e, qb=qb: e.tensor_scalar(out=ssq[qb][:], in0=ssq[qb][:], scalar1=1.0 / 128.0, scalar2=EPS, op0=ALU.mult, op1=ALU.add), reads=["ssq%d" % qb], writes=["ssq%d" % qb])
                    P.op("act", lambda e, qb=qb: e.activation(out=ssq[qb][:], in_=ssq[qb][:], func=AF.Sqrt), reads=["ssq%d" % qb], writes=["ssq%d" % qb])
                    P.op("dve", lambda e, qb=qb: e.reciprocal(out=ssq[qb][:], in_=ssq[qb][:]), reads=["ssq%d" % qb], writes=["ssq%d" % qb])
                    P.op("dve", lambda e, qb=qb, hb_=hb_, m=m: e.scalar_tensor_tensor(out=oh[hb_][:, m, :], in0=o1[qb][:], scalar=ssq[qb][:, 0:1], in1=subg[:], op0=ALU.mult, op1=ALU.mult),
                         reads=[o1res, "ssq%d" % qb, "subg"], writes=[ohres])
                else:
                    P.op("dve", lambda e, qb=qb: e.reciprocal(out=rl[qb][:, 0:1], in_=pO[qb][:, 128:129]), reads=[pOres], writes=[rres])
                    P.op("dve", lambda e, qb=qb, hb_=hb_, m=m: e.tensor_scalar(out=oh[hb_][:, m, :], in0=pO[qb][:, 0:128], scalar1=rl[qb][:, 0:1], scalar2=None, op0=ALU.mult),
                         reads=[pOres, rres], writes=[ohres])

            pending = None
            for m in range(NTO):
                for g in range(m + 1):
                    if g == 0:
                        emit_pre(m)
                    sidx = emit_qk(m, g)
                    if pending is not None:
                        emit_exp_pv(*pending)
                        if pending[1] == pending[0]:
                            emit_epi(pending[0])
                    pending = (m, g, sidx)
            emit_exp_pv(*pending)
            emit_epi(pending[0])
            P.dma("sp", lambda e, hb_=hb_, h=h: e.dma_start(out=A["o_d"][:, h * 128:(h + 1) * 128].rearrange("(m p) d -> p m d", p=128), in_=oh[hb_][:]),
                  reads=[ohres], writes=["dram_o"])
        P.run()


def phase_oproj(nc, A):
    with contextlib.ExitStack() as st:
        P = Phase(nc, "p5")
        ident = sb(st, nc, "p5_ident", [128, 128], BF16)
        Wd = sb(st, nc, "p5_Wd", [128, 8, D], BF16)
        Wm = sb(st, nc, "p5_Wm", [128, 8, D], BF16)
        ot = [sb(st, nc, "p5_ot%d" % i, [128, D], BF16) for i in range(2)]
        oT = [sb(st, nc, "p5_oT%d" % i, [128, KC, 512], BF16) for i in range(1)]
        Gd = [sb(st, nc, "p5_Gd%d" % i, [128, KC, 512], BF16) for i in range(1)]
        Gm = [sb(st, nc, "p5_Gm%d" % i, [128, KC, 512], BF16) for i in range(1)]
        zT = [sb(st, nc, "p5_zT%d" % i, [128, KC, 512], BF16) for i in range(1)]
        t1 = [sb(st, nc, "p5_t1%d" % i, [128, 512], F32) for i in range(2)]
        pT = [ps(st, nc, "p5_pT%d" % i, [128, KC, 128], BF16) for i in range(1)]
        pY = [ps(st, nc, "p5_pY%d" % i, [128, 512], F32) for i in range(4)]
        P.dma("sp", lambda e: e.dma_start(out=ident[:], in_=A["ident_bf"][:, :]), writes=["ident"])
        for q4 in range(2):
            P.dma("pool", lambda e, q4=q4: e.dma_start(out=Wd[:, q4 * 4:(q4 + 1) * 4, :], in_=A["w_o_diff"].rearrange("(k p) n -> p k n", p=128)[:, q4 * 4:(q4 + 1) * 4, :]), writes=["Wd"])
            P.dma("pool", lambda e, q4=q4: e.dma_start(out=Wm[:, q4 * 4:(q4 + 1) * 4, :], in_=A["w_o_moba"].rearrange("(k p) n -> p k n", p=128)[:, q4 * 4:(q4 + 1) * 4, :]), writes=["Wm"])
        tcount = 0
        ycount = 0
        for g in range(4):
            gi = 0
            P.dma("sp", lambda e, gi=gi, g=g: e.dma_start(out=Gd[gi][:], in_=A["GT"][0:16, :, g * 512:(g + 1) * 512].rearrange("c p t -> p c t")), writes=["Gd%d" % gi])
            P.dma("sp", lambda e, gi=gi, g=g: e.dma_start(out=Gm[gi][:], in_=A["GT"][16:32, :, g * 512:(g + 1) * 512].rearrange("c p t -> p c t")), writes=["Gm%d" % gi])
            for tt in range(4):
                ti = tcount % 2
                tcount += 1
                tile = g * 4 + tt
                P.dma("sp", lambda e, ti=ti, tile=tile: e.dma_start(out=ot[ti][:], in_=A["o_d"][tile * 128:(tile + 1) * 128, :]), writes=["ot%d" % ti])

                def tr(e, ti=ti):
                    ins = None
                    for k in range(KC):
                        ins = e.transpose(out=pT[0][:, k, :], in_=ot[ti][:, k * 128:(k + 1) * 128], identity=ident[:])
                    return ins
                P.op("pe", tr, reads=["ot%d" % ti, "ident"], writes=["pT"])
                P.op("act", lambda e, gi=gi, tt=tt: e.activation(out=oT[gi][:, :, tt * 128:(tt + 1) * 128], in_=pT[0][:], func=AF.Copy), reads=["pT"], writes=["oT%d" % gi])
            for c in range(KC):
                yd_i = ycount % 4
                ym_i = (ycount + 1) % 4
                ycount += 2
                t1i = c % 2

                def mmd(e, gi=gi, c=c, yd_i=yd_i):
                    ins = None
                    for k in range(8):
                        ins = e.matmul(pY[yd_i][:], lhsT=Wd[:, k, c * 128:(c + 1) * 128], rhs=oT[gi][:, k, :], start=(k == 0), stop=(k == 7))
                    return ins

                def mmm(e, gi=gi, c=c, ym_i=ym_i):
                    ins = None
                    for k in range(8):
                        ins = e.matmul(pY[ym_i][:], lhsT=Wm[:, k, c * 128:(c + 1) * 128], rhs=oT[gi][:, 8 + k, :], start=(k == 0), stop=(k == 7))
                    return ins
                P.op("pe", mmd, reads=["Wd", "oT%d" % gi], writes=["pY%d" % yd_i])
                P.op("pe", mmm, reads=["Wm", "oT%d" % gi], writes=["pY%d" % ym_i])
                P.op("dve", lambda e, gi=gi, c=c, yd_i=yd_i, t1i=t1i: e.tensor_tensor(out=t1[t1i][:], in0=pY[yd_i][:], in1=Gd[gi][:, c, :], op=ALU.mult),
                     reads=["pY%d" % yd_i, "Gd%d" % gi], writes=["t1%d" % t1i])
                P.op("dve", lambda e, gi=gi, c=c, ym_i=ym_i, t1i=t1i: e.tensor_tensor(out=zT[gi][:, c, :], in0=pY[ym_i][:], in1=Gm[gi][:, c, :], op=ALU.mult),
                     reads=["pY%d" % ym_i, "Gm%d" % gi], writes=["zT%d" % gi])
                P.op("pool", lambda e, gi=gi, c=c, t1i=t1i: e.tensor_tensor(out=zT[gi][:, c, :], in0=zT[gi][:, c, :], in1=t1[t1i][:], op=ALU.add),
                     reads=["t1%d" % t1i, "zT%d" % gi], writes=["zT%d" % gi])
            P.dma("sp", lambda e, gi=gi, g=g: e.dma_start(out=A["zT"][g], in_=zT[gi][:].rearrange("p c t -> p (c t)")), reads=["zT%d" % gi], writes=["dram_zT"])
        P.run()


def phase_wout(nc, A):
    with contextlib.ExitStack() as st:
        P = Phase(nc, "p6")
        Wo = sb(st, nc, "p6_Wo", [128, KC, D], BF16)
        G1 = sb(st, nc, "p6_G1", [128, D], F32)
        zT = [sb(st, nc, "p6_zT%d" % i, [128, KC, 512], BF16) for i in range(2)]
        xt = [sb(st, nc, "p6_x%d" % i, [128, D], F32) for i in range(2)]
        pY = [ps(st, nc, "p6_pY%d" % i, [128, 512], F32) for i in range(4)]
        for q4 in range(4):
            P.dma("pool", lambda e, q4=q4: e.dma_start(out=Wo[:, q4 * 4:(q4 + 1) * 4, :], in_=A["w_out"].rearrange("(k p) n -> p k n", p=128)[:, q4 * 4:(q4 + 1) * 4, :]), writes=["Wo"])
        P.dma("sp", lambda e: e.dma_start(out=G1[:], in_=A["modb"][:, MOD_G1:MOD_G1 + D]), writes=["G1"])
        ycount = 0
        tcount = 0
        for g in range(4):
            gi = g % 2
            P.dma("sp", lambda e, gi=gi, g=g: e.dma_start(out=zT[gi][:].rearrange("p c t -> p (c t)"), in_=A["zT"][g]), writes=["zT%d" % gi])
            for tt in range(4):
                ti = tcount % 2
                tcount += 1
                tile = g * 4 + tt
                P.dma("sp", lambda e, ti=ti, tile=tile: e.dma_start(out=xt[ti][:], in_=A["x_own"][tile * 128:(tile + 1) * 128, :]), writes=["x%d" % ti])
                for cg in range(4):
                    yi = ycount % 4
                    ycount += 1

                    def mm(e, gi=gi, tt=tt, cg=cg, yi=yi):
                        ins = None
                        for k in range(KC):
                            ins = e.matmul(pY[yi][:], lhsT=zT[gi][:, k, tt * 128:(tt + 1) * 128], rhs=Wo[:, k, cg * 512:(cg + 1) * 512], start=(k == 0), stop=(k == KC - 1))
                        return ins
                    P.op("pe", mm, reads=["zT%d" % gi, "Wo"], writes=["pY%d" % yi])
                    P.op("dve", lambda e, yi=yi, cg=cg, ti=ti: e.tensor_tensor(out=pY[yi][:], in0=pY[yi][:], in1=G1[:, cg * 512:(cg + 1) * 512], op=ALU.mult),
                         reads=["pY%d" % yi, "G1"], writes=["pY%d" % yi])
                    P.op("dve", lambda e, yi=yi, cg=cg, ti=ti: e.tensor_tensor(out=xt[ti][:, cg * 512:(cg + 1) * 512], in0=pY[yi][:], in1=xt[ti][:, cg * 512:(cg + 1) * 512], op=ALU.add),
                         reads=["pY%d" % yi, "x%d" % ti], writes=["x%d" % ti])
                P.dma("sp", lambda e, ti=ti, tile=tile: e.dma_start(out=A["x1"][tile * 128:(tile + 1) * 128, :], in_=xt[ti][:]), reads=["x%d" % ti], writes=["dram_x1"])
        P.run()


def phase_route(nc, A):
    with contextlib.ExitStack() as st:
        P = Phase(nc, "p7")
        ident = sb(st, nc, "p7_ident", [128, 128], BF16)
        ltri = sb(st, nc, "p7_ltri", [128, 128], BF16)
        onesb = sb(st, nc, "p7_ones", [128, 128], BF16)
        Amod = sb(st, nc, "p7_A", [128, D], F32)
        Smod = sb(st, nc, "p7_S", [128, D], F32)
        Wr = sb(st, nc, "p7_Wr", [128, KC, E], BF16)
        rbias = sb(st, nc, "p7_rbias", [128, E], F32)
        ecap = sb(st, nc, "p7_ecap", [128, E], F32)
        dumpidx = sb(st, nc, "p7_dump", [128, 1], F32)
        tokid = sb(st, nc, "p7_tokid", [128, NTO], I32)
        zero_i = sb(st, nc, "p7_zero", [128, CAP * 8], I32)
        zero_b = sb(st, nc, "p7_zerob", [128, D], BF16)
        xt = [sb(st, nc, "p7_x%d" % i, [128, D], F32) for i in range(2)]
        sq = [sb(st, nc, "p7_sq%d" % i, [128, D], BF16) for i in range(2)]
        ss = [sb(st, nc, "p7_ss%d" % i, [128, 1], F32) for i in range(2)]
        rstd = [sb(st, nc, "p7_rs%d" % i, [128, 1], F32) for i in range(2)]
        hf = [sb(st, nc, "p7_hf%d" % i, [128, D], F32) for i in range(2)]
        hb = [sb(st, nc, "p7_hb%d" % i, [128, D], BF16) for i in range(2)]
        hT = [sb(st, nc, "p7_hT%d" % i, [128, KC, 128], BF16) for i in range(2)]
        emask = sb(st, nc, "p7_emask", [128, NTO, E], BF16)
        scores = [sb(st, nc, "p7_sc%d" % i, [128, E], F32) for i in range(2)]
        selv = [sb(st, nc, "p7_sel%d" % i, [128, E], F32) for i in range(2)]
        g8 = [sb(st, nc, "p7_g8%d" % i, [128, 8], F32) for i in range(2)]
        gsc = [sb(st, nc, "p7_gsc%d" % i, [128, 8], F32) for i in range(2)]
        gm8 = [sb(st, nc, "p7_gm%d" % i, [128, 8], F32) for i in range(2)]
        gmask = [sb(st, nc, "p7_gmask%d" % i, [128, 8], F32) for i in range(2)]
        t8 = [sb(st, nc, "p7_t8%d" % i, [128, 8], F32) for i in range(2)]
        em = [sb(st, nc, "p7_em%d" % i, [128, E], F32) for i in range(2)]
        wt = sb(st, nc, "p7_wt", [128, NTO, E], F32)
        wsum = [sb(st, nc, "p7_ws%d" % i, [128, 1], F32) for i in range(2)]
        key = [sb(st, nc, "p7_key%d" % i, [128, E], F32) for i in range(2)]
        k8 = [sb(st, nc, "p7_k8%d" % i, [128, 8], F32) for i in range(2)]
        oh_ = [sb(st, nc, "p7_oh%d" % i, [128, E], F32) for i in range(2)]
        junk_ = [sb(st, nc, "p7_junk%d" % i, [128, E], F32) for i in range(2)]
        cnt_i = sb(st, nc, "p7_cnt_i", [128, E], I32)
        route = [sb(st, nc, "p7_route%d" % i, [128, 16], F32) for i in range(2)]
        sidx = [sb(st, nc, "p7_sidx%d" % i, [128, 8], I32) for i in range(2)]
        tokrow = [sb(st, nc, "p7_tokrow%d" % i, [128, 8], I32) for i in range(2)]
        valid = [sb(st, nc, "p7_valid%d" % i, [128, 8], F32) for i in range(2)]
        pT = [ps(st, nc, "p7_pT%d" % i, [128, KC, 128], BF16) for i in range(2)]
        pL = [ps(st, nc, "p7_pL%d" % i, [128, E], F32) for i in range(2)]
        pR = [ps(st, nc, "p7_pR%d" % i, [128, E], F32) for i in range(2)]

        P.dma("sp", lambda e: e.dma_start(out=ident[:], in_=A["ident_bf"][:, :]), writes=["ident"])
        P.dma("sp", lambda e: e.dma_start(out=ltri[:], in_=A["ltri"][:, :]), writes=["ltri"])
        P.dma("sp", lambda e: e.dma_start(out=Amod[:], in_=A["modb"][:, MOD_A2:MOD_A2 + D]), writes=["Amod"])
        P.dma("sp", lambda e: e.dma_start(out=Smod[:], in_=A["modb"][:, MOD_SHIFT2:MOD_SHIFT2 + D]), writes=["Smod"])
        P.dma("pool", lambda e: e.dma_start(out=Wr[:], in_=A["w_router"].rearrange("(k p) n -> p k n", p=128)), writes=["Wr"])
        P.dma("sp", lambda e: e.dma_start(out=rbias[:], in_=A["rbias_b"][:, :]), writes=["rbias"])
        P.dma("sp", lambda e: e.dma_start(out=ecap[:], in_=A["ecap"][:, :]), writes=["ecap"])
        P.dma("sp", lambda e: e.dma_start(out=dumpidx[:], in_=A["dumpidx"][:, :]), writes=["dumpidx"])
        P.dma("sp", lambda e: e.dma_start(out=tokid[:], in_=A["tokid"][:, :]), writes=["tokid"])
        P.op("dve", lambda e: e.memset(onesb[:], 1.0), writes=["onesb"])
        P.op("pool", lambda e: e.memset(zero_i[:], 0), writes=["zero_i"])
        P.op("pool", lambda e: e.memset(zero_b[:], 0.0), writes=["zero_b"])
        rt_view = A["rowtok"][0:NSLOT, :].rearrange("(e c) w -> e (c w)", e=E)
        P.dma("sp", lambda e: e.dma_start(out=rt_view, in_=zero_i[0:E, :]), reads=["zero_i"], writes=["dram_rowtok"])
        P.dma("sp", lambda e: e.dma_start(out=A["rowtok"][NSLOT:NSLOT + 128, :], in_=zero_i[:, 0:8]), reads=["zero_i"], writes=["dram_rowtok"])
        P.dma("sp", lambda e: e.dma_start(out=A["Ybuf"][NSLOT:NSLOT + 128, :], in_=zero_b[:]), reads=["zero_b"], writes=["dram_Ybuf"])

        for t in range(NTO):
            i = t % 2
            tag = str(i)
            P.dma("sp", lambda e, i=i, t=t: e.dma_start(out=xt[i][:], in_=A["x1"][t * 128:(t + 1) * 128, :]), writes=["x" + tag])
            emit_norm_mod_T(P, nc, xt[i], sq[i], ss[i], rstd[i], hf[i], hb[i], pT[i], hT[i], Amod, Smod, ident, tag, "x" + tag)
            P.dma("sp", lambda e, i=i, t=t: e.dma_start(out=A["h2"][t * 128:(t + 1) * 128, :], in_=hb[i][:]), reads=["hb" + tag], writes=["dram_h2"])
            P.dma("sp", lambda e, i=i, t=t: e.dma_start(out=A["h2T"][t], in_=hT[i][:].rearrange("p k t -> p (k t)")), reads=["hT" + tag], writes=["dram_h2T"])

            def mml(e, i=i):
                ins = None
                for k in range(KC):
                    ins = e.matmul(pL[i][:], lhsT=hT[i][:, k, :], rhs=Wr[:, k, :], start=(k == 0), stop=(k == KC - 1))
                return ins
            P.op("pe", mml, reads=["hT" + tag, "Wr"], writes=["pL" + tag])
            P.op("act", lambda e, i=i: e.activation(out=scores[i][:], in_=pL[i][:], func=AF.Sigmoid), reads=["pL" + tag], writes=["sc" + tag])
            P.op("dve", lambda e, i=i: e.tensor_tensor(out=selv[i][:], in0=scores[i][:], in1=rbias[:], op=ALU.add), reads=["sc" + tag, "rbias"], writes=["sel" + tag])
            for gq in range(8):
                P.op("dve", lambda e, i=i, gq=gq: e.max(out=g8[i][:], in_=selv[i][:, gq * 8:(gq + 1) * 8]), reads=["sel" + tag, "gsc" + tag], writes=["g8" + tag])
                P.op("dve", lambda e, i=i, gq=gq: e.tensor_tensor(out=gsc[i][:, gq:gq + 1], in0=g8[i][:, 0:1], in1=g8[i][:, 1:2], op=ALU.add), reads=["g8" + tag], writes=["gsc" + tag])
            P.op("dve", lambda e, i=i: e.max(out=gm8[i][:], in_=gsc[i][:]), reads=["gsc" + tag], writes=["gm8" + tag])
            P.op("dve", lambda e, i=i: e.tensor_scalar(out=gmask[i][:], in0=gsc[i][:], scalar1=gm8[i][:, 3:4], scalar2=None, op0=ALU.is_ge), reads=["gsc" + tag, "gm8" + tag], writes=["gmask" + tag])
            for gq in range(8):
                P.op("dve", lambda e, i=i, gq=gq: e.tensor_scalar(out=selv[i][:, gq * 8:(gq + 1) * 8], in0=selv[i][:, gq * 8:(gq + 1) * 8], scalar1=2.0, scalar2=gmask[i][:, gq:gq + 1],
                                                                 op0=ALU.add, op1=ALU.mult), reads=["sel" + tag, "gmask" + tag], writes=["sel" + tag])
            P.op("dve", lambda e, i=i: e.max(out=t8[i][:], in_=selv[i][:]), reads=["sel" + tag], writes=["t8" + tag])
            P.op("dve", lambda e, i=i: e.tensor_scalar(out=em[i][:], in0=selv[i][:], scalar1=t8[i][:, 7:8], scalar2=None, op0=ALU.is_ge), reads=["sel" + tag, "t8" + tag], writes=["em" + tag])
            P.op("dve", lambda e, i=i, t=t: e.tensor_copy(out=emask[:, t, :], in_=em[i][:]), reads=["em" + tag], writes=["emask"])
            P.op("dve", lambda e, i=i, t=t: e.tensor_tensor(out=wt[:, t, :], in0=scores[i][:], in1=em[i][:], op=ALU.mult), reads=["sc" + tag, "em" + tag], writes=["wt"])
            P.op("act", lambda e, i=i, t=t: e.activation(out=junk_[i][:], in_=wt[:, t, :], func=AF.Copy, accum_out=wsum[i][:]), reads=["wt"], writes=["ws" + tag, "junk" + tag])
            P.op("dve", lambda e, i=i: e.reciprocal(out=wsum[i][:], in_=wsum[i][:]), reads=["ws" + tag], writes=["ws" + tag])
            P.op("dve", lambda e, i=i, t=t: e.tensor_scalar(out=wt[:, t, :], in0=wt[:, t, :], scalar1=wsum[i][:, 0:1], scalar2=2.5, op0=ALU.mult, op1=ALU.mult), reads=["wt", "ws" + tag], writes=["wt"])

            def mmr(e, i=i, t=t):
                ins = None
                for j in range(t):
                    ins = e.matmul(pR[i][:], lhsT=onesb[:], rhs=emask[:, j, :], start=(j == 0), stop=False)
                ins = e.matmul(pR[i][:], lhsT=ltri[:], rhs=emask[:, t, :], start=(t == 0), stop=True)
                return ins
            P.op("pe", mmr, reads=["emask", "onesb", "ltri"], writes=["pR" + tag])
            P.op("dve", lambda e, i=i: e.scalar_tensor_tensor(out=key[i][:], in0=pR[i][:], scalar=1.0, in1=ecap[:], op0=ALU.add, op1=ALU.add), reads=["pR" + tag, "ecap"], writes=["key" + tag])
            P.op("dve", lambda e, i=i: e.tensor_scalar(out=oh_[i][:], in0=pR[i][:], scalar1=float(CAP) - 0.5, scalar2=None, op0=ALU.is_lt), reads=["pR" + tag], writes=["oh" + tag])
            P.op("dve", lambda e, i=i: e.tensor_tensor(out=oh_[i][:], in0=oh_[i][:], in1=em[i][:], op=ALU.mult), reads=["oh" + tag, "em" + tag], writes=["oh" + tag])
            P.op("dve", lambda e, i=i: e.tensor_tensor(out=key[i][:], in0=key[i][:], in1=oh_[i][:], op=ALU.mult), reads=["key" + tag, "oh" + tag], writes=["key" + tag])
            P.op("dve", lambda e, i=i: e.max(out=k8[i][:], in_=key[i][:]), reads=["key" + tag], writes=["k8" + tag])
            for kk in range(8):
                P.op("dve", lambda e, i=i, kk=kk: e.tensor_scalar(out=oh_[i][:], in0=key[i][:], scalar1=k8[i][:, kk:kk + 1], scalar2=None, op0=ALU.is_equal), reads=["key" + tag, "k8" + tag, "route" + tag], writes=["oh" + tag])
                P.op("dve", lambda e, i=i, kk=kk, t=t: e.tensor_tensor(out=oh_[i][:], in0=oh_[i][:], in1=wt[:, t, :], op=ALU.mult), reads=["oh" + tag, "wt"], writes=["oh" + tag])
                P.op("act", lambda e, i=i, kk=kk: e.activation(out=junk_[i][:], in_=oh_[i][:], func=AF.Copy, accum_out=route[i][:, 8 + kk:9 + kk]), reads=["oh" + tag], writes=["route" + tag, "junk" + tag])
            P.op("dve", lambda e, i=i: e.tensor_scalar(out=valid[i][:], in0=k8[i][:], scalar1=0.5, scalar2=None, op0=ALU.is_gt), reads=["k8" + tag], writes=["valid" + tag])
            P.op("dve", lambda e, i=i: e.tensor_tensor(out=route[i][:, 8:16], in0=route[i][:, 8:16], in1=valid[i][:], op=ALU.mult), reads=["route" + tag, "valid" + tag], writes=["route" + tag])
            P.op("dve", lambda e, i=i: e.tensor_scalar(out=route[i][:, 0:8], in0=k8[i][:], scalar1=-1.0, scalar2=dumpidx[:, 0:1], op0=ALU.add, op1=ALU.subtract), reads=["k8" + tag, "dumpidx", "route" + tag], writes=["route" + tag])
            P.op("dve", lambda e, i=i: e.tensor_tensor(out=route[i][:, 0:8], in0=route[i][:, 0:8], in1=valid[i][:], op=ALU.mult), reads=["route" + tag, "valid" + tag], writes=["route" + tag])
            P.op("dve", lambda e, i=i: e.tensor_scalar(out=route[i][:, 0:8], in0=route[i][:, 0:8], scalar1=dumpidx[:, 0:1], scalar2=None, op0=ALU.add), reads=["route" + tag, "dumpidx"], writes=["route" + tag])
            P.op("dve", lambda e, i=i: e.tensor_copy(out=sidx[i][:], in_=route[i][:, 0:8]), reads=["route" + tag], writes=["sidx" + tag])
            P.dma("sp", lambda e, i=i, t=t: e.dma_start(out=A["route"][:, t * 16:(t + 1) * 16], in_=route[i][:]), reads=["route" + tag], writes=["dram_route"])
            if t == NTO - 1:
                def mmc(e):
                    ins = None
                    for j in range(NTO):
                        ins = e.matmul(pL[0][:], lhsT=onesb[:], rhs=emask[:, j, :], start=(j == 0), stop=(j == NTO - 1))
                    return ins
                P.op("pe", mmc, reads=["emask", "onesb"], writes=["pL0"])
                P.op("dve", lambda e: e.tensor_copy(out=cnt_i[:], in_=pL[0][:]), reads=["pL0"], writes=["cnt_i"])
                P.dma("sp", lambda e: e.dma_start(out=A["cnt_d"][:, :], in_=cnt_i[0:1, :]), reads=["cnt_i"], writes=["dram_cnt"])
            for kk in range(8):
                P.op("pool", lambda e, i=i, kk=kk, t=t: e.tensor_copy(out=tokrow[i][:, kk:kk + 1], in_=tokid[:, t:t + 1]), reads=["tokid", "tokrow" + tag], writes=["tokrow" + tag])
            for kk in range(8):
                P.dma("pool", lambda e, i=i, kk=kk: e.indirect_dma_start(out=A["rowtok"][:, :], out_offset=bass.IndirectOffsetOnAxis(ap=sidx[i][:, kk:kk + 1], axis=0),
                                                                         in_=tokrow[i][:], in_offset=None),
                      reads=["sidx" + tag, "tokrow" + tag, "dram_rowtok"], writes=["dram_rowtok_s"])
        P.run()


def phase_experts(nc, A):
    NST = 4
    with contextlib.ExitStack() as st:
        P = Phase(nc, "p8")
        ident = sb(st, nc, "p8_ident", [128, 128], BF16)
        cnt_sb = sb(st, nc, "p8_cnt", [1, E], I32)
        P.cnt_ap = cnt_sb
        Wg = [sb(st, nc, "p8_Wg%d" % i, [128, KC, 512], BF16) for i in range(2)]
        Wu = [sb(st, nc, "p8_Wu%d" % i, [128, KC, 512], BF16) for i in range(2)]
        Wd = [sb(st, nc, "p8_Wd%d" % i, [128, 4, D], BF16) for i in range(2)]
        stage = [sb(st, nc, "p8_st%d" % i, [128, 2048], F32) for i in range(NST)]
        idx_all = sb(st, nc, "p8_idxall", [128, E * JMAX, 8], I32)
        xg = [sb(st, nc, "p8_xg%d" % i, [128, D], BF16) for i in range(3)]
        xT = [sb(st, nc, "p8_xT%d" % i, [128, KC, 128], BF16) for i in range(2)]
        sg = [sb(st, nc, "p8_sg%d" % i, [128, 512], F32) for i in range(2)]
        aT = [sb(st, nc, "p8_aT%d" % i, [128, 4, 128], BF16) for i in range(2)]
        Y = [sb(st, nc, "p8_Y%d" % i, [128, D], BF16) for i in range(2)]
        pT = [ps(st, nc, "p8_pT%d" % i, [128, KC, 128], BF16) for i in range(1)]
        pG = [ps(st, nc, "p8_pG%d" % i, [128, 512], F32) for i in range(2)]
        pU = [ps(st, nc, "p8_pU%d" % i, [128, 512], F32) for i in range(2)]
        pY = [ps(st, nc, "p8_pY%d" % i, [128, 512], F32) for i in range(2)]
        P.dma("pool", lambda e: e.dma_start(out=ident[:], in_=A["ident_bf"][:, :]), writes=["ident"])
        P.dma("pool", lambda e: e.dma_start(out=cnt_sb[:], in_=A["cnt_d"][:, :]), writes=["cnt_sb"])
        cn = {"g": 0, "y": 0, "yb": 0, "x": 0, "a": 0, "gu": 0, "st": 0, "ce": 0}

        def wsrc(ei):
            if ei < E:
                return A["w_exp_gate"][ei], A["w_exp_up"][ei], A["w_exp_down"][ei]
            return A["w_sh_gate"], A["w_sh_up"], A["w_sh_down"]

        def weight_chunks(ei):
            wi = ei % 2
            gsrc, usrc, dsrc = wsrc(ei)
            out = []
            for c in range(4):
                out.append((gsrc.rearrange("(k p) n -> p k n", p=128)[:, 4 * c:4 * c + 4, :], Wg[wi][:, 4 * c:4 * c + 4, :], "Wg%d_%d" % (wi, c), (4, 512)))
            for c in range(4):
                out.append((usrc.rearrange("(k p) n -> p k n", p=128)[:, 4 * c:4 * c + 4, :], Wu[wi][:, 4 * c:4 * c + 4, :], "Wu%d_%d" % (wi, c), (4, 512)))
            for c in range(4):
                out.append((dsrc.rearrange("(k p) n -> p k n", p=128)[:, c:c + 1, :], Wd[wi][:, c:c + 1, :], "Wd%d_%d" % (wi, c), (1, 2048)))
            return out

        def issue_chunk_dma(ch):
            src, dst, res, (a, b) = ch
            si = cn["st"] % NST
            cn["st"] += 1
            P.dma("sp", lambda e, si=si, src=src, a=a: e.dma_start(out=stage[si][:].rearrange("p (a b) -> p a b", a=a), in_=src), writes=["st%d" % si])
            return si

        def issue_chunk_cast(ch, si):
            src, dst, res, (a, b) = ch
            eng = "dve"
            view = stage[si][:].rearrange("p (a b) -> p a b", a=a)
            if eng == "act":
                P.op("act", lambda e, dst=dst, view=view: e.activation(out=dst, in_=view, func=AF.Copy), reads=["st%d" % si], writes=[res])
            else:
                P.op(eng, lambda e, dst=dst, view=view: e.tensor_copy(out=dst, in_=view), reads=["st%d" % si], writes=[res])

        def ffn(wi, xi, dst):
            xres = "xT%d" % xi
            ai = cn["a"] % 2
            cn["a"] += 1
            ares = "aT%d" % ai
            gb = cn["gu"] % 2
            cn["gu"] += 1

            def mmg(e, wi=wi, xi=xi, gb=gb):
                ins = None
                for fc in range(4):
                    for k in range(KC):
                        ins = e.matmul(pG[gb][:, fc * 128:(fc + 1) * 128], lhsT=Wg[wi][:, k, fc * 128:(fc + 1) * 128], rhs=xT[xi][:, k, :], start=(k == 0), stop=(k == KC - 1))
                return ins

            def mmu(e, wi=wi, xi=xi, gb=gb):
                ins = None
                for fc in range(4):
                    for k in range(KC):
                        ins = e.matmul(pU[gb][:, fc * 128:(fc + 1) * 128], lhsT=Wu[wi][:, k, fc * 128:(fc + 1) * 128], rhs=xT[xi][:, k, :], start=(k == 0), stop=(k == KC - 1))
                return ins
            P.op("pe", mmg, reads=["Wg%d_%d" % (wi, c) for c in range(4)] + [xres], writes=["pG%d" % gb])
            P.op("pe", mmu, reads=["Wu%d_%d" % (wi, c) for c in range(4)] + [xres], writes=["pU%d" % gb])
            P.op("act", lambda e, gb=gb: e.activation(out=sg[gb][:], in_=pG[gb][:], func=AF.Sigmoid), reads=["pG%d" % gb], writes=["sg%d" % gb])
            P.op("dve", lambda e, gb=gb: e.tensor_tensor(out=sg[gb][:], in0=pG[gb][:], in1=sg[gb][:], op=ALU.mult), reads=["pG%d" % gb, "sg%d" % gb], writes=["sg%d" % gb])
            P.op("dve", lambda e, gb=gb, ai=ai: e.tensor_tensor(out=aT[ai][:], in0=pU[gb][:].rearrange("p (f t) -> p f t", f=4), in1=sg[gb][:].rearrange("p (f t) -> p f t", f=4), op=ALU.mult),
                 reads=["pU%d" % gb, "sg%d" % gb], writes=[ares])
            yi = cn["yb"] % 2
            cn["yb"] += 1
            for cg in range(4):
                pi = cn["y"] % 2
                cn["y"] += 1

                def mmy(e, wi=wi, ai=ai, cg=cg, pi=pi):
                    ins = None
                    for fc in range(4):
                        ins = e.matmul(pY[pi][:], lhsT=aT[ai][:, fc, :], rhs=Wd[wi][:, fc, cg * 512:(cg + 1) * 512], start=(fc == 0), stop=(fc == 3))
                    return ins
                P.op("pe", mmy, reads=[ares] + ["Wd%d_%d" % (wi, c) for c in range(4)], writes=["pY%d" % pi])
                P.op("dve", lambda e, yi=yi, cg=cg, pi=pi: e.tensor_copy(out=Y[yi][:, cg * 512:(cg + 1) * 512], in_=pY[pi][:]), reads=["pY%d" % pi], writes=["Y%d_%d" % (yi, cg)])
            P.dma("act", lambda e, yi=yi, dst=dst: e.dma_start(out=dst, in_=Y[yi][:]), reads=["Y%d_%d" % (yi, c) for c in range(4)], writes=["dram_Y"])

        for ei in range(E):
            P.dma("pool", lambda e, ei=ei: e.dma_start(out=idx_all[:, ei * JMAX:(ei + 1) * JMAX, :], in_=A["rowtok"][ei * CAP:(ei + 1) * CAP, :].rearrange("(t p) w -> p t w", p=128)),
                  writes=["idxall%d" % ei])
        for ch in weight_chunks(0):
            si = issue_chunk_dma(ch)
            issue_chunk_cast(ch, si)
        for ei in range(E):
            wi = ei % 2
            P.load_cond_reg(ei, "cnt_sb")
            nxt = weight_chunks(ei + 1)
            pend = []

            def pump(ncast):
                for _ in range(ncast):
                    if pend:
                        ch, si = pend.pop(0)
                        issue_chunk_cast(ch, si)
                while nxt and len(pend) < NST:
                    ch = nxt.pop(0)
                    pend.append((ch, issue_chunk_dma(ch)))
            pump(0)
            for j in range(JMAX):
                P.begin_region(ei, 128 * j)
                gi = cn["g"] % 3
                cn["g"] += 1
                xi = cn["x"] % 2
                cn["x"] += 1
                s0 = ei * CAP + j * 128
                P.dma("pool", lambda e, ei=ei, j=j, gi=gi: e.indirect_dma_start(out=xg[gi][:], out_offset=None, in_=A["h2"][:, :],
                                                                              in_offset=bass.IndirectOffsetOnAxis(ap=idx_all[:, ei * JMAX + j, 0:1], axis=0)),
                      reads=["idxall%d" % ei], writes=["xg%d" % gi])

                def tr(e, gi=gi):
                    ins = None
                    for k in range(KC):
                        ins = e.transpose(out=pT[0][:, k, :], in_=xg[gi][:, k * 128:(k + 1) * 128], identity=ident[:])
                    return ins
                P.op("pe", tr, reads=["xg%d" % gi, "ident"], writes=["pT"])
                P.op("dve", lambda e, xi=xi: e.tensor_copy(out=xT[xi][:], in_=pT[0][:]), reads=["pT"], writes=["xT%d" % xi])
                P.end_region()
                pump(2)
                P.begin_region(ei, 128 * j)
                ffn(wi, xi, A["Ybuf"][s0:s0 + 128, :])
                P.end_region()
            while pend or nxt:
                pump(2)
        wi = E % 2
        for t in range(NTO):
            xi = cn["x"] % 2
            cn["x"] += 1
            P.dma("pool", lambda e, xi=xi, t=t: e.dma_start(out=xT[xi][:], in_=A["h2T"][t].rearrange("p (k q) -> p k q", k=KC)), writes=["xT%d" % xi])
            ffn(wi, xi, A["Ysh"][t * 128:(t + 1) * 128, :])
        P.run()


def phase_final(nc, A):
    with contextlib.ExitStack() as st:
        P = Phase(nc, "p9")
        G2 = sb(st, nc, "p9_G2", [128, D], F32)
        gf = sb(st, nc, "p9_gf", [128, D], F32)
        xt = [sb(st, nc, "p9_x%d" % i, [128, D], F32) for i in range(2)]
        ysh = [sb(st, nc, "p9_ysh%d" % i, [128, D], BF16) for i in range(2)]
        yk = [sb(st, nc, "p9_yk%d" % i, [128, D], BF16) for i in range(4)]
        acc = [sb(st, nc, "p9_acc%d" % i, [128, D], F32) for i in range(2)]
        route = [sb(st, nc, "p9_route%d" % i, [128, 16], F32) for i in range(2)]
        sidx = [sb(st, nc, "p9_sidx%d" % i, [128, 8], I32) for i in range(2)]
        sq = [sb(st, nc, "p9_sq%d" % i, [128, D], BF16) for i in range(2)]
        ss = [sb(st, nc, "p9_ss%d" % i, [128, 1], F32) for i in range(2)]
        P.dma("sp", lambda e: e.dma_start(out=G2[:], in_=A["modb"][:, MOD_G2:MOD_G2 + D]), writes=["G2"])
        P.dma("sp", lambda e: e.dma_start(out=gf[:], in_=A["g_final_b"][:, :]), writes=["gf"])
        kc = 0
        for t in range(NTO):
            i = t % 2
            tag = str(i)
            P.dma("sp", lambda e, i=i, t=t: e.dma_start(out=xt[i][:], in_=A["x1"][t * 128:(t + 1) * 128, :]), writes=["x" + tag])
            P.dma("sp", lambda e, i=i, t=t: e.dma_start(out=ysh[i][:], in_=A["Ysh"][t * 128:(t + 1) * 128, :]), writes=["ysh" + tag])
            P.dma("sp", lambda e, i=i, t=t: e.dma_start(out=route[i][:], in_=A["route"][:, t * 16:(t + 1) * 16]), writes=["route" + tag])
            P.op("dve", lambda e, i=i: e.tensor_copy(out=sidx[i][:], in_=route[i][:, 0:8]), reads=["route" + tag], writes=["sidx" + tag])
            P.op("dve", lambda e, i=i: e.tensor_copy(out=acc[i][:], in_=ysh[i][:]), reads=["ysh" + tag], writes=["acc" + tag])
            for kk in range(8):
                ki = kc % 4
                kc += 1
                P.dma("pool", lambda e, i=i, kk=kk, ki=ki: e.indirect_dma_start(out=yk[ki][:], out_offset=None, in_=A["Ybuf"][:, :],
                                                                              in_offset=bass.IndirectOffsetOnAxis(ap=sidx[i][:, kk:kk + 1], axis=0)),
                      reads=["sidx" + tag], writes=["yk%d" % ki])
                P.op("dve", lambda e, i=i, kk=kk, ki=ki: e.scalar_tensor_tensor(out=acc[i][:], in0=yk[ki][:], scalar=route[i][:, 8 + kk:9 + kk], in1=acc[i][:], op0=ALU.mult, op1=ALU.add),
                     reads=["yk%d" % ki, "route" + tag, "acc" + tag], writes=["acc" + tag])
            P.op("pool", lambda e, i=i: e.tensor_tensor(out=acc[i][:], in0=acc[i][:], in1=G2[:], op=ALU.mult), reads=["acc" + tag, "G2"], writes=["acc" + tag])
            P.op("pool", lambda e, i=i: e.tensor_tensor(out=xt[i][:], in0=xt[i][:], in1=acc[i][:], op=ALU.add), reads=["acc" + tag, "x" + tag], writes=["x" + tag])
            P.op("act", lambda e, i=i: e.activation(out=sq[i][:], in_=xt[i][:], func=AF.Square, accum_out=ss[i][:]), reads=["x" + tag], writes=["sq" + tag, "ss" + tag])
            P.op("dve", lambda e, i=i: e.tensor_scalar(out=ss[i][:], in0=ss[i][:], scalar1=1.0 / D, scalar2=EPS, op0=ALU.mult, op1=ALU.add), reads=["ss" + tag], writes=["ss" + tag])
            P.op("act", lambda e, i=i: e.activation(out=ss[i][:], in_=ss[i][:], func=AF.Sqrt), reads=["ss" + tag], writes=["ss" + tag])
            P.op("dve", lambda e, i=i: e.reciprocal(out=ss[i][:], in_=ss[i][:]), reads=["ss" + tag], writes=["ss" + tag])
            P.op("dve", lambda e, i=i: e.scalar_tensor_tensor(out=acc[i][:], in0=xt[i][:], scalar=ss[i][:, 0:1], in1=gf[:], op0=ALU.mult, op1=ALU.mult),
                 reads=["x" + tag, "ss" + tag, "gf", "acc" + tag], writes=["acc" + tag])
            P.dma("sp", lambda e, i=i, t=t: e.dma_start(out=A["out"][t * 128:(t + 1) * 128, :], in_=acc[i][:]), reads=["acc" + tag], writes=["dram_out"])
        P.run()


def _t5_bucket_np(rel):
    n = np.maximum(rel, 0)
    nf = np.maximum(n, 1).astype(np.float32)
    large = 16 + (np.log(nf / 16) / np.float32(np.log(128 / 16)) * 16).astype(np.int32)
    large = np.minimum(large, 31)
    return np.where(n < 16, n, large)


def _bucket_table():
    n = np.arange(0, 700, dtype=np.int64)
    nf = np.maximum(n, 1).astype(np.float32)
    large = 16 + (np.log(nf / np.float32(16)) / np.float32(np.log(8.0)) * np.float32(16)).astype(np.int32)
    large = np.minimum(large, 31)
    return np.where(n < 16, n, large)


def make_core_inputs(inp, c, shared):
    b, r = c // 4, c % 4
    f32 = np.float32
    x = inp["x"]
    own_tiles = [4 * m + r for m in range(NTO)]
    xb = np.ascontiguousarray(x[b])
    m_ = dict(shared)
    m_["x_all"] = xb
    m_["x_own"] = np.ascontiguousarray(xb.reshape(NTA, 128, D)[own_tiles].reshape(TO, D))
    m_["cT"] = np.ascontiguousarray(inp["c"][b].reshape(KC, 128).T)
    ext = shared["_ext_table"]
    bidx = np.zeros((128, 5, 128), np.int64)
    kk = np.arange(128)[:, None]
    qq = np.arange(128)[None, :]
    for jj in range(5):
        delta = r - (jj - 1)
        rel = delta * 128 + qq - kk
        bi = shared["_bucket"][np.clip(rel, 0, 699)]
        bidx[:, jj, :] = np.where(rel >= 0, bi, 32)
    BT = ext[bidx]
    m_["BT"] = np.ascontiguousarray(BT.transpose(0, 3, 1, 2).reshape(128, 16 * 5 * 128)).astype(ml_dtypes.bfloat16)
    cur = np.array([(4 * m + r) // 2 for m in range(NTO)])
    n = np.arange(32)[None, :]
    past = (n < cur[:, None])
    ownm = (n == cur[:, None])
    const_tbl = np.array([0.0, 1.0, -BIG], f32)
    m_["past"] = np.ascontiguousarray(np.broadcast_to(const_tbl[past.astype(np.int64)].reshape(1, NTO * 32), (128, NTO * 32)))
    m_["own"] = np.ascontiguousarray(np.broadcast_to(const_tbl[ownm.astype(np.int64)].reshape(1, NTO * 32), (128, NTO * 32)))
    m_["pen"] = np.ascontiguousarray(np.broadcast_to(const_tbl[np.where(past, 0, 2)].reshape(1, NTO * 32), (128, NTO * 32)))
    for k in list(m_.keys()):
        if k.startswith("_"):
            del m_[k]
    return m_


def make_shared(inp):
    f32 = np.float32
    bc = lambda v, n: np.ascontiguousarray(np.broadcast_to(np.asarray(v, f32).reshape(1, n), (128, n)))
    sh = {}
    sh["w_ada"] = np.ascontiguousarray(inp["w_ada"][0])
    sh["b_ada_b"] = bc(inp["b_ada"][0], 6 * D)
    sh["g_mix_b"] = bc(inp["g_mix"][0], D)
    sh["g_ffn_b"] = bc(inp["g_ffn"][0], D)
    sh["g_final_b"] = bc(inp["g_final"], D)
    sh["w_in"] = np.ascontiguousarray(inp["w_in"][0])
    sh["lam_b"] = bc(inp["diff_lambda"][0].reshape(-1), 256)
    sh["subln_b"] = bc(inp["diff_subln_g"][0], 128)
    rel_bias = np.asarray(inp["rel_bias"], f32)
    sh["b31"] = bc(rel_bias[31], 16)
    sh["_ext_table"] = np.concatenate([rel_bias, np.full((1, 16), -BIG, f32)], axis=0)
    sh["_bucket"] = _bucket_table()
    sh["w_o_diff"] = np.ascontiguousarray(inp["w_o_diff"][0])
    sh["w_o_moba"] = np.ascontiguousarray(inp["w_o_moba"][0])
    sh["w_out"] = np.ascontiguousarray(inp["w_out"][0])
    sh["w_router"] = np.ascontiguousarray(inp["w_router"][0])
    sh["rbias_b"] = bc(inp["router_bias"][0], E)
    sh["w_exp_gate"] = np.ascontiguousarray(inp["w_exp_gate"][0])
    sh["w_exp_up"] = np.ascontiguousarray(inp["w_exp_up"][0])
    sh["w_exp_down"] = np.ascontiguousarray(inp["w_exp_down"][0])
    sh["w_sh_gate"] = np.ascontiguousarray(inp["w_sh_gate"][0])
    sh["w_sh_up"] = np.ascontiguousarray(inp["w_sh_up"][0])
    sh["w_sh_down"] = np.ascontiguousarray(inp["w_sh_down"][0])
    sh["ident_bf"] = np.eye(128, dtype=f32).astype(ml_dtypes.bfloat16)
    sh["ident_f"] = np.eye(128, dtype=f32)
    tp = np.arange(128)
    sh["ltri"] = (tp[:, None] < tp[None, :]).astype(f32).astype(ml_dtypes.bfloat16)
    sel = np.zeros((32, 32, 128), f32)
    for n in range(32):
        sel[n, n, :] = 1.0
    sh["selc"] = sel.reshape(32, 32 * 128).astype(ml_dtypes.bfloat16)
    sh["ecap"] = bc(np.arange(E, dtype=f32) * CAP, E)
    sh["dumpidx"] = (NSLOT + np.arange(128, dtype=f32)).reshape(128, 1)
    sh["tokid"] = (np.arange(NTO, dtype=np.int32)[None, :] * 128 + np.arange(128, dtype=np.int32)[:, None]).astype(np.int32)
    return sh


_NC_CACHE = {}


def kernel(**inputs):
    inp = {k: np.asarray(v) for k, v in inputs.items()}
    stop_after = int(os.environ.get("MK_STOP", "99"))
    key = (stop_after, DEBUG)
    if key not in _NC_CACHE:
        _NC_CACHE[key] = build_program(stop_after)
    nc = _NC_CACHE[key]
    shared = make_shared(inp)
    in_maps = [make_core_inputs(inp, c, shared) for c in range(NCORES)]
    used = set(USED_INPUTS)
    in_maps = [{k: v for k, v in m.items() if k in used} for m in in_maps]
    res = run_bass_kernel_spmd(nc, in_maps, core_ids=list(range(NCORES)))
    out = np.zeros((2, S, D), np.float32)
    for c in range(NCORES):
        b, r = c // 4, c % 4
        if "out" not in res.results[c]:
            continue
        o = np.asarray(res.results[c]["out"]).reshape(NTO, 128, D)
        ov = out[b].reshape(NTA, 128, D)
        for m in range(NTO):
            ov[4 * m + r] = o[m]
    if DEBUG:
        kernel.last_results = res.results
    return out
```

```python
import os
import contextlib
import numpy as np
import ml_dtypes
import concourse.bass as bass
import concourse.mybir as mybir
from concourse.bass_utils import run_bass_kernel_spmd

F32 = mybir.dt.float32
BF16 = mybir.dt.bfloat16
I32 = mybir.dt.int32
AF = mybir.ActivationFunctionType
ALU = mybir.AluOpType
AX = mybir.AxisListType

D = 2048
KC = 16
S = 8192
NTA = 64
NTO = 16
TO = 2048
E = 64
CAP = 896
JMAX = CAP // 128
NSLOT = E * CAP
BIG = 30000.0
EPS = 1e-6
NCORES = int(os.environ.get("MK_CORES", "8"))
DEBUG = os.environ.get("MK_DEBUG", "")

COMPUTE = ("pe", "act", "dve", "pool")
NDSEM = 6
_SYNC = [None]
USED_INPUTS = set()


class Sync:
    def __init__(self, nc, st):
        self.nc = nc
        self.cnt = {e: 0 for e in COMPUTE}
        self.dcnt = {}
        self.seen = {e: {} for e in ("pe", "act", "dve", "pool", "sp")}
        self.all_tokens = {}
        self.sems = {}
        keys = list(COMPUTE) + ["d_%s_%d" % (q, j) for q in ("sp", "pool", "poolw", "act") for j in range(NDSEM)]
        for k in keys:
            self.sems[k] = st.enter_context(nc.semaphore("s_" + k))


class Phase:
    def __init__(self, nc, name):
        self.nc = nc
        self.name = name
        self.sy = _SYNC[0]
        self.streams = {e: [] for e in ("pe", "act", "dve", "pool", "sp")}
        self.last_w = {}
        self.readers = {}
        self.region = None
        self.cnt_ap = None

    def begin_region(self, expert, thresh):
        self.region = {"expert": expert, "thresh": thresh, "before": dict(self.sy.all_tokens)}

    def end_region(self):
        self.region = None

    def load_cond_reg(self, expert, res):
        t = self.last_w.get(res)
        for eng in self.streams:
            w = self._waits(eng, [t] if t is not None else [])
            self.streams[eng].append((w, ("reg", expert), None, None))

    def _deps(self, reads, writes):
        deps = []
        for r in reads:
            t = self.last_w.get(r)
            if t is not None:
                deps.append(t)
        for w in writes:
            t = self.last_w.get(w)
            if t is not None:
                deps.append(t)
            deps.extend(self.readers.get(w, ()))
        return deps

    def _waits(self, eng, deps):
        out = {}
        seen = self.sy.seen[eng]
        for (k, v) in deps:
            if eng == "pe" and k == "pe":
                continue
            if seen.get(k, 0) >= v:
                continue
            if out.get(k, 0) < v:
                out[k] = v
        for k, v in out.items():
            seen[k] = v
        return list(out.items())

    def _commit(self, tok, reads, writes):
        for r in reads:
            self.readers.setdefault(r, []).append(tok)
        for w in writes:
            self.last_w[w] = tok
            self.readers[w] = []
        k, v = tok
        if self.sy.all_tokens.get(k, 0) < v:
            self.sy.all_tokens[k] = v

    def op(self, eng, fn, reads=(), writes=()):
        deps = self._deps(reads, writes)
        waits = self._waits(eng, deps)
        self.sy.cnt[eng] += 1
        tok = (eng, self.sy.cnt[eng])
        self.streams[eng].append((waits, fn, tok, self.region))
        self._commit(tok, reads, writes)
        return tok

    def dma(self, q, fn, reads=(), writes=(), cls=""):
        deps = self._deps(reads, writes)
        i = self.sy.dcnt.get(q + cls, 0)
        self.sy.dcnt[q + cls] = i + 1
        semkey = "d_%s%s_%d" % (q, cls, i % NDSEM)
        tok = (semkey, 16 * (i // NDSEM + 1))
        if i >= NDSEM:
            deps.append((semkey, 16 * (i // NDSEM)))
        waits = self._waits(q, deps)
        self.streams[q].append((waits, fn, tok, self.region))
        self._commit(tok, reads, writes)
        return tok

    def run(self):
        nc = self.nc
        sems = self.sy.sems
        toks = list(self.sy.all_tokens.items())
        for eng in self.streams:
            w = self._waits(eng, toks)
            if w:
                self.streams[eng].append((w, None, None, None))
        cnt_ap = self.cnt_ap
        with nc.Block() as block:

            def emit(engine, entry):
                waits, fn, tok, _ = entry
                for (k, v) in waits:
                    engine.wait_ge(sems[k], v)
                if fn is None:
                    return
                ins = fn(engine)
                k, v = tok
                ins.then_inc(sems[k], 1 if k in COMPUTE else 16)

            def replay(engine, stream):
                reg = None
                i = 0
                n = len(stream)
                while i < n:
                    entry = stream[i]
                    waits, fn, tok, region = entry
                    if isinstance(fn, tuple):
                        for (k, v) in waits:
                            engine.wait_ge(sems[k], v)
                        if reg is None:
                            reg = engine.alloc_register("cnt_reg")
                        engine.reg_load(reg, cnt_ap[0:1, fn[1]:fn[1] + 1])
                        i += 1
                        continue
                    if region is None:
                        emit(engine, entry)
                        i += 1
                        continue
                    groups = []
                    j = i
                    while j < n and stream[j][3] is not None and stream[j][3]["expert"] == region["expert"]:
                        r_ = stream[j][3]
                        j2 = j
                        while j2 < n and stream[j2][3] is r_:
                            j2 += 1
                        groups.append((r_, stream[j:j2]))
                        j = j2

                    merged = []
                    for (r_, grp) in groups:
                        if merged and merged[-1][0]["thresh"] == r_["thresh"]:
                            merged[-1] = (merged[-1][0], merged[-1][1] + grp)
                        else:
                            merged.append((r_, list(grp)))
                    groups = merged

                    def compensate(rest):
                        before = rest[0][0]["before"]
                        ext = {}
                        incs = {}
                        for (_, grp) in rest:
                            for (w_, f_, t_, _r) in grp:
                                for (k, v) in w_:
                                    v2 = min(v, before.get(k, 0))
                                    if v2 > 0 and ext.get(k, 0) < v2:
                                        ext[k] = v2
                                if t_ is not None:
                                    k, v = t_
                                    incs[k] = incs.get(k, 0) + (1 if k in COMPUTE else 16)
                        for k in incs:
                            b = before.get(k, 0)
                            if b > 0 and ext.get(k, 0) < b:
                                ext[k] = b
                        for k, v in ext.items():
                            engine.wait_ge(sems[k], v)
                        for k, v in incs.items():
                            engine.sem_inc(sems[k], v)

                    def chain(gi_):
                        if gi_ == len(groups):
                            return
                        r_, grp = groups[gi_]
                        with engine.If_lt(reg, r_["thresh"] + 1):
                            compensate(groups[gi_:])
                        with engine.Else():
                            for e_ in grp:
                                emit(engine, e_)
                            chain(gi_ + 1)
                    chain(0)
                    i = j

            @block.tensor
            def _(e):
                replay(e, self.streams["pe"])

            @block.scalar
            def _(e):
                replay(e, self.streams["act"])

            @block.vector
            def _(e):
                replay(e, self.streams["dve"])

            @block.gpsimd
            def _(e):
                replay(e, self.streams["pool"])

            @block.sync
            def _(e):
                replay(e, self.streams["sp"])


def sb(st, nc, name, shape, dt):
    return st.enter_context(nc.sbuf_tensor(name, shape, dt))


def ps(st, nc, name, shape, dt):
    return st.enter_context(nc.psum_tensor(name, shape, dt))


def build_program(stop_after=99):
    nc = bass.Bass("TRN2", target_bir_lowering=False)
    USED_INPUTS.clear()

    def din(name, shape, dt=F32):
        A[name] = nc.dram_tensor(name, list(shape), dt, kind="ExternalInput").ap()

    def dscr(name, shape, dt):
        kind = "ExternalOutput" if name in DEBUG.split(",") else "Internal"
        A[name] = nc.dram_tensor(name, list(shape), dt, kind=kind).ap()

    in_specs = {
        "x_all": ([S, D], F32),
        "x_own": ([TO, D], F32),
        "cT": ([128, KC], F32),
        "w_ada": ([D, 6 * D], F32),
        "b_ada_b": ([128, 6 * D], F32),
        "g_mix_b": ([128, D], F32),
        "g_ffn_b": ([128, D], F32),
        "g_final_b": ([128, D], F32),
        "w_in": ([D, 10240], F32),
        "lam_b": ([128, 256], F32),
        "subln_b": ([128, 128], F32),
        "BT": ([128, 16 * 5 * 128], BF16),
        "b31": ([128, 16], F32),
        "pen": ([128, NTO * 32], F32),
        "past": ([128, NTO * 32], F32),
        "own": ([128, NTO * 32], F32),
        "w_o_diff": ([1024, D], F32),
        "w_o_moba": ([1024, D], F32),
        "w_out": ([D, D], F32),
        "w_router": ([D, E], F32),
        "rbias_b": ([128, E], F32),
        "w_exp_gate": ([E, D, 512], F32),
        "w_exp_up": ([E, D, 512], F32),
        "w_exp_down": ([E, 512, D], F32),
        "w_sh_gate": ([D, 512], F32),
        "w_sh_up": ([D, 512], F32),
        "w_sh_down": ([512, D], F32),
        "ident_bf": ([128, 128], BF16),
        "ident_f": ([128, 128], F32),
        "ltri": ([128, 128], BF16),
        "selc": ([32, 32 * 128], BF16),
        "ecap": ([128, E], F32),
        "dumpidx": ([128, 1], F32),
        "tokid": ([128, NTO], I32),
    }

    class LazyA(dict):
        def __missing__(self, name):
            shape, dt = in_specs[name]
            ap = nc.dram_tensor(name, list(shape), dt, kind="ExternalInput").ap()
            self[name] = ap
            USED_INPUTS.add(name)
            return ap
    A = LazyA()
    A["out"] = nc.dram_tensor("out", [TO, D], F32, kind="ExternalOutput").ap()

    dscr("modb", [128, 6 * D], F32)
    dscr("hT_all", [NTA, 128, KC * 128], BF16)
    dscr("hT_own", [NTO, 128, KC * 128], BF16)
    dscr("QTd", [8, 128, TO], BF16); dscr("KTd", [8, 128, S], BF16); dscr("Vd", [S, 1024], BF16)
    dscr("QTm", [8, 128, TO], BF16); dscr("KTm", [8, 128, S], BF16); dscr("Vm", [S, 1024], BF16)
    dscr("KMT", [8, 128, 32], F32)
    dscr("GT", [32, 128, TO], BF16)
    dscr("o_d", [TO, D], BF16)
    dscr("zT", [4, 128, KC * 512], BF16)
    dscr("x1", [TO, D], F32)
    dscr("h2", [TO, D], BF16)
    dscr("h2T", [NTO, 128, KC * 128], BF16)
    dscr("rowtok", [NSLOT + 128, 8], I32)
    dscr("Ybuf", [NSLOT + 128, D], BF16)
    dscr("Ysh", [TO, D], BF16)
    dscr("route", [128, NTO * 16], F32)
    dscr("cnt_d", [1, E], I32)

    gst = contextlib.ExitStack()
    gst.__enter__()
    _SYNC[0] = Sync(nc, gst)
    if stop_after >= 1:
        phase_mod(nc, A)
    if stop_after >= 2:
        phase_h(nc, A)
    if stop_after >= 3:
        phase_proj(nc, A)
    if stop_after >= 4:
        phase_attn(nc, A)
    if stop_after >= 5:
        phase_oproj(nc, A)
    if stop_after >= 6:
        phase_wout(nc, A)
    if stop_after >= 7:
        phase_route(nc, A)
    if stop_after >= 8:
        phase_experts(nc, A)
    if stop_after >= 9:
        phase_final(nc, A)
    gst.__exit__(None, None, None)
    return nc


def phase_mod(nc, A):
    with contextlib.ExitStack() as st:
        P = Phase(nc, "p1")
        cT = sb(st, nc, "p1_cT", [128, KC], F32)
        cact = sb(st, nc, "p1_cact", [128, KC], F32)
        ones = sb(st, nc, "p1_ones", [128, 128], F32)
        L = sb(st, nc, "p1_L", [128, KC, 128], BF16)
        wt = [sb(st, nc, "p1_w%d" % i, [128, KC, 512], BF16) for i in range(2)]
        bt = [sb(st, nc, "p1_b%d" % i, [128, 512], F32) for i in range(2)]
        gt = [sb(st, nc, "p1_g%d" % i, [128, 512], F32) for i in range(2)]
        ot = [sb(st, nc, "p1_o%d" % i, [128, 512], F32) for i in range(2)]
        pp = [ps(st, nc, "p1_ps%d" % i, [128, 512], F32) for i in range(2)]
        P.dma("sp", lambda e: e.dma_start(out=cT[:], in_=A["cT"][:, :]), writes=["cT"])
        P.op("dve", lambda e: e.memset(ones[:], 1.0), writes=["ones"])
        P.op("act", lambda e: e.activation(out=cact[:], in_=cT[:], func=AF.Silu), reads=["cT"], writes=["cact"])
        for j in range(KC):
            P.op("dve", lambda e, j=j: e.tensor_scalar(out=L[:, j, :], in0=ones[:], scalar1=cact[:, j:j + 1], scalar2=None, op0=ALU.mult),
                 reads=["cact", "ones"], writes=["L"])
        w_view = A["w_ada"].rearrange("(k p) n -> p k n", p=128)
        for n in range(24):
            i = n % 2
            P.dma("pool", lambda e, n=n, i=i: e.dma_start(out=wt[i][:], in_=w_view[:, :, n * 512:(n + 1) * 512]), writes=["w%d" % i])
            P.dma("sp", lambda e, n=n, i=i: e.dma_start(out=bt[i][:], in_=A["b_ada_b"][:, n * 512:(n + 1) * 512]), writes=["b%d" % i])
            kind = n // 4
            if kind in (1, 4):
                gsrc = A["g_mix_b"] if kind == 1 else A["g_ffn_b"]
                c0 = (n % 4) * 512
                P.dma("sp", lambda e, i=i, gsrc=gsrc, c0=c0: e.dma_start(out=gt[i][:], in_=gsrc[:, c0:c0 + 512]), writes=["g%d" % i])

            def mm(e, i=i):
                ins = None
                for k in range(KC):
                    ins = e.matmul(pp[i][:], lhsT=L[:, k, :], rhs=wt[i][:, k, :], start=(k == 0), stop=(k == KC - 1))
                return ins
            P.op("pe", mm, reads=["L", "w%d" % i], writes=["ps%d" % i])
            if kind in (1, 4):
                P.op("dve", lambda e, i=i: e.tensor_tensor(out=ot[i][:], in0=pp[i][:], in1=bt[i][:], op=ALU.add),
                     reads=["ps%d" % i, "b%d" % i], writes=["o%d" % i])
                P.op("dve", lambda e, i=i: e.scalar_tensor_tensor(out=ot[i][:], in0=ot[i][:], scalar=1.0, in1=gt[i][:], op0=ALU.add, op1=ALU.mult),
                     reads=["o%d" % i, "g%d" % i], writes=["o%d" % i])
            else:
                P.op("dve", lambda e, i=i: e.tensor_tensor(out=ot[i][:], in0=pp[i][:], in1=bt[i][:], op=ALU.add),
                     reads=["ps%d" % i, "b%d" % i], writes=["o%d" % i])
            P.dma("sp", lambda e, n=n, i=i: e.dma_start(out=A["modb"][:, n * 512:(n + 1) * 512], in_=ot[i][:]), reads=["o%d" % i], writes=["modb"])
        P.run()


MOD_SHIFT1, MOD_A1, MOD_G1, MOD_SHIFT2, MOD_A2, MOD_G2 = [i * D for i in range(6)]


def emit_norm_mod_T(P, nc, xt, sq, ss, rstd, hf, hb, pT, hT, Amod, Smod, ident, tag, xres, extra_reads=()):
    P.op("act", lambda e: e.activation(out=sq[:], in_=xt[:], func=AF.Square, accum_out=ss[:]),
         reads=[xres], writes=["sq" + tag, "ss" + tag])
    P.op("dve", lambda e: e.tensor_scalar(out=rstd[:], in0=ss[:], scalar1=1.0 / D, scalar2=EPS, op0=ALU.mult, op1=ALU.add),
         reads=["ss" + tag], writes=["rstd" + tag])
    P.op("act", lambda e: e.activation(out=rstd[:], in_=rstd[:], func=AF.Sqrt), reads=["rstd" + tag], writes=["rstd" + tag])
    P.op("dve", lambda e: e.reciprocal(out=rstd[:], in_=rstd[:]), reads=["rstd" + tag], writes=["rstd" + tag])
    P.op("dve", lambda e: e.scalar_tensor_tensor(out=hf[:], in0=xt[:], scalar=rstd[:, 0:1], in1=Amod[:], op0=ALU.mult, op1=ALU.mult),
         reads=[xres, "rstd" + tag, "Amod"] + list(extra_reads), writes=["hf" + tag])
    P.op("dve", lambda e: e.tensor_tensor(out=hb[:], in0=hf[:], in1=Smod[:], op=ALU.add),
         reads=["hf" + tag, "Smod"], writes=["hb" + tag])

    def tr(e):
        ins = None
        for k in range(KC):
            ins = e.transpose(out=pT[:, k, :], in_=hb[:, k * 128:(k + 1) * 128], identity=ident[:])
        return ins
    P.op("pe", tr, reads=["hb" + tag, "ident"], writes=["pT" + tag])
    P.op("act", lambda e: e.activation(out=hT[:], in_=pT[:], func=AF.Copy), reads=["pT" + tag], writes=["hT" + tag])


def phase_h(nc, A):
    with contextlib.ExitStack() as st:
        P = Phase(nc, "p2")
        ident = sb(st, nc, "p2_ident", [128, 128], BF16)
        Amod = sb(st, nc, "p2_A", [128, D], F32)
        Smod = sb(st, nc, "p2_S", [128, D], F32)
        xt = [sb(st, nc, "p2_x%d" % i, [128, D], F32) for i in range(3)]
        sq = [sb(st, nc, "p2_sq%d" % i, [128, D], BF16) for i in range(3)]
        ss = [sb(st, nc, "p2_ss%d" % i, [128, 1], F32) for i in range(3)]
        rstd = [sb(st, nc, "p2_rs%d" % i, [128, 1], F32) for i in range(3)]
        hf = [sb(st, nc, "p2_hf%d" % i, [128, D], F32) for i in range(3)]
        hb = [sb(st, nc, "p2_hb%d" % i, [128, D], BF16) for i in range(3)]
        hT = [sb(st, nc, "p2_hT%d" % i, [128, KC, 128], BF16) for i in range(3)]
        pT = [ps(st, nc, "p2_pT%d" % i, [128, KC, 128], BF16) for i in range(3)]
        P.dma("sp", lambda e: e.dma_start(out=ident[:], in_=A["ident_bf"][:, :]), writes=["ident"])
        P.dma("sp", lambda e: e.dma_start(out=Amod[:], in_=A["modb"][:, MOD_A1:MOD_A1 + D]), writes=["Amod"])
        P.dma("sp", lambda e: e.dma_start(out=Smod[:], in_=A["modb"][:, MOD_SHIFT1:MOD_SHIFT1 + D]), writes=["Smod"])
        for t in range(NTA + NTO):
            i = t % 3
            tag = str(i)
            if t < NTA:
                src = A["x_all"][t * 128:(t + 1) * 128, :]
                dst = A["hT_all"][t]
            else:
                src = A["x_own"][(t - NTA) * 128:(t - NTA + 1) * 128, :]
                dst = A["hT_own"][t - NTA]
            P.dma("sp", lambda e, i=i, src=src: e.dma_start(out=xt[i][:], in_=src), writes=["x" + tag])
            emit_norm_mod_T(P, nc, xt[i], sq[i], ss[i], rstd[i], hf[i], hb[i], pT[i], hT[i], Amod, Smod, ident, tag, "x" + tag)
            P.dma("pool", lambda e, i=i, dst=dst: e.dma_start(out=dst, in_=hT[i][:].rearrange("p k t -> p (k t)")),
                  reads=["hT" + tag], writes=["hTd"])
        P.run()


def phase_proj(nc, A):
    with contextlib.ExitStack() as st:
        P = Phase(nc, "p3")
        W = [sb(st, nc, "p3_W%d" % i, [128, KC, 1024], BF16) for i in range(2)]
        H = [sb(st, nc, "p3_H%d" % i, [128, 4, KC * 128], BF16) for i in range(2)]
        O = [sb(st, nc, "p3_O%d" % i, [128, 8, 512], BF16) for i in range(2)]
        KM = sb(st, nc, "p3_KM", [128, 8, 32], F32)
        pp = [ps(st, nc, "p3_ps%d" % i, [128, 512], F32) for i in range(6)]
        w_view = A["w_in"].rearrange("(k p) n -> p k n", p=128)
        passes = [
            ("qd", 0, "own", "fm"), ("kd", 1024, "all", "fm"), ("vd", 2048, "all", "tm"),
            ("qm", 3072, "own", "fm"), ("km", 4096, "all", "fm"), ("vm", 5120, "all", "tm"),
            ("g0", 6144, "own", "fm"), ("g1", 7168, "own", "fm"), ("g2", 8192, "own", "fm"), ("g3", 9216, "own", "fm"),
        ]
        gcount = 0
        pcount = 0
        if os.environ.get("MK_P3"):
            passes = [passes[int(i)] for i in os.environ["MK_P3"].split(",")]
        for pi, (pname, c0, tset, kind) in enumerate(passes):
            wi = pi % 2
            wres = "W%d" % wi

            def loadW(pj):
                wj = pj % 2
                cj = passes[pj][1]
                for q4 in range(4):
                    P.dma("pool", lambda e, wj=wj, cj=cj, q4=q4: e.dma_start(out=W[wj][:, q4 * 4:(q4 + 1) * 4, :], in_=w_view[:, q4 * 4:(q4 + 1) * 4, cj:cj + 1024]),
                          writes=["W%d" % wj], cls="w")
            if pi == 0:
                loadW(0)
            if pi + 1 < len(passes):
                loadW(pi + 1)
            ngroups = 16 if tset == "all" else 4
            src = A["hT_all"] if tset == "all" else A["hT_own"]
            for g in range(ngroups):
                hi = gcount % 2
                gcount += 1
                hres = "H%d" % hi
                P.dma("sp", lambda e, hi=hi, src=src, g=g: e.dma_start(out=H[hi][:], in_=src[g * 4:(g + 1) * 4].rearrange("t p f -> p t f")),
                      writes=[hres])
                oi = g % 2
                ores = "O%d" % oi
                if kind == "fm":
                    for ch in range(8):
                        pidx = pcount % 6
                        pcount += 1
                        pres = "ps%d" % pidx

                        def mm(e, wi=wi, hi=hi, ch=ch, pidx=pidx):
                            ins = None
                            for k in range(KC):
                                ins = e.matmul(pp[pidx][:].rearrange("p (t q) -> p t q", t=4), lhsT=W[wi][:, k, ch * 128:(ch + 1) * 128],
                                               rhs=H[hi][:, :, k * 128:(k + 1) * 128], start=(k == 0), stop=(k == KC - 1))
                            return ins
                        P.op("pe", mm, reads=[wres, hres], writes=[pres])
                        if pname in ("qd", "qm"):
                            sc = 0.125 if pname == "qd" else float(128 ** -0.5)
                            P.op("act", lambda e, oi=oi, ch=ch, pidx=pidx, sc=sc: e.activation(out=O[oi][:, ch, :], in_=pp[pidx][:], func=AF.Copy, scale=sc),
                                 reads=[pres], writes=[ores])
                        elif pname.startswith("g"):
                            P.op("act", lambda e, oi=oi, ch=ch, pidx=pidx: e.activation(out=O[oi][:, ch, :], in_=pp[pidx][:], func=AF.Sigmoid),
                                 reads=[pres], writes=[ores])
                        elif pname == "km":
                            for bb in range(2):
                                P.op("act", lambda e, oi=oi, ch=ch, pidx=pidx, g=g, bb=bb: e.activation(out=O[oi][:, ch, bb * 256:(bb + 1) * 256], in_=pp[pidx][:, bb * 256:(bb + 1) * 256],
                                                                                                    func=AF.Copy, accum_out=KM[:, ch, 2 * g + bb:2 * g + bb + 1]),
                                     reads=[pres], writes=[ores, "KM"])
                        else:
                            eng = "dve" if ch % 2 == 0 else "act"
                            if eng == "dve":
                                P.op("dve", lambda e, oi=oi, ch=ch, pidx=pidx: e.tensor_copy(out=O[oi][:, ch, :], in_=pp[pidx][:]), reads=[pres], writes=[ores])
                            else:
                                P.op("act", lambda e, oi=oi, ch=ch, pidx=pidx: e.activation(out=O[oi][:, ch, :], in_=pp[pidx][:], func=AF.Copy), reads=[pres], writes=[ores])
                    if pname == "qd":
                        dst = A["QTd"][:, :, g * 512:(g + 1) * 512]
                    elif pname == "kd":
                        dst = A["KTd"][:, :, g * 512:(g + 1) * 512]
                    elif pname == "qm":
                        dst = A["QTm"][:, :, g * 512:(g + 1) * 512]
                    elif pname == "km":
                        dst = A["KTm"][:, :, g * 512:(g + 1) * 512]
                    else:
                        gi = int(pname[1])
                        dst = A["GT"][gi * 8:(gi + 1) * 8, :, g * 512:(g + 1) * 512]
                    P.dma("pool", lambda e, oi=oi, dst=dst: e.dma_start(out=dst.rearrange("c p t -> p c t"), in_=O[oi][:]), reads=[ores], writes=["dram_" + pname])
                else:
                    Ov = O[oi][:].rearrange("p c t -> p (c t)").rearrange("p (t n) -> p t n", t=4)
                    for tt in range(4):
                        for half in range(2):
                            pidx = pcount % 6
                            pcount += 1
                            pres = "ps%d" % pidx

                            def mm(e, wi=wi, hi=hi, tt=tt, half=half, pidx=pidx):
                                ins = None
                                for k in range(KC):
                                    ins = e.matmul(pp[pidx][:], lhsT=H[hi][:, tt, k * 128:(k + 1) * 128], rhs=W[wi][:, k, half * 512:(half + 1) * 512],
                                                   start=(k == 0), stop=(k == KC - 1))
                                return ins
                            P.op("pe", mm, reads=[wres, hres], writes=[pres])
                            if (tt * 2 + half) % 2 == 0:
                                P.op("dve", lambda e, Ov=Ov, tt=tt, half=half, pidx=pidx: e.tensor_copy(out=Ov[:, tt, half * 512:(half + 1) * 512], in_=pp[pidx][:]),
                                     reads=[pres], writes=[ores])
                            else:
                                P.op("act", lambda e, Ov=Ov, tt=tt, half=half, pidx=pidx: e.activation(out=Ov[:, tt, half * 512:(half + 1) * 512], in_=pp[pidx][:], func=AF.Copy),
                                     reads=[pres], writes=[ores])
                    dstT = A["Vd"] if pname == "vd" else A["Vm"]
                    dst = dstT[g * 512:(g + 1) * 512, :].rearrange("(t p) n -> p t n", p=128)
                    P.dma("pool", lambda e, Ov=Ov, dst=dst: e.dma_start(out=dst, in_=Ov), reads=[ores], writes=["dram_" + pname])
            if pname == "km":
                P.op("dve", lambda e: e.tensor_scalar(out=KM[:], in0=KM[:], scalar1=1.0 / 256.0, scalar2=None, op0=ALU.mult), reads=["KM"], writes=["KM"])
                P.dma("sp", lambda e: e.dma_start(out=A["KMT"].rearrange("h p n -> p h n"), in_=KM[:]), reads=["KM"], writes=["dram_KMT"])
        P.run()


def phase_attn(nc, A):
    with contextlib.ExitStack() as st:
        P = Phase(nc, "p4")
        ident = sb(st, nc, "p4_ident", [128, 128], BF16)
        identf = sb(st, nc, "p4_identf", [128, 128], F32)
        BT = sb(st, nc, "p4_BT", [128, 16, 5, 128], BF16)
        b31 = sb(st, nc, "p4_b31", [128, 16], F32)
        selc = sb(st, nc, "p4_sel", [32, 32, 128], BF16)
        pen = sb(st, nc, "p4_pen", [128, NTO, 32], F32)
        past = sb(st, nc, "p4_past", [128, NTO, 32], F32)
        own = sb(st, nc, "p4_own", [128, NTO, 32], F32)
        lamb = sb(st, nc, "p4_lamb", [128, 256], F32)
        lamt = sb(st, nc, "p4_lamt", [128, 128], F32)
        lam2 = sb(st, nc, "p4_lam2", [128, 2], F32)
        nlam = sb(st, nc, "p4_nlam", [128, 1], F32)
        subg = sb(st, nc, "p4_subg", [128, 128], F32)
        KTb = [sb(st, nc, "p4_KT%d" % i, [128, 2 * S], BF16) for i in range(2)]
        QTb = [sb(st, nc, "p4_QT%d" % i, [128, 2 * TO], BF16) for i in range(2)]
        Vb = [sb(st, nc, "p4_V%d" % i, [128, NTA, 130], BF16) for i in range(2)]
        KMb = [sb(st, nc, "p4_KM%d" % i, [128, 32], F32) for i in range(2)]
        QTf = [sb(st, nc, "p4_QTf%d" % i, [128, 128], F32) for i in range(2)]
        Pb = [sb(st, nc, "p4_P%d" % i, [128, 512], BF16) for i in range(6)]
        oh = [sb(st, nc, "p4_oh%d" % i, [128, NTO, 128], BF16) for i in range(2)]
        gate = [sb(st, nc, "p4_gate%d" % i, [128, 32], F32) for i in range(2)]
        top8 = [sb(st, nc, "p4_top8%d" % i, [128, 8], F32) for i in range(2)]
        mb = [sb(st, nc, "p4_mb%d" % i, [128, 32], F32) for i in range(2)]
        mbT = [sb(st, nc, "p4_mbT%d" % i, [32, 128], BF16) for i in range(2)]
        rl = [sb(st, nc, "p4_rl%d" % i, [128, 2], F32) for i in range(2)]
        o1 = [sb(st, nc, "p4_o1%d" % i, [128, 128], F32) for i in range(2)]
        o2 = [sb(st, nc, "p4_o2%d" % i, [128, 128], F32) for i in range(2)]
        junk = [sb(st, nc, "p4_junk%d" % i, [128, 128], F32) for i in range(2)]
        ssq = [sb(st, nc, "p4_ssq%d" % i, [128, 1], F32) for i in range(2)]
        pS = [ps(st, nc, "p4_pS%d" % i, [128, 512], F32) for i in range(4)]
        pO = [ps(st, nc, "p4_pO%d" % i, [128, 512], F32) for i in range(2)]
        pOb = [ps(st, nc, "p4_pOb%d" % i, [128, 512], F32) for i in range(2)]
        pG = pOb[0][:, 0:32]
        pM = pOb[1][0:32, 0:128]

        P.dma("sp", lambda e: e.dma_start(out=ident[:], in_=A["ident_bf"][:, :]), writes=["ident"])
        P.dma("sp", lambda e: e.dma_start(out=identf[:], in_=A["ident_f"][:, :]), writes=["identf"])
        P.dma("sp", lambda e: e.dma_start(out=BT[:].rearrange("p h j q -> p (h j q)"), in_=A["BT"][:, :]), writes=["BT"])
        P.dma("sp", lambda e: e.dma_start(out=b31[:], in_=A["b31"][:, :]), writes=["b31"])
        P.dma("sp", lambda e: e.dma_start(out=selc[:].rearrange("p n k -> p (n k)"), in_=A["selc"][:, :]), writes=["selc"])
        P.dma("sp", lambda e: e.dma_start(out=pen[:].rearrange("p m n -> p (m n)"), in_=A["pen"][:, :]), writes=["pen"])
        P.dma("sp", lambda e: e.dma_start(out=past[:].rearrange("p m n -> p (m n)"), in_=A["past"][:, :]), writes=["past"])
        P.dma("sp", lambda e: e.dma_start(out=own[:].rearrange("p m n -> p (m n)"), in_=A["own"][:, :]), writes=["own"])
        P.dma("sp", lambda e: e.dma_start(out=lamb[:], in_=A["lam_b"][:, :]), writes=["lamb"])
        P.dma("sp", lambda e: e.dma_start(out=subg[:], in_=A["subln_b"][:, :]), writes=["subg"])
        lv = lamb[:].rearrange("p (a d) -> p a d", a=4)
        P.op("dve", lambda e: e.tensor_tensor(out=lamt[:, 0:64], in0=lv[:, 0, :], in1=lv[:, 1, :], op=ALU.mult), reads=["lamb"], writes=["lamt"])
        P.op("dve", lambda e: e.tensor_tensor(out=lamt[:, 64:128], in0=lv[:, 2, :], in1=lv[:, 3, :], op=ALU.mult), reads=["lamb", "lamt"], writes=["lamt"])
        for a_ in range(2):
            P.op("act", lambda e, a_=a_: e.activation(out=lamb[:, a_ * 64:(a_ + 1) * 64], in_=lamt[:, a_ * 64:(a_ + 1) * 64], func=AF.Copy, accum_out=lam2[:, a_:a_ + 1]),
                 reads=["lamt"], writes=["lam2", "lamb"])
        P.op("act", lambda e: e.activation(out=lam2[:], in_=lam2[:], func=AF.Exp), reads=["lam2"], writes=["lam2"])
        P.op("dve", lambda e: e.tensor_tensor(out=nlam[:], in0=lam2[:, 1:2], in1=lam2[:, 0:1], op=ALU.subtract), reads=["lam2"], writes=["nlam"])
        P.op("dve", lambda e: e.tensor_scalar(out=nlam[:], in0=nlam[:], scalar1=-0.2, scalar2=None, op0=ALU.add), reads=["nlam"], writes=["nlam"])
        P.op("dve", lambda e: e.tensor_scalar(out=subg[:], in0=subg[:], scalar1=0.8, scalar2=None, op0=ALU.mult), reads=["subg"], writes=["subg"])
        for i in range(2):
            P.op("pool", lambda e, i=i: e.memset(Vb[i][:, :, 128:130], 1.0), writes=["V%d" % i])

        scount = 0
        pcount = 0
        qcount = 0
        def load_head(h):
            hb_ = h % 2
            diff = h < 8
            hh = h if diff else h - 8
            kres, qres, vres, kmres = "KT%d" % hb_, "QT%d" % hb_, "V%d" % hb_, "KM%d" % hb_
            if diff:
                P.dma("sp", lambda e, hb_=hb_, hh=hh: e.dma_start(out=KTb[hb_][:, 0:S], in_=A["KTd"][hh]), writes=[kres])
                for mp in range(2):
                    P.dma("sp", lambda e, hb_=hb_, hh=hh, mp=mp: e.dma_start(
                        out=QTb[hb_][mp * 64:(mp + 1) * 64, :].rearrange("p (m a q) -> p m a q", m=NTO, a=2)[:, :, mp, :],
                        in_=A["QTd"][hh, mp * 64:(mp + 1) * 64, :].rearrange("p (m q) -> p m q", q=128)), writes=[qres])
                vsrc = A["Vd"]
            else:
                P.dma("sp", lambda e, hb_=hb_, hh=hh: e.dma_start(out=KTb[hb_][:, 0:S], in_=A["KTm"][hh]), writes=[kres])
                P.dma("sp", lambda e, hb_=hb_, hh=hh: e.dma_start(out=QTb[hb_][:, 0:TO], in_=A["QTm"][hh]), writes=[qres])
                P.dma("sp", lambda e, hb_=hb_, hh=hh: e.dma_start(out=KMb[hb_][:], in_=A["KMT"][hh]), writes=[kmres])
                vsrc = A["Vm"]
            P.dma("sp", lambda e, hb_=hb_, hh=hh, vsrc=vsrc: e.dma_start(out=Vb[hb_][:, :, 0:128], in_=vsrc[:, hh * 128:(hh + 1) * 128].rearrange("(t p) d -> p t d", p=128)),
                  writes=[vres])

        for i in range(2):
            P.op("pool", lambda e, i=i: e.memset(QTb[i][:], 0.0), writes=["QT%d" % i])
        load_head(0)
        for h in range(16):
            hb_ = h % 2
            diff = h < 8
            hh = h if diff else h - 8
            kres, qres, vres, kmres = "KT%d" % hb_, "QT%d" % hb_, "V%d" % hb_, "KM%d" % hb_
            if h + 1 < 16:
                load_head(h + 1)
            nmaps = 2 if diff else 1
            ohres = "oh%d" % hb_

            def emit_pre(m, hb_=hb_, kmres=kmres, qres=qres, diff=diff):
                qb = m % 2
                if diff:
                    return
                gres, tres, mres, mtres = "gate%d" % qb, "top8%d" % qb, "mb%d" % qb, "mbT%d" % qb
                P.op("act", lambda e, qb=qb, hb_=hb_, m=m: e.activation(out=QTf[qb][:], in_=QTb[hb_][:, m * 128:(m + 1) * 128], func=AF.Copy), reads=[qres], writes=["QTf%d" % qb])
                P.op("pe", lambda e, qb=qb, hb_=hb_: e.matmul(pG, lhsT=QTf[qb][:], rhs=KMb[hb_][:], start=True, stop=True), reads=["QTf%d" % qb, kmres], writes=["pOb0"])
                P.op("dve", lambda e, qb=qb, m=m: e.tensor_tensor(out=gate[qb][:], in0=pG, in1=pen[:, m, :], op=ALU.add), reads=["pOb0", "pen"], writes=[gres])
                P.op("dve", lambda e, qb=qb: e.max(out=top8[qb][:], in_=gate[qb][:]), reads=[gres], writes=[tres])
                P.op("dve", lambda e, qb=qb: e.tensor_scalar(out=mb[qb][:], in0=gate[qb][:], scalar1=top8[qb][:, 2:3], scalar2=None, op0=ALU.is_ge), reads=[gres, tres], writes=[mres])
                P.op("dve", lambda e, qb=qb, m=m: e.tensor_tensor(out=mb[qb][:], in0=mb[qb][:], in1=past[:, m, :], op=ALU.mult), reads=[mres, "past"], writes=[mres])
                P.op("dve", lambda e, qb=qb, m=m: e.tensor_tensor(out=mb[qb][:], in0=mb[qb][:], in1=own[:, m, :], op=ALU.add), reads=[mres, "own"], writes=[mres])
                P.op("dve", lambda e, qb=qb: e.tensor_scalar(out=mb[qb][:], in0=mb[qb][:], scalar1=-1.0, scalar2=BIG, op0=ALU.add, op1=ALU.mult), reads=[mres], writes=[mres])
                P.op("pe", lambda e, qb=qb: e.transpose(out=pM, in_=mb[qb][:], identity=identf[:]), reads=[mres, "identf"], writes=["pOb1"])
                P.op("dve", lambda e, qb=qb: e.tensor_copy(out=mbT[qb][:], in_=pM), reads=["pOb1"], writes=[mtres])

            def emit_qk(m, g, hb_=hb_, h=h, diff=diff, nmaps=nmaps, kres=kres, qres=qres):
                nonlocal scount
                qb = m % 2
                near_last = (g == m)
                prev_grp = (g == m - 1)
                sidx = []
                if diff:
                    for b2 in range(2):
                        si = scount % 4
                        scount += 1
                        sidx.append(si)

                        def mmqk2(e, si=si, b2=b2, g=g, m=m, hb_=hb_, h=h, near_last=near_last, prev_grp=prev_grp):
                            ins = None
                            for jj in range(2):
                                j4 = 2 * b2 + jj
                                kj = 4 * g + j4
                                bt_j = None
                                if near_last:
                                    bt_j = j4 + 1
                                elif prev_grp and j4 == 3:
                                    bt_j = 0
                                out = pS[si][:, jj * 256:(jj + 1) * 256]
                                ins = e.matmul(out, lhsT=KTb[hb_][:, kj * 128:(kj + 1) * 128], rhs=QTb[hb_][:, m * 256:(m + 1) * 256], start=True, stop=(bt_j is None))
                                if bt_j is not None:
                                    for mp in range(2):
                                        ins = e.matmul(out[:, mp * 128:(mp + 1) * 128], lhsT=ident[:], rhs=BT[:, h, bt_j, :], start=False, stop=True)
                            return ins
                        P.op("pe", mmqk2, reads=[kres, qres, "ident", "BT"], writes=["pS%d" % si])
                    return sidx
                for mp in range(nmaps):
                    si = scount % 4
                    scount += 1
                    sidx.append(si)
                    sres = "pS%d" % si

                    def mmqk(e, si=si, mp=mp, g=g, m=m, hb_=hb_, h=h, diff=diff, near_last=near_last, prev_grp=prev_grp, qb=qb):
                        ins = None
                        for j4 in range(4):
                            kj = 4 * g + j4
                            if diff:
                                lhs = KTb[hb_][0:64, mp * S + kj * 128: mp * S + (kj + 1) * 128]
                                rhs = QTb[hb_][0:64, mp * TO + m * 128: mp * TO + (m + 1) * 128]
                            else:
                                lhs = KTb[hb_][:, kj * 128:(kj + 1) * 128]
                                rhs = QTb[hb_][:, m * 128:(m + 1) * 128]
                            bt_j = None
                            if near_last:
                                bt_j = j4 + 1
                            elif prev_grp and j4 == 3:
                                bt_j = 0
                            last = (bt_j is None) and diff
                            out = pS[si][:, j4 * 128:(j4 + 1) * 128]
                            ins = e.matmul(out, lhsT=lhs, rhs=rhs, start=True, stop=last)
                            if not diff:
                                ins = e.matmul(out, lhsT=selc[:, kj // 2, :], rhs=mbT[qb][:], start=False, stop=(bt_j is None))
                            if bt_j is not None:
                                ins = e.matmul(out, lhsT=ident[:], rhs=BT[:, h, bt_j, :], start=False, stop=True)
                        return ins
                    rd = [kres, qres, "ident", "BT"]
                    if not diff:
                        rd += ["selc", "mbT%d" % qb]
                    P.op("pe", mmqk, reads=rd, writes=[sres])
                return sidx

            def emit_exp_pv(m, g, sidx, hb_=hb_, h=h, diff=diff, nmaps=nmaps, vres=vres):
                nonlocal pcount
                qb = m % 2
                pOres = "pO%d" % qb
                pObres = "pOb%d" % qb
                near_last = (g == m)
                prev_grp = (g == m - 1)
                pidx = []
                if diff:
                    for b2 in range(2):
                        si = sidx[b2]
                        pi = pcount % 6
                        pcount += 1
                        pidx.append(pi)
                        sres, pres = "pS%d" % si, "P%d" % pi
                        if near_last:
                            P.op("act", lambda e, pi=pi, si=si: e.activation(out=Pb[pi][:], in_=pS[si][:], func=AF.Exp), reads=[sres], writes=[pres])
                        elif prev_grp and b2 == 1:
                            P.op("act", lambda e, pi=pi, si=si, h=h: e.activation(out=Pb[pi][:, 0:256], in_=pS[si][:, 0:256], func=AF.Exp, bias=b31[:, h:h + 1]), reads=[sres, "b31"], writes=[pres])
                            P.op("act", lambda e, pi=pi, si=si: e.activation(out=Pb[pi][:, 256:512], in_=pS[si][:, 256:512], func=AF.Exp), reads=[sres], writes=[pres])
                        else:
                            P.op("act", lambda e, pi=pi, si=si, h=h: e.activation(out=Pb[pi][:], in_=pS[si][:], func=AF.Exp, bias=b31[:, h:h + 1]), reads=[sres, "b31"], writes=[pres])
                    for b2 in range(2):
                        pi = pidx[b2]

                        def mmpv2(e, pi=pi, b2=b2, g=g, qb=qb, hb_=hb_, m=m):
                            ins = None
                            for jj in range(2):
                                kj = 4 * g + 2 * b2 + jj
                                for mp in range(2):
                                    ins = e.matmul((pO if mp == 0 else pOb)[qb][:, 0:130], lhsT=Pb[pi][:, jj * 256 + mp * 128: jj * 256 + (mp + 1) * 128], rhs=Vb[hb_][:, kj, :],
                                                   start=(kj == 0), stop=(kj == 4 * m + 3))
                            return ins
                        P.op("pe", mmpv2, reads=["P%d" % pi, vres], writes=[pOres, pObres])
                    return
                for mp in range(nmaps):
                    si = sidx[mp]
                    pi = pcount % 6
                    pcount += 1
                    pidx.append(pi)
                    sres, pres = "pS%d" % si, "P%d" % pi
                    if near_last:
                        P.op("act", lambda e, pi=pi, si=si: e.activation(out=Pb[pi][:], in_=pS[si][:], func=AF.Exp), reads=[sres], writes=[pres])
                    elif prev_grp:
                        P.op("act", lambda e, pi=pi, si=si, h=h: e.activation(out=Pb[pi][:, 0:384], in_=pS[si][:, 0:384], func=AF.Exp, bias=b31[:, h:h + 1]), reads=[sres, "b31"], writes=[pres])
                        P.op("act", lambda e, pi=pi, si=si: e.activation(out=Pb[pi][:, 384:512], in_=pS[si][:, 384:512], func=AF.Exp), reads=[sres], writes=[pres])
                    else:
                        P.op("act", lambda e, pi=pi, si=si, h=h: e.activation(out=Pb[pi][:], in_=pS[si][:], func=AF.Exp, bias=b31[:, h:h + 1]), reads=[sres, "b31"], writes=[pres])
                for mp in range(nmaps):
                    pi = pidx[mp]

                    def mmpv(e, pi=pi, mp=mp, g=g, qb=qb, hb_=hb_, m=m):
                        ins = None
                        for j4 in range(4):
                            kj = 4 * g + j4
                            ins = e.matmul((pO if mp == 0 else pOb)[qb][:, 0:130], lhsT=Pb[pi][:, j4 * 128:(j4 + 1) * 128], rhs=Vb[hb_][:, kj, :],
                                           start=(kj == 0), stop=(kj == 4 * m + 3))
                        return ins
                    P.op("pe", mmpv, reads=["P%d" % pi, vres], writes=[pOres if mp == 0 else pObres])

            def emit_epi(m, hb_=hb_, diff=diff, ohres=ohres):
                qb = m % 2
                pOres = "pO%d" % qb
                pObres = "pOb%d" % qb
                rres, o1res, o2res = "rl%d" % qb, "o1%d" % qb, "o2%d" % qb
                if diff:
                    P.op("dve", lambda e, qb=qb: e.reciprocal(out=rl[qb][:, 0:1], in_=pO[qb][:, 128:129]), reads=[pOres], writes=[rres])
                    P.op("dve", lambda e, qb=qb: e.reciprocal(out=rl[qb][:, 1:2], in_=pOb[qb][:, 128:129]), reads=[pObres, rres], writes=[rres])
                    P.op("dve", lambda e, qb=qb: e.tensor_scalar(out=o1[qb][:], in0=pO[qb][:, 0:128], scalar1=rl[qb][:, 0:1], scalar2=None, op0=ALU.mult), reads=[pOres, rres], writes=[o1res])
                    P.op("dve", lambda e, qb=qb: e.tensor_scalar(out=o2[qb][:], in0=pOb[qb][:, 0:128], scalar1=rl[qb][:, 1:2], scalar2=nlam[:, 0:1], op0=ALU.mult, op1=ALU.mult),
                         reads=[pObres, rres, "nlam"], writes=[o2res])
                    P.op("dve", lambda e, qb=qb: e.tensor_tensor(out=o1[qb][:], in0=o1[qb][:], in1=o2[qb][:], op=ALU.add), reads=[o1res, o2res], writes=[o1res])
                    P.op("act", lambda e, qb=qb: e.activation(out=junk[qb][:], in_=o1[qb][:], func=AF.Square, accum_out=ssq[qb][:]), reads=[o1res], writes=["junk%d" % qb, "ssq%d" % qb])
                    P.op("dve", lambda e, qb=qb: e.tensor_scalar(out=ssq[qb][:], in0=ssq[qb][:], scalar1=1.0 / 128.0, scalar2=EPS, op0=ALU.mult, op1=ALU.add), reads=["ssq%d" % qb], writes=["ssq%d" % qb])
                    P.op("act", lambda e, qb=qb: e.activation(out=ssq[qb][:], in_=ssq[qb][:], func=AF.Sqrt), reads=["ssq%d" % qb], writes=["ssq%d" % qb])
                    P.op("dve", lambda e, qb=qb: e.reciprocal(out=ssq[qb][:], in_=ssq[qb][:]), reads=["ssq%d" % qb], writes=["ssq%d" % qb])
                    P.op("dve", lambda e, qb=qb, hb_=hb_, m=m: e.scalar_tensor_tensor(out=oh[hb_][:, m, :], in0=o1[qb][:], scalar=ssq[qb][:, 0:1], in1=subg[:], op0=ALU.mult, op1=ALU.mult),
                         reads=[o1res, "ssq%d" % qb, "subg"], writes=[ohres])
                else:
                    P.op("dve", lambda e, qb=qb: e.reciprocal(out=rl[qb][:, 0:1], in_=pO[qb][:, 128:129]), reads=[pOres], writes=[rres])
                    P.op("dve", lambda e, qb=qb, hb_=hb_, m=m: e.tensor_scalar(out=oh[hb_][:, m, :], in0=pO[qb][:, 0:128], scalar1=rl[qb][:, 0:1], scalar2=None, op0=ALU.mult),
                         reads=[pOres, rres], writes=[ohres])

            pending = None
            for m in range(NTO):
                for g in range(m + 1):
                    if g == 0:
                        emit_pre(m)
                    sidx = emit_qk(m, g)
                    if pending is not None:
                        emit_exp_pv(*pending)
                        if pending[1] == pending[0]:
                            emit_epi(pending[0])
                    pending = (m, g, sidx)
            emit_exp_pv(*pending)
            emit_epi(pending[0])
            P.dma("sp", lambda e, hb_=hb_, h=h: e.dma_start(out=A["o_d"][:, h * 128:(h + 1) * 128].rearrange("(m p) d -> p m d", p=128), in_=oh[hb_][:]),
                  reads=[ohres], writes=["dram_o"])
        P.run()


def phase_oproj(nc, A):
    with contextlib.ExitStack() as st:
        P = Phase(nc, "p5")
        ident = sb(st, nc, "p5_ident", [128, 128], BF16)
        Wd = sb(st, nc, "p5_Wd", [128, 8, D], BF16)
        Wm = sb(st, nc, "p5_Wm", [128, 8, D], BF16)
        ot = [sb(st, nc, "p5_ot%d" % i, [128, D], BF16) for i in range(2)]
        oT = [sb(st, nc, "p5_oT%d" % i, [128, KC, 512], BF16) for i in range(1)]
        Gd = [sb(st, nc, "p5_Gd%d" % i, [128, KC, 512], BF16) for i in range(1)]
        Gm = [sb(st, nc, "p5_Gm%d" % i, [128, KC, 512], BF16) for i in range(1)]
        zT = [sb(st, nc, "p5_zT%d" % i, [128, KC, 512], BF16) for i in range(1)]
        t1 = [sb(st, nc, "p5_t1%d" % i, [128, 512], F32) for i in range(2)]
        pT = [ps(st, nc, "p5_pT%d" % i, [128, KC, 128], BF16) for i in range(1)]
        pY = [ps(st, nc, "p5_pY%d" % i, [128, 512], F32) for i in range(4)]
        P.dma("sp", lambda e: e.dma_start(out=ident[:], in_=A["ident_bf"][:, :]), writes=["ident"])
        for q4 in range(2):
            P.dma("pool", lambda e, q4=q4: e.dma_start(out=Wd[:, q4 * 4:(q4 + 1) * 4, :], in_=A["w_o_diff"].rearrange("(k p) n -> p k n", p=128)[:, q4 * 4:(q4 + 1) * 4, :]), writes=["Wd"])
            P.dma("pool", lambda e, q4=q4: e.dma_start(out=Wm[:, q4 * 4:(q4 + 1) * 4, :], in_=A["w_o_moba"].rearrange("(k p) n -> p k n", p=128)[:, q4 * 4:(q4 + 1) * 4, :]), writes=["Wm"])
        tcount = 0
        ycount = 0
        for g in range(4):
            gi = 0
            P.dma("sp", lambda e, gi=gi, g=g: e.dma_start(out=Gd[gi][:], in_=A["GT"][0:16, :, g * 512:(g + 1) * 512].rearrange("c p t -> p c t")), writes=["Gd%d" % gi])
            P.dma("sp", lambda e, gi=gi, g=g: e.dma_start(out=Gm[gi][:], in_=A["GT"][16:32, :, g * 512:(g + 1) * 512].rearrange("c p t -> p c t")), writes=["Gm%d" % gi])
            for tt in range(4):
                ti = tcount % 2
                tcount += 1
                tile = g * 4 + tt
                P.dma("sp", lambda e, ti=ti, tile=tile: e.dma_start(out=ot[ti][:], in_=A["o_d"][tile * 128:(tile + 1) * 128, :]), writes=["ot%d" % ti])

                def tr(e, ti=ti):
                    ins = None
                    for k in range(KC):
                        ins = e.transpose(out=pT[0][:, k, :], in_=ot[ti][:, k * 128:(k + 1) * 128], identity=ident[:])
                    return ins
                P.op("pe", tr, reads=["ot%d" % ti, "ident"], writes=["pT"])
                P.op("act", lambda e, gi=gi, tt=tt: e.activation(out=oT[gi][:, :, tt * 128:(tt + 1) * 128], in_=pT[0][:], func=AF.Copy), reads=["pT"], writes=["oT%d" % gi])
            for c in range(KC):
                yd_i = ycount % 4
                ym_i = (ycount + 1) % 4
                ycount += 2
                t1i = c % 2

                def mmd(e, gi=gi, c=c, yd_i=yd_i):
                    ins = None
                    for k in range(8):
                        ins = e.matmul(pY[yd_i][:], lhsT=Wd[:, k, c * 128:(c + 1) * 128], rhs=oT[gi][:, k, :], start=(k == 0), stop=(k == 7))
                    return ins

                def mmm(e, gi=gi, c=c, ym_i=ym_i):
                    ins = None
                    for k in range(8):
                        ins = e.matmul(pY[ym_i][:], lhsT=Wm[:, k, c * 128:(c + 1) * 128], rhs=oT[gi][:, 8 + k, :], start=(k == 0), stop=(k == 7))
                    return ins
                P.op("pe", mmd, reads=["Wd", "oT%d" % gi], writes=["pY%d" % yd_i])
                P.op("pe", mmm, reads=["Wm", "oT%d" % gi], writes=["pY%d" % ym_i])
                P.op("dve", lambda e, gi=gi, c=c, yd_i=yd_i, t1i=t1i: e.tensor_tensor(out=t1[t1i][:], in0=pY[yd_i][:], in1=Gd[gi][:, c, :], op=ALU.mult),
                     reads=["pY%d" % yd_i, "Gd%d" % gi], writes=["t1%d" % t1i])
                P.op("dve", lambda e, gi=gi, c=c, ym_i=ym_i, t1i=t1i: e.tensor_tensor(out=zT[gi][:, c, :], in0=pY[ym_i][:], in1=Gm[gi][:, c, :], op=ALU.mult),
                     reads=["pY%d" % ym_i, "Gm%d" % gi], writes=["zT%d" % gi])
                P.op("pool", lambda e, gi=gi, c=c, t1i=t1i: e.tensor_tensor(out=zT[gi][:, c, :], in0=zT[gi][:, c, :], in1=t1[t1i][:], op=ALU.add),
                     reads=["t1%d" % t1i, "zT%d" % gi], writes=["zT%d" % gi])
            P.dma("sp", lambda e, gi=gi, g=g: e.dma_start(out=A["zT"][g], in_=zT[gi][:].rearrange("p c t -> p (c t)")), reads=["zT%d" % gi], writes=["dram_zT"])
        P.run()


def phase_wout(nc, A):
    with contextlib.ExitStack() as st:
        P = Phase(nc, "p6")
        Wo = sb(st, nc, "p6_Wo", [128, KC, D], BF16)
        G1 = sb(st, nc, "p6_G1", [128, D], F32)
        zT = [sb(st, nc, "p6_zT%d" % i, [128, KC, 512], BF16) for i in range(2)]
        xt = [sb(st, nc, "p6_x%d" % i, [128, D], F32) for i in range(2)]
        pY = [ps(st, nc, "p6_pY%d" % i, [128, 512], F32) for i in range(4)]
        for q4 in range(4):
            P.dma("pool", lambda e, q4=q4: e.dma_start(out=Wo[:, q4 * 4:(q4 + 1) * 4, :], in_=A["w_out"].rearrange("(k p) n -> p k n", p=128)[:, q4 * 4:(q4 + 1) * 4, :]), writes=["Wo"])
        P.dma("sp", lambda e: e.dma_start(out=G1[:], in_=A["modb"][:, MOD_G1:MOD_G1 + D]), writes=["G1"])
        ycount = 0
        tcount = 0
        for g in range(4):
            gi = g % 2
            P.dma("sp", lambda e, gi=gi, g=g: e.dma_start(out=zT[gi][:].rearrange("p c t -> p (c t)"), in_=A["zT"][g]), writes=["zT%d" % gi])
            for tt in range(4):
                ti = tcount % 2
                tcount += 1
                tile = g * 4 + tt
                P.dma("sp", lambda e, ti=ti, tile=tile: e.dma_start(out=xt[ti][:], in_=A["x_own"][tile * 128:(tile + 1) * 128, :]), writes=["x%d" % ti])
                for cg in range(4):
                    yi = ycount % 4
                    ycount += 1

                    def mm(e, gi=gi, tt=tt, cg=cg, yi=yi):
                        ins = None
                        for k in range(KC):
                            ins = e.matmul(pY[yi][:], lhsT=zT[gi][:, k, tt * 128:(tt + 1) * 128], rhs=Wo[:, k, cg * 512:(cg + 1) * 512], start=(k == 0), stop=(k == KC - 1))
                        return ins
                    P.op("pe", mm, reads=["zT%d" % gi, "Wo"], writes=["pY%d" % yi])
                    P.op("dve", lambda e, yi=yi, cg=cg, ti=ti: e.tensor_tensor(out=pY[yi][:], in0=pY[yi][:], in1=G1[:, cg * 512:(cg + 1) * 512], op=ALU.mult),
                         reads=["pY%d" % yi, "G1"], writes=["pY%d" % yi])
                    P.op("dve", lambda e, yi=yi, cg=cg, ti=ti: e.tensor_tensor(out=xt[ti][:, cg * 512:(cg + 1) * 512], in0=pY[yi][:], in1=xt[ti][:, cg * 512:(cg + 1) * 512], op=ALU.add),
                         reads=["pY%d" % yi, "x%d" % ti], writes=["x%d" % ti])
                P.dma("sp", lambda e, ti=ti, tile=tile: e.dma_start(out=A["x1"][tile * 128:(tile + 1) * 128, :], in_=xt[ti][:]), reads=["x%d" % ti], writes=["dram_x1"])
        P.run()


def phase_route(nc, A):
    with contextlib.ExitStack() as st:
        P = Phase(nc, "p7")
        ident = sb(st, nc, "p7_ident", [128, 128], BF16)
        ltri = sb(st, nc, "p7_ltri", [128, 128], BF16)
        onesb = sb(st, nc, "p7_ones", [128, 128], BF16)
        Amod = sb(st, nc, "p7_A", [128, D], F32)
        Smod = sb(st, nc, "p7_S", [128, D], F32)
        Wr = sb(st, nc, "p7_Wr", [128, KC, E], BF16)
        rbias = sb(st, nc, "p7_rbias", [128, E], F32)
        ecap = sb(st, nc, "p7_ecap", [128, E], F32)
        dumpidx = sb(st, nc, "p7_dump", [128, 1], F32)
        tokid = sb(st, nc, "p7_tokid", [128, NTO], I32)
        zero_i = sb(st, nc, "p7_zero", [128, CAP * 8], I32)
        zero_b = sb(st, nc, "p7_zerob", [128, D], BF16)
        xt = [sb(st, nc, "p7_x%d" % i, [128, D], F32) for i in range(2)]
        sq = [sb(st, nc, "p7_sq%d" % i, [128, D], BF16) for i in range(2)]
        ss = [sb(st, nc, "p7_ss%d" % i, [128, 1], F32) for i in range(2)]
        rstd = [sb(st, nc, "p7_rs%d" % i, [128, 1], F32) for i in range(2)]
        hf = [sb(st, nc, "p7_hf%d" % i, [128, D], F32) for i in range(2)]
        hb = [sb(st, nc, "p7_hb%d" % i, [128, D], BF16) for i in range(2)]
        hT = [sb(st, nc, "p7_hT%d" % i, [128, KC, 128], BF16) for i in range(2)]
        emask = sb(st, nc, "p7_emask", [128, NTO, E], BF16)
        scores = [sb(st, nc, "p7_sc%d" % i, [128, E], F32) for i in range(2)]
        selv = [sb(st, nc, "p7_sel%d" % i, [128, E], F32) for i in range(2)]
        g8 = [sb(st, nc, "p7_g8%d" % i, [128, 8], F32) for i in range(2)]
        gsc = [sb(st, nc, "p7_gsc%d" % i, [128, 8], F32) for i in range(2)]
        gm8 = [sb(st, nc, "p7_gm%d" % i, [128, 8], F32) for i in range(2)]
        gmask = [sb(st, nc, "p7_gmask%d" % i, [128, 8], F32) for i in range(2)]
        t8 = [sb(st, nc, "p7_t8%d" % i, [128, 8], F32) for i in range(2)]
        em = [sb(st, nc, "p7_em%d" % i, [128, E], F32) for i in range(2)]
        wt = sb(st, nc, "p7_wt", [128, NTO, E], F32)
        wsum = [sb(st, nc, "p7_ws%d" % i, [128, 1], F32) for i in range(2)]
        key = [sb(st, nc, "p7_key%d" % i, [128, E], F32) for i in range(2)]
        k8 = [sb(st, nc, "p7_k8%d" % i, [128, 8], F32) for i in range(2)]
        oh_ = [sb(st, nc, "p7_oh%d" % i, [128, E], F32) for i in range(2)]
        junk_ = [sb(st, nc, "p7_junk%d" % i, [128, E], F32) for i in range(2)]
        cnt_i = sb(st, nc, "p7_cnt_i", [128, E], I32)
        route = [sb(st, nc, "p7_route%d" % i, [128, 16], F32) for i in range(2)]
        sidx = [sb(st, nc, "p7_sidx%d" % i, [128, 8], I32) for i in range(2)]
        tokrow = [sb(st, nc, "p7_tokrow%d" % i, [128, 8], I32) for i in range(2)]
        valid = [sb(st, nc, "p7_valid%d" % i, [128, 8], F32) for i in range(2)]
        pT = [ps(st, nc, "p7_pT%d" % i, [128, KC, 128], BF16) for i in range(2)]
        pL = [ps(st, nc, "p7_pL%d" % i, [128, E], F32) for i in range(2)]
        pR = [ps(st, nc, "p7_pR%d" % i, [128, E], F32) for i in range(2)]

        P.dma("sp", lambda e: e.dma_start(out=ident[:], in_=A["ident_bf"][:, :]), writes=["ident"])
        P.dma("sp", lambda e: e.dma_start(out=ltri[:], in_=A["ltri"][:, :]), writes=["ltri"])
        P.dma("sp", lambda e: e.dma_start(out=Amod[:], in_=A["modb"][:, MOD_A2:MOD_A2 + D]), writes=["Amod"])
        P.dma("sp", lambda e: e.dma_start(out=Smod[:], in_=A["modb"][:, MOD_SHIFT2:MOD_SHIFT2 + D]), writes=["Smod"])
        P.dma("pool", lambda e: e.dma_start(out=Wr[:], in_=A["w_router"].rearrange("(k p) n -> p k n", p=128)), writes=["Wr"])
        P.dma("sp", lambda e: e.dma_start(out=rbias[:], in_=A["rbias_b"][:, :]), writes=["rbias"])
        P.dma("sp", lambda e: e.dma_start(out=ecap[:], in_=A["ecap"][:, :]), writes=["ecap"])
        P.dma("sp", lambda e: e.dma_start(out=dumpidx[:], in_=A["dumpidx"][:, :]), writes=["dumpidx"])
        P.dma("sp", lambda e: e.dma_start(out=tokid[:], in_=A["tokid"][:, :]), writes=["tokid"])
        P.op("dve", lambda e: e.memset(onesb[:], 1.0), writes=["onesb"])
        P.op("pool", lambda e: e.memset(zero_i[:], 0), writes=["zero_i"])
        P.op("pool", lambda e: e.memset(zero_b[:], 0.0), writes=["zero_b"])
        rt_view = A["rowtok"][0:NSLOT, :].rearrange("(e c) w -> e (c w)", e=E)
        P.dma("sp", lambda e: e.dma_start(out=rt_view, in_=zero_i[0:E, :]), reads=["zero_i"], writes=["dram_rowtok"])
        P.dma("sp", lambda e: e.dma_start(out=A["rowtok"][NSLOT:NSLOT + 128, :], in_=zero_i[:, 0:8]), reads=["zero_i"], writes=["dram_rowtok"])
        P.dma("sp", lambda e: e.dma_start(out=A["Ybuf"][NSLOT:NSLOT + 128, :], in_=zero_b[:]), reads=["zero_b"], writes=["dram_Ybuf"])

        for t in range(NTO):
            i = t % 2
            tag = str(i)
            P.dma("sp", lambda e, i=i, t=t: e.dma_start(out=xt[i][:], in_=A["x1"][t * 128:(t + 1) * 128, :]), writes=["x" + tag])
            emit_norm_mod_T(P, nc, xt[i], sq[i], ss[i], rstd[i], hf[i], hb[i], pT[i], hT[i], Amod, Smod, ident, tag, "x" + tag)
            P.dma("sp", lambda e, i=i, t=t: e.dma_start(out=A["h2"][t * 128:(t + 1) * 128, :], in_=hb[i][:]), reads=["hb" + tag], writes=["dram_h2"])
            P.dma("sp", lambda e, i=i, t=t: e.dma_start(out=A["h2T"][t], in_=hT[i][:].rearrange("p k t -> p (k t)")), reads=["hT" + tag], writes=["dram_h2T"])

            def mml(e, i=i):
                ins = None
                for k in range(KC):
                    ins = e.matmul(pL[i][:], lhsT=hT[i][:, k, :], rhs=Wr[:, k, :], start=(k == 0), stop=(k == KC - 1))
                return ins
            P.op("pe", mml, reads=["hT" + tag, "Wr"], writes=["pL" + tag])
            P.op("act", lambda e, i=i: e.activation(out=scores[i][:], in_=pL[i][:], func=AF.Sigmoid), reads=["pL" + tag], writes=["sc" + tag])
            P.op("dve", lambda e, i=i: e.tensor_tensor(out=selv[i][:], in0=scores[i][:], in1=rbias[:], op=ALU.add), reads=["sc" + tag, "rbias"], writes=["sel" + tag])
            for gq in range(8):
                P.op("dve", lambda e, i=i, gq=gq: e.max(out=g8[i][:], in_=selv[i][:, gq * 8:(gq + 1) * 8]), reads=["sel" + tag, "gsc" + tag], writes=["g8" + tag])
                P.op("dve", lambda e, i=i, gq=gq: e.tensor_tensor(out=gsc[i][:, gq:gq + 1], in0=g8[i][:, 0:1], in1=g8[i][:, 1:2], op=ALU.add), reads=["g8" + tag], writes=["gsc" + tag])
            P.op("dve", lambda e, i=i: e.max(out=gm8[i][:], in_=gsc[i][:]), reads=["gsc" + tag], writes=["gm8" + tag])
            P.op("dve", lambda e, i=i: e.tensor_scalar(out=gmask[i][:], in0=gsc[i][:], scalar1=gm8[i][:, 3:4], scalar2=None, op0=ALU.is_ge), reads=["gsc" + tag, "gm8" + tag], writes=["gmask" + tag])
            for gq in range(8):
                P.op("dve", lambda e, i=i, gq=gq: e.tensor_scalar(out=selv[i][:, gq * 8:(gq + 1) * 8], in0=selv[i][:, gq * 8:(gq + 1) * 8], scalar1=2.0, scalar2=gmask[i][:, gq:gq + 1],
                                                                 op0=ALU.add, op1=ALU.mult), reads=["sel" + tag, "gmask" + tag], writes=["sel" + tag])
            P.op("dve", lambda e, i=i: e.max(out=t8[i][:], in_=selv[i][:]), reads=["sel" + tag], writes=["t8" + tag])
            P.op("dve", lambda e, i=i: e.tensor_scalar(out=em[i][:], in0=selv[i][:], scalar1=t8[i][:, 7:8], scalar2=None, op0=ALU.is_ge), reads=["sel" + tag, "t8" + tag], writes=["em" + tag])
            P.op("dve", lambda e, i=i, t=t: e.tensor_copy(out=emask[:, t, :], in_=em[i][:]), reads=["em" + tag], writes=["emask"])
            P.op("dve", lambda e, i=i, t=t: e.tensor_tensor(out=wt[:, t, :], in0=scores[i][:], in1=em[i][:], op=ALU.mult), reads=["sc" + tag, "em" + tag], writes=["wt"])
            P.op("act", lambda e, i=i, t=t: e.activation(out=junk_[i][:], in_=wt[:, t, :], func=AF.Copy, accum_out=wsum[i][:]), reads=["wt"], writes=["ws" + tag, "junk" + tag])
            P.op("dve", lambda e, i=i: e.reciprocal(out=wsum[i][:], in_=wsum[i][:]), reads=["ws" + tag], writes=["ws" + tag])
            P.op("dve", lambda e, i=i, t=t: e.tensor_scalar(out=wt[:, t, :], in0=wt[:, t, :], scalar1=wsum[i][:, 0:1], scalar2=2.5, op0=ALU.mult, op1=ALU.mult), reads=["wt", "ws" + tag], writes=["wt"])

            def mmr(e, i=i, t=t):
                ins = None
                for j in range(t):
                    ins = e.matmul(pR[i][:], lhsT=onesb[:], rhs=emask[:, j, :], start=(j == 0), stop=False)
                ins = e.matmul(pR[i][:], lhsT=ltri[:], rhs=emask[:, t, :], start=(t == 0), stop=True)
                return ins
            P.op("pe", mmr, reads=["emask", "onesb", "ltri"], writes=["pR" + tag])
            P.op("dve", lambda e, i=i: e.scalar_tensor_tensor(out=key[i][:], in0=pR[i][:], scalar=1.0, in1=ecap[:], op0=ALU.add, op1=ALU.add), reads=["pR" + tag, "ecap"], writes=["key" + tag])
            P.op("dve", lambda e, i=i: e.tensor_scalar(out=oh_[i][:], in0=pR[i][:], scalar1=float(CAP) - 0.5, scalar2=None, op0=ALU.is_lt), reads=["pR" + tag], writes=["oh" + tag])
            P.op("dve", lambda e, i=i: e.tensor_tensor(out=oh_[i][:], in0=oh_[i][:], in1=em[i][:], op=ALU.mult), reads=["oh" + tag, "em" + tag], writes=["oh" + tag])
            P.op("dve", lambda e, i=i: e.tensor_tensor(out=key[i][:], in0=key[i][:], in1=oh_[i][:], op=ALU.mult), reads=["key" + tag, "oh" + tag], writes=["key" + tag])
            P.op("dve", lambda e, i=i: e.max(out=k8[i][:], in_=key[i][:]), reads=["key" + tag], writes=["k8" + tag])
            for kk in range(8):
                P.op("dve", lambda e, i=i, kk=kk: e.tensor_scalar(out=oh_[i][:], in0=key[i][:], scalar1=k8[i][:, kk:kk + 1], scalar2=None, op0=ALU.is_equal), reads=["key" + tag, "k8" + tag, "route" + tag], writes=["oh" + tag])
                P.op("dve", lambda e, i=i, kk=kk, t=t: e.tensor_tensor(out=oh_[i][:], in0=oh_[i][:], in1=wt[:, t, :], op=ALU.mult), reads=["oh" + tag, "wt"], writes=["oh" + tag])
                P.op("act", lambda e, i=i, kk=kk: e.activation(out=junk_[i][:], in_=oh_[i][:], func=AF.Copy, accum_out=route[i][:, 8 + kk:9 + kk]), reads=["oh" + tag], writes=["route" + tag, "junk" + tag])
            P.op("dve", lambda e, i=i: e.tensor_scalar(out=valid[i][:], in0=k8[i][:], scalar1=0.5, scalar2=None, op0=ALU.is_gt), reads=["k8" + tag], writes=["valid" + tag])
            P.op("dve", lambda e, i=i: e.tensor_tensor(out=route[i][:, 8:16], in0=route[i][:, 8:16], in1=valid[i][:], op=ALU.mult), reads=["route" + tag, "valid" + tag], writes=["route" + tag])
            P.op("dve", lambda e, i=i: e.tensor_scalar(out=route[i][:, 0:8], in0=k8[i][:], scalar1=-1.0, scalar2=dumpidx[:, 0:1], op0=ALU.add, op1=ALU.subtract), reads=["k8" + tag, "dumpidx", "route" + tag], writes=["route" + tag])
            P.op("dve", lambda e, i=i: e.tensor_tensor(out=route[i][:, 0:8], in0=route[i][:, 0:8], in1=valid[i][:], op=ALU.mult), reads=["route" + tag, "valid" + tag], writes=["route" + tag])
            P.op("dve", lambda e, i=i: e.tensor_scalar(out=route[i][:, 0:8], in0=route[i][:, 0:8], scalar1=dumpidx[:, 0:1], scalar2=None, op0=ALU.add), reads=["route" + tag, "dumpidx"], writes=["route" + tag])
            P.op("dve", lambda e, i=i: e.tensor_copy(out=sidx[i][:], in_=route[i][:, 0:8]), reads=["route" + tag], writes=["sidx" + tag])
            P.dma("sp", lambda e, i=i, t=t: e.dma_start(out=A["route"][:, t * 16:(t + 1) * 16], in_=route[i][:]), reads=["route" + tag], writes=["dram_route"])
            if t == NTO - 1:
                def mmc(e):
                    ins = None
                    for j in range(NTO):
                        ins = e.matmul(pL[0][:], lhsT=onesb[:], rhs=emask[:, j, :], start=(j == 0), stop=(j == NTO - 1))
                    return ins
                P.op("pe", mmc, reads=["emask", "onesb"], writes=["pL0"])
                P.op("dve", lambda e: e.tensor_copy(out=cnt_i[:], in_=pL[0][:]), reads=["pL0"], writes=["cnt_i"])
                P.dma("sp", lambda e: e.dma_start(out=A["cnt_d"][:, :], in_=cnt_i[0:1, :]), reads=["cnt_i"], writes=["dram_cnt"])
            for kk in range(8):
                P.op("pool", lambda e, i=i, kk=kk, t=t: e.tensor_copy(out=tokrow[i][:, kk:kk + 1], in_=tokid[:, t:t + 1]), reads=["tokid", "tokrow" + tag], writes=["tokrow" + tag])
            for kk in range(8):
                P.dma("pool", lambda e, i=i, kk=kk: e.indirect_dma_start(out=A["rowtok"][:, :], out_offset=bass.IndirectOffsetOnAxis(ap=sidx[i][:, kk:kk + 1], axis=0),
                                                                         in_=tokrow[i][:], in_offset=None),
                      reads=["sidx" + tag, "tokrow" + tag, "dram_rowtok"], writes=["dram_rowtok_s"])
        P.run()


def phase_experts(nc, A):
    NST = 4
    with contextlib.ExitStack() as st:
        P = Phase(nc, "p8")
        ident = sb(st, nc, "p8_ident", [128, 128], BF16)
        cnt_sb = sb(st, nc, "p8_cnt", [1, E], I32)
        P.cnt_ap = cnt_sb
        Wg = [sb(st, nc, "p8_Wg%d" % i, [128, KC, 512], BF16) for i in range(2)]
        Wu = [sb(st, nc, "p8_Wu%d" % i, [128, KC, 512], BF16) for i in range(2)]
        Wd = [sb(st, nc, "p8_Wd%d" % i, [128, 4, D], BF16) for i in range(2)]
        stage = [sb(st, nc, "p8_st%d" % i, [128, 2048], F32) for i in range(NST)]
        idx_all = sb(st, nc, "p8_idxall", [128, E * JMAX, 8], I32)
        xg = [sb(st, nc, "p8_xg%d" % i, [128, D], BF16) for i in range(3)]
        xT = [sb(st, nc, "p8_xT%d" % i, [128, KC, 128], BF16) for i in range(2)]
        sg = [sb(st, nc, "p8_sg%d" % i, [128, 512], F32) for i in range(2)]
        aT = [sb(st, nc, "p8_aT%d" % i, [128, 4, 128], BF16) for i in range(2)]
        Y = [sb(st, nc, "p8_Y%d" % i, [128, D], BF16) for i in range(2)]
        pT = [ps(st, nc, "p8_pT%d" % i, [128, KC, 128], BF16) for i in range(1)]
        pG = [ps(st, nc, "p8_pG%d" % i, [128, 512], F32) for i in range(2)]
        pU = [ps(st, nc, "p8_pU%d" % i, [128, 512], F32) for i in range(2)]
        pY = [ps(st, nc, "p8_pY%d" % i, [128, 512], F32) for i in range(2)]
        P.dma("pool", lambda e: e.dma_start(out=ident[:], in_=A["ident_bf"][:, :]), writes=["ident"])
        P.dma("pool", lambda e: e.dma_start(out=cnt_sb[:], in_=A["cnt_d"][:, :]), writes=["cnt_sb"])
        cn = {"g": 0, "y": 0, "yb": 0, "x": 0, "a": 0, "gu": 0, "st": 0, "ce": 0}

        def wsrc(ei):
            if ei < E:
                return A["w_exp_gate"][ei], A["w_exp_up"][ei], A["w_exp_down"][ei]
            return A["w_sh_gate"], A["w_sh_up"], A["w_sh_down"]

        def weight_chunks(ei):
            wi = ei % 2
            gsrc, usrc, dsrc = wsrc(ei)
            out = []
            for c in range(4):
                out.append((gsrc.rearrange("(k p) n -> p k n", p=128)[:, 4 * c:4 * c + 4, :], Wg[wi][:, 4 * c:4 * c + 4, :], "Wg%d_%d" % (wi, c), (4, 512)))
            for c in range(4):
                out.append((usrc.rearrange("(k p) n -> p k n", p=128)[:, 4 * c:4 * c + 4, :], Wu[wi][:, 4 * c:4 * c + 4, :], "Wu%d_%d" % (wi, c), (4, 512)))
            for c in range(4):
                out.append((dsrc.rearrange("(k p) n -> p k n", p=128)[:, c:c + 1, :], Wd[wi][:, c:c + 1, :], "Wd%d_%d" % (wi, c), (1, 2048)))
            return out

        def issue_chunk_dma(ch):
            src, dst, res, (a, b) = ch
            si = cn["st"] % NST
            cn["st"] += 1
            P.dma("sp", lambda e, si=si, src=src, a=a: e.dma_start(out=stage[si][:].rearrange("p (a b) -> p a b", a=a), in_=src), writes=["st%d" % si])
            return si

        def issue_chunk_cast(ch, si):
            src, dst, res, (a, b) = ch
            eng = "dve"
            view = stage[si][:].rearrange("p (a b) -> p a b", a=a)
            if eng == "act":
                P.op("act", lambda e, dst=dst, view=view: e.activation(out=dst, in_=view, func=AF.Copy), reads=["st%d" % si], writes=[res])
            else:
                P.op(eng, lambda e, dst=dst, view=view: e.tensor_copy(out=dst, in_=view), reads=["st%d" % si], writes=[res])

        def ffn(wi, xi, dst):
            xres = "xT%d" % xi
            ai = cn["a"] % 2
            cn["a"] += 1
            ares = "aT%d" % ai
            gb = cn["gu"] % 2
            cn["gu"] += 1

            def mmg(e, wi=wi, xi=xi, gb=gb):
                ins = None
                for fc in range(4):
                    for k in range(KC):
                        ins = e.matmul(pG[gb][:, fc * 128:(fc + 1) * 128], lhsT=Wg[wi][:, k, fc * 128:(fc + 1) * 128], rhs=xT[xi][:, k, :], start=(k == 0), stop=(k == KC - 1))
                return ins

            def mmu(e, wi=wi, xi=xi, gb=gb):
                ins = None
                for fc in range(4):
                    for k in range(KC):
                        ins = e.matmul(pU[gb][:, fc * 128:(fc + 1) * 128], lhsT=Wu[wi][:, k, fc * 128:(fc + 1) * 128], rhs=xT[xi][:, k, :], start=(k == 0), stop=(k == KC - 1))
                return ins
            P.op("pe", mmg, reads=["Wg%d_%d" % (wi, c) for c in range(4)] + [xres], writes=["pG%d" % gb])
            P.op("pe", mmu, reads=["Wu%d_%d" % (wi, c) for c in range(4)] + [xres], writes=["pU%d" % gb])
            P.op("act", lambda e, gb=gb: e.activation(out=sg[gb][:], in_=pG[gb][:], func=AF.Sigmoid), reads=["pG%d" % gb], writes=["sg%d" % gb])
            P.op("dve", lambda e, gb=gb: e.tensor_tensor(out=sg[gb][:], in0=pG[gb][:], in1=sg[gb][:], op=ALU.mult), reads=["pG%d" % gb, "sg%d" % gb], writes=["sg%d" % gb])
            P.op("dve", lambda e, gb=gb, ai=ai: e.tensor_tensor(out=aT[ai][:], in0=pU[gb][:].rearrange("p (f t) -> p f t", f=4), in1=sg[gb][:].rearrange("p (f t) -> p f t", f=4), op=ALU.mult),
                 reads=["pU%d" % gb, "sg%d" % gb], writes=[ares])
            yi = cn["yb"] % 2
            cn["yb"] += 1
            for cg in range(4):
                pi = cn["y"] % 2
                cn["y"] += 1

                def mmy(e, wi=wi, ai=ai, cg=cg, pi=pi):
                    ins = None
                    for fc in range(4):
                        ins = e.matmul(pY[pi][:], lhsT=aT[ai][:, fc, :], rhs=Wd[wi][:, fc, cg * 512:(cg + 1) * 512], start=(fc == 0), stop=(fc == 3))
                    return ins
                P.op("pe", mmy, reads=[ares] + ["Wd%d_%d" % (wi, c) for c in range(4)], writes=["pY%d" % pi])
                P.op("dve", lambda e, yi=yi, cg=cg, pi=pi: e.tensor_copy(out=Y[yi][:, cg * 512:(cg + 1) * 512], in_=pY[pi][:]), reads=["pY%d" % pi], writes=["Y%d_%d" % (yi, cg)])
            P.dma("act", lambda e, yi=yi, dst=dst: e.dma_start(out=dst, in_=Y[yi][:]), reads=["Y%d_%d" % (yi, c) for c in range(4)], writes=["dram_Y"])

        for ei in range(E):
            P.dma("pool", lambda e, ei=ei: e.dma_start(out=idx_all[:, ei * JMAX:(ei + 1) * JMAX, :], in_=A["rowtok"][ei * CAP:(ei + 1) * CAP, :].rearrange("(t p) w -> p t w", p=128)),
                  writes=["idxall%d" % ei])
        for ch in weight_chunks(0):
            si = issue_chunk_dma(ch)
            issue_chunk_cast(ch, si)
        for ei in range(E):
            wi = ei % 2
            P.load_cond_reg(ei, "cnt_sb")
            nxt = weight_chunks(ei + 1)
            pend = []

            def pump(ncast):
                for _ in range(ncast):
                    if pend:
                        ch, si = pend.pop(0)
                        issue_chunk_cast(ch, si)
                while nxt and len(pend) < NST:
                    ch = nxt.pop(0)
                    pend.append((ch, issue_chunk_dma(ch)))
            pump(0)
            for j in range(JMAX):
                P.begin_region(ei, 128 * j)
                gi = cn["g"] % 3
                cn["g"] += 1
                xi = cn["x"] % 2
                cn["x"] += 1
                s0 = ei * CAP + j * 128
                P.dma("pool", lambda e, ei=ei, j=j, gi=gi: e.indirect_dma_start(out=xg[gi][:], out_offset=None, in_=A["h2"][:, :],
                                                                              in_offset=bass.IndirectOffsetOnAxis(ap=idx_all[:, ei * JMAX + j, 0:1], axis=0)),
                      reads=["idxall%d" % ei], writes=["xg%d" % gi])

                def tr(e, gi=gi):
                    ins = None
                    for k in range(KC):
                        ins = e.transpose(out=pT[0][:, k, :], in_=xg[gi][:, k * 128:(k + 1) * 128], identity=ident[:])
                    return ins
                P.op("pe", tr, reads=["xg%d" % gi, "ident"], writes=["pT"])
                P.op("dve", lambda e, xi=xi: e.tensor_copy(out=xT[xi][:], in_=pT[0][:]), reads=["pT"], writes=["xT%d" % xi])
                P.end_region()
                pump(2)
                P.begin_region(ei, 128 * j)
                ffn(wi, xi, A["Ybuf"][s0:s0 + 128, :])
                P.end_region()
            while pend or nxt:
                pump(2)
        wi = E % 2
        for t in range(NTO):
            xi = cn["x"] % 2
            cn["x"] += 1
            P.dma("pool", lambda e, xi=xi, t=t: e.dma_start(out=xT[xi][:], in_=A["h2T"][t].rearrange("p (k q) -> p k q", k=KC)), writes=["xT%d" % xi])
            ffn(wi, xi, A["Ysh"][t * 128:(t + 1) * 128, :])
        P.run()


def phase_final(nc, A):
    with contextlib.ExitStack() as st:
        P = Phase(nc, "p9")
        G2 = sb(st, nc, "p9_G2", [128, D], F32)
        gf = sb(st, nc, "p9_gf", [128, D], F32)
        xt = [sb(st, nc, "p9_x%d" % i, [128, D], F32) for i in range(2)]
        ysh = [sb(st, nc, "p9_ysh%d" % i, [128, D], BF16) for i in range(2)]
        yk = [sb(st, nc, "p9_yk%d" % i, [128, D], BF16) for i in range(8)]
        acc = [sb(st, nc, "p9_acc%d" % i, [128, D], F32) for i in range(2)]
        route = [sb(st, nc, "p9_route%d" % i, [128, 16], F32) for i in range(2)]
        sidx = [sb(st, nc, "p9_sidx%d" % i, [128, 8], I32) for i in range(2)]
        sq = [sb(st, nc, "p9_sq%d" % i, [128, D], BF16) for i in range(2)]
        ss = [sb(st, nc, "p9_ss%d" % i, [128, 1], F32) for i in range(2)]
        P.dma("sp", lambda e: e.dma_start(out=G2[:], in_=A["modb"][:, MOD_G2:MOD_G2 + D]), writes=["G2"])
        P.dma("sp", lambda e: e.dma_start(out=gf[:], in_=A["g_final_b"][:, :]), writes=["gf"])
        kc = 0
        for t in range(NTO):
            i = t % 2
            tag = str(i)
            P.dma("sp", lambda e, i=i, t=t: e.dma_start(out=xt[i][:], in_=A["x1"][t * 128:(t + 1) * 128, :]), writes=["x" + tag])
            P.dma("sp", lambda e, i=i, t=t: e.dma_start(out=ysh[i][:], in_=A["Ysh"][t * 128:(t + 1) * 128, :]), writes=["ysh" + tag])
            P.dma("sp", lambda e, i=i, t=t: e.dma_start(out=route[i][:], in_=A["route"][:, t * 16:(t + 1) * 16]), writes=["route" + tag])
            P.op("dve", lambda e, i=i: e.tensor_copy(out=sidx[i][:], in_=route[i][:, 0:8]), reads=["route" + tag], writes=["sidx" + tag])
            P.op("dve", lambda e, i=i: e.tensor_copy(out=acc[i][:], in_=ysh[i][:]), reads=["ysh" + tag], writes=["acc" + tag])
            for kk in range(8):
                ki = kc % 8
                kc += 1
                P.dma("pool", lambda e, i=i, kk=kk, ki=ki: e.indirect_dma_start(out=yk[ki][:], out_offset=None, in_=A["Ybuf"][:, :],
                                                                              in_offset=bass.IndirectOffsetOnAxis(ap=sidx[i][:, kk:kk + 1], axis=0)),
                      reads=["sidx" + tag], writes=["yk%d" % ki])
                P.op("dve", lambda e, i=i, kk=kk, ki=ki: e.scalar_tensor_tensor(out=acc[i][:], in0=yk[ki][:], scalar=route[i][:, 8 + kk:9 + kk], in1=acc[i][:], op0=ALU.mult, op1=ALU.add),
                     reads=["yk%d" % ki, "route" + tag, "acc" + tag], writes=["acc" + tag])
            P.op("pool", lambda e, i=i: e.tensor_tensor(out=acc[i][:], in0=acc[i][:], in1=G2[:], op=ALU.mult), reads=["acc" + tag, "G2"], writes=["acc" + tag])
            P.op("pool", lambda e, i=i: e.tensor_tensor(out=xt[i][:], in0=xt[i][:], in1=acc[i][:], op=ALU.add), reads=["acc" + tag, "x" + tag], writes=["x" + tag])
            P.op("act", lambda e, i=i: e.activation(out=sq[i][:], in_=xt[i][:], func=AF.Square, accum_out=ss[i][:]), reads=["x" + tag], writes=["sq" + tag, "ss" + tag])
            P.op("dve", lambda e, i=i: e.tensor_scalar(out=ss[i][:], in0=ss[i][:], scalar1=1.0 / D, scalar2=EPS, op0=ALU.mult, op1=ALU.add), reads=["ss" + tag], writes=["ss" + tag])
            P.op("act", lambda e, i=i: e.activation(out=ss[i][:], in_=ss[i][:], func=AF.Sqrt), reads=["ss" + tag], writes=["ss" + tag])
            P.op("dve", lambda e, i=i: e.reciprocal(out=ss[i][:], in_=ss[i][:]), reads=["ss" + tag], writes=["ss" + tag])
            P.op("dve", lambda e, i=i: e.scalar_tensor_tensor(out=acc[i][:], in0=xt[i][:], scalar=ss[i][:, 0:1], in1=gf[:], op0=ALU.mult, op1=ALU.mult),
                 reads=["x" + tag, "ss" + tag, "gf", "acc" + tag], writes=["acc" + tag])
            P.dma("sp", lambda e, i=i, t=t: e.dma_start(out=A["out"][t * 128:(t + 1) * 128, :], in_=acc[i][:]), reads=["acc" + tag], writes=["dram_out"])
        P.run()


def _t5_bucket_np(rel):
    n = np.maximum(rel, 0)
    nf = np.maximum(n, 1).astype(np.float32)
    large = 16 + (np.log(nf / 16) / np.float32(np.log(128 / 16)) * 16).astype(np.int32)
    large = np.minimum(large, 31)
    return np.where(n < 16, n, large)


def _bucket_table():
    n = np.arange(0, 700, dtype=np.int64)
    nf = np.maximum(n, 1).astype(np.float32)
    large = 16 + (np.log(nf / np.float32(16)) / np.float32(np.log(8.0)) * np.float32(16)).astype(np.int32)
    large = np.minimum(large, 31)
    return np.where(n < 16, n, large)


def make_core_inputs(inp, c, shared):
    b, r = c // 4, c % 4
    f32 = np.float32
    x = inp["x"]
    own_tiles = [4 * m + r for m in range(NTO)]
    xb = np.ascontiguousarray(x[b])
    m_ = dict(shared)
    m_["x_all"] = xb
    m_["x_own"] = np.ascontiguousarray(xb.reshape(NTA, 128, D)[own_tiles].reshape(TO, D))
    m_["cT"] = np.ascontiguousarray(inp["c"][b].reshape(KC, 128).T)
    ext = shared["_ext_table"]
    bidx = np.zeros((128, 5, 128), np.int64)
    kk = np.arange(128)[:, None]
    qq = np.arange(128)[None, :]
    for jj in range(5):
        delta = r - (jj - 1)
        rel = delta * 128 + qq - kk
        bi = shared["_bucket"][np.clip(rel, 0, 699)]
        bidx[:, jj, :] = np.where(rel >= 0, bi, 32)
    BT = ext[bidx]
    m_["BT"] = np.ascontiguousarray(BT.transpose(0, 3, 1, 2).reshape(128, 16 * 5 * 128)).astype(ml_dtypes.bfloat16)
    cur = np.array([(4 * m + r) // 2 for m in range(NTO)])
    n = np.arange(32)[None, :]
    past = (n < cur[:, None])
    ownm = (n == cur[:, None])
    const_tbl = np.array([0.0, 1.0, -BIG], f32)
    m_["past"] = np.ascontiguousarray(np.broadcast_to(const_tbl[past.astype(np.int64)].reshape(1, NTO * 32), (128, NTO * 32)))
    m_["own"] = np.ascontiguousarray(np.broadcast_to(const_tbl[ownm.astype(np.int64)].reshape(1, NTO * 32), (128, NTO * 32)))
    m_["pen"] = np.ascontiguousarray(np.broadcast_to(const_tbl[np.where(past, 0, 2)].reshape(1, NTO * 32), (128, NTO * 32)))
    for k in list(m_.keys()):
        if k.startswith("_"):
            del m_[k]
    return m_


def make_shared(inp):
    f32 = np.float32
    bc = lambda v, n: np.ascontiguousarray(np.broadcast_to(np.asarray(v, f32).reshape(1, n), (128, n)))
    sh = {}
    sh["w_ada"] = np.ascontiguousarray(inp["w_ada"][0])
    sh["b_ada_b"] = bc(inp["b_ada"][0], 6 * D)
    sh["g_mix_b"] = bc(inp["g_mix"][0], D)
    sh["g_ffn_b"] = bc(inp["g_ffn"][0], D)
    sh["g_final_b"] = bc(inp["g_final"], D)
    sh["w_in"] = np.ascontiguousarray(inp["w_in"][0])
    sh["lam_b"] = bc(inp["diff_lambda"][0].reshape(-1), 256)
    sh["subln_b"] = bc(inp["diff_subln_g"][0], 128)
    rel_bias = np.asarray(inp["rel_bias"], f32)
    sh["b31"] = bc(rel_bias[31], 16)
    sh["_ext_table"] = np.concatenate([rel_bias, np.full((1, 16), -BIG, f32)], axis=0)
    sh["_bucket"] = _bucket_table()
    sh["w_o_diff"] = np.ascontiguousarray(inp["w_o_diff"][0])
    sh["w_o_moba"] = np.ascontiguousarray(inp["w_o_moba"][0])
    sh["w_out"] = np.ascontiguousarray(inp["w_out"][0])
    sh["w_router"] = np.ascontiguousarray(inp["w_router"][0])
    sh["rbias_b"] = bc(inp["router_bias"][0], E)
    sh["w_exp_gate"] = np.ascontiguousarray(inp["w_exp_gate"][0])
    sh["w_exp_up"] = np.ascontiguousarray(inp["w_exp_up"][0])
    sh["w_exp_down"] = np.ascontiguousarray(inp["w_exp_down"][0])
    sh["w_sh_gate"] = np.ascontiguousarray(inp["w_sh_gate"][0])
    sh["w_sh_up"] = np.ascontiguousarray(inp["w_sh_up"][0])
    sh["w_sh_down"] = np.ascontiguousarray(inp["w_sh_down"][0])
    sh["ident_bf"] = np.eye(128, dtype=f32).astype(ml_dtypes.bfloat16)
    sh["ident_f"] = np.eye(128, dtype=f32)
    tp = np.arange(128)
    sh["ltri"] = (tp[:, None] < tp[None, :]).astype(f32).astype(ml_dtypes.bfloat16)
    sel = np.zeros((32, 32, 128), f32)
    for n in range(32):
        sel[n, n, :] = 1.0
    sh["selc"] = sel.reshape(32, 32 * 128).astype(ml_dtypes.bfloat16)
    sh["ecap"] = bc(np.arange(E, dtype=f32) * CAP, E)
    sh["dumpidx"] = (NSLOT + np.arange(128, dtype=f32)).reshape(128, 1)
    sh["tokid"] = (np.arange(NTO, dtype=np.int32)[None, :] * 128 + np.arange(128, dtype=np.int32)[:, None]).astype(np.int32)
    return sh


_NC_CACHE = {}


def kernel(**inputs):
    inp = {k: np.asarray(v) for k, v in inputs.items()}
    stop_after = int(os.environ.get("MK_STOP", "99"))
    key = (stop_after, DEBUG)
    if key not in _NC_CACHE:
        _NC_CACHE[key] = build_program(stop_after)
    nc = _NC_CACHE[key]
    shared = make_shared(inp)
    in_maps = [make_core_inputs(inp, c, shared) for c in range(NCORES)]
    used = set(USED_INPUTS)
    in_maps = [{k: v for k, v in m.items() if k in used} for m in in_maps]
    res = run_bass_kernel_spmd(nc, in_maps, core_ids=list(range(NCORES)))
    out = np.zeros((2, S, D), np.float32)
    for c in range(NCORES):
        b, r = c // 4, c % 4
        if "out" not in res.results[c]:
            continue
        o = np.asarray(res.results[c]["out"]).reshape(NTO, 128, D)
        ov = out[b].reshape(NTA, 128, D)
        for m in range(NTO):
            ov[4 * m + r] = o[m]
    if DEBUG:
        kernel.last_results = res.results
    return out
```

```python
import os
import contextlib
import numpy as np
import ml_dtypes
import concourse.bass as bass
import concourse.mybir as mybir
from concourse.bass_utils import run_bass_kernel_spmd

F32 = mybir.dt.float32
BF16 = mybir.dt.bfloat16
I32 = mybir.dt.int32
AF = mybir.ActivationFunctionType
ALU = mybir.AluOpType
AX = mybir.AxisListType

D = 2048
KC = 16
S = 8192
NTA = 64
NTO = 16
TO = 2048
E = 64
CAP = 896
JMAX = CAP // 128
NSLOT = E * CAP
BIG = 30000.0
EPS = 1e-6
NCORES = int(os.environ.get("MK_CORES", "8"))
DEBUG = os.environ.get("MK_DEBUG", "")

COMPUTE = ("pe", "act", "dve", "pool")
NDSEM = 6
_SYNC = [None]
USED_INPUTS = set()


class Sync:
    def __init__(self, nc, st):
        self.nc = nc
        self.cnt = {e: 0 for e in COMPUTE}
        self.dcnt = {}
        self.seen = {e: {} for e in ("pe", "act", "dve", "pool", "sp")}
        self.all_tokens = {}
        self.sems = {}
        keys = list(COMPUTE) + ["d_%s_%d" % (q, j) for q in ("sp", "pool", "poolw", "act") for j in range(NDSEM)]
        for k in keys:
            self.sems[k] = st.enter_context(nc.semaphore("s_" + k))


class Phase:
    def __init__(self, nc, name):
        self.nc = nc
        self.name = name
        self.sy = _SYNC[0]
        self.streams = {e: [] for e in ("pe", "act", "dve", "pool", "sp")}
        self.last_w = {}
        self.readers = {}
        self.region = None
        self.cnt_ap = None

    def begin_region(self, expert, thresh):
        self.region = {"expert": expert, "thresh": thresh, "before": dict(self.sy.all_tokens)}

    def end_region(self):
        self.region = None

    def load_cond_reg(self, expert, res):
        t = self.last_w.get(res)
        for eng in self.streams:
            w = self._waits(eng, [t] if t is not None else [])
            self.streams[eng].append((w, ("reg", expert), None, None))

    def _deps(self, reads, writes):
        deps = []
        for r in reads:
            t = self.last_w.get(r)
            if t is not None:
                deps.append(t)
        for w in writes:
            t = self.last_w.get(w)
            if t is not None:
                deps.append(t)
            deps.extend(self.readers.get(w, ()))
        return deps

    def _waits(self, eng, deps):
        out = {}
        seen = self.sy.seen[eng]
        for (k, v) in deps:
            if eng == "pe" and k == "pe":
                continue
            if seen.get(k, 0) >= v:
                continue
            if out.get(k, 0) < v:
                out[k] = v
        for k, v in out.items():
            seen[k] = v
        return list(out.items())

    def _commit(self, tok, reads, writes):
        for r in reads:
            self.readers.setdefault(r, []).append(tok)
        for w in writes:
            self.last_w[w] = tok
            self.readers[w] = []
        k, v = tok
        if self.sy.all_tokens.get(k, 0) < v:
            self.sy.all_tokens[k] = v

    def op(self, eng, fn, reads=(), writes=()):
        deps = self._deps(reads, writes)
        waits = self._waits(eng, deps)
        self.sy.cnt[eng] += 1
        tok = (eng, self.sy.cnt[eng])
        self.streams[eng].append((waits, fn, tok, self.region))
        self._commit(tok, reads, writes)
        return tok

    def dma(self, q, fn, reads=(), writes=(), cls=""):
        deps = self._deps(reads, writes)
        i = self.sy.dcnt.get(q + cls, 0)
        self.sy.dcnt[q + cls] = i + 1
        semkey = "d_%s%s_%d" % (q, cls, i % NDSEM)
        tok = (semkey, 16 * (i // NDSEM + 1))
        if i >= NDSEM:
            deps.append((semkey, 16 * (i // NDSEM)))
        waits = self._waits(q, deps)
        self.streams[q].append((waits, fn, tok, self.region))
        self._commit(tok, reads, writes)
        return tok

    def run(self):
        nc = self.nc
        sems = self.sy.sems
        toks = list(self.sy.all_tokens.items())
        for eng in self.streams:
            w = self._waits(eng, toks)
            if w:
                self.streams[eng].append((w, None, None, None))
        cnt_ap = self.cnt_ap
        with nc.Block() as block:

            def emit(engine, entry):
                waits, fn, tok, _ = entry
                for (k, v) in waits:
                    engine.wait_ge(sems[k], v)
                if fn is None:
                    return
                ins = fn(engine)
                k, v = tok
                ins.then_inc(sems[k], 1 if k in COMPUTE else 16)

            def replay(engine, stream):
                reg = None
                i = 0
                n = len(stream)
                while i < n:
                    entry = stream[i]
                    waits, fn, tok, region = entry
                    if isinstance(fn, tuple):
                        for (k, v) in waits:
                            engine.wait_ge(sems[k], v)
                        if reg is None:
                            reg = engine.alloc_register("cnt_reg")
                        engine.reg_load(reg, cnt_ap[0:1, fn[1]:fn[1] + 1])
                        i += 1
                        continue
                    if region is None:
                        emit(engine, entry)
                        i += 1
                        continue
                    groups = []
                    j = i
                    while j < n and stream[j][3] is not None and stream[j][3]["expert"] == region["expert"]:
                        r_ = stream[j][3]
                        j2 = j
                        while j2 < n and stream[j2][3] is r_:
                            j2 += 1
                        groups.append((r_, stream[j:j2]))
                        j = j2

                    merged = []
                    for (r_, grp) in groups:
                        if merged and merged[-1][0]["thresh"] == r_["thresh"]:
                            merged[-1] = (merged[-1][0], merged[-1][1] + grp)
                        else:
                            merged.append((r_, list(grp)))
                    groups = merged

                    def compensate(rest):
                        before = rest[0][0]["before"]
                        ext = {}
                        incs = {}
                        for (_, grp) in rest:
                            for (w_, f_, t_, _r) in grp:
                                for (k, v) in w_:
                                    v2 = min(v, before.get(k, 0))
                                    if v2 > 0 and ext.get(k, 0) < v2:
                                        ext[k] = v2
                                if t_ is not None:
                                    k, v = t_
                                    incs[k] = incs.get(k, 0) + (1 if k in COMPUTE else 16)
                        for k in incs:
                            b = before.get(k, 0)
                            if b > 0 and ext.get(k, 0) < b:
                                ext[k] = b
                        for k, v in ext.items():
                            engine.wait_ge(sems[k], v)
                        for k, v in incs.items():
                            engine.sem_inc(sems[k], v)

                    def chain(gi_):
                        if gi_ == len(groups):
                            return
                        r_, grp = groups[gi_]
                        with engine.If_lt(reg, r_["thresh"] + 1):
                            compensate(groups[gi_:])
                        with engine.Else():
                            for e_ in grp:
                                emit(engine, e_)
                            chain(gi_ + 1)
                    chain(0)
                    i = j

            @block.tensor
            def _(e):
                replay(e, self.streams["pe"])

            @block.scalar
            def _(e):
                replay(e, self.streams["act"])

            @block.vector
            def _(e):
                replay(e, self.streams["dve"])

            @block.gpsimd
            def _(e):
                replay(e, self.streams["pool"])

            @block.sync
            def _(e):
                replay(e, self.streams["sp"])


def sb(st, nc, name, shape, dt):
    return st.enter_context(nc.sbuf_tensor(name, shape, dt))


def ps(st, nc, name, shape, dt):
    return st.enter_context(nc.psum_tensor(name, shape, dt))


def build_program(stop_after=99):
    nc = bass.Bass("TRN2", target_bir_lowering=False)
    USED_INPUTS.clear()

    def din(name, shape, dt=F32):
        A[name] = nc.dram_tensor(name, list(shape), dt, kind="ExternalInput").ap()

    def dscr(name, shape, dt):
        kind = "ExternalOutput" if name in DEBUG.split(",") else "Internal"
        A[name] = nc.dram_tensor(name, list(shape), dt, kind=kind).ap()

    in_specs = {
        "x_all": ([S, D], F32),
        "x_own": ([TO, D], F32),
        "cT": ([128, KC], F32),
        "w_ada": ([D, 6 * D], F32),
        "b_ada_b": ([128, 6 * D], F32),
        "g_mix_b": ([128, D], F32),
        "g_ffn_b": ([128, D], F32),
        "g_final_b": ([128, D], F32),
        "w_in": ([D, 10240], F32),
        "lam_b": ([128, 256], F32),
        "subln_b": ([128, 128], F32),
        "BT": ([128, 16 * 5 * 128], BF16),
        "b31": ([128, 16], F32),
        "pen": ([128, NTO * 32], F32),
        "past": ([128, NTO * 32], F32),
        "own": ([128, NTO * 32], F32),
        "w_o_diff": ([1024, D], F32),
        "w_o_moba": ([1024, D], F32),
        "w_out": ([D, D], F32),
        "w_router": ([D, E], F32),
        "rbias_b": ([128, E], F32),
        "w_exp_gate": ([E, D, 512], F32),
        "w_exp_up": ([E, D, 512], F32),
        "w_exp_down": ([E, 512, D], F32),
        "w_sh_gate": ([D, 512], F32),
        "w_sh_up": ([D, 512], F32),
        "w_sh_down": ([512, D], F32),
        "ident_bf": ([128, 128], BF16),
        "ident_f": ([128, 128], F32),
        "ltri": ([128, 128], BF16),
        "selc": ([32, 32 * 128], BF16),
        "ecap": ([128, E], F32),
        "dumpidx": ([128, 1], F32),
        "tokid": ([128, NTO], I32),
    }

    class LazyA(dict):
        def __missing__(self, name):
            shape, dt = in_specs[name]
            ap = nc.dram_tensor(name, list(shape), dt, kind="ExternalInput").ap()
            self[name] = ap
            USED_INPUTS.add(name)
            return ap
    A = LazyA()
    A["out"] = nc.dram_tensor("out", [TO, D], F32, kind="ExternalOutput").ap()

    dscr("modb", [128, 6 * D], F32)
    dscr("hT_all", [NTA, 128, KC * 128], BF16)
    dscr("hT_own", [NTO, 128, KC * 128], BF16)
    dscr("QTd", [8, 128, TO], BF16); dscr("KTd", [8, 128, S], BF16); dscr("Vd", [S, 1024], BF16)
    dscr("QTm", [8, 128, TO], BF16); dscr("KTm", [8, 128, S], BF16); dscr("Vm", [S, 1024], BF16)
    dscr("KMT", [8, 128, 32], F32)
    dscr("GT", [32, 128, TO], BF16)
    dscr("o_d", [TO, D], BF16)
    dscr("zT", [4, 128, KC * 512], BF16)
    dscr("x1", [TO, D], F32)
    dscr("h2", [TO, D], BF16)
    dscr("h2T", [NTO, 128, KC * 128], BF16)
    dscr("rowtok", [NSLOT + 128, 8], I32)
    dscr("Ybuf", [NSLOT + 128, D], BF16)
    dscr("Ysh", [TO, D], BF16)
    dscr("route", [128, NTO * 16], F32)
    dscr("cnt_d", [1, E], I32)

    gst = contextlib.ExitStack()
    gst.__enter__()
    _SYNC[0] = Sync(nc, gst)
    if stop_after >= 1:
        phase_mod(nc, A)
    if stop_after >= 2:
        phase_h(nc, A)
    if stop_after >= 3:
        phase_proj(nc, A)
    if stop_after >= 4:
        phase_attn(nc, A)
    if stop_after >= 5:
        phase_oproj(nc, A)
    if stop_after >= 6:
        phase_wout(nc, A)
    if stop_after >= 7:
        phase_route(nc, A)
    if stop_after >= 8:
        phase_experts(nc, A)
    if stop_after >= 9:
        phase_final(nc, A)
    gst.__exit__(None, None, None)
    return nc


def phase_mod(nc, A):
    with contextlib.ExitStack() as st:
        P = Phase(nc, "p1")
        cT = sb(st, nc, "p1_cT", [128, KC], F32)
        cact = sb(st, nc, "p1_cact", [128, KC], F32)
        ones = sb(st, nc, "p1_ones", [128, 128], F32)
        L = sb(st, nc, "p1_L", [128, KC, 128], BF16)
        wt = [sb(st, nc, "p1_w%d" % i, [128, KC, 512], BF16) for i in range(2)]
        bt = [sb(st, nc, "p1_b%d" % i, [128, 512], F32) for i in range(2)]
        gt = [sb(st, nc, "p1_g%d" % i, [128, 512], F32) for i in range(2)]
        ot = [sb(st, nc, "p1_o%d" % i, [128, 512], F32) for i in range(2)]
        pp = [ps(st, nc, "p1_ps%d" % i, [128, 512], F32) for i in range(2)]
        P.dma("sp", lambda e: e.dma_start(out=cT[:], in_=A["cT"][:, :]), writes=["cT"])
        P.op("dve", lambda e: e.memset(ones[:], 1.0), writes=["ones"])
        P.op("act", lambda e: e.activation(out=cact[:], in_=cT[:], func=AF.Silu), reads=["cT"], writes=["cact"])
        for j in range(KC):
            P.op("dve", lambda e, j=j: e.tensor_scalar(out=L[:, j, :], in0=ones[:], scalar1=cact[:, j:j + 1], scalar2=None, op0=ALU.mult),
                 reads=["cact", "ones"], writes=["L"])
        w_view = A["w_ada"].rearrange("(k p) n -> p k n", p=128)
        for n in range(24):
            i = n % 2
            P.dma("pool", lambda e, n=n, i=i: e.dma_start(out=wt[i][:], in_=w_view[:, :, n * 512:(n + 1) * 512]), writes=["w%d" % i])
            P.dma("sp", lambda e, n=n, i=i: e.dma_start(out=bt[i][:], in_=A["b_ada_b"][:, n * 512:(n + 1) * 512]), writes=["b%d" % i])
            kind = n // 4
            if kind in (1, 4):
                gsrc = A["g_mix_b"] if kind == 1 else A["g_ffn_b"]
                c0 = (n % 4) * 512
                P.dma("sp", lambda e, i=i, gsrc=gsrc, c0=c0: e.dma_start(out=gt[i][:], in_=gsrc[:, c0:c0 + 512]), writes=["g%d" % i])

            def mm(e, i=i):
                ins = None
                for k in range(KC):
                    ins = e.matmul(pp[i][:], lhsT=L[:, k, :], rhs=wt[i][:, k, :], start=(k == 0), stop=(k == KC - 1))
                return ins
            P.op("pe", mm, reads=["L", "w%d" % i], writes=["ps%d" % i])
            if kind in (1, 4):
                P.op("dve", lambda e, i=i: e.tensor_tensor(out=ot[i][:], in0=pp[i][:], in1=bt[i][:], op=ALU.add),
                     reads=["ps%d" % i, "b%d" % i], writes=["o%d" % i])
                P.op("dve", lambda e, i=i: e.scalar_tensor_tensor(out=ot[i][:], in0=ot[i][:], scalar=1.0, in1=gt[i][:], op0=ALU.add, op1=ALU.mult),
                     reads=["o%d" % i, "g%d" % i], writes=["o%d" % i])
            else:
                P.op("dve", lambda e, i=i: e.tensor_tensor(out=ot[i][:], in0=pp[i][:], in1=bt[i][:], op=ALU.add),
                     reads=["ps%d" % i, "b%d" % i], writes=["o%d" % i])
            P.dma("sp", lambda e, n=n, i=i: e.dma_start(out=A["modb"][:, n * 512:(n + 1) * 512], in_=ot[i][:]), reads=["o%d" % i], writes=["modb"])
        P.run()


MOD_SHIFT1, MOD_A1, MOD_G1, MOD_SHIFT2, MOD_A2, MOD_G2 = [i * D for i in range(6)]


def emit_norm_mod_T(P, nc, xt, sq, ss, rstd, hf, hb, pT, hT, Amod, Smod, ident, tag, xres, extra_reads=()):
    P.op("act", lambda e: e.activation(out=sq[:], in_=xt[:], func=AF.Square, accum_out=ss[:]),
         reads=[xres], writes=["sq" + tag, "ss" + tag])
    P.op("dve", lambda e: e.tensor_scalar(out=rstd[:], in0=ss[:], scalar1=1.0 / D, scalar2=EPS, op0=ALU.mult, op1=ALU.add),
         reads=["ss" + tag], writes=["rstd" + tag])
    P.op("act", lambda e: e.activation(out=rstd[:], in_=rstd[:], func=AF.Sqrt), reads=["rstd" + tag], writes=["rstd" + tag])
    P.op("dve", lambda e: e.reciprocal(out=rstd[:], in_=rstd[:]), reads=["rstd" + tag], writes=["rstd" + tag])
    P.op("dve", lambda e: e.scalar_tensor_tensor(out=hf[:], in0=xt[:], scalar=rstd[:, 0:1], in1=Amod[:], op0=ALU.mult, op1=ALU.mult),
         reads=[xres, "rstd" + tag, "Amod"] + list(extra_reads), writes=["hf" + tag])
    P.op("dve", lambda e: e.tensor_tensor(out=hb[:], in0=hf[:], in1=Smod[:], op=ALU.add),
         reads=["hf" + tag, "Smod"], writes=["hb" + tag])

    def tr(e):
        ins = None
        for k in range(KC):
            ins = e.transpose(out=pT[:, k, :], in_=hb[:, k * 128:(k + 1) * 128], identity=ident[:])
        return ins
    P.op("pe", tr, reads=["hb" + tag, "ident"], writes=["pT" + tag])
    P.op("act", lambda e: e.activation(out=hT[:], in_=pT[:], func=AF.Copy), reads=["pT" + tag], writes=["hT" + tag])


def phase_h(nc, A):
    with contextlib.ExitStack() as st:
        P = Phase(nc, "p2")
        ident = sb(st, nc, "p2_ident", [128, 128], BF16)
        Amod = sb(st, nc, "p2_A", [128, D], F32)
        Smod = sb(st, nc, "p2_S", [128, D], F32)
        xt = [sb(st, nc, "p2_x%d" % i, [128, D], F32) for i in range(3)]
        sq = [sb(st, nc, "p2_sq%d" % i, [128, D], BF16) for i in range(3)]
        ss = [sb(st, nc, "p2_ss%d" % i, [128, 1], F32) for i in range(3)]
        rstd = [sb(st, nc, "p2_rs%d" % i, [128, 1], F32) for i in range(3)]
        hf = [sb(st, nc, "p2_hf%d" % i, [128, D], F32) for i in range(3)]
        hb = [sb(st, nc, "p2_hb%d" % i, [128, D], BF16) for i in range(3)]
        hT = [sb(st, nc, "p2_hT%d" % i, [128, KC, 128], BF16) for i in range(3)]
        pT = [ps(st, nc, "p2_pT%d" % i, [128, KC, 128], BF16) for i in range(3)]
        P.dma("sp", lambda e: e.dma_start(out=ident[:], in_=A["ident_bf"][:, :]), writes=["ident"])
        P.dma("sp", lambda e: e.dma_start(out=Amod[:], in_=A["modb"][:, MOD_A1:MOD_A1 + D]), writes=["Amod"])
        P.dma("sp", lambda e: e.dma_start(out=Smod[:], in_=A["modb"][:, MOD_SHIFT1:MOD_SHIFT1 + D]), writes=["Smod"])
        for t in range(NTA + NTO):
            i = t % 3
            tag = str(i)
            if t < NTA:
                src = A["x_all"][t * 128:(t + 1) * 128, :]
                dst = A["hT_all"][t]
            else:
                src = A["x_own"][(t - NTA) * 128:(t - NTA + 1) * 128, :]
                dst = A["hT_own"][t - NTA]
            P.dma("sp", lambda e, i=i, src=src: e.dma_start(out=xt[i][:], in_=src), writes=["x" + tag])
            emit_norm_mod_T(P, nc, xt[i], sq[i], ss[i], rstd[i], hf[i], hb[i], pT[i], hT[i], Amod, Smod, ident, tag, "x" + tag)
            P.dma("pool", lambda e, i=i, dst=dst: e.dma_start(out=dst, in_=hT[i][:].rearrange("p k t -> p (k t)")),
                  reads=["hT" + tag], writes=["hTd"])
        P.run()


def phase_proj(nc, A):
    with contextlib.ExitStack() as st:
        P = Phase(nc, "p3")
        W = [sb(st, nc, "p3_W%d" % i, [128, KC, 1024], BF16) for i in range(2)]
        H = [sb(st, nc, "p3_H%d" % i, [128, 4, KC * 128], BF16) for i in range(2)]
        O = [sb(st, nc, "p3_O%d" % i, [128, 8, 512], BF16) for i in range(2)]
        KM = sb(st, nc, "p3_KM", [128, 8, 32], F32)
        pp = [ps(st, nc, "p3_ps%d" % i, [128, 512], F32) for i in range(6)]
        w_view = A["w_in"].rearrange("(k p) n -> p k n", p=128)
        passes = [
            ("qd", 0, "own", "fm"), ("kd", 1024, "all", "fm"), ("vd", 2048, "all", "tm"),
            ("qm", 3072, "own", "fm"), ("km", 4096, "all", "fm"), ("vm", 5120, "all", "tm"),
            ("g0", 6144, "own", "fm"), ("g1", 7168, "own", "fm"), ("g2", 8192, "own", "fm"), ("g3", 9216, "own", "fm"),
        ]
        gcount = 0
        pcount = 0
        if os.environ.get("MK_P3"):
            passes = [passes[int(i)] for i in os.environ["MK_P3"].split(",")]
        for pi, (pname, c0, tset, kind) in enumerate(passes):
            wi = pi % 2
            wres = "W%d" % wi

            def loadW(pj):
                wj = pj % 2
                cj = passes[pj][1]
                for q4 in range(4):
                    P.dma("pool", lambda e, wj=wj, cj=cj, q4=q4: e.dma_start(out=W[wj][:, q4 * 4:(q4 + 1) * 4, :], in_=w_view[:, q4 * 4:(q4 + 1) * 4, cj:cj + 1024]),
                          writes=["W%d" % wj], cls="w")
            if pi == 0:
                loadW(0)
            if pi + 1 < len(passes):
                loadW(pi + 1)
            ngroups = 16 if tset == "all" else 4
            src = A["hT_all"] if tset == "all" else A["hT_own"]
            for g in range(ngroups):
                hi = gcount % 2
                gcount += 1
                hres = "H%d" % hi
                P.dma("sp", lambda e, hi=hi, src=src, g=g: e.dma_start(out=H[hi][:], in_=src[g * 4:(g + 1) * 4].rearrange("t p f -> p t f")),
                      writes=[hres])
                oi = g % 2
                ores = "O%d" % oi
                if kind == "fm":
                    for ch in range(8):
                        pidx = pcount % 6
                        pcount += 1
                        pres = "ps%d" % pidx

                        def mm(e, wi=wi, hi=hi, ch=ch, pidx=pidx):
                            ins = None
                            for k in range(KC):
                                ins = e.matmul(pp[pidx][:].rearrange("p (t q) -> p t q", t=4), lhsT=W[wi][:, k, ch * 128:(ch + 1) * 128],
                                               rhs=H[hi][:, :, k * 128:(k + 1) * 128], start=(k == 0), stop=(k == KC - 1))
                            return ins
                        P.op("pe", mm, reads=[wres, hres], writes=[pres])
                        if pname in ("qd", "qm"):
                            sc = 0.125 if pname == "qd" else float(128 ** -0.5)
                            P.op("act", lambda e, oi=oi, ch=ch, pidx=pidx, sc=sc: e.activation(out=O[oi][:, ch, :], in_=pp[pidx][:], func=AF.Copy, scale=sc),
                                 reads=[pres], writes=[ores])
                        elif pname.startswith("g"):
                            P.op("act", lambda e, oi=oi, ch=ch, pidx=pidx: e.activation(out=O[oi][:, ch, :], in_=pp[pidx][:], func=AF.Sigmoid),
                                 reads=[pres], writes=[ores])
                        elif pname == "km":
                            for bb in range(2):
                                P.op("act", lambda e, oi=oi, ch=ch, pidx=pidx, g=g, bb=bb: e.activation(out=O[oi][:, ch, bb * 256:(bb + 1) * 256], in_=pp[pidx][:, bb * 256:(bb + 1) * 256],
                                                                                                    func=AF.Copy, accum_out=KM[:, ch, 2 * g + bb:2 * g + bb + 1]),
                                     reads=[pres], writes=[ores, "KM"])
                        else:
                            eng = "dve" if ch % 2 == 0 else "act"
                            if eng == "dve":
                                P.op("dve", lambda e, oi=oi, ch=ch, pidx=pidx: e.tensor_copy(out=O[oi][:, ch, :], in_=pp[pidx][:]), reads=[pres], writes=[ores])
                            else:
                                P.op("act", lambda e, oi=oi, ch=ch, pidx=pidx: e.activation(out=O[oi][:, ch, :], in_=pp[pidx][:], func=AF.Copy), reads=[pres], writes=[ores])
                    if pname == "qd":
                        dst = A["QTd"][:, :, g * 512:(g + 1) * 512]
                    elif pname == "kd":
                        dst = A["KTd"][:, :, g * 512:(g + 1) * 512]
                    elif pname == "qm":
                        dst = A["QTm"][:, :, g * 512:(g + 1) * 512]
                    elif pname == "km":
                        dst = A["KTm"][:, :, g * 512:(g + 1) * 512]
                    else:
                        gi = int(pname[1])
                        dst = A["GT"][gi * 8:(gi + 1) * 8, :, g * 512:(g + 1) * 512]
                    P.dma("pool", lambda e, oi=oi, dst=dst: e.dma_start(out=dst.rearrange("c p t -> p c t"), in_=O[oi][:]), reads=[ores], writes=["dram_" + pname])
                else:
                    Ov = O[oi][:].rearrange("p c t -> p (c t)").rearrange("p (t n) -> p t n", t=4)
                    for tt in range(4):
                        for half in range(2):
                            pidx = pcount % 6
                            pcount += 1
                            pres = "ps%d" % pidx

                            def mm(e, wi=wi, hi=hi, tt=tt, half=half, pidx=pidx):
                                ins = None
                                for k in range(KC):
                                    ins = e.matmul(pp[pidx][:], lhsT=H[hi][:, tt, k * 128:(k + 1) * 128], rhs=W[wi][:, k, half * 512:(half + 1) * 512],
                                                   start=(k == 0), stop=(k == KC - 1))
                                return ins
                            P.op("pe", mm, reads=[wres, hres], writes=[pres])
                            if (tt * 2 + half) % 2 == 0:
                                P.op("dve", lambda e, Ov=Ov, tt=tt, half=half, pidx=pidx: e.tensor_copy(out=Ov[:, tt, half * 512:(half + 1) * 512], in_=pp[pidx][:]),
                                     reads=[pres], writes=[ores])
                            else:
                                P.op("act", lambda e, Ov=Ov, tt=tt, half=half, pidx=pidx: e.activation(out=Ov[:, tt, half * 512:(half + 1) * 512], in_=pp[pidx][:], func=AF.Copy),
                                     reads=[pres], writes=[ores])
                    dstT = A["Vd"] if pname == "vd" else A["Vm"]
                    dst = dstT[g * 512:(g + 1) * 512, :].rearrange("(t p) n -> p t n", p=128)
                    P.dma("pool", lambda e, Ov=Ov, dst=dst: e.dma_start(out=dst, in_=Ov), reads=[ores], writes=["dram_" + pname])
            if pname == "km":
                P.op("dve", lambda e: e.tensor_scalar(out=KM[:], in0=KM[:], scalar1=1.0 / 256.0, scalar2=None, op0=ALU.mult), reads=["KM"], writes=["KM"])
                P.dma("sp", lambda e: e.dma_start(out=A["KMT"].rearrange("h p n -> p h n"), in_=KM[:]), reads=["KM"], writes=["dram_KMT"])
        P.run()


def phase_attn(nc, A):
    with contextlib.ExitStack() as st:
        P = Phase(nc, "p4")
        ident = sb(st, nc, "p4_ident", [128, 128], BF16)
        identf = sb(st, nc, "p4_identf", [128, 128], F32)
        BT = sb(st, nc, "p4_BT", [128, 16, 5, 128], BF16)
        b31 = sb(st, nc, "p4_b31", [128, 16], F32)
        selc = sb(st, nc, "p4_sel", [32, 32, 128], BF16)
        pen = sb(st, nc, "p4_pen", [128, NTO, 32], F32)
        past = sb(st, nc, "p4_past", [128, NTO, 32], F32)
        own = sb(st, nc, "p4_own", [128, NTO, 32], F32)
        lamb = sb(st, nc, "p4_lamb", [128, 256], F32)
        lamt = sb(st, nc, "p4_lamt", [128, 128], F32)
        lam2 = sb(st, nc, "p4_lam2", [128, 2], F32)
        nlam = sb(st, nc, "p4_nlam", [128, 1], F32)
        subg = sb(st, nc, "p4_subg", [128, 128], F32)
        KTb = [sb(st, nc, "p4_KT%d" % i, [128, 2 * S], BF16) for i in range(2)]
        QTb = [sb(st, nc, "p4_QT%d" % i, [128, 2 * TO], BF16) for i in range(2)]
        Vb = [sb(st, nc, "p4_V%d" % i, [128, NTA, 130], BF16) for i in range(2)]
        KMb = [sb(st, nc, "p4_KM%d" % i, [128, 32], F32) for i in range(2)]
        QTf = [sb(st, nc, "p4_QTf%d" % i, [128, 128], F32) for i in range(2)]
        Pb = [sb(st, nc, "p4_P%d" % i, [128, 512], BF16) for i in range(6)]
        oh = [sb(st, nc, "p4_oh%d" % i, [128, NTO, 128], BF16) for i in range(2)]
        gate = [sb(st, nc, "p4_gate%d" % i, [128, 32], F32) for i in range(2)]
        top8 = [sb(st, nc, "p4_top8%d" % i, [128, 8], F32) for i in range(2)]
        mb = [sb(st, nc, "p4_mb%d" % i, [128, 32], F32) for i in range(2)]
        mbT = [sb(st, nc, "p4_mbT%d" % i, [32, 128], BF16) for i in range(2)]
        rl = [sb(st, nc, "p4_rl%d" % i, [128, 2], F32) for i in range(2)]
        o1 = [sb(st, nc, "p4_o1%d" % i, [128, 128], F32) for i in range(2)]
        o2 = [sb(st, nc, "p4_o2%d" % i, [128, 128], F32) for i in range(2)]
        junk = [sb(st, nc, "p4_junk%d" % i, [128, 128], F32) for i in range(2)]
        ssq = [sb(st, nc, "p4_ssq%d" % i, [128, 1], F32) for i in range(2)]
        pS = [ps(st, nc, "p4_pS%d" % i, [128, 512], F32) for i in range(4)]
        pO = [ps(st, nc, "p4_pO%d" % i, [128, 512], F32) for i in range(2)]
        pOb = [ps(st, nc, "p4_pOb%d" % i, [128, 512], F32) for i in range(2)]
        pG = pOb[0][:, 0:32]
        pM = pOb[1][0:32, 0:128]

        P.dma("sp", lambda e: e.dma_start(out=ident[:], in_=A["ident_bf"][:, :]), writes=["ident"])
        P.dma("sp", lambda e: e.dma_start(out=identf[:], in_=A["ident_f"][:, :]), writes=["identf"])
        P.dma("sp", lambda e: e.dma_start(out=BT[:].rearrange("p h j q -> p (h j q)"), in_=A["BT"][:, :]), writes=["BT"])
        P.dma("sp", lambda e: e.dma_start(out=b31[:], in_=A["b31"][:, :]), writes=["b31"])
        P.dma("sp", lambda e: e.dma_start(out=selc[:].rearrange("p n k -> p (n k)"), in_=A["selc"][:, :]), writes=["selc"])
        P.dma("sp", lambda e: e.dma_start(out=pen[:].rearrange("p m n -> p (m n)"), in_=A["pen"][:, :]), writes=["pen"])
        P.dma("sp", lambda e: e.dma_start(out=past[:].rearrange("p m n -> p (m n)"), in_=A["past"][:, :]), writes=["past"])
        P.dma("sp", lambda e: e.dma_start(out=own[:].rearrange("p m n -> p (m n)"), in_=A["own"][:, :]), writes=["own"])
        P.dma("sp", lambda e: e.dma_start(out=lamb[:], in_=A["lam_b"][:, :]), writes=["lamb"])
        P.dma("sp", lambda e: e.dma_start(out=subg[:], in_=A["subln_b"][:, :]), writes=["subg"])
        lv = lamb[:].rearrange("p (a d) -> p a d", a=4)
        P.op("dve", lambda e: e.tensor_tensor(out=lamt[:, 0:64], in0=lv[:, 0, :], in1=lv[:, 1, :], op=ALU.mult), reads=["lamb"], writes=["lamt"])
        P.op("dve", lambda e: e.tensor_tensor(out=lamt[:, 64:128], in0=lv[:, 2, :], in1=lv[:, 3, :], op=ALU.mult), reads=["lamb", "lamt"], writes=["lamt"])
        for a_ in range(2):
            P.op("act", lambda e, a_=a_: e.activation(out=lamb[:, a_ * 64:(a_ + 1) * 64], in_=lamt[:, a_ * 64:(a_ + 1) * 64], func=AF.Copy, accum_out=lam2[:, a_:a_ + 1]),
                 reads=["lamt"], writes=["lam2", "lamb"])
        P.op("act", lambda e: e.activation(out=lam2[:], in_=lam2[:], func=AF.Exp), reads=["lam2"], writes=["lam2"])
        P.op("dve", lambda e: e.tensor_tensor(out=nlam[:], in0=lam2[:, 1:2], in1=lam2[:, 0:1], op=ALU.subtract), reads=["lam2"], writes=["nlam"])
        P.op("dve", lambda e: e.tensor_scalar(out=nlam[:], in0=nlam[:], scalar1=-0.2, scalar2=None, op0=ALU.add), reads=["nlam"], writes=["nlam"])
        P.op("dve", lambda e: e.tensor_scalar(out=subg[:], in0=subg[:], scalar1=0.8, scalar2=None, op0=ALU.mult), reads=["subg"], writes=["subg"])
        for i in range(2):
            P.op("pool", lambda e, i=i: e.memset(Vb[i][:, :, 128:130], 1.0), writes=["V%d" % i])

        scount = 0
        pcount = 0
        qcount = 0
        def load_head(h):
            hb_ = h % 2
            diff = h < 8
            hh = h if diff else h - 8
            kres, qres, vres, kmres = "KT%d" % hb_, "QT%d" % hb_, "V%d" % hb_, "KM%d" % hb_
            if diff:
                P.dma("sp", lambda e, hb_=hb_, hh=hh: e.dma_start(out=KTb[hb_][:, 0:S], in_=A["KTd"][hh]), writes=[kres])
                for mp in range(2):
                    P.dma("sp", lambda e, hb_=hb_, hh=hh, mp=mp: e.dma_start(
                        out=QTb[hb_][mp * 64:(mp + 1) * 64, :].rearrange("p (m a q) -> p m a q", m=NTO, a=2)[:, :, mp, :],
                        in_=A["QTd"][hh, mp * 64:(mp + 1) * 64, :].rearrange("p (m q) -> p m q", q=128)), writes=[qres])
                vsrc = A["Vd"]
            else:
                P.dma("sp", lambda e, hb_=hb_, hh=hh: e.dma_start(out=KTb[hb_][:, 0:S], in_=A["KTm"][hh]), writes=[kres])
                P.dma("sp", lambda e, hb_=hb_, hh=hh: e.dma_start(out=QTb[hb_][:, 0:TO], in_=A["QTm"][hh]), writes=[qres])
                P.dma("sp", lambda e, hb_=hb_, hh=hh: e.dma_start(out=KMb[hb_][:], in_=A["KMT"][hh]), writes=[kmres])
                vsrc = A["Vm"]
            P.dma("sp", lambda e, hb_=hb_, hh=hh, vsrc=vsrc: e.dma_start(out=Vb[hb_][:, :, 0:128], in_=vsrc[:, hh * 128:(hh + 1) * 128].rearrange("(t p) d -> p t d", p=128)),
                  writes=[vres])

        for i in range(2):
            P.op("pool", lambda e, i=i: e.memset(QTb[i][:], 0.0), writes=["QT%d" % i])
        load_head(0)
        for h in range(16):
            hb_ = h % 2
            diff = h < 8
            hh = h if diff else h - 8
            kres, qres, vres, kmres = "KT%d" % hb_, "QT%d" % hb_, "V%d" % hb_, "KM%d" % hb_
            if h + 1 < 16:
                load_head(h + 1)
            nmaps = 2 if diff else 1
            ohres = "oh%d" % hb_

            def emit_pre(m, hb_=hb_, kmres=kmres, qres=qres, diff=diff):
                qb = m % 2
                if diff:
                    return
                gres, tres, mres, mtres = "gate%d" % qb, "top8%d" % qb, "mb%d" % qb, "mbT%d" % qb
                P.op("act", lambda e, qb=qb, hb_=hb_, m=m: e.activation(out=QTf[qb][:], in_=QTb[hb_][:, m * 128:(m + 1) * 128], func=AF.Copy), reads=[qres], writes=["QTf%d" % qb])
                P.op("pe", lambda e, qb=qb, hb_=hb_: e.matmul(pG, lhsT=QTf[qb][:], rhs=KMb[hb_][:], start=True, stop=True), reads=["QTf%d" % qb, kmres], writes=["pOb0"])
                P.op("dve", lambda e, qb=qb, m=m: e.tensor_tensor(out=gate[qb][:], in0=pG, in1=pen[:, m, :], op=ALU.add), reads=["pOb0", "pen"], writes=[gres])
                P.op("dve", lambda e, qb=qb: e.max(out=top8[qb][:], in_=gate[qb][:]), reads=[gres], writes=[tres])
                P.op("dve", lambda e, qb=qb: e.tensor_scalar(out=mb[qb][:], in0=gate[qb][:], scalar1=top8[qb][:, 2:3], scalar2=None, op0=ALU.is_ge), reads=[gres, tres], writes=[mres])
                P.op("dve", lambda e, qb=qb, m=m: e.tensor_tensor(out=mb[qb][:], in0=mb[qb][:], in1=past[:, m, :], op=ALU.mult), reads=[mres, "past"], writes=[mres])
                P.op("dve", lambda e, qb=qb, m=m: e.tensor_tensor(out=mb[qb][:], in0=mb[qb][:], in1=own[:, m, :], op=ALU.add), reads=[mres, "own"], writes=[mres])
                P.op("dve", lambda e, qb=qb: e.tensor_scalar(out=mb[qb][:], in0=mb[qb][:], scalar1=-1.0, scalar2=BIG, op0=ALU.add, op1=ALU.mult), reads=[mres], writes=[mres])
                P.op("pe", lambda e, qb=qb: e.transpose(out=pM, in_=mb[qb][:], identity=identf[:]), reads=[mres, "identf"], writes=["pOb1"])
                P.op("dve", lambda e, qb=qb: e.tensor_copy(out=mbT[qb][:], in_=pM), reads=["pOb1"], writes=[mtres])

            def emit_qk(m, g, hb_=hb_, h=h, diff=diff, nmaps=nmaps, kres=kres, qres=qres):
                nonlocal scount
                qb = m % 2
                near_last = (g == m)
                prev_grp = (g == m - 1)
                sidx = []
                if diff:
                    for b2 in range(2):
                        si = scount % 4
                        scount += 1
                        sidx.append(si)

                        def mmqk2(e, si=si, b2=b2, g=g, m=m, hb_=hb_, h=h, near_last=near_last, prev_grp=prev_grp):
                            ins = None
                            for jj in range(2):
                                j4 = 2 * b2 + jj
                                kj = 4 * g + j4
                                bt_j = None
                                if near_last:
                                    bt_j = j4 + 1
                                elif prev_grp and j4 == 3:
                                    bt_j = 0
                                out = pS[si][:, jj * 256:(jj + 1) * 256]
                                ins = e.matmul(out, lhsT=KTb[hb_][:, kj * 128:(kj + 1) * 128], rhs=QTb[hb_][:, m * 256:(m + 1) * 256], start=True, stop=(bt_j is None))
                                if bt_j is not None:
                                    for mp in range(2):
                                        ins = e.matmul(out[:, mp * 128:(mp + 1) * 128], lhsT=ident[:], rhs=BT[:, h, bt_j, :], start=False, stop=True)
                            return ins
                        P.op("pe", mmqk2, reads=[kres, qres, "ident", "BT"], writes=["pS%d" % si])
                    return sidx
                for mp in range(nmaps):
                    si = scount % 4
                    scount += 1
                    sidx.append(si)
                    sres = "pS%d" % si

                    def mmqk(e, si=si, mp=mp, g=g, m=m, hb_=hb_, h=h, diff=diff, near_last=near_last, prev_grp=prev_grp, qb=qb):
                        ins = None
                        for j4 in range(4):
                            kj = 4 * g + j4
                            if diff:
                                lhs = KTb[hb_][0:64, mp * S + kj * 128: mp * S + (kj + 1) * 128]
                                rhs = QTb[hb_][0:64, mp * TO + m * 128: mp * TO + (m + 1) * 128]
                            else:
                                lhs = KTb[hb_][:, kj * 128:(kj + 1) * 128]
                                rhs = QTb[hb_][:, m * 128:(m + 1) * 128]
                            bt_j = None
                            if near_last:
                                bt_j = j4 + 1
                            elif prev_grp and j4 == 3:
                                bt_j = 0
                            last = (bt_j is None) and diff
                            out = pS[si][:, j4 * 128:(j4 + 1) * 128]
                            ins = e.matmul(out, lhsT=lhs, rhs=rhs, start=True, stop=last)
                            if not diff:
                                ins = e.matmul(out, lhsT=selc[:, kj // 2, :], rhs=mbT[qb][:], start=False, stop=(bt_j is None))
                            if bt_j is not None:
                                ins = e.matmul(out, lhsT=ident[:], rhs=BT[:, h, bt_j, :], start=False, stop=True)
                        return ins
                    rd = [kres, qres, "ident", "BT"]
                    if not diff:
                        rd += ["selc", "mbT%d" % qb]
                    P.op("pe", mmqk, reads=rd, writes=[sres])
                return sidx

            def emit_exp_pv(m, g, sidx, hb_=hb_, h=h, diff=diff, nmaps=nmaps, vres=vres):
                nonlocal pcount
                qb = m % 2
                pOres = "pO%d" % qb
                pObres = "pOb%d" % qb
                near_last = (g == m)
                prev_grp = (g == m - 1)
                pidx = []
                if diff:
                    for b2 in range(2):
                        si = sidx[b2]
                        pi = pcount % 6
                        pcount += 1
                        pidx.append(pi)
                        sres, pres = "pS%d" % si, "P%d" % pi
                        if near_last:
                            P.op("act", lambda e, pi=pi, si=si: e.activation(out=Pb[pi][:], in_=pS[si][:], func=AF.Exp), reads=[sres], writes=[pres])
                        elif prev_grp and b2 == 1:
                            P.op("act", lambda e, pi=pi, si=si, h=h: e.activation(out=Pb[pi][:, 0:256], in_=pS[si][:, 0:256], func=AF.Exp, bias=b31[:, h:h + 1]), reads=[sres, "b31"], writes=[pres])
                            P.op("act", lambda e, pi=pi, si=si: e.activation(out=Pb[pi][:, 256:512], in_=pS[si][:, 256:512], func=AF.Exp), reads=[sres], writes=[pres])
                        else:
                            P.op("act", lambda e, pi=pi, si=si, h=h: e.activation(out=Pb[pi][:], in_=pS[si][:], func=AF.Exp, bias=b31[:, h:h + 1]), reads=[sres, "b31"], writes=[pres])
                    for b2 in range(2):
                        pi = pidx[b2]

                        def mmpv2(e, pi=pi, b2=b2, g=g, qb=qb, hb_=hb_, m=m):
                            ins = None
                            for jj in range(2):
                                kj = 4 * g + 2 * b2 + jj
                                for mp in range(2):
                                    ins = e.matmul((pO if mp == 0 else pOb)[qb][:, 0:130], lhsT=Pb[pi][:, jj * 256 + mp * 128: jj * 256 + (mp + 1) * 128], rhs=Vb[hb_][:, kj, :],
                                                   start=(kj == 0), stop=(kj == 4 * m + 3))
                            return ins
                        P.op("pe", mmpv2, reads=["P%d" % pi, vres], writes=[pOres, pObres])
                    return
                for mp in range(nmaps):
                    si = sidx[mp]
                    pi = pcount % 6
                    pcount += 1
                    pidx.append(pi)
                    sres, pres = "pS%d" % si, "P%d" % pi
                    if near_last:
                        P.op("act", lambda e, pi=pi, si=si: e.activation(out=Pb[pi][:], in_=pS[si][:], func=AF.Exp), reads=[sres], writes=[pres])
                    elif prev_grp:
                        P.op("act", lambda e, pi=pi, si=si, h=h: e.activation(out=Pb[pi][:, 0:384], in_=pS[si][:, 0:384], func=AF.Exp, bias=b31[:, h:h + 1]), reads=[sres, "b31"], writes=[pres])
                        P.op("act", lambda e, pi=pi, si=si: e.activation(out=Pb[pi][:, 384:512], in_=pS[si][:, 384:512], func=AF.Exp), reads=[sres], writes=[pres])
                    else:
                        P.op("act", lambda e, pi=pi, si=si, h=h: e.activation(out=Pb[pi][:], in_=pS[si][:], func=AF.Exp, bias=b31[:, h:h + 1]), reads=[sres, "b31"], writes=[pres])
                for mp in range(nmaps):
                    pi = pidx[mp]

                    def mmpv(e, pi=pi, mp=mp, g=g, qb=qb, hb_=hb_, m=m):
                        ins = None
                        for j4 in range(4):
                            kj = 4 * g + j4
                            ins = e.matmul((pO if mp == 0 else pOb)[qb][:, 0:130], lhsT=Pb[pi][:, j4 * 128:(j4 + 1) * 128], rhs=Vb[hb_][:, kj, :],
                                           start=(kj == 0), stop=(kj == 4 * m + 3))
                        return ins
                    P.op("pe", mmpv, reads=["P%d" % pi, vres], writes=[pOres if mp == 0 else pObres])

            def emit_epi(m, hb_=hb_, diff=diff, ohres=ohres):
                qb = m % 2
                pOres = "pO%d" % qb
                pObres = "pOb%d" % qb
                rres, o1res, o2res = "rl%d" % qb, "o1%d" % qb, "o2%d" % qb
                if diff:
                    P.op("dve", lambda e, qb=qb: e.reciprocal(out=rl[qb][:, 0:1], in_=pO[qb][:, 128:129]), reads=[pOres], writes=[rres])
                    P.op("dve", lambda e, qb=qb: e.reciprocal(out=rl[qb][:, 1:2], in_=pOb[qb][:, 128:129]), reads=[pObres, rres], writes=[rres])
                    P.op("dve", lambda e, qb=qb: e.tensor_scalar(out=o1[qb][:], in0=pO[qb][:, 0:128], scalar1=rl[qb][:, 0:1], scalar2=None, op0=ALU.mult), reads=[pOres, rres], writes=[o1res])
                    P.op("dve", lambda e, qb=qb: e.tensor_scalar(out=o2[qb][:], in0=pOb[qb][:, 0:128], scalar1=rl[qb][:, 1:2], scalar2=nlam[:, 0:1], op0=ALU.mult, op1=ALU.mult),
                         reads=[pObres, rres, "nlam"], writes=[o2res])
                    P.op("dve", lambda e, qb=qb: e.tensor_tensor(out=o1[qb][:], in0=o1[qb][:], in1=o2[qb][:], op=ALU.add), reads=[o1res, o2res], writes=[o1res])
                    P.op("act", lambda e, qb=qb: e.activation(out=junk[qb][:], in_=o1[qb][:], func=AF.Square, accum_out=ssq[qb][:]), reads=[o1res], writes=["junk%d" % qb, "ssq%d" % qb])
                    P.op("dve", lambda e, qb=qb: e.tensor_scalar(out=ssq[qb][:], in0=ssq[qb][:], scalar1=1.0 / 128.0, scalar2=EPS, op0=ALU.mult, op1=ALU.add), reads=["ssq%d" % qb], writes=["ssq%d" % qb])
                    P.op("act", lambda e, qb=qb: e.activation(out=ssq[qb][:], in_=ssq[qb][:], func=AF.Ln), reads=["ssq%d" % qb], writes=["ssq%d" % qb])
                    P.op("act", lambda e, qb=qb: e.activation(out=ssq[qb][:], in_=ssq[qb][:], func=AF.Exp, scale=-0.5), reads=["ssq%d" % qb], writes=["ssq%d" % qb])
                    P.op("dve", lambda e, qb=qb, hb_=hb_, m=m: e.scalar_tensor_tensor(out=oh[hb_][:, m, :], in0=o1[qb][:], scalar=ssq[qb][:, 0:1], in1=subg[:], op0=ALU.mult, op1=ALU.mult),
                         reads=[o1res, "ssq%d" % qb, "subg"], writes=[ohres])
                else:
                    P.op("dve", lambda e, qb=qb: e.reciprocal(out=rl[qb][:, 0:1], in_=pO[qb][:, 128:129]), reads=[pOres], writes=[rres])
                    P.op("dve", lambda e, qb=qb, hb_=hb_, m=m: e.tensor_scalar(out=oh[hb_][:, m, :], in0=pO[qb][:, 0:128], scalar1=rl[qb][:, 0:1], scalar2=None, op0=ALU.mult),
                         reads=[pOres, rres], writes=[ohres])

            pending = None
            for m in range(NTO):
                for g in range(m + 1):
                    if g == 0:
                        emit_pre(m)
                    sidx = emit_qk(m, g)
                    if pending is not None:
                        emit_exp_pv(*pending)
                        if pending[1] == pending[0]:
                            emit_epi(pending[0])
                    pending = (m, g, sidx)
            emit_exp_pv(*pending)
            emit_epi(pending[0])
            P.dma("sp", lambda e, hb_=hb_, h=h: e.dma_start(out=A["o_d"][:, h * 128:(h + 1) * 128].rearrange("(m p) d -> p m d", p=128), in_=oh[hb_][:]),
                  reads=[ohres], writes=["dram_o"])
        P.run()


def phase_oproj(nc, A):
    with contextlib.ExitStack() as st:
        P = Phase(nc, "p5")
        ident = sb(st, nc, "p5_ident", [128, 128], BF16)
        Wd = sb(st, nc, "p5_Wd", [128, 8, D], BF16)
        Wm = sb(st, nc, "p5_Wm", [128, 8, D], BF16)
        ot = [sb(st, nc, "p5_ot%d" % i, [128, D], BF16) for i in range(2)]
        oT = [sb(st, nc, "p5_oT%d" % i, [128, KC, 512], BF16) for i in range(1)]
        Gd = [sb(st, nc, "p5_Gd%d" % i, [128, KC, 512], BF16) for i in range(1)]
        Gm = [sb(st, nc, "p5_Gm%d" % i, [128, KC, 512], BF16) for i in range(1)]
        zT = [sb(st, nc, "p5_zT%d" % i, [128, KC, 512], BF16) for i in range(1)]
        t1 = [sb(st, nc, "p5_t1%d" % i, [128, 512], F32) for i in range(2)]
        pT = [ps(st, nc, "p5_pT%d" % i, [128, KC, 128], BF16) for i in range(1)]
        pY = [ps(st, nc, "p5_pY%d" % i, [128, 512], F32) for i in range(4)]
        P.dma("sp", lambda e: e.dma_start(out=ident[:], in_=A["ident_bf"][:, :]), writes=["ident"])
        for q4 in range(2):
            P.dma("pool", lambda e, q4=q4: e.dma_start(out=Wd[:, q4 * 4:(q4 + 1) * 4, :], in_=A["w_o_diff"].rearrange("(k p) n -> p k n", p=128)[:, q4 * 4:(q4 + 1) * 4, :]), writes=["Wd"])
            P.dma("pool", lambda e, q4=q4: e.dma_start(out=Wm[:, q4 * 4:(q4 + 1) * 4, :], in_=A["w_o_moba"].rearrange("(k p) n -> p k n", p=128)[:, q4 * 4:(q4 + 1) * 4, :]), writes=["Wm"])
        tcount = 0
        ycount = 0
        for g in range(4):
            gi = 0
            P.dma("sp", lambda e, gi=gi, g=g: e.dma_start(out=Gd[gi][:], in_=A["GT"][0:16, :, g * 512:(g + 1) * 512].rearrange("c p t -> p c t")), writes=["Gd%d" % gi])
            P.dma("sp", lambda e, gi=gi, g=g: e.dma_start(out=Gm[gi][:], in_=A["GT"][16:32, :, g * 512:(g + 1) * 512].rearrange("c p t -> p c t")), writes=["Gm%d" % gi])
            for tt in range(4):
                ti = tcount % 2
                tcount += 1
                tile = g * 4 + tt
                P.dma("sp", lambda e, ti=ti, tile=tile: e.dma_start(out=ot[ti][:], in_=A["o_d"][tile * 128:(tile + 1) * 128, :]), writes=["ot%d" % ti])

                def tr(e, ti=ti):
                    ins = None
                    for k in range(KC):
                        ins = e.transpose(out=pT[0][:, k, :], in_=ot[ti][:, k * 128:(k + 1) * 128], identity=ident[:])
                    return ins
                P.op("pe", tr, reads=["ot%d" % ti, "ident"], writes=["pT"])
                P.op("act", lambda e, gi=gi, tt=tt: e.activation(out=oT[gi][:, :, tt * 128:(tt + 1) * 128], in_=pT[0][:], func=AF.Copy), reads=["pT"], writes=["oT%d" % gi])
            for c in range(KC):
                yd_i = ycount % 4
                ym_i = (ycount + 1) % 4
                ycount += 2
                t1i = c % 2

                def mmd(e, gi=gi, c=c, yd_i=yd_i):
                    ins = None
                    for k in range(8):
                        ins = e.matmul(pY[yd_i][:], lhsT=Wd[:, k, c * 128:(c + 1) * 128], rhs=oT[gi][:, k, :], start=(k == 0), stop=(k == 7))
                    return ins

                def mmm(e, gi=gi, c=c, ym_i=ym_i):
                    ins = None
                    for k in range(8):
                        ins = e.matmul(pY[ym_i][:], lhsT=Wm[:, k, c * 128:(c + 1) * 128], rhs=oT[gi][:, 8 + k, :], start=(k == 0), stop=(k == 7))
                    return ins
                P.op("pe", mmd, reads=["Wd", "oT%d" % gi], writes=["pY%d" % yd_i])
                P.op("pe", mmm, reads=["Wm", "oT%d" % gi], writes=["pY%d" % ym_i])
                P.op("dve", lambda e, gi=gi, c=c, yd_i=yd_i, t1i=t1i: e.tensor_tensor(out=t1[t1i][:], in0=pY[yd_i][:], in1=Gd[gi][:, c, :], op=ALU.mult),
                     reads=["pY%d" % yd_i, "Gd%d" % gi], writes=["t1%d" % t1i])
                P.op("dve", lambda e, gi=gi, c=c, ym_i=ym_i, t1i=t1i: e.tensor_tensor(out=zT[gi][:, c, :], in0=pY[ym_i][:], in1=Gm[gi][:, c, :], op=ALU.mult),
                     reads=["pY%d" % ym_i, "Gm%d" % gi], writes=["zT%d" % gi])
                P.op("pool", lambda e, gi=gi, c=c, t1i=t1i: e.tensor_tensor(out=zT[gi][:, c, :], in0=zT[gi][:, c, :], in1=t1[t1i][:], op=ALU.add),
                     reads=["t1%d" % t1i, "zT%d" % gi], writes=["zT%d" % gi])
            P.dma("sp", lambda e, gi=gi, g=g: e.dma_start(out=A["zT"][g], in_=zT[gi][:].rearrange("p c t -> p (c t)")), reads=["zT%d" % gi], writes=["dram_zT"])
        P.run()


def phase_wout(nc, A):
    with contextlib.ExitStack() as st:
        P = Phase(nc, "p6")
        Wo = sb(st, nc, "p6_Wo", [128, KC, D], BF16)
        G1 = sb(st, nc, "p6_G1", [128, D], F32)
        zT = [sb(st, nc, "p6_zT%d" % i, [128, KC, 512], BF16) for i in range(2)]
        xt = [sb(st, nc, "p6_x%d" % i, [128, D], F32) for i in range(2)]
        pY = [ps(st, nc, "p6_pY%d" % i, [128, 512], F32) for i in range(4)]
        for q4 in range(4):
            P.dma("pool", lambda e, q4=q4: e.dma_start(out=Wo[:, q4 * 4:(q4 + 1) * 4, :], in_=A["w_out"].rearrange("(k p) n -> p k n", p=128)[:, q4 * 4:(q4 + 1) * 4, :]), writes=["Wo"])
        P.dma("sp", lambda e: e.dma_start(out=G1[:], in_=A["modb"][:, MOD_G1:MOD_G1 + D]), writes=["G1"])
        ycount = 0
        tcount = 0
        for g in range(4):
            gi = g % 2
            P.dma("sp", lambda e, gi=gi, g=g: e.dma_start(out=zT[gi][:].rearrange("p c t -> p (c t)"), in_=A["zT"][g]), writes=["zT%d" % gi])
            for tt in range(4):
                ti = tcount % 2
                tcount += 1
                tile = g * 4 + tt
                P.dma("sp", lambda e, ti=ti, tile=tile: e.dma_start(out=xt[ti][:], in_=A["x_own"][tile * 128:(tile + 1) * 128, :]), writes=["x%d" % ti])
                for cg in range(4):
                    yi = ycount % 4
                    ycount += 1

                    def mm(e, gi=gi, tt=tt, cg=cg, yi=yi):
                        ins = None
                        for k in range(KC):
                            ins = e.matmul(pY[yi][:], lhsT=zT[gi][:, k, tt * 128:(tt + 1) * 128], rhs=Wo[:, k, cg * 512:(cg + 1) * 512], start=(k == 0), stop=(k == KC - 1))
                        return ins
                    P.op("pe", mm, reads=["zT%d" % gi, "Wo"], writes=["pY%d" % yi])
                    P.op("dve", lambda e, yi=yi, cg=cg, ti=ti: e.tensor_tensor(out=pY[yi][:], in0=pY[yi][:], in1=G1[:, cg * 512:(cg + 1) * 512], op=ALU.mult),
                         reads=["pY%d" % yi, "G1"], writes=["pY%d" % yi])
                    P.op("dve", lambda e, yi=yi, cg=cg, ti=ti: e.tensor_tensor(out=xt[ti][:, cg * 512:(cg + 1) * 512], in0=pY[yi][:], in1=xt[ti][:, cg * 512:(cg + 1) * 512], op=ALU.add),
                         reads=["pY%d" % yi, "x%d" % ti], writes=["x%d" % ti])
                P.dma("sp", lambda e, ti=ti, tile=tile: e.dma_start(out=A["x1"][tile * 128:(tile + 1) * 128, :], in_=xt[ti][:]), reads=["x%d" % ti], writes=["dram_x1"])
        P.run()


def phase_route(nc, A):
    with contextlib.ExitStack() as st:
        P = Phase(nc, "p7")
        ident = sb(st, nc, "p7_ident", [128, 128], BF16)
        ltri = sb(st, nc, "p7_ltri", [128, 128], BF16)
        onesb = sb(st, nc, "p7_ones", [128, 128], BF16)
        Amod = sb(st, nc, "p7_A", [128, D], F32)
        Smod = sb(st, nc, "p7_S", [128, D], F32)
        Wr = sb(st, nc, "p7_Wr", [128, KC, E], BF16)
        rbias = sb(st, nc, "p7_rbias", [128, E], F32)
        ecap = sb(st, nc, "p7_ecap", [128, E], F32)
        dumpidx = sb(st, nc, "p7_dump", [128, 1], F32)
        tokid = sb(st, nc, "p7_tokid", [128, NTO], I32)
        zero_i = sb(st, nc, "p7_zero", [128, CAP * 8], I32)
        zero_b = sb(st, nc, "p7_zerob", [128, D], BF16)
        xt = [sb(st, nc, "p7_x%d" % i, [128, D], F32) for i in range(2)]
        sq = [sb(st, nc, "p7_sq%d" % i, [128, D], BF16) for i in range(2)]
        ss = [sb(st, nc, "p7_ss%d" % i, [128, 1], F32) for i in range(2)]
        rstd = [sb(st, nc, "p7_rs%d" % i, [128, 1], F32) for i in range(2)]
        hf = [sb(st, nc, "p7_hf%d" % i, [128, D], F32) for i in range(2)]
        hb = [sb(st, nc, "p7_hb%d" % i, [128, D], BF16) for i in range(2)]
        hT = [sb(st, nc, "p7_hT%d" % i, [128, KC, 128], BF16) for i in range(2)]
        emask = sb(st, nc, "p7_emask", [128, NTO, E], BF16)
        scores = [sb(st, nc, "p7_sc%d" % i, [128, E], F32) for i in range(2)]
        selv = [sb(st, nc, "p7_sel%d" % i, [128, E], F32) for i in range(2)]
        g8 = [sb(st, nc, "p7_g8%d" % i, [128, 8], F32) for i in range(2)]
        gsc = [sb(st, nc, "p7_gsc%d" % i, [128, 8], F32) for i in range(2)]
        gm8 = [sb(st, nc, "p7_gm%d" % i, [128, 8], F32) for i in range(2)]
        gmask = [sb(st, nc, "p7_gmask%d" % i, [128, 8], F32) for i in range(2)]
        t8 = [sb(st, nc, "p7_t8%d" % i, [128, 8], F32) for i in range(2)]
        em = [sb(st, nc, "p7_em%d" % i, [128, E], F32) for i in range(2)]
        wt = sb(st, nc, "p7_wt", [128, NTO, E], F32)
        wsum = [sb(st, nc, "p7_ws%d" % i, [128, 1], F32) for i in range(2)]
        key = [sb(st, nc, "p7_key%d" % i, [128, E], F32) for i in range(2)]
        k8 = [sb(st, nc, "p7_k8%d" % i, [128, 8], F32) for i in range(2)]
        oh_ = [sb(st, nc, "p7_oh%d" % i, [128, E], F32) for i in range(2)]
        junk_ = [sb(st, nc, "p7_junk%d" % i, [128, E], F32) for i in range(2)]
        cnt_i = sb(st, nc, "p7_cnt_i", [128, E], I32)
        route = [sb(st, nc, "p7_route%d" % i, [128, 16], F32) for i in range(2)]
        sidx = [sb(st, nc, "p7_sidx%d" % i, [128, 8], I32) for i in range(2)]
        tokrow = [sb(st, nc, "p7_tokrow%d" % i, [128, 8], I32) for i in range(2)]
        valid = [sb(st, nc, "p7_valid%d" % i, [128, 8], F32) for i in range(2)]
        pT = [ps(st, nc, "p7_pT%d" % i, [128, KC, 128], BF16) for i in range(2)]
        pL = [ps(st, nc, "p7_pL%d" % i, [128, E], F32) for i in range(2)]
        pR = [ps(st, nc, "p7_pR%d" % i, [128, E], F32) for i in range(2)]

        P.dma("sp", lambda e: e.dma_start(out=ident[:], in_=A["ident_bf"][:, :]), writes=["ident"])
        P.dma("sp", lambda e: e.dma_start(out=ltri[:], in_=A["ltri"][:, :]), writes=["ltri"])
        P.dma("sp", lambda e: e.dma_start(out=Amod[:], in_=A["modb"][:, MOD_A2:MOD_A2 + D]), writes=["Amod"])
        P.dma("sp", lambda e: e.dma_start(out=Smod[:], in_=A["modb"][:, MOD_SHIFT2:MOD_SHIFT2 + D]), writes=["Smod"])
        P.dma("pool", lambda e: e.dma_start(out=Wr[:], in_=A["w_router"].rearrange("(k p) n -> p k n", p=128)), writes=["Wr"])
        P.dma("sp", lambda e: e.dma_start(out=rbias[:], in_=A["rbias_b"][:, :]), writes=["rbias"])
        P.dma("sp", lambda e: e.dma_start(out=ecap[:], in_=A["ecap"][:, :]), writes=["ecap"])
        P.dma("sp", lambda e: e.dma_start(out=dumpidx[:], in_=A["dumpidx"][:, :]), writes=["dumpidx"])
        P.dma("sp", lambda e: e.dma_start(out=tokid[:], in_=A["tokid"][:, :]), writes=["tokid"])
        P.op("dve", lambda e: e.memset(onesb[:], 1.0), writes=["onesb"])
        P.op("pool", lambda e: e.memset(zero_i[:], 0), writes=["zero_i"])
        P.op("pool", lambda e: e.memset(zero_b[:], 0.0), writes=["zero_b"])
        rt_view = A["rowtok"][0:NSLOT, :].rearrange("(e c) w -> e (c w)", e=E)
        P.dma("sp", lambda e: e.dma_start(out=rt_view, in_=zero_i[0:E, :]), reads=["zero_i"], writes=["dram_rowtok"])
        P.dma("sp", lambda e: e.dma_start(out=A["rowtok"][NSLOT:NSLOT + 128, :], in_=zero_i[:, 0:8]), reads=["zero_i"], writes=["dram_rowtok"])
        P.dma("sp", lambda e: e.dma_start(out=A["Ybuf"][NSLOT:NSLOT + 128, :], in_=zero_b[:]), reads=["zero_b"], writes=["dram_Ybuf"])

        for t in range(NTO):
            i = t % 2
            tag = str(i)
            P.dma("sp", lambda e, i=i, t=t: e.dma_start(out=xt[i][:], in_=A["x1"][t * 128:(t + 1) * 128, :]), writes=["x" + tag])
            emit_norm_mod_T(P, nc, xt[i], sq[i], ss[i], rstd[i], hf[i], hb[i], pT[i], hT[i], Amod, Smod, ident, tag, "x" + tag)
            P.dma("sp", lambda e, i=i, t=t: e.dma_start(out=A["h2"][t * 128:(t + 1) * 128, :], in_=hb[i][:]), reads=["hb" + tag], writes=["dram_h2"])
            P.dma("sp", lambda e, i=i, t=t: e.dma_start(out=A["h2T"][t], in_=hT[i][:].rearrange("p k t -> p (k t)")), reads=["hT" + tag], writes=["dram_h2T"])

            def mml(e, i=i):
                ins = None
                for k in range(KC):
                    ins = e.matmul(pL[i][:], lhsT=hT[i][:, k, :], rhs=Wr[:, k, :], start=(k == 0), stop=(k == KC - 1))
                return ins
            P.op("pe", mml, reads=["hT" + tag, "Wr"], writes=["pL" + tag])
            P.op("act", lambda e, i=i: e.activation(out=scores[i][:], in_=pL[i][:], func=AF.Sigmoid), reads=["pL" + tag], writes=["sc" + tag])
            P.op("dve", lambda e, i=i: e.tensor_tensor(out=selv[i][:], in0=scores[i][:], in1=rbias[:], op=ALU.add), reads=["sc" + tag, "rbias"], writes=["sel" + tag])
            for gq in range(8):
                P.op("dve", lambda e, i=i, gq=gq: e.max(out=g8[i][:], in_=selv[i][:, gq * 8:(gq + 1) * 8]), reads=["sel" + tag, "gsc" + tag], writes=["g8" + tag])
                P.op("dve", lambda e, i=i, gq=gq: e.tensor_tensor(out=gsc[i][:, gq:gq + 1], in0=g8[i][:, 0:1], in1=g8[i][:, 1:2], op=ALU.add), reads=["g8" + tag], writes=["gsc" + tag])
            P.op("dve", lambda e, i=i: e.max(out=gm8[i][:], in_=gsc[i][:]), reads=["gsc" + tag], writes=["gm8" + tag])
            P.op("dve", lambda e, i=i: e.tensor_scalar(out=gmask[i][:], in0=gsc[i][:], scalar1=gm8[i][:, 3:4], scalar2=None, op0=ALU.is_ge), reads=["gsc" + tag, "gm8" + tag], writes=["gmask" + tag])
            for gq in range(8):
                P.op("dve", lambda e, i=i, gq=gq: e.tensor_scalar(out=selv[i][:, gq * 8:(gq + 1) * 8], in0=selv[i][:, gq * 8:(gq + 1) * 8], scalar1=2.0, scalar2=gmask[i][:, gq:gq + 1],
                                                                 op0=ALU.add, op1=ALU.mult), reads=["sel" + tag, "gmask" + tag], writes=["sel" + tag])
            P.op("dve", lambda e, i=i: e.max(out=t8[i][:], in_=selv[i][:]), reads=["sel" + tag], writes=["t8" + tag])
            P.op("dve", lambda e, i=i: e.tensor_scalar(out=em[i][:], in0=selv[i][:], scalar1=t8[i][:, 7:8], scalar2=None, op0=ALU.is_ge), reads=["sel" + tag, "t8" + tag], writes=["em" + tag])
            P.op("dve", lambda e, i=i, t=t: e.tensor_copy(out=emask[:, t, :], in_=em[i][:]), reads=["em" + tag], writes=["emask"])
            P.op("dve", lambda e, i=i, t=t: e.tensor_tensor(out=wt[:, t, :], in0=scores[i][:], in1=em[i][:], op=ALU.mult), reads=["sc" + tag, "em" + tag], writes=["wt"])
            P.op("act", lambda e, i=i, t=t: e.activation(out=junk_[i][:], in_=wt[:, t, :], func=AF.Copy, accum_out=wsum[i][:]), reads=["wt"], writes=["ws" + tag, "junk" + tag])
            P.op("dve", lambda e, i=i: e.reciprocal(out=wsum[i][:], in_=wsum[i][:]), reads=["ws" + tag], writes=["ws" + tag])
            P.op("dve", lambda e, i=i, t=t: e.tensor_scalar(out=wt[:, t, :], in0=wt[:, t, :], scalar1=wsum[i][:, 0:1], scalar2=2.5, op0=ALU.mult, op1=ALU.mult), reads=["wt", "ws" + tag], writes=["wt"])

            def mmr(e, i=i, t=t):
                ins = None
                for j in range(t):
                    ins = e.matmul(pR[i][:], lhsT=onesb[:], rhs=emask[:, j, :], start=(j == 0), stop=False)
                ins = e.matmul(pR[i][:], lhsT=ltri[:], rhs=emask[:, t, :], start=(t == 0), stop=True)
                return ins
            P.op("pe", mmr, reads=["emask", "onesb", "ltri"], writes=["pR" + tag])
            P.op("dve", lambda e, i=i: e.scalar_tensor_tensor(out=key[i][:], in0=pR[i][:], scalar=1.0, in1=ecap[:], op0=ALU.add, op1=ALU.add), reads=["pR" + tag, "ecap"], writes=["key" + tag])
            P.op("dve", lambda e, i=i: e.tensor_scalar(out=oh_[i][:], in0=pR[i][:], scalar1=float(CAP) - 0.5, scalar2=None, op0=ALU.is_lt), reads=["pR" + tag], writes=["oh" + tag])
            P.op("dve", lambda e, i=i: e.tensor_tensor(out=oh_[i][:], in0=oh_[i][:], in1=em[i][:], op=ALU.mult), reads=["oh" + tag, "em" + tag], writes=["oh" + tag])
            P.op("dve", lambda e, i=i: e.tensor_tensor(out=key[i][:], in0=key[i][:], in1=oh_[i][:], op=ALU.mult), reads=["key" + tag, "oh" + tag], writes=["key" + tag])
            P.op("dve", lambda e, i=i: e.max(out=k8[i][:], in_=key[i][:]), reads=["key" + tag], writes=["k8" + tag])
            for kk in range(8):
                P.op("dve", lambda e, i=i, kk=kk: e.tensor_scalar(out=oh_[i][:], in0=key[i][:], scalar1=k8[i][:, kk:kk + 1], scalar2=None, op0=ALU.is_equal), reads=["key" + tag, "k8" + tag, "route" + tag], writes=["oh" + tag])
                P.op("dve", lambda e, i=i, kk=kk, t=t: e.tensor_tensor(out=oh_[i][:], in0=oh_[i][:], in1=wt[:, t, :], op=ALU.mult), reads=["oh" + tag, "wt"], writes=["oh" + tag])
                P.op("act", lambda e, i=i, kk=kk: e.activation(out=junk_[i][:], in_=oh_[i][:], func=AF.Copy, accum_out=route[i][:, 8 + kk:9 + kk]), reads=["oh" + tag], writes=["route" + tag, "junk" + tag])
            P.op("dve", lambda e, i=i: e.tensor_scalar(out=valid[i][:], in0=k8[i][:], scalar1=0.5, scalar2=None, op0=ALU.is_gt), reads=["k8" + tag], writes=["valid" + tag])
            P.op("dve", lambda e, i=i: e.tensor_tensor(out=route[i][:, 8:16], in0=route[i][:, 8:16], in1=valid[i][:], op=ALU.mult), reads=["route" + tag, "valid" + tag], writes=["route" + tag])
            P.op("dve", lambda e, i=i: e.tensor_scalar(out=route[i][:, 0:8], in0=k8[i][:], scalar1=-1.0, scalar2=dumpidx[:, 0:1], op0=ALU.add, op1=ALU.subtract), reads=["k8" + tag, "dumpidx", "route" + tag], writes=["route" + tag])
            P.op("dve", lambda e, i=i: e.tensor_tensor(out=route[i][:, 0:8], in0=route[i][:, 0:8], in1=valid[i][:], op=ALU.mult), reads=["route" + tag, "valid" + tag], writes=["route" + tag])
            P.op("dve", lambda e, i=i: e.tensor_scalar(out=route[i][:, 0:8], in0=route[i][:, 0:8], scalar1=dumpidx[:, 0:1], scalar2=None, op0=ALU.add), reads=["route" + tag, "dumpidx"], writes=["route" + tag])
            P.op("dve", lambda e, i=i: e.tensor_copy(out=sidx[i][:], in_=route[i][:, 0:8]), reads=["route" + tag], writes=["sidx" + tag])
            P.dma("sp", lambda e, i=i, t=t: e.dma_start(out=A["route"][:, t * 16:(t + 1) * 16], in_=route[i][:]), reads=["route" + tag], writes=["dram_route"])
            if t == NTO - 1:
                def mmc(e):
                    ins = None
                    for j in range(NTO):
                        ins = e.matmul(pL[0][:], lhsT=onesb[:], rhs=emask[:, j, :], start=(j == 0), stop=(j == NTO - 1))
                    return ins
                P.op("pe", mmc, reads=["emask", "onesb"], writes=["pL0"])
                P.op("dve", lambda e: e.tensor_copy(out=cnt_i[:], in_=pL[0][:]), reads=["pL0"], writes=["cnt_i"])
                P.dma("sp", lambda e: e.dma_start(out=A["cnt_d"][:, :], in_=cnt_i[0:1, :]), reads=["cnt_i"], writes=["dram_cnt"])
            for kk in range(8):
                P.op("pool", lambda e, i=i, kk=kk, t=t: e.tensor_copy(out=tokrow[i][:, kk:kk + 1], in_=tokid[:, t:t + 1]), reads=["tokid", "tokrow" + tag], writes=["tokrow" + tag])
            for kk in range(8):
                P.dma("pool", lambda e, i=i, kk=kk: e.indirect_dma_start(out=A["rowtok"][:, :], out_offset=bass.IndirectOffsetOnAxis(ap=sidx[i][:, kk:kk + 1], axis=0),
                                                                         in_=tokrow[i][:], in_offset=None),
                      reads=["sidx" + tag, "tokrow" + tag, "dram_rowtok"], writes=["dram_rowtok_s"])
        P.run()


def phase_experts(nc, A):
    NST = 4
    with contextlib.ExitStack() as st:
        P = Phase(nc, "p8")
        ident = sb(st, nc, "p8_ident", [128, 128], BF16)
        cnt_sb = sb(st, nc, "p8_cnt", [1, E], I32)
        P.cnt_ap = cnt_sb
        Wg = [sb(st, nc, "p8_Wg%d" % i, [128, KC, 512], BF16) for i in range(2)]
        Wu = [sb(st, nc, "p8_Wu%d" % i, [128, KC, 512], BF16) for i in range(2)]
        Wd = [sb(st, nc, "p8_Wd%d" % i, [128, 4, D], BF16) for i in range(2)]
        stage = [sb(st, nc, "p8_st%d" % i, [128, 2048], F32) for i in range(NST)]
        idx_all = sb(st, nc, "p8_idxall", [128, E * JMAX, 8], I32)
        xg = [sb(st, nc, "p8_xg%d" % i, [128, D], BF16) for i in range(3)]
        xT = [sb(st, nc, "p8_xT%d" % i, [128, KC, 128], BF16) for i in range(2)]
        sg = [sb(st, nc, "p8_sg%d" % i, [128, 512], F32) for i in range(2)]
        aT = [sb(st, nc, "p8_aT%d" % i, [128, 4, 128], BF16) for i in range(2)]
        Y = [sb(st, nc, "p8_Y%d" % i, [128, D], BF16) for i in range(2)]
        pT = [ps(st, nc, "p8_pT%d" % i, [128, KC, 128], BF16) for i in range(1)]
        pG = [ps(st, nc, "p8_pG%d" % i, [128, 512], F32) for i in range(2)]
        pU = [ps(st, nc, "p8_pU%d" % i, [128, 512], F32) for i in range(2)]
        pY = [ps(st, nc, "p8_pY%d" % i, [128, 512], F32) for i in range(2)]
        P.dma("pool", lambda e: e.dma_start(out=ident[:], in_=A["ident_bf"][:, :]), writes=["ident"])
        P.dma("pool", lambda e: e.dma_start(out=cnt_sb[:], in_=A["cnt_d"][:, :]), writes=["cnt_sb"])
        cn = {"g": 0, "y": 0, "yb": 0, "x": 0, "a": 0, "gu": 0, "st": 0, "ce": 0}

        def wsrc(ei):
            if ei < E:
                return A["w_exp_gate"][ei], A["w_exp_up"][ei], A["w_exp_down"][ei]
            return A["w_sh_gate"], A["w_sh_up"], A["w_sh_down"]

        def weight_chunks(ei):
            wi = ei % 2
            gsrc, usrc, dsrc = wsrc(ei)
            out = []
            for c in range(4):
                out.append((gsrc.rearrange("(k p) n -> p k n", p=128)[:, 4 * c:4 * c + 4, :], Wg[wi][:, 4 * c:4 * c + 4, :], "Wg%d_%d" % (wi, c), (4, 512)))
            for c in range(4):
                out.append((usrc.rearrange("(k p) n -> p k n", p=128)[:, 4 * c:4 * c + 4, :], Wu[wi][:, 4 * c:4 * c + 4, :], "Wu%d_%d" % (wi, c), (4, 512)))
            for c in range(4):
                out.append((dsrc.rearrange("(k p) n -> p k n", p=128)[:, c:c + 1, :], Wd[wi][:, c:c + 1, :], "Wd%d_%d" % (wi, c), (1, 2048)))
            return out

        def issue_chunk_dma(ch):
            src, dst, res, (a, b) = ch
            si = cn["st"] % NST
            cn["st"] += 1
            P.dma("sp", lambda e, si=si, src=src, a=a: e.dma_start(out=stage[si][:].rearrange("p (a b) -> p a b", a=a), in_=src), writes=["st%d" % si])
            return si

        def issue_chunk_cast(ch, si):
            src, dst, res, (a, b) = ch
            eng = "dve"
            view = stage[si][:].rearrange("p (a b) -> p a b", a=a)
            if eng == "act":
                P.op("act", lambda e, dst=dst, view=view: e.activation(out=dst, in_=view, func=AF.Copy), reads=["st%d" % si], writes=[res])
            else:
                P.op(eng, lambda e, dst=dst, view=view: e.tensor_copy(out=dst, in_=view), reads=["st%d" % si], writes=[res])

        def ffn(wi, xi, dst):
            xres = "xT%d" % xi
            ai = cn["a"] % 2
            cn["a"] += 1
            ares = "aT%d" % ai
            gb = cn["gu"] % 2
            cn["gu"] += 1

            def mmg(e, wi=wi, xi=xi, gb=gb):
                ins = None
                for fc in range(4):
                    for k in range(KC):
                        ins = e.matmul(pG[gb][:, fc * 128:(fc + 1) * 128], lhsT=Wg[wi][:, k, fc * 128:(fc + 1) * 128], rhs=xT[xi][:, k, :], start=(k == 0), stop=(k == KC - 1))
                return ins

            def mmu(e, wi=wi, xi=xi, gb=gb):
                ins = None
                for fc in range(4):
                    for k in range(KC):
                        ins = e.matmul(pU[gb][:, fc * 128:(fc + 1) * 128], lhsT=Wu[wi][:, k, fc * 128:(fc + 1) * 128], rhs=xT[xi][:, k, :], start=(k == 0), stop=(k == KC - 1))
                return ins
            P.op("pe", mmg, reads=["Wg%d_%d" % (wi, c) for c in range(4)] + [xres], writes=["pG%d" % gb])
            P.op("pe", mmu, reads=["Wu%d_%d" % (wi, c) for c in range(4)] + [xres], writes=["pU%d" % gb])
            P.op("act", lambda e, gb=gb: e.activation(out=sg[gb][:], in_=pG[gb][:], func=AF.Sigmoid), reads=["pG%d" % gb], writes=["sg%d" % gb])
            P.op("dve", lambda e, gb=gb: e.tensor_tensor(out=sg[gb][:], in0=pG[gb][:], in1=sg[gb][:], op=ALU.mult), reads=["pG%d" % gb, "sg%d" % gb], writes=["sg%d" % gb])
            P.op("dve", lambda e, gb=gb, ai=ai: e.tensor_tensor(out=aT[ai][:], in0=pU[gb][:].rearrange("p (f t) -> p f t", f=4), in1=sg[gb][:].rearrange("p (f t) -> p f t", f=4), op=ALU.mult),
                 reads=["pU%d" % gb, "sg%d" % gb], writes=[ares])
            yi = cn["yb"] % 2
            cn["yb"] += 1
            for cg in range(4):
                pi = cn["y"] % 2
                cn["y"] += 1

                def mmy(e, wi=wi, ai=ai, cg=cg, pi=pi):
                    ins = None
                    for fc in range(4):
                        ins = e.matmul(pY[pi][:], lhsT=aT[ai][:, fc, :], rhs=Wd[wi][:, fc, cg * 512:(cg + 1) * 512], start=(fc == 0), stop=(fc == 3))
                    return ins
                P.op("pe", mmy, reads=[ares] + ["Wd%d_%d" % (wi, c) for c in range(4)], writes=["pY%d" % pi])
                P.op("dve", lambda e, yi=yi, cg=cg, pi=pi: e.tensor_copy(out=Y[yi][:, cg * 512:(cg + 1) * 512], in_=pY[pi][:]), reads=["pY%d" % pi], writes=["Y%d_%d" % (yi, cg)])
            P.dma("act", lambda e, yi=yi, dst=dst: e.dma_start(out=dst, in_=Y[yi][:]), reads=["Y%d_%d" % (yi, c) for c in range(4)], writes=["dram_Y"])

        for ei in range(E):
            P.dma("pool", lambda e, ei=ei: e.dma_start(out=idx_all[:, ei * JMAX:(ei + 1) * JMAX, :], in_=A["rowtok"][ei * CAP:(ei + 1) * CAP, :].rearrange("(t p) w -> p t w", p=128)),
                  writes=["idxall%d" % ei])
        for ch in weight_chunks(0):
            si = issue_chunk_dma(ch)
            issue_chunk_cast(ch, si)
        for ei in range(E):
            wi = ei % 2
            P.load_cond_reg(ei, "cnt_sb")
            nxt = weight_chunks(ei + 1)
            pend = []

            def pump(ncast):
                for _ in range(ncast):
                    if pend:
                        ch, si = pend.pop(0)
                        issue_chunk_cast(ch, si)
                while nxt and len(pend) < NST:
                    ch = nxt.pop(0)
                    pend.append((ch, issue_chunk_dma(ch)))
            pump(0)
            for j in range(JMAX):
                P.begin_region(ei, 128 * j)
                gi = cn["g"] % 3
                cn["g"] += 1
                xi = cn["x"] % 2
                cn["x"] += 1
                s0 = ei * CAP + j * 128
                P.dma("pool", lambda e, ei=ei, j=j, gi=gi: e.indirect_dma_start(out=xg[gi][:], out_offset=None, in_=A["h2"][:, :],
                                                                              in_offset=bass.IndirectOffsetOnAxis(ap=idx_all[:, ei * JMAX + j, 0:1], axis=0)),
                      reads=["idxall%d" % ei], writes=["xg%d" % gi])

                def tr(e, gi=gi):
                    ins = None
                    for k in range(KC):
                        ins = e.transpose(out=pT[0][:, k, :], in_=xg[gi][:, k * 128:(k + 1) * 128], identity=ident[:])
                    return ins
                P.op("pe", tr, reads=["xg%d" % gi, "ident"], writes=["pT"])
                P.op("dve", lambda e, xi=xi: e.tensor_copy(out=xT[xi][:], in_=pT[0][:]), reads=["pT"], writes=["xT%d" % xi])
                P.end_region()
                pump(2)
                P.begin_region(ei, 128 * j)
                ffn(wi, xi, A["Ybuf"][s0:s0 + 128, :])
                P.end_region()
            while pend or nxt:
                pump(2)
        wi = E % 2
        for t in range(NTO):
            xi = cn["x"] % 2
            cn["x"] += 1
            P.dma("pool", lambda e, xi=xi, t=t: e.dma_start(out=xT[xi][:], in_=A["h2T"][t].rearrange("p (k q) -> p k q", k=KC)), writes=["xT%d" % xi])
            ffn(wi, xi, A["Ysh"][t * 128:(t + 1) * 128, :])
        P.run()


def phase_final(nc, A):
    with contextlib.ExitStack() as st:
        P = Phase(nc, "p9")
        G2 = sb(st, nc, "p9_G2", [128, D], F32)
        gf = sb(st, nc, "p9_gf", [128, D], F32)
        xt = [sb(st, nc, "p9_x%d" % i, [128, D], F32) for i in range(2)]
        ysh = [sb(st, nc, "p9_ysh%d" % i, [128, D], BF16) for i in range(2)]
        yk = [sb(st, nc, "p9_yk%d" % i, [128, D], BF16) for i in range(8)]
        acc = [sb(st, nc, "p9_acc%d" % i, [128, D], F32) for i in range(2)]
        route = [sb(st, nc, "p9_route%d" % i, [128, 16], F32) for i in range(2)]
        sidx = [sb(st, nc, "p9_sidx%d" % i, [128, 8], I32) for i in range(2)]
        sq = [sb(st, nc, "p9_sq%d" % i, [128, D], BF16) for i in range(2)]
        ss = [sb(st, nc, "p9_ss%d" % i, [128, 1], F32) for i in range(2)]
        P.dma("sp", lambda e: e.dma_start(out=G2[:], in_=A["modb"][:, MOD_G2:MOD_G2 + D]), writes=["G2"])
        P.dma("sp", lambda e: e.dma_start(out=gf[:], in_=A["g_final_b"][:, :]), writes=["gf"])
        kc = 0
        for t in range(NTO):
            i = t % 2
            tag = str(i)
            P.dma("sp", lambda e, i=i, t=t: e.dma_start(out=xt[i][:], in_=A["x1"][t * 128:(t + 1) * 128, :]), writes=["x" + tag])
            P.dma("sp", lambda e, i=i, t=t: e.dma_start(out=ysh[i][:], in_=A["Ysh"][t * 128:(t + 1) * 128, :]), writes=["ysh" + tag])
            P.dma("sp", lambda e, i=i, t=t: e.dma_start(out=route[i][:], in_=A["route"][:, t * 16:(t + 1) * 16]), writes=["route" + tag])
            P.op("dve", lambda e, i=i: e.tensor_copy(out=sidx[i][:], in_=route[i][:, 0:8]), reads=["route" + tag], writes=["sidx" + tag])
            P.op("dve", lambda e, i=i: e.tensor_copy(out=acc[i][:], in_=ysh[i][:]), reads=["ysh" + tag], writes=["acc" + tag])
            for kk in range(8):
                ki = kc % 8
                kc += 1
                P.dma("pool", lambda e, i=i, kk=kk, ki=ki: e.indirect_dma_start(out=yk[ki][:], out_offset=None, in_=A["Ybuf"][:, :],
                                                                              in_offset=bass.IndirectOffsetOnAxis(ap=sidx[i][:, kk:kk + 1], axis=0)),
                      reads=["sidx" + tag], writes=["yk%d" % ki])
                P.op("dve", lambda e, i=i, kk=kk, ki=ki: e.scalar_tensor_tensor(out=acc[i][:], in0=yk[ki][:], scalar=route[i][:, 8 + kk:9 + kk], in1=acc[i][:], op0=ALU.mult, op1=ALU.add),
                     reads=["yk%d" % ki, "route" + tag, "acc" + tag], writes=["acc" + tag])
            P.op("pool", lambda e, i=i: e.tensor_tensor(out=acc[i][:], in0=acc[i][:], in1=G2[:], op=ALU.mult), reads=["acc" + tag, "G2"], writes=["acc" + tag])
            P.op("pool", lambda e, i=i: e.tensor_tensor(out=xt[i][:], in0=xt[i][:], in1=acc[i][:], op=ALU.add), reads=["acc" + tag, "x" + tag], writes=["x" + tag])
            P.op("act", lambda e, i=i: e.activation(out=sq[i][:], in_=xt[i][:], func=AF.Square, accum_out=ss[i][:]), reads=["x" + tag], writes=["sq" + tag, "ss" + tag])
            P.op("dve", lambda e, i=i: e.tensor_scalar(out=ss[i][:], in0=ss[i][:], scalar1=1.0 / D, scalar2=EPS, op0=ALU.mult, op1=ALU.add), reads=["ss" + tag], writes=["ss" + tag])
            P.op("act", lambda e, i=i: e.activation(out=ss[i][:], in_=ss[i][:], func=AF.Sqrt), reads=["ss" + tag], writes=["ss" + tag])
            P.op("dve", lambda e, i=i: e.reciprocal(out=ss[i][:], in_=ss[i][:]), reads=["ss" + tag], writes=["ss" + tag])
            P.op("dve", lambda e, i=i: e.scalar_tensor_tensor(out=acc[i][:], in0=xt[i][:], scalar=ss[i][:, 0:1], in1=gf[:], op0=ALU.mult, op1=ALU.mult),
                 reads=["x" + tag, "ss" + tag, "gf", "acc" + tag], writes=["acc" + tag])
            P.dma("sp", lambda e, i=i, t=t: e.dma_start(out=A["out"][t * 128:(t + 1) * 128, :], in_=acc[i][:]), reads=["acc" + tag], writes=["dram_out"])
        P.run()


def _t5_bucket_np(rel):
    n = np.maximum(rel, 0)
    nf = np.maximum(n, 1).astype(np.float32)
    large = 16 + (np.log(nf / 16) / np.float32(np.log(128 / 16)) * 16).astype(np.int32)
    large = np.minimum(large, 31)
    return np.where(n < 16, n, large)


def _bucket_table():
    n = np.arange(0, 700, dtype=np.int64)
    nf = np.maximum(n, 1).astype(np.float32)
    large = 16 + (np.log(nf / np.float32(16)) / np.float32(np.log(8.0)) * np.float32(16)).astype(np.int32)
    large = np.minimum(large, 31)
    return np.where(n < 16, n, large)


def make_core_inputs(inp, c, shared):
    b, r = c // 4, c % 4
    f32 = np.float32
    x = inp["x"]
    own_tiles = [4 * m + r for m in range(NTO)]
    xb = np.ascontiguousarray(x[b])
    m_ = dict(shared)
    m_["x_all"] = xb
    m_["x_own"] = np.ascontiguousarray(xb.reshape(NTA, 128, D)[own_tiles].reshape(TO, D))
    m_["cT"] = np.ascontiguousarray(inp["c"][b].reshape(KC, 128).T)
    ext = shared["_ext_table"]
    bidx = np.zeros((128, 5, 128), np.int64)
    kk = np.arange(128)[:, None]
    qq = np.arange(128)[None, :]
    for jj in range(5):
        delta = r - (jj - 1)
        rel = delta * 128 + qq - kk
        bi = shared["_bucket"][np.clip(rel, 0, 699)]
        bidx[:, jj, :] = np.where(rel >= 0, bi, 32)
    BT = ext[bidx]
    m_["BT"] = np.ascontiguousarray(BT.transpose(0, 3, 1, 2).reshape(128, 16 * 5 * 128)).astype(ml_dtypes.bfloat16)
    cur = np.array([(4 * m + r) // 2 for m in range(NTO)])
    n = np.arange(32)[None, :]
    past = (n < cur[:, None])
    ownm = (n == cur[:, None])
    const_tbl = np.array([0.0, 1.0, -BIG], f32)
    m_["past"] = np.ascontiguousarray(np.broadcast_to(const_tbl[past.astype(np.int64)].reshape(1, NTO * 32), (128, NTO * 32)))
    m_["own"] = np.ascontiguousarray(np.broadcast_to(const_tbl[ownm.astype(np.int64)].reshape(1, NTO * 32), (128, NTO * 32)))
    m_["pen"] = np.ascontiguousarray(np.broadcast_to(const_tbl[np.where(past, 0, 2)].reshape(1, NTO * 32), (128, NTO * 32)))
    for k in list(m_.keys()):
        if k.startswith("_"):
            del m_[k]
    return m_


def make_shared(inp):
    f32 = np.float32
    bc = lambda v, n: np.ascontiguousarray(np.broadcast_to(np.asarray(v, f32).reshape(1, n), (128, n)))
    sh = {}
    sh["w_ada"] = np.ascontiguousarray(inp["w_ada"][0])
    sh["b_ada_b"] = bc(inp["b_ada"][0], 6 * D)
    sh["g_mix_b"] = bc(inp["g_mix"][0], D)
    sh["g_ffn_b"] = bc(inp["g_ffn"][0], D)
    sh["g_final_b"] = bc(inp["g_final"], D)
    sh["w_in"] = np.ascontiguousarray(inp["w_in"][0])
    sh["lam_b"] = bc(inp["diff_lambda"][0].reshape(-1), 256)
    sh["subln_b"] = bc(inp["diff_subln_g"][0], 128)
    rel_bias = np.asarray(inp["rel_bias"], f32)
    sh["b31"] = bc(rel_bias[31], 16)
    sh["_ext_table"] = np.concatenate([rel_bias, np.full((1, 16), -BIG, f32)], axis=0)
    sh["_bucket"] = _bucket_table()
    sh["w_o_diff"] = np.ascontiguousarray(inp["w_o_diff"][0])
    sh["w_o_moba"] = np.ascontiguousarray(inp["w_o_moba"][0])
    sh["w_out"] = np.ascontiguousarray(inp["w_out"][0])
    sh["w_router"] = np.ascontiguousarray(inp["w_router"][0])
    sh["rbias_b"] = bc(inp["router_bias"][0], E)
    sh["w_exp_gate"] = np.ascontiguousarray(inp["w_exp_gate"][0])
    sh["w_exp_up"] = np.ascontiguousarray(inp["w_exp_up"][0])
    sh["w_exp_down"] = np.ascontiguousarray(inp["w_exp_down"][0])
    sh["w_sh_gate"] = np.ascontiguousarray(inp["w_sh_gate"][0])
    sh["w_sh_up"] = np.ascontiguousarray(inp["w_sh_up"][0])
    sh["w_sh_down"] = np.ascontiguousarray(inp["w_sh_down"][0])
    sh["ident_bf"] = np.eye(128, dtype=f32).astype(ml_dtypes.bfloat16)
    sh["ident_f"] = np.eye(128, dtype=f32)
    tp = np.arange(128)
    sh["ltri"] = (tp[:, None] < tp[None, :]).astype(f32).astype(ml_dtypes.bfloat16)
    sel = np.zeros((32, 32, 128), f32)
    for n in range(32):
        sel[n, n, :] = 1.0
    sh["selc"] = sel.reshape(32, 32 * 128).astype(ml_dtypes.bfloat16)
    sh["ecap"] = bc(np.arange(E, dtype=f32) * CAP, E)
    sh["dumpidx"] = (NSLOT + np.arange(128, dtype=f32)).reshape(128, 1)
    sh["tokid"] = (np.arange(NTO, dtype=np.int32)[None, :] * 128 + np.arange(128, dtype=np.int32)[:, None]).astype(np.int32)
    return sh


_NC_CACHE = {}


def kernel(**inputs):
    inp = {k: np.asarray(v) for k, v in inputs.items()}
    stop_after = int(os.environ.get("MK_STOP", "99"))
    key = (stop_after, DEBUG)
    if key not in _NC_CACHE:
        _NC_CACHE[key] = build_program(stop_after)
    nc = _NC_CACHE[key]
    shared = make_shared(inp)
    in_maps = [make_core_inputs(inp, c, shared) for c in range(NCORES)]
    used = set(USED_INPUTS)
    in_maps = [{k: v for k, v in m.items() if k in used} for m in in_maps]
    res = run_bass_kernel_spmd(nc, in_maps, core_ids=list(range(NCORES)))
    out = np.zeros((2, S, D), np.float32)
    for c in range(NCORES):
        b, r = c // 4, c % 4
        if "out" not in res.results[c]:
            continue
        o = np.asarray(res.results[c]["out"]).reshape(NTO, 128, D)
        ov = out[b].reshape(NTA, 128, D)
        for m in range(NTO):
            ov[4 * m + r] = o[m]
    if DEBUG:
        kernel.last_results = res.results
    return out
```
